# Optimizing a Trainium2 kernel written in Bass

```python
import jax
import jax.numpy as jnp
from jax import lax
import numpy as np

D_MODEL = 1024
BATCH = 8
SEQ = 2048
DEPTH = 1

GRID_W = 64
CTX_LEN = 256
EPS = 1e-6

S5_WIDTH = D_MODEL // 2
S5_GROUP = 16
S5_GROUPS = S5_WIDTH // S5_GROUP
S5_STATE = 64

GDN_HEADS = 4
GDN_DK = 128
GDN_DV = 128
GDN_QK = GDN_HEADS * GDN_DK
GDN_V = GDN_HEADS * GDN_DV
GDN_CONV_CH = 2 * GDN_QK + GDN_V
CONV_K = 5
CHUNK = 64

N_EXPERTS = 32
TOP_K = 4
D_EXPERT = D_MODEL
SWIGLU_LIMIT = 7.0
SWIGLU_ALPHA = 1.702

N_BRANCH = 2
IN_SPLITS = (S5_WIDTH, GDN_CONV_CH, GDN_V, 2 * GDN_HEADS, 2 * GDN_HEADS, N_BRANCH * D_MODEL)
IN_COLS = sum(IN_SPLITS)

kernel_name = "hybrid_s5_gdn_moe_prefix_dit_block"


def _rmsnorm(x, gain):
    xf = x.astype(jnp.float32)
    y = xf * lax.rsqrt(jnp.mean(xf * xf, axis=-1, keepdims=True) + EPS)
    return (y * gain.astype(jnp.float32)).astype(x.dtype)


def _split_in(z):
    offs = np.cumsum(IN_SPLITS)[:-1].tolist()
    u, qkv, gate, b_raw, a_raw, br = jnp.split(z, offs, axis=-1)
    lead = z.shape[:-1]
    return (u, qkv, gate, b_raw.reshape(lead + (2, GDN_HEADS)),
            a_raw.reshape(lead + (2, GDN_HEADS)), br)


def _short_conv(u, w):
    y = lax.conv_general_dilated(u, w[:, None, :].astype(u.dtype), window_strides=(1,), padding='SAME',
                                 dimension_numbers=('NWC', 'WIO', 'NWC'),
                                 feature_group_count=u.shape[-1])
    return jax.nn.silu(y)


def _s5_discretise(lam_re, lam_im, log_step, b_re, b_im):
    f32 = jnp.float32
    lr = jnp.minimum(lam_re.astype(f32), -1e-4)
    li = lam_im.astype(f32)
    dt = jnp.exp(log_step.astype(f32))[:, None]
    mag = jnp.exp(lr * dt)
    ar, ai = mag * jnp.cos(li * dt), mag * jnp.sin(li * dt)
    den = lr * lr + li * li
    fr = ((ar - 1.0) * lr + ai * li) / den
    fi = (ai * lr - (ar - 1.0) * li) / den
    br, bi = b_re.astype(f32), b_im.astype(f32)
    bbr = fr[..., None] * br - fi[..., None] * bi
    bbi = fr[..., None] * bi + fi[..., None] * br
    return ar, ai, bbr, bbi


def _cmul_combine(e1, e2):
    a1r, a1i, b1r, b1i = e1
    a2r, a2i, b2r, b2i = e2
    return (a2r * a1r - a2i * a1i, a2r * a1i + a2i * a1r,
            a2r * b1r - a2i * b1i + b2r, a2r * b1i + a2i * b1r + b2i)


def _complex_scan(ar, ai, bur, bui, reverse):
    ar = jnp.broadcast_to(ar, bur.shape)
    ai = jnp.broadcast_to(ai, bur.shape)
    return lax.associative_scan(_cmul_combine, (ar, ai, bur, bui), axis=1, reverse=reverse)


def _s5_drive(ug, bbr, bbi):
    return (jnp.einsum('blgc,gnc->blgn', ug, bbr), jnp.einsum('blgc,gnc->blgn', ug, bbi))


def _s5_readout(hr, hi, cr, ci):
    return jnp.einsum('blgn,gcn->blgc', hr, cr) - jnp.einsum('blgn,gcn->blgc', hi, ci)


def _s5_glu(y, w_glu, b_glu, dtype):
    b, l = y.shape[:2]
    z = jax.nn.gelu(y.reshape(b, l, S5_WIDTH)).astype(dtype)
    return z * jax.nn.sigmoid(z @ w_glu + b_glu)


def _s5_mixer(u_ctx, u_lat, lam_re, lam_im, log_step, b_re, b_im, c_re, c_im, d_skip, w_glu, b_glu,
              with_ctx_out):
    dtype = u_lat.dtype
    bsz, lat_len, _ = u_lat.shape
    ctx_len = u_ctx.shape[1]
    ug_c = u_ctx.astype(jnp.float32).reshape(bsz, ctx_len, S5_GROUPS, S5_GROUP)
    ug_l = u_lat.astype(jnp.float32).reshape(bsz, lat_len, S5_GROUPS, S5_GROUP)
    d = d_skip.astype(jnp.float32).reshape(S5_GROUPS, S5_GROUP)
    y_l = d * ug_l
    y_c = d * ug_c
    for direction in range(2):
        rev = direction == 1
        ar, ai, bbr, bbi = _s5_discretise(lam_re[direction], lam_im[direction], log_step[direction],
                                          b_re[direction], b_im[direction])
        cr = c_re[direction].astype(jnp.float32)
        ci = c_im[direction].astype(jnp.float32)
        _, _, hcr, hci = _complex_scan(ar, ai, *_s5_drive(ug_c, bbr, bbi), rev)
        end = 0 if rev else -1
        h0r, h0i = hcr[:, end][:, None], hci[:, end][:, None]
        pr, pi, hr, hi = _complex_scan(ar, ai, *_s5_drive(ug_l, bbr, bbi), rev)
        hr, hi = hr + pr * h0r - pi * h0i, hi + pr * h0i + pi * h0r
        y_l = y_l + _s5_readout(hr, hi, cr, ci)
        if with_ctx_out:
            y_c = y_c + _s5_readout(hcr, hci, cr, ci)
    out_l = _s5_glu(y_l, w_glu, b_glu, dtype)
    out_c = _s5_glu(y_c, w_glu, b_glu, dtype) if with_ctx_out else None
    return out_c, out_l


def _l2norm(t):
    return t * lax.rsqrt(jnp.sum(t * t, axis=-1, keepdims=True) + EPS)


def _gdn_inputs(qkv, b_raw, a_raw, a_log, dt_bias):
    bsz, length, _ = qkv.shape
    f32 = jnp.float32
    q, k, v = jnp.split(qkv.astype(f32), [GDN_QK, 2 * GDN_QK], axis=-1)
    q = _l2norm(q.reshape(bsz, length, GDN_HEADS, GDN_DK)) * (GDN_DK ** -0.5)
    k = _l2norm(k.reshape(bsz, length, GDN_HEADS, GDN_DK))
    v = v.reshape(bsz, length, GDN_HEADS, GDN_DV)
    beta = jax.nn.sigmoid(b_raw.astype(f32))
    g = -jnp.exp(a_log.astype(f32)) * jax.nn.softplus(a_raw.astype(f32) + dt_bias.astype(f32))
    return q, k, v, beta, g


def _gdn_chunked(q, k, v, g, beta, s0):
    bsz, length, heads, _ = q.shape
    dv = v.shape[-1]
    n = length // CHUNK

    def blocks(t):
        t = t.reshape((bsz, n, CHUNK) + t.shape[2:])
        return jnp.moveaxis(t, 3, 1)

    qb, kb, vb, gb, bb = (blocks(t) for t in (q, k, v, g, beta))
    gcum = jnp.cumsum(gb, axis=-1)
    idx = jnp.arange(CHUNK)
    incl = idx[:, None] >= idx[None, :]
    strict = idx[:, None] > idx[None, :]
    decay = jnp.exp(jnp.where(incl, gcum[..., :, None] - gcum[..., None, :], -jnp.inf))
    kbeta = kb * bb[..., None]
    a_mat = jnp.where(strict, jnp.einsum('bhncd,bhnsd->bhncs', kbeta, kb) * decay, 0.0)
    rhs = jnp.concatenate([vb * bb[..., None], kbeta * jnp.exp(gcum)[..., None]], axis=-1)
    sol = lax.linalg.triangular_solve(a_mat, rhs, left_side=True, lower=True, unit_diagonal=True)
    u_blk, w_blk = sol[..., :dv], sol[..., dv:]
    qk = jnp.einsum('bhncd,bhnsd->bhncs', qb, kb) * decay
    glast = gcum[..., -1]
    q_dec = qb * jnp.exp(gcum)[..., None]
    k_dec = kb * jnp.exp(glast[..., None] - gcum)[..., None]
    xs = tuple(jnp.moveaxis(t, 2, 0) for t in (q_dec, k_dec, u_blk, w_blk, qk, jnp.exp(glast)))

    def step(s, inp):
        qg, kd, u_i, w_i, qk_i, dl = inp
        v_new = u_i - jnp.einsum('bhck,bhkv->bhcv', w_i, s)
        o = jnp.einsum('bhck,bhkv->bhcv', qg, s) + jnp.einsum('bhcs,bhsv->bhcv', qk_i, v_new)
        s = s * dl[..., None, None] + jnp.einsum('bhck,bhcv->bhkv', kd, v_new)
        return s, o

    s_final, o = lax.scan(step, s0, xs)
    o = jnp.moveaxis(jnp.moveaxis(o, 0, 2), 1, 3).reshape(bsz, length, heads, dv)
    return o, s_final


def _gdn_direction(inp, direction, s_init):
    q, k, v, beta, g = inp
    flip = (lambda t: jnp.flip(t, axis=1)) if direction == 1 else (lambda t: t)
    o, s = _gdn_chunked(flip(q), flip(k), flip(v), flip(g[:, :, direction]), flip(beta[:, :, direction]), s_init)
    return flip(o), s


def _gdn_out(o, gate, norm_w, dtype):
    bsz, length = o.shape[:2]
    on = o * lax.rsqrt(jnp.mean(o * o, axis=-1, keepdims=True) + EPS) * norm_w.astype(jnp.float32)
    return (on.reshape(bsz, length, GDN_V) * jax.nn.silu(gate.astype(jnp.float32))).astype(dtype)


def _gdn_mixer(qkv_ctx, b_ctx, a_ctx, gate_ctx, qkv_lat, b_lat, a_lat, gate_lat, a_log, dt_bias, norm_w,
               with_ctx_out):
    dtype = qkv_lat.dtype
    ctx_in = _gdn_inputs(qkv_ctx, b_ctx, a_ctx, a_log, dt_bias)
    lat_in = _gdn_inputs(qkv_lat, b_lat, a_lat, a_log, dt_bias)
    s0 = jnp.zeros((qkv_lat.shape[0], GDN_HEADS, GDN_DK, GDN_DV), jnp.float32)
    o_c, o_l = 0.0, 0.0
    for direction in range(2):
        oc, sc = _gdn_direction(ctx_in, direction, s0)
        ol, _ = _gdn_direction(lat_in, direction, sc)
        o_c, o_l = o_c + oc, o_l + ol
    y_l = _gdn_out(o_l, gate_lat, norm_w, dtype)
    y_c = _gdn_out(o_c, gate_ctx, norm_w, dtype) if with_ctx_out else None
    return y_c, y_l


def _token_mixer(h, hc, rows, w_in, s5_lam_re, s5_lam_im, s5_log_step, s5_b_re, s5_b_im, s5_c_re, s5_c_im,
                 s5_d, s5_w_glu, s5_b_glu, gdn_conv, gdn_a_log, gdn_dt_bias, gdn_norm,
                 w_branch_a, w_branch_b, w_out, with_ctx_out):
    bsz, length, _ = h.shape
    u, qkv, gate, b_raw, a_raw, br = _split_in(h @ w_in)
    uc, qkvc, gatec, b_rawc, a_rawc, brc = _split_in(hc @ w_in)
    qkv = _short_conv(qkv.reshape(bsz * rows, GRID_W, GDN_CONV_CH), gdn_conv).reshape(bsz, length, GDN_CONV_CH)
    qkvc = _short_conv(qkvc, gdn_conv)
    ya_c, ya = _s5_mixer(uc, u, s5_lam_re, s5_lam_im, s5_log_step, s5_b_re, s5_b_im, s5_c_re, s5_c_im,
                         s5_d, s5_w_glu, s5_b_glu, with_ctx_out)
    yb_c, yb = _gdn_mixer(qkvc, b_rawc, a_rawc, gatec, qkv, b_raw, a_raw, gate, gdn_a_log, gdn_dt_bias,
                          gdn_norm, with_ctx_out)

    def merge(ya_, yb_, br_):
        ga, gb = jnp.split(jax.nn.sigmoid(br_), N_BRANCH, axis=-1)
        return (ga * (ya_ @ w_branch_a) + gb * (yb_ @ w_branch_b)) @ w_out

    out = merge(ya, yb, br)
    out_c = merge(ya_c, yb_c, brc) if with_ctx_out else None
    return out, out_c


def _moe(h, w_router, b_router, w_gate_up, b_gate_up, w_down, b_down):
    shp = h.shape
    t = h.reshape(-1, shp[-1])
    logits = (t @ w_router + b_router).astype(jnp.float32)
    top_val, top_idx = lax.top_k(logits, TOP_K)
    weights = jax.nn.softmax(top_val, axis=-1)
    combine = jnp.einsum('tk,tke->te', weights,
                         jax.nn.one_hot(top_idx, N_EXPERTS, dtype=jnp.float32)).astype(h.dtype)
    out = jnp.zeros_like(t)
    for e in range(N_EXPERTS):
        gu = t @ w_gate_up[e] + b_gate_up[e]
        gate, up = gu[:, :D_EXPERT], gu[:, D_EXPERT:]
        gate = jnp.minimum(gate, SWIGLU_LIMIT)
        up = jnp.clip(up, -SWIGLU_LIMIT, SWIGLU_LIMIT)
        act = (up + 1.0) * gate * jax.nn.sigmoid(gate * SWIGLU_ALPHA)
        out = out + combine[:, e:e + 1] * (act @ w_down[e] + b_down[e])
    return out.reshape(shp)


def setup_inputs(seed: int = 0) -> dict:
    key = jax.random.key(seed)
    ks = iter(jax.random.split(key, 48))
    f32 = jnp.float32

    def nrm(shape, scale):
        return jax.random.normal(next(ks), shape, f32) * scale

    def unif(shape, lo, hi):
        return jax.random.uniform(next(ks), shape, f32, lo, hi)

    d = D_MODEL
    lam_im_base = jnp.pi * jnp.arange(S5_STATE, dtype=f32)
    dt_init = jnp.exp(unif((DEPTH, 2, GDN_HEADS), float(np.log(1e-3)), float(np.log(1e-1))))
    return {
        'x': nrm((BATCH, SEQ, d), 1.0),
        'c': nrm((BATCH, d), 1.0),
        'ctx': nrm((BATCH, CTX_LEN, d), 1.0),
        'c_ctx': nrm((d,), 1.0),
        'w_mod': nrm((DEPTH, d, 6 * d), 0.5 * d ** -0.5),
        'b_mod': nrm((DEPTH, 6 * d), 0.01),
        'norm1': 1.0 + nrm((DEPTH, d), 0.02),
        'w_in': nrm((DEPTH, d, IN_COLS), d ** -0.5),
        's5_lam_re': -0.5 + nrm((DEPTH, 2, S5_GROUPS, S5_STATE), 0.01),
        's5_lam_im': lam_im_base + nrm((DEPTH, 2, S5_GROUPS, S5_STATE), 0.01),
        's5_log_step': unif((DEPTH, 2, S5_GROUPS), float(np.log(1e-3)), float(np.log(1e-1))),
        's5_b_re': nrm((DEPTH, 2, S5_GROUPS, S5_STATE, S5_GROUP), (2 * S5_GROUP) ** -0.5),
        's5_b_im': nrm((DEPTH, 2, S5_GROUPS, S5_STATE, S5_GROUP), (2 * S5_GROUP) ** -0.5),
        's5_c_re': nrm((DEPTH, 2, S5_GROUPS, S5_GROUP, S5_STATE), 0.5),
        's5_c_im': nrm((DEPTH, 2, S5_GROUPS, S5_GROUP, S5_STATE), 0.5),
        's5_d': nrm((DEPTH, S5_WIDTH), 0.5),
        's5_w_glu': nrm((DEPTH, S5_WIDTH, S5_WIDTH), S5_WIDTH ** -0.5),
        's5_b_glu': nrm((DEPTH, S5_WIDTH), 0.01),
        'gdn_conv': nrm((DEPTH, CONV_K, GDN_CONV_CH), CONV_K ** -0.5),
        'gdn_a_log': jnp.log(unif((DEPTH, 2, GDN_HEADS), 1.0, 16.0)),
        'gdn_dt_bias': jnp.log(jnp.expm1(dt_init)),
        'gdn_norm': 1.0 + nrm((DEPTH, GDN_DV), 0.02),
        'w_branch_a': nrm((DEPTH, S5_WIDTH, d), S5_WIDTH ** -0.5),
        'w_branch_b': nrm((DEPTH, GDN_V, d), GDN_V ** -0.5),
        'w_out': nrm((DEPTH, d, d), d ** -0.5),
        'norm2': 1.0 + nrm((DEPTH, d), 0.02),
        'w_router': nrm((DEPTH, d, N_EXPERTS), d ** -0.5),
        'b_router': nrm((DEPTH, N_EXPERTS), 0.01),
        'w_gate_up': nrm((DEPTH, N_EXPERTS, d, 2 * D_EXPERT), d ** -0.5),
        'b_gate_up': nrm((DEPTH, N_EXPERTS, 2 * D_EXPERT), 0.01),
        'w_down': nrm((DEPTH, N_EXPERTS, D_EXPERT, d), D_EXPERT ** -0.5),
        'b_down': nrm((DEPTH, N_EXPERTS, d), 0.01),
        'norm_f': 1.0 + nrm((d,), 0.02),
    }


def reference(x, c, ctx, c_ctx, w_mod, b_mod, norm1, w_in, s5_lam_re, s5_lam_im, s5_log_step, s5_b_re,
              s5_b_im, s5_c_re, s5_c_im, s5_d, s5_w_glu, s5_b_glu, gdn_conv, gdn_a_log, gdn_dt_bias, gdn_norm,
              w_branch_a, w_branch_b, w_out, norm2, w_router, b_router, w_gate_up, b_gate_up, w_down, b_down,
              norm_f):
    rows = x.shape[1] // GRID_W
    for layer in range(DEPTH):
        last = layer == DEPTH - 1
        mod = jax.nn.silu(c) @ w_mod[layer] + b_mod[layer]
        sh1, sc1, gt1, sh2, sc2, gt2 = jnp.split(mod[:, None, :], 6, axis=-1)
        mod_c = jax.nn.silu(c_ctx) @ w_mod[layer] + b_mod[layer]
        csh1, csc1, cgt1, csh2, csc2, cgt2 = jnp.split(mod_c, 6, axis=-1)
        h = _rmsnorm(x, norm1[layer]) * (1.0 + sc1) + sh1
        hc = _rmsnorm(ctx, norm1[layer]) * (1.0 + csc1) + csh1
        mix, mix_c = _token_mixer(h, hc, rows, w_in[layer], s5_lam_re[layer], s5_lam_im[layer],
                                  s5_log_step[layer], s5_b_re[layer], s5_b_im[layer], s5_c_re[layer],
                                  s5_c_im[layer], s5_d[layer], s5_w_glu[layer], s5_b_glu[layer],
                                  gdn_conv[layer], gdn_a_log[layer], gdn_dt_bias[layer], gdn_norm[layer],
                                  w_branch_a[layer], w_branch_b[layer], w_out[layer], not last)
        x = x + gt1 * mix
        h2 = _rmsnorm(x, norm2[layer]) * (1.0 + sc2) + sh2
        x = x + gt2 * _moe(h2, w_router[layer], b_router[layer], w_gate_up[layer], b_gate_up[layer],
                           w_down[layer], b_down[layer])
        if not last:
            ctx = ctx + cgt1 * mix_c
            hc2 = _rmsnorm(ctx, norm2[layer]) * (1.0 + csc2) + csh2
            ctx = ctx + cgt2 * _moe(hc2, w_router[layer], b_router[layer], w_gate_up[layer],
                                    b_gate_up[layer], w_down[layer], b_down[layer])
    return _rmsnorm(x, norm_f)
```

```python
import os
import numpy as np
from contextlib import ExitStack
import concourse.bass as bass
import concourse.mybir as mybir
from concourse.bass_utils import run_bass_kernel_spmd

F32 = mybir.dt.float32
BF16 = mybir.dt.bfloat16
I32 = mybir.dt.int32
AF = mybir.ActivationFunctionType
ALU = mybir.AluOpType
AX = mybir.AxisListType

D = 1024
L = 2048
CTX = 256
TOK = L + CTX
NT = TOK // 128
EPS = 1e-6
NE = 32
TS5 = 128
IN_COLS = 4624
OFF_U, OFF_QKV, OFF_GATE, OFF_B, OFF_A, OFF_BR = 0, 512, 2048, 2560, 2568, 2576
BLOCKS = [(0, 256)] + [(256 + 512 * i, 512) for i in range(4)]


class Buf:
    __slots__ = ("name", "w", "r")

    def __init__(self, name):
        self.name = name
        self.w = None
        self.r = {}


class KB:
    def __init__(self, nc, same_engine_sync=True):
        self.nc = nc
        self.es = ExitStack()
        self.root = self.es
        self.same = same_engine_sync
        self.engs = {}
        self.sems = {}
        for name in ("tensor", "vector", "scalar", "gpsimd", "sync"):
            eng = getattr(nc, name)
            sem = self.root.enter_context(nc.semaphore("s_" + name))
            self.sems["E" + name] = sem
            self.engs[name] = dict(eng=eng, key="E" + name, count=0, waited={})
        self.nbuf = 0
        self.dmasems = {}
        self.nalloc = 0

    def sb(self, name, shape, dt=F32):
        nb = int(np.prod(shape[1:])) * (2 if dt == BF16 else 4)
        self.nalloc += (nb + 31) // 32 * 32
        if os.environ.get("KALLOC"):
            print("alloc", name, shape, nb, "total", self.nalloc)
        self.nnames = getattr(self, "nnames", 0) + 1
        return self.es.enter_context(self.nc.sbuf_tensor("sb%d_%s" % (self.nnames, name), list(shape), dt))

    def ps(self, name, shape, dt=F32):
        return self.es.enter_context(self.nc.psum_tensor("pp_" + name, list(shape), dt))

    def buf(self, name=None):
        self.nbuf += 1
        return Buf((name or "b") + "_%d" % self.nbuf)

    def _wait(self, en, key, val):
        e = self.engs[en]
        if key == e["key"] and ((not self.same) or en == "tensor"):
            return
        if e["waited"].get(key, 0) >= val:
            return
        if key in self.dmasems:
            val = self.dmasems[key]
        e["eng"].wait_ge(self.sems[key], val)
        e["waited"][key] = val

    def _deps(self, en, reads, writes):
        need = {}
        for b in reads:
            if b.w is not None:
                k, v = b.w
                need[k] = max(need.get(k, 0), v)
        for b in writes:
            if b.w is not None:
                k, v = b.w
                need[k] = max(need.get(k, 0), v)
            for k, v in b.r.items():
                need[k] = max(need.get(k, 0), v)
        for k, v in need.items():
            self._wait(en, k, v)

    def _post(self, sig, reads, writes):
        k, v = sig
        for b in reads:
            b.r[k] = max(b.r.get(k, 0), v)
        for b in writes:
            b.w = sig
            b.r = {}

    def op(self, en, fn, reads=(), writes=(), sig=True):
        e = self.engs[en]
        self._deps(en, reads, writes)
        inst = fn(e["eng"])
        if sig:
            e["count"] += 1
            inst.then_inc(self.sems[e["key"]], 1)
            e["pending"] = False
            self._post((e["key"], e["count"]), reads, writes)
        else:
            assert en == "tensor"
            e["pending"] = True
            self._post((e["key"], e["count"] + 1), reads, writes)
        return inst

    NDSEM = 64

    def dma(self, en, out, in_, reads=(), writes=(), **kw):
        e = self.engs[en]
        self._deps(en, reads, writes)
        tgt = writes[0] if writes else reads[0]
        if not hasattr(self, "bufsem"):
            self.bufsem = {}
            self.dsem_list = []
        semkey = self.bufsem.get(tgt.name)
        if semkey is None:
            idx = len(self.bufsem) % self.NDSEM
            semkey = "DS%d" % idx
            self.bufsem[tgt.name] = semkey
            if semkey not in self.sems:
                self.sems[semkey] = self.root.enter_context(self.nc.semaphore("d%d" % idx))
                self.dmasems[semkey] = 0
        self.dmasems[semkey] += 16
        inst = e["eng"].dma_start(out=out, in_=in_, **kw)
        inst.then_inc(self.sems[semkey], 16)
        self._post((semkey, self.dmasems[semkey]), reads, writes)
        return inst

    def barrier(self):
        for en, e in self.engs.items():
            for en2, e2 in self.engs.items():
                if en2 != en and e2["count"] > 0:
                    self._wait(en, e2["key"], e2["count"])
            for k, v in self.dmasems.items():
                self._wait(en, k, v)

    def scope(self):
        kb = self

        class _S:
            def __enter__(s):
                s.old = kb.es
                s.base = kb.nalloc
                kb.es = ExitStack()
                kb.scopes = getattr(kb, "scopes", [])
                kb.scopes.append(kb.es)

            def __exit__(s, *a):
                if getattr(kb, "finished", False):
                    return
                kb.barrier()
                kb.es.close()
                kb.scopes.pop()
                kb.es = s.old
                kb.nalloc = s.base
        return _S()

    def finish(self, bufs):
        for b in bufs:
            if b.w is not None:
                self._wait("sync", b.w[0], b.w[1])
            for k, v in b.r.items():
                self._wait("sync", k, v)


class Pool:
    def __init__(self, kb, name, shape, dt, n, psum=False):
        self.tiles = []
        for i in range(n):
            t = (kb.ps if psum else kb.sb)("%s%d" % (name, i), shape, dt)
            self.tiles.append((t, kb.buf("%s%d" % (name, i))))
        self.i = 0

    def get(self):
        t = self.tiles[self.i % len(self.tiles)]
        self.i += 1
        return t


def _rev(ap2):
    return ap2[:, ::-1]


class Builder:
    def __init__(self, dbg=(), stop=None, same=True):
        self.dbg = set(dbg)
        self.stop = stop
        self.nc = bass.Bass("TRN2", target_bir_lowering=False)
        self.kb = KB(self.nc, same_engine_sync=same)
        self.ins = {}
        self.outs = {}
        self.fin = []

    def inp(self, name, shape):
        t = self.nc.dram_tensor(name, list(shape), F32, kind="ExternalInput").ap()
        self.ins[name] = t
        return t

    def outp(self, name, shape):
        t = self.nc.dram_tensor(name, list(shape), F32, kind="ExternalOutput").ap()
        self.outs[name] = t
        return t

    def dump(self, name, tile_ap, b, shape):
        if name not in self.dbg:
            return
        o = self.outp("dbg_" + name, shape)
        bo = self.kb.buf("dbg_" + name)
        self.kb.dma("sync", o, tile_ap, reads=[b], writes=[bo])
        self.fin.append(bo)

    def dump_bf(self, name, tile_ap, b, shape):
        if name not in self.dbg:
            return
        kb = self.kb
        t = kb.sb("dbgt_" + name, shape, F32)
        bt = kb.buf("dbgt_" + name)
        kb.op("vector", lambda e: e.tensor_copy(out=t[:], in_=tile_ap), reads=[b], writes=[bt])
        self.dump(name, t[:], bt, shape)

    def declare(self):
        i = self.inp
        self.x = i("x", [L, D])
        self.ctx = i("ctx", [CTX, D])
        self.cs_in = i("cs", [128, 8, 2])
        self.w_mod = i("w_mod", [D, 6 * D])
        self.b_mod_t = i("b_mod_t", [128, 48])
        self.b_mod = i("b_mod", [6 * D])
        self.norm1_t = i("norm1_t", [128, 8])
        self.norm2_t = i("norm2_t", [128, 8])
        self.w_in = i("w_in", [D, IN_COLS])
        self.ident = i("ident", [128, 128])
        self.s5_d_t = i("s5_d_t", [128, 4])
        self.lam_re_t = i("lam_re_t", [128, 32])
        self.lam_im_t = i("lam_im_t", [128, 32])
        self.logstep_t = i("logstep_t", [128, 32])
        self.Bblk = i("Bblk", [128, 2, 32, 32])
        self.Cblk = i("Cblk", [128, 2, 32, 32])
        self.iota1 = i("iota1", [128, TS5])
        self.gmask = i("gmask", [128, 11, 128])
        self.conv_t = i("conv_t", [128, 12, 5])
        self.alog_dtb = i("alog_dtb", [16])
        self.gdn_norm = i("gdn_norm", [128])
        self.w_glu = i("w_glu", [512, 512])
        self.b_glu_t = i("b_glu_t", [128, 4])
        self.w_ba = i("w_ba", [512, D])
        self.w_bb = i("w_bb", [512, D])
        self.w_out = i("w_out", [D, D])
        self.w_router = i("w_router", [D, NE])
        self.b_router = i("b_router", [NE])
        self.w_gu = i("w_gu", [NE, D, 2 * D])
        self.b_gu_t = i("b_gu_t", [128, NE, 16])
        self.w_dn = i("w_dn", [NE, D, D])
        self.b_dn = i("b_dn", [NE, D])
        self.norm_f = i("norm_f", [D])
        self.out = self.outp("out", [L, D])
        self.x1s = self.nc.dram_tensor("x1_scratch", [L, D], F32, kind="Internal").ap()

    def common(self):
        kb = self.kb
        self.PS = Pool(kb, "ps", [128, 512], F32, 8, psum=True)
        self.modT = kb.sb("modT", [128, 32, 2], F32)
        self.b_modT = kb.buf("modT")
        self.gt1_bc = kb.sb("gt1_bc", [128, D], F32)
        self.gt2_bc = kb.sb("gt2_bc", [128, D], F32)
        self.b_gt1 = kb.buf("gt1")
        self.b_gt2 = kb.buf("gt2")
        self.A1 = kb.sb("A1", [128, 8, 2], F32)
        self.b_A1 = kb.buf("A1")
        self.identf = kb.sb("identf", [128, 128], F32)
        self.b_ident = kb.buf("ident")
        kb.dma("sync", self.identf[:], self.ident, writes=[self.b_ident])

    def phase_mod(self):
        kb, nc = self.kb, self.nc
        cs = kb.sb("cs", [128, 8, 2], F32)
        b_cs = kb.buf("cs")
        kb.dma("sync", cs[:], self.cs_in, writes=[b_cs])
        sg = kb.sb("cs_sg", [128, 8, 2], F32)
        b_sg = kb.buf("cs_sg")
        kb.op("scalar", lambda e: e.activation(out=sg[:], in_=cs[:], func=AF.Sigmoid), reads=[b_cs], writes=[b_sg])
        css = kb.sb("css", [128, 8, 2], F32)
        b_css = kb.buf("css")
        kb.op("vector", lambda e: e.tensor_tensor(out=css[:], in0=cs[:], in1=sg[:], op=ALU.mult), reads=[b_cs, b_sg], writes=[b_css])
        csb = kb.sb("csb", [128, 8, 128], F32)
        b_csb = kb.buf("csb")
        kb.op("vector", lambda e: e.tensor_copy(out=csb[:], in_=css[:, :, 0:1].to_broadcast([128, 8, 128])), reads=[b_css], writes=[b_csb])
        bmt = kb.sb("bmt", [128, 48], F32)
        b_bmt = kb.buf("bmt")
        kb.dma("sync", bmt[:], self.b_mod_t, writes=[b_bmt])
        wpool = Pool(kb, "wmod", [128, 8, 512], F32, 2)
        wv = self.w_mod.rearrange("(kc p) n -> p kc n", p=128)
        pm, b_pm = self.PS.get()
        fm_groups = [0, 1, 2, 3, 6, 7, 8, 9]
        for gi, g in enumerate(fm_groups):
            wt, b_wt = wpool.get()
            kb.dma("sync" if gi % 2 == 0 else "scalar", wt[:], wv[:, :, 512 * g:512 * (g + 1)], writes=[b_wt])
            for cc in range(4):
                j = gi * 4 + cc
                for kc in range(8):
                    kb.op("tensor", lambda e, j=j, kc=kc, cc=cc, wt=wt: e.matmul(
                        pm[:, 2 * j:2 * j + 2], wt[:, kc, cc * 128:(cc + 1) * 128], css[:, kc, :],
                        start=(kc == 0), stop=(kc == 7)), reads=[b_wt, b_css], writes=[b_pm], sig=(kc == 7))
        kb.op("vector", lambda e: e.tensor_tensor(
            out=self.modT[:], in0=pm[:, 0:64].rearrange("p (j t) -> p j t", t=2),
            in1=self._bmt_sel(bmt), op=ALU.add), reads=[b_pm, b_bmt], writes=[self.b_modT])
        for which, (g0, dst, bdst) in enumerate([(4, self.gt1_bc, self.b_gt1), (10, self.gt2_bc, self.b_gt2)]):
            bb = kb.sb("bmodbc%d" % which, [128, D], F32)
            b_bb = kb.buf("bmodbc")
            kb.dma("sync", bb[:], self.b_mod[512 * g0:512 * g0 + D].partition_broadcast(128), writes=[b_bb])
            for half in range(2):
                g = g0 + half
                wt, b_wt = wpool.get()
                kb.dma("sync" if half == 0 else "scalar", wt[:], wv[:, :, 512 * g:512 * (g + 1)], writes=[b_wt])
                pg, b_pg = self.PS.get()
                for kc in range(8):
                    kb.op("tensor", lambda e, kc=kc, wt=wt, pg=pg: e.matmul(
                        pg[:], csb[:, kc, :], wt[:, kc, :], start=(kc == 0), stop=(kc == 7)),
                        reads=[b_wt, b_csb], writes=[b_pg], sig=(kc == 7))
                kb.op("vector", lambda e, pg=pg, half=half, dst=dst, bb=bb: e.tensor_tensor(
                    out=dst[:, 512 * half:512 * (half + 1)], in0=pg[:], in1=bb[:, 512 * half:512 * (half + 1)], op=ALU.add),
                    reads=[b_pg, b_bb], writes=[bdst])
        self.dump("modT", self.modT[:], self.b_modT, [128, 32, 2])
        self.dump("gt1", self.gt1_bc[:], self.b_gt1, [128, D])

    def _bmt_sel(self, bmt):
        return bmt[:, 0:32].unsqueeze(2).to_broadcast([128, 32, 2])

    def norm_to_T(self, src_tiles, dstT, b_dstT_blocks, scale_ap_fn, shift_ap_fn, tile_base, tag):
        raise NotImplementedError

    def phase_norm1(self):
        kb = self.kb
        n1 = kb.sb("norm1", [128, 8], F32)
        b_n1 = kb.buf("n1")
        kb.dma("sync", n1[:], self.norm1_t, writes=[b_n1])
        kb.op("vector", lambda e: e.scalar_tensor_tensor(
            out=self.A1[:], in0=self.modT[:, 8:16, :], scalar=1.0, in1=n1[:].unsqueeze(2).to_broadcast([128, 8, 2]),
            op0=ALU.add, op1=ALU.mult), reads=[self.b_modT, b_n1], writes=[self.b_A1])
        xin = Pool(kb, "xin", [128, D], F32, 3)
        xnp = Pool(kb, "xn", [128, D], F32, 2)
        junk = Pool(kb, "junk", [128, D], F32, 1)
        stat = Pool(kb, "stat", [128, 4], F32, 4)
        for tt in range(NT):
            which = 1 if tt < 2 else 0
            src = self.ctx[tt * 128:(tt + 1) * 128, :] if tt < 2 else self.x[(tt - 2) * 128:(tt - 1) * 128, :]
            xt, b_xt = xin.get()
            kb.dma("sync" if tt % 2 == 0 else "scalar", xt[:], src, writes=[b_xt])
            self._norm_tile(xt, b_xt, tt, which, self.A1, self.b_A1, 0, xnp, junk, stat, self.hT, self.b_hT[tt])
        self.dump_bf("hT", self.hT[:, :, 0:512], self.b_hT[3], [128, 8, 512]) if False else None
        if "hT" in self.dbg:
            allb = kb.buf("hTall")
            t = kb.sb("dbg_hT_t", [128, 8, 384], F32)
            kb.op("vector", lambda e: e.tensor_copy(out=t[:], in_=self.hT[:, :, 128:512]), reads=self.b_hT[1:4], writes=[allb])
            self.dump("hT", t[:], allb, [128, 8, 384])

    def _norm_tile(self, xt, b_xt, tt, which, A, b_A, shift_chunk0, xnp, junk, stat, dstT, b_dst):
        kb = self.kb
        jt, b_jt = junk.get()
        st, b_st = stat.get()
        kb.op("scalar", lambda e: e.activation(out=jt[:], in_=xt[:], func=AF.Square, accum_out=st[:, 0:1]),
              reads=[b_xt], writes=[b_jt, b_st])
        kb.op("scalar", lambda e: e.activation(out=st[:, 1:2], in_=st[:, 0:1], func=AF.Sqrt, bias=EPS, scale=1.0 / D),
              reads=[b_st], writes=[b_st])
        kb.op("vector", lambda e: e.reciprocal(out=st[:, 2:3], in_=st[:, 1:2]), reads=[b_st], writes=[b_st])
        xn, b_xn = xnp.get()
        kb.op("scalar", lambda e: e.activation(out=xn[:], in_=xt[:], func=AF.Identity, scale=st[:, 2:3]),
              reads=[b_xt, b_st], writes=[b_xn])
        for half in range(2):
            pt, b_pt = self.PS.get()
            for q in range(4):
                kc = half * 4 + q
                kb.op("tensor", lambda e, kc=kc, q=q, pt=pt: e.transpose(
                    pt[:, q * 128:(q + 1) * 128], xn[:, kc * 128:(kc + 1) * 128], self.identf[:]),
                    reads=[b_xn, self.b_ident], writes=[b_pt])
            for q in range(4):
                kc = half * 4 + q
                kb.op("vector", lambda e, kc=kc, q=q, pt=pt: e.tensor_scalar(
                    out=dstT[:, kc, tt * 128:(tt + 1) * 128], in0=pt[:, q * 128:(q + 1) * 128],
                    scalar1=A[:, kc, which:which + 1], scalar2=self.modT[:, shift_chunk0 + kc, which:which + 1],
                    op0=ALU.mult, op1=ALU.add), reads=[b_pt, b_A, self.b_modT], writes=[b_dst])

    def load_w_bf16(self, dst, b_dst, src_rows_ap, ncols, col0=0):
        kb = self.kb
        kcs = dst.shape[1]
        v = src_rows_ap.rearrange("(kc p) n -> p kc n", p=128)
        for kc in range(kcs):
            kb.dma("gpsimd", dst[:, kc, :], v[:, kc, col0:col0 + ncols], writes=[b_dst])

    def phase_u(self):
        kb = self.kb
        wu = kb.sb("wu", [128, 8, 512], BF16)
        b_wu = kb.buf("wu")
        self.load_w_bf16(wu, b_wu, self.w_in, 512, OFF_U)
        dsk = kb.sb("s5d", [128, 4], F32)
        b_dsk = kb.buf("s5d")
        kb.dma("sync", dsk[:], self.s5_d_t, writes=[b_dsk])
        for oc in range(4):
            for (s0, n) in BLOCKS:
                pt, b_pt = self.PS.get()
                tiles = range(s0 // 128, (s0 + n) // 128)
                for kc in range(8):
                    kb.op("tensor", lambda e, kc=kc, pt=pt, oc=oc, s0=s0, n=n: e.matmul(
                        pt[:, 0:n], wu[:, kc, oc * 128:(oc + 1) * 128], self.hT[:, kc, s0:s0 + n],
                        start=(kc == 0), stop=(kc == 7)), reads=[b_wu] + [self.b_hT[t] for t in tiles], writes=[b_pt], sig=(kc == 7))
                kb.op("scalar", lambda e, pt=pt, oc=oc, s0=s0, n=n: e.activation(
                    out=self.uT[:, oc, s0:s0 + n], in_=pt[:, 0:n], func=AF.Identity), reads=[b_pt], writes=[self.b_uT])
                if s0 >= CTX:
                    kb.op("scalar", lambda e, pt=pt, oc=oc, s0=s0, n=n: e.activation(
                        out=self.yT[:, oc, s0 - CTX:s0 - CTX + n], in_=pt[:, 0:n], func=AF.Identity, scale=dsk[:, oc:oc + 1]),
                        reads=[b_pt, b_dsk], writes=[self.b_yT[oc]])
        if "uT" in self.dbg:
            t = kb.sb("dbg_uT_t", [128, 4, 512], F32)
            bt = kb.buf("dbg_uT")
            kb.op("vector", lambda e: e.tensor_copy(out=t[:], in_=self.uT[:, :, 0:512]), reads=[self.b_uT], writes=[bt])
            self.dump("uT", t[:], bt, [128, 4, 512])


    def sincos(self, ang, b_ang, n, want_cos, out_ap, b_out, scale=1.0):
        kb = self.kb
        with kb.scope():
            y = kb.sb("sc_y", [128, n], F32)
            ki = kb.sb("sc_k", [128, n], I32)
            kf = kb.sb("sc_kf", [128, n], F32)
            b = kb.buf("sc")
            off = 0.75 if want_cos else 0.5
            kb.op("vector", lambda e: e.tensor_scalar(out=y[:], in0=ang, scalar1=1.0 / (2 * np.pi), scalar2=off + 64.0,
                                                     op0=ALU.mult, op1=ALU.add), reads=[b_ang], writes=[b])
            kb.op("vector", lambda e: e.tensor_copy(out=ki[:], in_=y[:]), reads=[b], writes=[b])
            kb.op("vector", lambda e: e.tensor_copy(out=kf[:], in_=ki[:]), reads=[b], writes=[b])
            kb.op("vector", lambda e: e.tensor_tensor(out=y[:], in0=y[:], in1=kf[:], op=ALU.subtract), reads=[b], writes=[b])
            kb.op("vector", lambda e: e.scalar_tensor_tensor(out=kf[:], in0=y[:], scalar=0.0, in1=y[:], op0=ALU.is_lt, op1=ALU.add),
                  reads=[b], writes=[b])
            kb.op("scalar", lambda e: e.activation(out=y[:], in_=kf[:], func=AF.Sin, scale=6.2831, bias=-3.14155),
                  reads=[b], writes=[b])
            yv = y[:] if len(out_ap.shape) == 2 else y[:].rearrange("p (a t) -> p a t", t=out_ap.shape[-1])
            kb.op("scalar", lambda e: e.activation(out=out_ap, in_=yv, func=AF.Identity, scale=float(scale)),
                  reads=[b], writes=[b_out])

    def phase_s5_setup(self):
        kb = self.kb
        T = TS5
        self.rho = kb.sb("s5_rho", [128, 32], F32)
        self.Bdrv = kb.sb("Bdrv", [128, 2, 4, 2, 128], BF16)
        self.b_Bdrv = kb.buf("Bdrv")
        self.Crd = kb.sb("Crd", [128, 32, 2, 32], BF16)
        self.b_Crd = kb.buf("Crd")
        self.Tc = kb.sb("Tc", [128, 32 * T], BF16)
        self.b_Tc = kb.buf("Tc")
        self.Tsn = kb.sb("Tsn", [128, 32, 2, T], BF16)
        self.b_Tsn = kb.buf("Tsn")
        self.b_s5c = kb.buf("s5setup")
        with kb.scope():
            self._s5_setup_body()

    def _s5_setup_body(self):
        kb = self.kb
        T = TS5
        ld = lambda name, src, shape: self._ld(name, src, shape)
        lre, b_lre = ld("lre", self.lam_re_t, [128, 32])
        lim, b_lim = ld("lim", self.lam_im_t, [128, 32])
        lst, b_lst = ld("lst", self.logstep_t, [128, 32])
        Bb, b_Bb = ld("Bblk", self.Bblk, [128, 2, 32, 32])
        Cb, b_Cb = ld("Cblk", self.Cblk, [128, 2, 32, 32])
        io, b_io = ld("iota1", self.iota1, [128, T])
        V = lambda name: (kb.sb("s5v_" + name, [128, 32], F32))
        b = self.b_s5c
        deps = [b_lre, b_lim, b_lst, b]
        mag = self.rho
        lr, dt, ang, ar, ai, den, am1, fr, fi, t1, t2 = [V(n) for n in
            ("lr", "dt", "ang", "ar", "ai", "den", "am1", "fr", "fi", "t1", "t2")]
        v = lambda fn: kb.op("vector", fn, reads=deps, writes=[b])
        a = lambda fn: kb.op("scalar", fn, reads=deps, writes=[b])
        v(lambda e: e.tensor_scalar(out=lr[:], in0=lre[:], scalar1=-1e-4, scalar2=0.0, op0=ALU.min, op1=ALU.add))
        a(lambda e: e.activation(out=dt[:], in_=lst[:], func=AF.Exp))
        v(lambda e: e.tensor_tensor(out=t1[:], in0=lr[:], in1=dt[:], op=ALU.mult))
        a(lambda e: e.activation(out=mag[:], in_=t1[:], func=AF.Exp))
        v(lambda e: e.tensor_tensor(out=ang[:], in0=lim[:], in1=dt[:], op=ALU.mult))
        sn = V("sn0"); cs = V("cs0")
        self.sincos(ang[:], b, 32, False, sn[:], b)
        self.sincos(ang[:], b, 32, True, cs[:], b)
        v(lambda e: e.tensor_tensor(out=ar[:], in0=mag[:], in1=cs[:], op=ALU.mult))
        v(lambda e: e.tensor_tensor(out=ai[:], in0=mag[:], in1=sn[:], op=ALU.mult))
        v(lambda e: e.tensor_tensor(out=t1[:], in0=lr[:], in1=lr[:], op=ALU.mult))
        v(lambda e: e.tensor_tensor(out=t2[:], in0=lim[:], in1=lim[:], op=ALU.mult))
        v(lambda e: e.tensor_tensor(out=den[:], in0=t1[:], in1=t2[:], op=ALU.add))
        v(lambda e: e.reciprocal(out=den[:], in_=den[:]))
        v(lambda e: e.tensor_scalar(out=am1[:], in0=ar[:], scalar1=-1.0, scalar2=0.0, op0=ALU.add, op1=ALU.add))
        v(lambda e: e.tensor_tensor(out=t1[:], in0=am1[:], in1=lr[:], op=ALU.mult))
        v(lambda e: e.tensor_tensor(out=t2[:], in0=ai[:], in1=lim[:], op=ALU.mult))
        v(lambda e: e.tensor_tensor(out=fr[:], in0=t1[:], in1=t2[:], op=ALU.add))
        v(lambda e: e.tensor_tensor(out=fr[:], in0=fr[:], in1=den[:], op=ALU.mult))
        v(lambda e: e.tensor_tensor(out=t1[:], in0=ai[:], in1=lr[:], op=ALU.mult))
        v(lambda e: e.tensor_tensor(out=t2[:], in0=am1[:], in1=lim[:], op=ALU.mult))
        v(lambda e: e.tensor_tensor(out=fi[:], in0=t1[:], in1=t2[:], op=ALU.subtract))
        v(lambda e: e.tensor_tensor(out=fi[:], in0=fi[:], in1=den[:], op=ALU.mult))
        Bbar = kb.sb("Bbar", [128, 2, 32, 32], F32)
        tb = kb.sb("Bbar_t", [128, 32, 32], F32)
        b_Bbar = kb.buf("Bbar")
        frb = fr[:].unsqueeze(2).to_broadcast([128, 32, 32])
        fib = fi[:].unsqueeze(2).to_broadcast([128, 32, 32])
        vb = lambda fn: kb.op("vector", fn, reads=[b, b_Bb], writes=[b_Bbar])
        vb(lambda e: e.tensor_tensor(out=Bbar[:, 0], in0=Bb[:, 0], in1=frb, op=ALU.mult))
        vb(lambda e: e.tensor_tensor(out=tb[:], in0=Bb[:, 1], in1=fib, op=ALU.mult))
        vb(lambda e: e.tensor_tensor(out=Bbar[:, 0], in0=Bbar[:, 0], in1=tb[:], op=ALU.subtract))
        vb(lambda e: e.tensor_tensor(out=Bbar[:, 1], in0=Bb[:, 1], in1=frb, op=ALU.mult))
        vb(lambda e: e.tensor_tensor(out=tb[:], in0=Bb[:, 0], in1=fib, op=ALU.mult))
        vb(lambda e: e.tensor_tensor(out=Bbar[:, 1], in0=Bbar[:, 1], in1=tb[:], op=ALU.add))
        for d in range(2):
            for qd in range(4):
                pt, b_pt = self.PS.get()
                for ri in range(2):
                    blk = (d * 4 + qd) * 4
                    kb.op("tensor", lambda e, ri=ri, blk=blk, pt=pt: e.transpose(
                        pt[:, ri * 128:(ri + 1) * 128], Bbar[:, ri, blk:blk + 4, :], self.identf[:]),
                        reads=[b_Bbar, self.b_ident], writes=[b_pt])
                kb.op("scalar", lambda e, d=d, qd=qd, pt=pt: e.activation(
                    out=self.Bdrv[:, d, qd, :, :], in_=pt[:, 0:256].rearrange("p (r n) -> p r n", r=2), func=AF.Identity),
                    reads=[b_pt], writes=[self.b_Bdrv])
        kb.op("scalar", lambda e: e.activation(out=self.Crd[:, :, 0, :], in_=Cb[:, 0], func=AF.Identity),
              reads=[b_Cb], writes=[self.b_Crd])
        kb.op("scalar", lambda e: e.activation(out=self.Crd[:, :, 1, :], in_=Cb[:, 1], func=AF.Identity, scale=-1.0),
              reads=[b_Cb], writes=[self.b_Crd])
        ph = kb.sb("s5_ph", [128, 32, T], F32)
        b_ph = kb.buf("s5ph")
        kb.op("vector", lambda e: e.tensor_tensor(out=ph[:], in0=ang[:].unsqueeze(2).to_broadcast([128, 32, T]),
                                                 in1=io[:].unsqueeze(1).to_broadcast([128, 32, T]), op=ALU.mult),
              reads=[b, b_io], writes=[b_ph])
        for a0 in range(0, 32, 8):
            pha = ph[:, a0:a0 + 8, :].rearrange("p a t -> p (a t)")
            self.sincos(pha, b_ph, 8 * T, True, self.Tc[:, a0 * T:(a0 + 8) * T], self.b_Tc)
            self.sincos(pha, b_ph, 8 * T, False, self.Tsn[:, a0:a0 + 8, 0, :], self.b_Tsn)
            self.sincos(pha, b_ph, 8 * T, False, self.Tsn[:, a0:a0 + 8, 1, :], self.b_Tsn, scale=-1.0)
        self.dump("rho", self.rho[:], b, [128, 32])
        self.dump("fr", fr[:], b, [128, 32])
        self.dump("fi", fi[:], b, [128, 32])
        self.dump("Tc", self.Tc[:, 0:4 * T], self.b_Tc, [128, 4 * T])

    def _ld(self, name, src, shape, dt=F32, q="sync"):
        t = self.kb.sb(name, shape, dt)
        b = self.kb.buf(name)
        self.kb.dma(q, t[:], src, writes=[b])
        return t, b

    def phase_s5(self):
        kb = self.kb
        T = TS5
        NCK = TOK // T
        Tc3 = self.Tc[:].rearrange("p (a t) -> p a t", t=T)
        G = kb.sb("s5_G", [128, 32, 2, T], F32)
        b_G = [kb.buf("s5G%d" % i) for i in range(32)]
        carry = kb.sb("s5_carry", [128, 32, 2], F32)
        b_carry = kb.buf("s5carry")
        kb.op("vector", lambda e: e.memset(carry[:], 0.0), writes=[b_carry])
        P1p = Pool(kb, "s5P1", [128, 2, T], F32, 2)
        P2p = Pool(kb, "s5P2", [128, 2, T], F32, 2)
        Vp = Pool(kb, "s5V", [128, 2, T], F32, 2)
        Hp = Pool(kb, "s5H", [128, 2, T], BF16, 3)
        ctmp = kb.sb("s5_ctmp", [128, 32, 2], F32)
        ctmp2 = kb.sb("s5_ctmp2", [128, 32, 2], F32)
        tabs = [self.b_Tc, self.b_Tsn]
        for ck in range(NCK):
            is_lat = ck >= CTX // T
            for qd in range(4):
                for d in range(2):
                    if is_lat:
                        py, b_py = self.PS.get()
                    for ppq in range(4):
                        pd = d * 16 + qd * 4 + ppq
                        if d == 0:
                            s0 = ck * T
                            rhs = self.uT[ppq * 32:(ppq + 1) * 32, qd, s0:s0 + T]
                        else:
                            if not is_lat:
                                s0 = CTX - (ck + 1) * T
                            else:
                                s0 = TOK - (ck - CTX // T + 1) * T
                            rhs = self.uT[ppq * 32:(ppq + 1) * 32, qd, s0:s0 + T][:, ::-1]
                        pdv, b_pdv = self.PS.get()
                        for ri in range(2):
                            kb.op("tensor", lambda e, ri=ri, pdv=pdv, rhs=rhs, d=d, qd=qd, ppq=ppq: e.matmul(
                                pdv[:, ri * T:(ri + 1) * T], self.Bdrv[ppq * 32:(ppq + 1) * 32, d, qd, ri, :], rhs,
                                start=True, stop=True, tile_position=(ppq * 32, 0)),
                                reads=[self.b_Bdrv, self.b_uT], writes=[b_pdv])
                        Dv = pdv[:, 0:2 * T].rearrange("p (r t) -> p r t", r=2)
                        cosb = Tc3[:, pd:pd + 1, :].to_broadcast([128, 2, T])
                        P1, b_P1 = P1p.get()
                        P2, b_P2 = P2p.get()
                        Vt, b_V = Vp.get()
                        kb.op("vector", lambda e, P1=P1, Dv=Dv, cosb=cosb: e.tensor_tensor(out=P1[:], in0=Dv, in1=cosb, op=ALU.mult),
                              reads=[b_pdv] + tabs, writes=[b_P1])
                        kb.op("vector", lambda e, P2=P2, Dv=Dv, pd=pd: e.tensor_tensor(out=P2[:], in0=Dv[:, ::-1, :], in1=self.Tsn[:, pd], op=ALU.mult),
                              reads=[b_pdv] + tabs, writes=[b_P2])
                        kb.op("vector", lambda e, P1=P1, P2=P2, Vt=Vt: e.tensor_tensor(out=Vt[:], in0=P1[:], in1=P2[:], op=ALU.add),
                              reads=[b_P1, b_P2], writes=[b_V])
                        for ri in range(2):
                            kb.op("vector", lambda e, ri=ri, Vt=Vt, pd=pd: e.tensor_tensor_scan(
                                out=G[:, pd, ri, :], data0=self.rho[:, pd:pd + 1].to_broadcast([128, T]), data1=Vt[:, ri, :],
                                initial=carry[:, pd, ri:ri + 1], op0=ALU.mult, op1=ALU.add),
                                reads=[b_V, b_carry, self.b_s5c], writes=[b_G[pd]])
                        if is_lat:
                            P1, b_P1 = P1p.get()
                            P2, b_P2 = P2p.get()
                            Ht, b_H = Hp.get()
                            kb.op("vector", lambda e, P1=P1, pd=pd, cosb=cosb: e.tensor_tensor(out=P1[:], in0=G[:, pd], in1=cosb, op=ALU.mult),
                                  reads=[b_G[pd]] + tabs, writes=[b_P1])
                            kb.op("vector", lambda e, P2=P2, pd=pd: e.tensor_tensor(out=P2[:], in0=G[:, pd, ::-1, :], in1=self.Tsn[:, pd], op=ALU.mult),
                                  reads=[b_G[pd]] + tabs, writes=[b_P2])
                            kb.op("vector", lambda e, P1=P1, P2=P2, Ht=Ht: e.tensor_tensor(out=Ht[:], in0=P1[:], in1=P2[:], op=ALU.subtract),
                                  reads=[b_P1, b_P2], writes=[b_H])
                            for ri in range(2):
                                kb.op("tensor", lambda e, ri=ri, Ht=Ht, pd=pd, ppq=ppq, py=py: e.matmul(
                                    py[ppq * 32:(ppq + 1) * 32, 0:T], self.Crd[:, pd, ri, :], Ht[:, ri, :],
                                    start=(ri == 0), stop=(ri == 1), tile_position=(0, ppq * 32)),
                                    reads=[self.b_Crd, b_H], writes=[b_py])
                    if is_lat:
                        l0 = s0 - CTX
                        src = py[:, 0:T] if d == 0 else py[:, 0:T][:, ::-1]
                        kb.op("vector", lambda e, src=src, qd=qd, l0=l0: e.tensor_tensor(
                            out=self.yT[:, qd, l0:l0 + T], in0=src, in1=self.yT[:, qd, l0:l0 + T], op=ALU.add),
                            reads=[b_py], writes=[self.b_yT[qd]])
            if ck < NCK - 1:
                Gl = G[:, :, :, T - 1]
                cl = Tc3[:, :, T - 1:T].to_broadcast([128, 32, 2])
                sl = self.Tsn[:, :, :, T - 1]
                kb.op("vector", lambda e, Gl=Gl, cl=cl: e.tensor_tensor(out=ctmp[:], in0=Gl, in1=cl, op=ALU.mult),
                      reads=b_G + tabs, writes=[b_carry])
                kb.op("vector", lambda e, Gl=Gl, sl=sl: e.tensor_tensor(out=ctmp2[:], in0=Gl[:, :, ::-1], in1=sl, op=ALU.mult),
                      reads=b_G + tabs, writes=[b_carry])
                kb.op("vector", lambda e: e.tensor_tensor(out=carry[:], in0=ctmp[:], in1=ctmp2[:], op=ALU.subtract),
                      reads=[b_carry], writes=[b_carry])
        if "yT" in self.dbg:
            allb = kb.buf("yTall")
            t = kb.sb("dbg_yT_t", [128, 4, 512], F32)
            kb.op("vector", lambda e: e.tensor_copy(out=t[:], in_=self.yT[:, :, 0:512]), reads=self.b_yT, writes=[allb])
            self.dump("yT", t[:], allb, [128, 4, 512])
            t2 = kb.sb("dbg_yT_t2", [128, 4, 512], F32)
            allb2 = kb.buf("yTall2")
            kb.op("vector", lambda e: e.tensor_copy(out=t2[:], in_=self.yT[:, :, 1536:2048]), reads=self.b_yT, writes=[allb2])
            self.dbg.add("yT2")
            self.dump("yT2", t2[:], allb2, [128, 4, 512])


    def phase_gdn_setup(self):
        kb = self.kb
        self.gm, self.b_gm = self._ld("gmask", self.gmask, [128, 11, 128])
        self.cw, self.b_cw = self._ld("convw", self.conv_t, [128, 12, 5])
        self.beta = kb.sb("g_beta", [128, NT, 8], F32)
        self.gg = kb.sb("g_g", [128, NT, 8], F32)
        self.E1 = kb.sb("g_E1", [128, NT, 8], F32)
        self.E2 = kb.sb("g_E2", [128, NT, 8], F32)
        self.DL = kb.sb("g_DL", [128, 2, NT, 8], F32)
        self.b_gs = kb.buf("gscal")
        b = self.b_gs
        with kb.scope():
            wab = kb.sb("wab", [128, 8, 16], BF16)
            b_wab = kb.buf("wab")
            self.load_w_bf16(wab, b_wab, self.w_in, 16, OFF_B)
            ab = kb.sb("alogdtb", [128, 16], F32)
            b_ab = kb.buf("alogdtb")
            kb.dma("sync", ab[:], self.alog_dtb.partition_broadcast(128), writes=[b_ab])
            pz, b_pz = self.PS.get()
            for tt in range(NT):
                for kc in range(8):
                    kb.op("tensor", lambda e, tt=tt, kc=kc: e.matmul(
                        pz[:, tt * 16:(tt + 1) * 16], self.hT[:, kc, tt * 128:(tt + 1) * 128], wab[:, kc, :],
                        start=(kc == 0), stop=(kc == 7)), reads=[b_wab, self.b_hT[tt]], writes=[b_pz], sig=(kc == 7))
            zab = pz[:, 0:NT * 16].rearrange("p (t c) -> p t c", c=16)
            kb.op("scalar", lambda e: e.activation(out=self.beta[:], in_=zab[:, :, 0:8], func=AF.Sigmoid), reads=[b_pz], writes=[b])
            if self.stop == "gs1":
                return
            t1 = kb.sb("g_t1", [128, NT, 8], F32)
            ea = kb.sb("g_ea", [128, 8], F32)
            kb.op("vector", lambda e: e.tensor_tensor(out=t1[:], in0=zab[:, :, 8:16], in1=ab[:, 8:16].unsqueeze(1).to_broadcast([128, NT, 8]),
                                                     op=ALU.add), reads=[b_pz, b_ab], writes=[b])
            kb.op("scalar", lambda e: e.activation(out=t1[:], in_=t1[:], func=AF.Exp), reads=[b], writes=[b])
            kb.op("scalar", lambda e: e.activation(out=t1[:], in_=t1[:], func=AF.Ln, bias=1.0), reads=[b], writes=[b])
            kb.op("scalar", lambda e: e.activation(out=ea[:], in_=ab[:, 0:8], func=AF.Exp), reads=[b_ab], writes=[b])
            kb.op("vector", lambda e: e.scalar_tensor_tensor(out=self.gg[:], in0=t1[:], scalar=-1.0,
                                                            in1=ea[:].unsqueeze(1).to_broadcast([128, NT, 8]), op0=ALU.mult, op1=ALU.mult),
                  reads=[b], writes=[b])
            if self.stop == "gs2":
                return
            gflat = self.gg[:].rearrange("p t x -> p (t x)")
            gc = kb.sb("g_gcum", [128, 2, NT, 8], F32)
            for d in range(2):
                pc, b_pc = self.PS.get()
                kb.op("tensor", lambda e, d=d, pc=pc: e.matmul(pc[:, 0:NT * 8], self.gm[:, d, :], gflat, start=True, stop=True),
                      reads=[b, self.b_gm], writes=[b_pc])
                kb.op("vector", lambda e, d=d, pc=pc: e.tensor_copy(out=gc[:, d].rearrange("p t x -> p (t x)"), in_=pc[:, 0:NT * 8]),
                      reads=[b_pc], writes=[b])
            if self.stop == "gs3":
                return
            pl, b_pl = self.PS.get()
            kb.op("tensor", lambda e: e.matmul(pl[:, 0:NT * 8], self.gm[:, 7, :], gflat, start=True, stop=True),
                  reads=[b, self.b_gm], writes=[b_pl])
            glv = pl[:, 0:NT * 8].rearrange("p (t x) -> p t x", x=8)
            for d in range(2):
                xs = slice(d * 4, d * 4 + 4)
                kb.op("scalar", lambda e, d=d, xs=xs: e.activation(out=self.E1[:, :, xs], in_=gc[:, d, :, xs], func=AF.Exp),
                      reads=[b], writes=[b])
                kb.op("vector", lambda e, d=d, xs=xs: e.tensor_tensor(out=t1[:, :, xs], in0=glv[:, :, xs], in1=gc[:, d, :, xs], op=ALU.subtract),
                      reads=[b, b_pl], writes=[b])
                kb.op("scalar", lambda e, xs=xs: e.activation(out=self.E2[:, :, xs], in_=t1[:, :, xs], func=AF.Exp), reads=[b], writes=[b])
            if self.stop == "gs4":
                return
            for c2 in ([1] if self.stop == "gs7" else range(2)):
                ph, b_ph = self.PS.get()
                kb.op("tensor", lambda e, c2=c2, ph=ph: e.matmul(ph[:, 0:NT * 8], self.gm[:, 5 + c2, :], gflat, start=True, stop=True),
                      reads=[b, self.b_gm], writes=[b_ph])
                if self.stop == "gs5":
                    return
                kb.op("scalar", lambda e, c2=c2, ph=ph: e.activation(out=self.DL[:, c2].rearrange("p t x -> p (t x)"), in_=ph[:, 0:NT * 8],
                                                                      func=AF.Exp), reads=[b_ph], writes=[b])
                if self.stop == "gs6":
                    return
        self.dump("g_g", self.gg[:], b, [128, NT, 8])
        self.dump("g_E1", self.E1[:], b, [128, NT, 8])
        self.dump("g_E2", self.E2[:], b, [128, NT, 8])
        self.dump("g_DL", self.DL[:], b, [128, 2, NT, 8])

    def gdn_head_front(self, h):
        kb = self.kb
        self.qh = kb.sb("g_qh%d" % h, [128, NT, 128], F32)
        self.kh = kb.sb("g_kh%d" % h, [128, NT, 128], F32)
        self.vh = kb.sb("g_vh%d" % h, [128, NT, 128], F32)
        self.kT = kb.sb("g_kT%d" % h, [128, TOK], F32)
        self.qT = kb.sb("g_qT%d" % h, [128, TOK], F32)
        self.b_qh, self.b_kh, self.b_vh, self.b_kT, self.b_qT = [kb.buf("g_%s%d" % (n, h)) for n in ("qh", "kh", "vh", "kT", "qT")]
        with kb.scope():
            wq = kb.sb("g_wqkv", [128, 8, 3, 128], BF16)
            b_wq = kb.buf("g_wqkv")
            v = self.w_in.rearrange("(kc p) n -> p kc n", p=128)
            for c in range(3):
                for kc in range(8):
                    col = OFF_QKV + c * 512 + h * 128
                    kb.dma("gpsimd", wq[:, kc, c, :], v[:, kc, col:col + 128], writes=[b_wq])
            zb = kb.sb("g_z", [128, TOK], F32)
            acc = kb.sb("g_acc", [128, TOK], F32)
            b_z = kb.buf("g_z")
            b_acc = kb.buf("g_acc")
            dsts = [(self.qh, self.b_qh), (self.kh, self.b_kh), (self.vh, self.b_vh)]
            for c in range(3):
                ch = c * 4 + h
                for (s0, n) in BLOCKS:
                    pt, b_pt = self.PS.get()
                    tiles = range(s0 // 128, (s0 + n) // 128)
                    for kc in range(8):
                        kb.op("tensor", lambda e, kc=kc, pt=pt, c=c, s0=s0, n=n: e.matmul(
                            pt[:, 0:n], wq[:, kc, c, :], self.hT[:, kc, s0:s0 + n], start=(kc == 0), stop=(kc == 7)),
                            reads=[b_wq] + [self.b_hT[t] for t in tiles], writes=[b_pt], sig=(kc == 7))
                    kb.op("scalar", lambda e, pt=pt, s0=s0, n=n: e.activation(out=zb[:, s0:s0 + n], in_=pt[:, 0:n], func=AF.Identity),
                          reads=[b_pt], writes=[b_z])
                kb.op("vector", lambda e, ch=ch: e.tensor_scalar(out=acc[:], in0=zb[:], scalar1=self.cw[:, ch, 2:3], scalar2=0.0,
                                                                 op0=ALU.mult, op1=ALU.add), reads=[b_z, self.b_cw], writes=[b_acc])
                segs = [(zb[:, 0:CTX].rearrange("p (r w) -> p r w", w=CTX), acc[:, 0:CTX].rearrange("p (r w) -> p r w", w=CTX), CTX),
                        (zb[:, CTX:TOK].rearrange("p (r w) -> p r w", w=64), acc[:, CTX:TOK].rearrange("p (r w) -> p r w", w=64), 64)]
                for k in (0, 1, 3, 4):
                    sh = k - 2
                    for (zv, av, w) in segs:
                        lo, hi = max(0, -sh), w - max(0, sh)
                        kb.op("vector", lambda e, zv=zv, av=av, lo=lo, hi=hi, sh=sh, k=k, ch=ch: e.scalar_tensor_tensor(
                            out=av[:, :, lo:hi], in0=zv[:, :, lo + sh:hi + sh], scalar=self.cw[:, ch, k:k + 1], in1=av[:, :, lo:hi],
                            op0=ALU.mult, op1=ALU.add), reads=[b_z, b_acc, self.b_cw], writes=[b_acc])
                kb.op("scalar", lambda e: e.activation(out=zb[:], in_=acc[:], func=AF.Silu), reads=[b_acc], writes=[b_z])
                if c == 0 and h == 0:
                    self.dump("g_cq", zb[:, 0:512], b_z, [128, 512])
                dst, b_dst = dsts[c]
                for t0 in range(0, NT, 4):
                    nt = min(4, NT - t0)
                    pt, b_pt = self.PS.get()
                    for q in range(nt):
                        kb.op("tensor", lambda e, q=q, t0=t0, pt=pt: e.transpose(
                            pt[:, q * 128:(q + 1) * 128], zb[:, (t0 + q) * 128:(t0 + q + 1) * 128], self.identf[:]),
                            reads=[b_z, self.b_ident], writes=[b_pt])
                    kb.op("scalar" if (t0 // 4) % 2 == 0 else "vector", lambda e, t0=t0, nt=nt, pt=pt, dst=dst: (
                        e.activation(out=dst[:, t0:t0 + nt, :], in_=pt[:, 0:nt * 128].rearrange("p (t c) -> p t c", c=128), func=AF.Identity)
                        if e is self.nc.scalar else
                        e.tensor_copy(out=dst[:, t0:t0 + nt, :], in_=pt[:, 0:nt * 128].rearrange("p (t c) -> p t c", c=128))),
                        reads=[b_pt], writes=[b_dst])
            sq = acc[:, 0:NT * 128].rearrange("p (t c) -> p t c", c=128)
            ss = kb.sb("g_ss", [128, 2, NT], F32)
            b_ss = kb.buf("g_ss")
            for qi, (src, b_src) in enumerate([(self.qh, self.b_qh), (self.kh, self.b_kh)]):
                kb.op("vector", lambda e, src=src: e.tensor_tensor(out=sq, in0=src[:], in1=src[:], op=ALU.mult), reads=[b_src], writes=[b_acc])
                kb.op("vector", lambda e, qi=qi: e.tensor_reduce(out=ss[:, qi, :], in_=sq, axis=AX.X, op=ALU.add), reads=[b_acc], writes=[b_ss])
            kb.op("scalar", lambda e: e.activation(out=ss[:], in_=ss[:], func=AF.Sqrt, bias=EPS, scale=1.0), reads=[b_ss], writes=[b_ss])
            kb.op("vector", lambda e: e.reciprocal(out=ss[:], in_=ss[:]), reads=[b_ss], writes=[b_ss])
            kb.op("vector", lambda e: e.tensor_scalar(out=ss[:, 0, :], in0=ss[:, 0, :], scalar1=float(128 ** -0.5), scalar2=0.0,
                                                     op0=ALU.mult, op1=ALU.add), reads=[b_ss], writes=[b_ss])
            for qi, (src, b_src) in enumerate([(self.qh, self.b_qh), (self.kh, self.b_kh)]):
                kb.op("vector", lambda e, src=src, qi=qi: e.tensor_tensor(
                    out=src[:], in0=src[:], in1=ss[:, qi, :].unsqueeze(2).to_broadcast([128, NT, 128]), op=ALU.mult),
                    reads=[b_src, b_ss], writes=[b_src])
            for (src, b_src, dstT, b_dT) in [(self.kh, self.b_kh, self.kT, self.b_kT), (self.qh, self.b_qh, self.qT, self.b_qT)]:
                for t0 in range(0, NT, 4):
                    nt = min(4, NT - t0)
                    pt, b_pt = self.PS.get()
                    for q in range(nt):
                        kb.op("tensor", lambda e, q=q, t0=t0, pt=pt, src=src: e.transpose(
                            pt[:, q * 128:(q + 1) * 128], src[:, t0 + q, :], self.identf[:]), reads=[b_src, self.b_ident], writes=[b_pt])
                    kb.op("scalar", lambda e, t0=t0, nt=nt, pt=pt, dstT=dstT: e.activation(
                        out=dstT[:, t0 * 128:(t0 + nt) * 128], in_=pt[:, 0:nt * 128], func=AF.Identity), reads=[b_pt], writes=[b_dT])
        if h == 0:
            self.dump("g_qh", self.qh[:, 0:4, :], self.b_qh, [128, 4, 128])
            self.dump("g_kh", self.kh[:, 0:4, :], self.b_kh, [128, 4, 128])
            self.dump("g_vh", self.vh[:, 0:4, :], self.b_vh, [128, 4, 128])


    def gdn_head_core(self, h):
        kb = self.kb
        gm = self.gm
        T_ = lambda name, n=1: Pool(kb, "gc_%s_%d" % (name, h), [128, 128], F32, n)
        R = 4
        rings = {n: [T_("%s%d" % (n, d), R) for d in range(2)] for n in ("wT", "ub", "qkT", "qdT", "kd")}
        tmp = {n: [T_("%s%d" % (n, d), 1) for d in range(2)] for n in
               ("kbt", "kw", "vb", "qd", "kbT", "Dls", "DTs", "DTi", "A", "N", "X0", "X1", "P0", "P1", "Q0", "Q1")}
        vnp = [T_("vn%d" % d, 2) for d in range(2)]
        S = [kb.sb("g_S%d_%d" % (h, d), [128, 128], F32) for d in range(2)]
        b_S = [kb.buf("g_S%d" % d) for d in range(2)]
        for d in range(2):
            kb.op("vector", lambda e, d=d: e.memset(S[d][:], 0.0), writes=[b_S[d]])
        order = {0: list(range(NT)), 1: [1, 0] + list(range(NT - 1, 1, -1))}
        ringent = {}
        gs = self.b_gs
        LS, US, UI, LI = 2, 3, 4, 10

        def prep(i):
            ents = []
            for d in range(2):
                tt = order[d][i]
                x = d * 4 + h
                ts_ = slice(tt * 128, (tt + 1) * 128)
                bsc = self.beta[:, tt, x:x + 1]
                e1 = self.E1[:, tt, x:x + 1]
                e2 = self.E2[:, tt, x:x + 1]
                g = lambda n: tmp[n][d].get()
                kbt, b_kbt = g("kbt"); kw, b_kw = g("kw"); vb, b_vb = g("vb"); qd, b_qd = g("qd"); kbT, b_kbT = g("kbT")
                kd, b_kd = rings["kd"][d].get(); qdT, b_qdT = rings["qdT"][d].get()
                ts1 = lambda e, o, i_, sc: e.tensor_scalar(out=o[:], in0=i_, scalar1=sc, scalar2=0.0, op0=ALU.mult, op1=ALU.add)
                kb.op("vector", lambda e: ts1(e, kbt, self.kh[:, tt, :], bsc), reads=[self.b_kh, gs], writes=[b_kbt])
                kb.op("vector", lambda e: ts1(e, kw, kbt[:], e1), reads=[b_kbt, gs], writes=[b_kw])
                kb.op("vector", lambda e: ts1(e, vb, self.vh[:, tt, :], bsc), reads=[self.b_vh, gs], writes=[b_vb])
                kb.op("vector", lambda e: ts1(e, kd, self.kh[:, tt, :], e2), reads=[self.b_kh, gs], writes=[b_kd])
                kb.op("vector", lambda e: ts1(e, qd, self.qh[:, tt, :], e1), reads=[self.b_qh, gs], writes=[b_qd])
                pt, b_pt = self.PS.get()
                kb.op("tensor", lambda e: e.transpose(pt[:, 0:128], kbt[:], self.identf[:]), reads=[b_kbt, self.b_ident], writes=[b_pt])
                kb.op("tensor", lambda e: e.transpose(pt[:, 128:256], qd[:], self.identf[:]), reads=[b_qd, self.b_ident], writes=[b_pt])
                kb.op("scalar", lambda e: e.activation(out=kbT[:], in_=pt[:, 0:128], func=AF.Identity), reads=[b_pt], writes=[b_kbT])
                kb.op("scalar", lambda e: e.activation(out=qdT[:], in_=pt[:, 128:256], func=AF.Identity), reads=[b_pt], writes=[b_qdT])
                gb = self.gg[:, tt, x:x + 1].to_broadcast([128, 128])
                M, nM = gm[:, d, :], gm[:, 8 + d, :]
                pD, b_pD = self.PS.get()
                kb.op("tensor", lambda e: e.matmul(pD[:, 0:128], M, gb, start=True, stop=False), reads=[gs, self.b_gm], writes=[b_pD])
                kb.op("tensor", lambda e: e.matmul(pD[:, 0:128], gb, nM, start=False, stop=True), reads=[gs, self.b_gm], writes=[b_pD])
                kb.op("tensor", lambda e: e.matmul(pD[:, 128:256], nM, gb, start=True, stop=False), reads=[gs, self.b_gm], writes=[b_pD])
                kb.op("tensor", lambda e: e.matmul(pD[:, 128:256], gb, M, start=False, stop=True), reads=[gs, self.b_gm], writes=[b_pD])
                Dls, b_Dls = g("Dls"); DTs, b_DTs = g("DTs"); DTi, b_DTi = g("DTi")
                mD, mDTs, mDTi = (LS, US, UI) if d == 0 else (US, LS, LI)
                for (dst, b_dst, src, mk) in [(Dls, b_Dls, pD[:, 0:128], mD), (DTs, b_DTs, pD[:, 128:256], mDTs), (DTi, b_DTi, pD[:, 128:256], mDTi)]:
                    kb.op("vector", lambda e, dst=dst, src=src, mk=mk: e.scalar_tensor_tensor(
                        out=dst[:], in0=src, scalar=0.0, in1=gm[:, mk, :], op0=ALU.min, op1=ALU.add), reads=[b_pD, self.b_gm], writes=[b_dst])
                    kb.op("scalar", lambda e, dst=dst: e.activation(out=dst[:], in_=dst[:], func=AF.Exp), reads=[b_dst], writes=[b_dst])
                pK, b_pK = self.PS.get()
                kTt, qTt = self.kT[:, ts_], self.qT[:, ts_]
                kb.op("tensor", lambda e: e.matmul(pK[:, 0:128], kbT[:], kTt, start=True, stop=True), reads=[b_kbT, self.b_kT], writes=[b_pK])
                kb.op("tensor", lambda e: e.matmul(pK[:, 128:256], kTt, kbT[:], start=True, stop=True), reads=[b_kbT, self.b_kT], writes=[b_pK])
                kb.op("tensor", lambda e: e.matmul(pK[:, 256:384], kTt, qTt, start=True, stop=True), reads=[self.b_qT, self.b_kT], writes=[b_pK])
                A, b_A = g("A"); N, b_N = g("N"); qkT, b_qkT = rings["qkT"][d].get()
                kb.op("vector", lambda e: e.tensor_tensor(out=A[:], in0=pK[:, 0:128], in1=Dls[:], op=ALU.mult), reads=[b_pK, b_Dls], writes=[b_A])
                kb.op("vector", lambda e: e.tensor_tensor(out=N[:], in0=pK[:, 128:256], in1=DTs[:], op=ALU.mult), reads=[b_pK, b_DTs], writes=[b_N])
                kb.op("vector", lambda e: e.tensor_tensor(out=qkT[:], in0=pK[:, 256:384], in1=DTi[:], op=ALU.mult), reads=[b_pK, b_DTi], writes=[b_qkT])
                X, b_X = g("X0")
                kb.op("vector", lambda e: e.tensor_tensor(out=X[:], in0=self.identf[:], in1=N[:], op=ALU.subtract), reads=[b_N, self.b_ident], writes=[b_X])
                ents.append(dict(d=d, tt=tt, x=x, kw=(kw, b_kw), vb=(vb, b_vb), kd=(kd, b_kd), qdT=(qdT, b_qdT), qkT=(qkT, b_qkT),
                                 P=(N, b_N), PT=(A, b_A), X=(X, b_X)))
            for sidx in range(1, 6):
                for en in ents:
                    d = en["d"]
                    P, b_P = en["P"]; PT, b_PT = en["PT"]; X, b_X = en["X"]
                    pp, b_pp = self.PS.get()
                    if sidx < 5:
                        kb.op("tensor", lambda e: e.matmul(pp[:, 0:128], PT[:], P[:], start=True, stop=True), reads=[b_P, b_PT], writes=[b_pp])
                    kb.op("tensor", lambda e: e.matmul(pp[:, 128:256], P[:], PT[:], start=True, stop=True), reads=[b_P, b_PT], writes=[b_pp])
                    nP, b_nP = tmp["P%d" % (sidx % 2)][d].get()
                    nPT, b_nPT = tmp["Q%d" % (sidx % 2)][d].get()
                    if sidx < 5:
                        kb.op("scalar", lambda e: e.activation(out=nP[:], in_=pp[:, 0:128], func=AF.Identity), reads=[b_pp], writes=[b_nP])
                    kb.op("scalar", lambda e: e.activation(out=nPT[:], in_=pp[:, 128:256], func=AF.Identity), reads=[b_pp], writes=[b_nPT])
                    kb.op("tensor", lambda e: e.matmul(pp[:, 256:384], nPT[:], X[:], start=True, stop=True), reads=[b_nPT, b_X], writes=[b_pp])
                    nX, b_nX = tmp["X%d" % (sidx % 2)][d].get()
                    kb.op("vector", lambda e: e.tensor_tensor(out=nX[:], in0=pp[:, 256:384], in1=X[:], op=ALU.add), reads=[b_pp, b_X], writes=[b_nX])
                    en["P"], en["PT"], en["X"] = (nP, b_nP), (nPT, b_nPT), (nX, b_nX)
            for en in ents:
                d = en["d"]
                X, b_X = en["X"]
                kw, b_kw = en["kw"]; vb, b_vb = en["vb"]
                pu, b_pu = self.PS.get()
                kb.op("tensor", lambda e: e.matmul(pu[:, 0:128], X[:], vb[:], start=True, stop=True), reads=[b_X, b_vb], writes=[b_pu])
                kb.op("tensor", lambda e: e.matmul(pu[:, 128:256], kw[:], X[:], start=True, stop=True), reads=[b_X, b_kw], writes=[b_pu])
                ub, b_ub = rings["ub"][d].get(); wT, b_wT = rings["wT"][d].get()
                kb.op("scalar", lambda e: e.activation(out=ub[:], in_=pu[:, 0:128], func=AF.Identity), reads=[b_pu], writes=[b_ub])
                kb.op("scalar", lambda e: e.activation(out=wT[:], in_=pu[:, 128:256], func=AF.Identity), reads=[b_pu], writes=[b_wT])
                en["ub"], en["wT"] = (ub, b_ub), (wT, b_wT)
                ringent[(d, i)] = en
                if h == 0 and i == 0 and d == 0:
                    self.dump("g_ub", ub[:], b_ub, [128, 128])
                    self.dump("g_wT", wT[:], b_wT, [128, 128])
                    self.dump("g_qkT", en["qkT"][0][:], en["qkT"][1], [128, 128])

        def chain(i):
            for hi in range(2):
                for d in range(2):
                    en = ringent[(d, i)]
                    tt, x = en["tt"], en["x"]
                    c2 = hi if d == 0 else 1 - hi
                    r = slice(c2 * 64, c2 * 64 + 64)
                    wT, b_wT = en["wT"]; ub, b_ub = en["ub"]; qdT, b_qdT = en["qdT"]; qkT, b_qkT = en["qkT"]; kd, b_kd = en["kd"]
                    pw, b_pw = self.PS.get()
                    kb.op("tensor", lambda e: e.matmul(pw[r, 0:128], wT[:, r], S[d][:], start=True, stop=True), reads=[b_wT, b_S[d]], writes=[b_pw])
                    vn, b_vn = vnp[d].get()
                    kb.op("vector", lambda e: e.tensor_tensor(out=vn[r, :], in0=ub[r, :], in1=pw[r, 0:128], op=ALU.subtract),
                          reads=[b_ub, b_pw], writes=[b_vn])
                    if tt >= 2:
                        kb.op("tensor", lambda e: e.matmul(pw[r, 128:256], qdT[:, r], S[d][:], start=True, stop=False),
                              reads=[b_qdT, b_S[d]], writes=[b_pw])
                        kb.op("tensor", lambda e: e.matmul(pw[r, 128:256], qkT[r, r], vn[r, :], start=False, stop=True),
                              reads=[b_qkT, b_vn], writes=[b_pw])
                        od = self.osum[r, tt - 2, h * 128:(h + 1) * 128]
                        kb.op("vector", lambda e: e.tensor_tensor(out=od, in0=pw[r, 128:256], in1=od, op=ALU.add),
                              reads=[b_pw], writes=[self.b_osum[tt - 2]])
                    ps_, b_ps = self.PS.get()
                    kb.op("tensor", lambda e: e.matmul(ps_[:, 0:128], kd[r, :], vn[r, :], start=True, stop=True), reads=[b_kd, b_vn], writes=[b_ps])
                    kb.op("vector", lambda e: e.scalar_tensor_tensor(out=S[d][:], in0=S[d][:], scalar=self.DL[:, c2, tt, x:x + 1], in1=ps_[:, 0:128],
                                                                    op0=ALU.mult, op1=ALU.add), reads=[b_ps, b_S[d], gs], writes=[b_S[d]])

        LA = 2
        for i in range(NT + LA):
            if i < NT:
                prep(i)
            if i - LA >= 0:
                chain(i - LA)

    def phase_gdn(self):
        kb = self.kb
        self.osum = kb.sb("g_osum", [128, 16, 512], F32)
        self.b_osum = [kb.buf("g_osum%d" % t) for t in range(16)]
        for t in range(16):
            kb.op("vector", lambda e, t=t: e.memset(self.osum[:, t, :], 0.0), writes=[self.b_osum[t]])
        for h in range(4):
            with kb.scope():
                self.gdn_head_front(h)
                self.gdn_head_core(h)
            if self.stop == "gdn_h0":
                break
        if "g_osum" in self.dbg:
            self.dump("g_osum", self.osum[:, 0:4, 0:128], self.b_osum[3], [128, 4, 128])
        if "g_osum2" in self.dbg:
            self.dump("g_osum2", self.osum[:, 12:16, 0:128], self.b_osum[15], [128, 4, 128])


    def phase_glu(self):
        kb = self.kb
        wg = kb.sb("wglu", [128, 4, 512], BF16)
        b_wg = kb.buf("wglu")
        self.load_w_bf16(wg, b_wg, self.w_glu, 512, 0)
        bg, b_bg = self._ld("bglu", self.b_glu_t, [128, 4])
        zT = kb.sb("glu_z", [128, 4, L], BF16)
        b_zT = [kb.buf("glu_z%d" % q) for q in range(4)]
        t1 = kb.sb("glu_t1", [128, L], F32)
        t2 = kb.sb("glu_t2", [128, L], F32)
        b_t = kb.buf("glu_t")
        for q in range(4):
            y = self.yT[:, q, :]
            kb.op("vector", lambda e, y=y: e.tensor_tensor(out=t1[:], in0=y, in1=y, op=ALU.mult), reads=[self.b_yT[q]], writes=[b_t])
            kb.op("vector", lambda e: e.tensor_scalar(out=t1[:], in0=t1[:], scalar1=0.044715, scalar2=1.0, op0=ALU.mult, op1=ALU.add), reads=[b_t], writes=[b_t])
            kb.op("vector", lambda e, y=y: e.tensor_tensor(out=t1[:], in0=t1[:], in1=y, op=ALU.mult), reads=[b_t, self.b_yT[q]], writes=[b_t])
            kb.op("scalar", lambda e: e.activation(out=t2[:], in_=t1[:], func=AF.Sigmoid, scale=1.5957691216057308), reads=[b_t], writes=[b_t])
            kb.op("vector", lambda e, y=y, q=q: e.tensor_tensor(out=zT[:, q, :], in0=t2[:], in1=y, op=ALU.mult), reads=[b_t, self.b_yT[q]], writes=[b_zT[q]])
        glp = Pool(kb, "glu_g", [128, 512], F32, 2)
        for oc in range(4):
            for n in range(4):
                pt, b_pt = self.PS.get()
                for kc in range(4):
                    kb.op("tensor", lambda e, kc=kc, pt=pt, oc=oc, n=n: e.matmul(
                        pt[:], wg[:, kc, oc * 128:(oc + 1) * 128], zT[:, kc, n * 512:(n + 1) * 512], start=(kc == 0), stop=(kc == 3)),
                        reads=[b_wg] + b_zT, writes=[b_pt], sig=(kc == 3))
                gl, b_gl = glp.get()
                kb.op("scalar", lambda e, pt=pt, gl=gl, oc=oc: e.activation(out=gl[:], in_=pt[:], func=AF.Sigmoid, bias=bg[:, oc:oc + 1]),
                      reads=[b_pt, b_bg], writes=[b_gl])
                kb.op("vector", lambda e, gl=gl, oc=oc, n=n: e.tensor_tensor(
                    out=self.yaT[:, oc, n * 512:(n + 1) * 512], in0=zT[:, oc, n * 512:(n + 1) * 512], in1=gl[:], op=ALU.mult),
                    reads=[b_gl, b_zT[oc]], writes=[self.b_yaT])
        if "yaT" in self.dbg:
            t = kb.sb("dbg_yaT_t", [128, 4, 512], F32)
            bt = kb.buf("dbg_yaT")
            kb.op("vector", lambda e: e.tensor_copy(out=t[:], in_=self.yaT[:, :, 0:512]), reads=[self.b_yaT], writes=[bt])
            self.dump("yaT", t[:], bt, [128, 4, 512])

    def phase_gdn_out(self):
        kb = self.kb
        wgt = kb.sb("wgate", [128, 8, 512], BF16)
        b_wgt = kb.buf("wgate")
        self.load_w_bf16(wgt, b_wgt, self.w_in, 512, OFF_GATE)
        nw = kb.sb("gnw", [128, 128], F32)
        b_nw = kb.buf("gnw")
        kb.dma("sync", nw[:], self.gdn_norm.partition_broadcast(128), writes=[b_nw])
        ss = kb.sb("go_ss", [128, 16, 4], F32)
        b_ss = kb.buf("go_ss")
        sqp = Pool(kb, "go_sq", [128, 512], F32, 2)
        for t in range(16):
            sq, b_sq = sqp.get()
            kb.op("vector", lambda e, t=t, sq=sq: e.tensor_tensor(out=sq[:], in0=self.osum[:, t, :], in1=self.osum[:, t, :], op=ALU.mult),
                  reads=[self.b_osum[t]], writes=[b_sq])
            kb.op("vector", lambda e, t=t, sq=sq: e.tensor_reduce(out=ss[:, t, :], in_=sq[:].rearrange("p (h c) -> p h c", c=128), axis=AX.X, op=ALU.add),
                  reads=[b_sq], writes=[b_ss])
        ssf = ss[:].rearrange("p t h -> p (t h)")
        kb.op("scalar", lambda e: e.activation(out=ssf, in_=ssf, func=AF.Sqrt, bias=EPS, scale=1.0 / 128), reads=[b_ss], writes=[b_ss])
        kb.op("vector", lambda e: e.reciprocal(out=ssf, in_=ssf), reads=[b_ss], writes=[b_ss])
        sgp = Pool(kb, "go_sg", [128, 512], F32, 2)
        onp = Pool(kb, "go_on", [128, 512], F32, 2)
        for t in range(16):
            pg, b_pg = self.PS.get()
            for kc in range(8):
                kb.op("tensor", lambda e, kc=kc, t=t, pg=pg: e.matmul(pg[:], self.hT[:, kc, (t + 2) * 128:(t + 3) * 128], wgt[:, kc, :],
                                                                     start=(kc == 0), stop=(kc == 7)), reads=[b_wgt, self.b_hT[t + 2]], writes=[b_pg], sig=(kc == 7))
            sg, b_sg = sgp.get()
            kb.op("scalar", lambda e, pg=pg, sg=sg: e.activation(out=sg[:], in_=pg[:], func=AF.Silu), reads=[b_pg], writes=[b_sg])
            on, b_on = onp.get()
            on3 = on[:].rearrange("p (h c) -> p h c", c=128)
            kb.op("vector", lambda e, t=t, on3=on3: e.tensor_tensor(out=on3, in0=self.osum[:, t, :].rearrange("p (h c) -> p h c", c=128),
                                                                   in1=ss[:, t, :].unsqueeze(2).to_broadcast([128, 4, 128]), op=ALU.mult),
                  reads=[self.b_osum[t], b_ss], writes=[b_on])
            kb.op("vector", lambda e, on3=on3: e.tensor_tensor(out=on3, in0=on3, in1=nw[:].unsqueeze(1).to_broadcast([128, 4, 128]), op=ALU.mult),
                  reads=[b_on, b_nw], writes=[b_on])
            kb.op("vector", lambda e, on=on, sg=sg: e.tensor_tensor(out=on[:], in0=on[:], in1=sg[:], op=ALU.mult), reads=[b_on, b_sg], writes=[b_on])
            pt, b_pt = self.PS.get()
            for c in range(4):
                kb.op("tensor", lambda e, c=c, pt=pt, on=on: e.transpose(pt[:, c * 128:(c + 1) * 128], on[:, c * 128:(c + 1) * 128], self.identf[:]),
                      reads=[b_on, self.b_ident], writes=[b_pt])
            kb.op("scalar", lambda e, pt=pt, t=t: e.activation(out=self.ybT[:, :, t * 128:(t + 1) * 128], in_=pt[:].rearrange("p (c k) -> p c k", k=128),
                                                              func=AF.Identity), reads=[b_pt], writes=[self.b_ybT])
        if "ybT" in self.dbg:
            t_ = kb.sb("dbg_ybT_t", [128, 4, 512], F32)
            bt = kb.buf("dbg_ybT")
            kb.op("vector", lambda e: e.tensor_copy(out=t_[:], in_=self.ybT[:, :, 0:512]), reads=[self.b_ybT], writes=[bt])
            self.dump("ybT", t_[:], bt, [128, 4, 512])

    def phase_merge(self):
        kb = self.kb
        mT = kb.sb("mergedT", [128, 8, L], BF16)
        b_mT = [kb.buf("mergedT%d" % n) for n in range(4)]
        with kb.scope():
            wba = kb.sb("wba", [128, 4, D], BF16); b_wba = kb.buf("wba")
            wbb = kb.sb("wbb", [128, 4, D], BF16); b_wbb = kb.buf("wbb")
            self.load_w_bf16(wba, b_wba, self.w_ba, D, 0)
            self.load_w_bf16(wbb, b_wbb, self.w_bb, D, 0)
            wbrp = Pool(kb, "wbr", [128, 8, 2, 128], BF16, 2)
            gp = Pool(kb, "mg_g", [128, 2, 512], F32, 2)
            m1p = Pool(kb, "mg_m", [128, 2, 512], F32, 2)
            v = self.w_in.rearrange("(kc p) n -> p kc n", p=128)
            for oc in range(8):
                wbr, b_wbr = wbrp.get()
                for ab in range(2):
                    for kc in range(8):
                        col = OFF_BR + ab * D + oc * 128
                        kb.dma("gpsimd", wbr[:, kc, ab, :], v[:, kc, col:col + 128], writes=[b_wbr])
                for n in range(4):
                    tiles = [self.b_hT[2 + 4 * n + j] for j in range(4)]
                    tok = slice(n * 512, (n + 1) * 512)
                    stok = slice(CTX + n * 512, CTX + (n + 1) * 512)
                    g, b_g = gp.get()
                    m1, b_m1 = m1p.get()
                    for ab, (wb, b_wb, yT_, b_y) in enumerate([(wba, b_wba, self.yaT, self.b_yaT), (wbb, b_wbb, self.ybT, self.b_ybT)]):
                        pbr, b_pbr = self.PS.get()
                        for kc in range(8):
                            kb.op("tensor", lambda e, kc=kc, pbr=pbr, ab=ab, wbr=wbr: e.matmul(
                                pbr[:], wbr[:, kc, ab, :], self.hT[:, kc, stok], start=(kc == 0), stop=(kc == 7)),
                                reads=[b_wbr] + tiles, writes=[b_pbr], sig=(kc == 7))
                        kb.op("scalar", lambda e, pbr=pbr, g=g, ab=ab: e.activation(out=g[:, ab, :], in_=pbr[:], func=AF.Sigmoid),
                              reads=[b_pbr], writes=[b_g])
                        pp, b_pp = self.PS.get()
                        for kc in range(4):
                            kb.op("tensor", lambda e, kc=kc, pp=pp, wb=wb, yT_=yT_: e.matmul(
                                pp[:], wb[:, kc, oc * 128:(oc + 1) * 128], yT_[:, kc, tok], start=(kc == 0), stop=(kc == 3)),
                                reads=[b_wb, b_y], writes=[b_pp], sig=(kc == 3))
                        kb.op("vector", lambda e, pp=pp, g=g, m1=m1, ab=ab: e.tensor_tensor(out=m1[:, ab, :], in0=pp[:], in1=g[:, ab, :], op=ALU.mult),
                              reads=[b_pp, b_g], writes=[b_m1])
                    kb.op("vector", lambda e, m1=m1, oc=oc: e.tensor_tensor(out=mT[:, oc, tok], in0=m1[:, 0, :], in1=m1[:, 1, :], op=ALU.add),
                          reads=[b_m1], writes=[b_mT[n]])
        if "mergedT" in self.dbg:
            t_ = kb.sb("dbg_mT_t", [128, 8, 256], F32)
            bt = kb.buf("dbg_mT")
            kb.op("vector", lambda e: e.tensor_copy(out=t_[:], in_=mT[:, :, 0:256]), reads=[b_mT[0]], writes=[bt])
            self.dump("mergedT", t_[:], bt, [128, 8, 256])
        with kb.scope():
            wo = kb.sb("wout", [128, 8, D], BF16); b_wo = kb.buf("wout")
            self.load_w_bf16(wo, b_wo, self.w_out, D, 0)
            xp = Pool(kb, "mg_x", [128, D], F32, 3)
            tp = Pool(kb, "mg_t", [128, D], F32, 2)
            self.b_x1s = [kb.buf("x1s%d" % t) for t in range(16)]
            for t in range(16):
                xt, b_xt = xp.get()
                kb.dma("sync" if t % 2 == 0 else "scalar", xt[:], self.x[t * 128:(t + 1) * 128, :], writes=[b_xt])
                tm_, b_tm = tp.get()
                for cb in range(2):
                    pm, b_pm = self.PS.get()
                    for kc in range(8):
                        kb.op("tensor", lambda e, kc=kc, pm=pm, cb=cb, t=t: e.matmul(
                            pm[:], mT[:, kc, t * 128:(t + 1) * 128], wo[:, kc, cb * 512:(cb + 1) * 512], start=(kc == 0), stop=(kc == 7)),
                            reads=[b_wo, b_mT[t // 4]], writes=[b_pm], sig=(kc == 7))
                    cs_ = slice(cb * 512, (cb + 1) * 512)
                    kb.op("vector", lambda e, pm=pm, tm_=tm_, cs_=cs_: e.tensor_tensor(out=tm_[:, cs_], in0=pm[:], in1=self.gt1_bc[:, cs_], op=ALU.mult),
                          reads=[b_pm, self.b_gt1], writes=[b_tm])
                kb.op("vector", lambda e, tm_=tm_, xt=xt: e.tensor_tensor(out=xt[:], in0=tm_[:], in1=xt[:], op=ALU.add), reads=[b_tm, b_xt], writes=[b_xt])
                kb.dma("sync", self.x1s[t * 128:(t + 1) * 128, :], xt[:], reads=[b_xt], writes=[self.b_x1s[t]])
                if t == 0:
                    self.dump("x1", xt[:], b_xt, [128, D])

    def phase_moe_half(self, hp):
        kb = self.kb
        NTL = 8
        X = kb.sb("X%d" % hp, [128, NTL, D], F32)
        b_X = [kb.buf("X%d_%d" % (hp, t)) for t in range(NTL)]
        h2T = kb.sb("h2T%d" % hp, [128, 8, NTL * 128], BF16)
        b_h2 = [kb.buf("h2T%d_%d" % (hp, t)) for t in range(NTL)]
        comb = kb.sb("comb%d" % hp, [128, NTL, NE], F32)
        b_comb = kb.buf("comb%d" % hp)
        combs = kb.sb("combs%d" % hp, [128, NTL, NE], F32)
        combT = kb.sb("combT%d" % hp, [NE, NTL * 128], F32)
        b_combT = kb.buf("combT%d" % hp)
        for t in range(NTL):
            gt = hp * NTL + t
            kb.dma("sync" if t % 2 == 0 else "scalar", X[:, t, :], self.x1s[gt * 128:(gt + 1) * 128, :], reads=[self.b_x1s[gt]], writes=[b_X[t]])
        with kb.scope():
            n2, b_n2 = self._ld("norm2", self.norm2_t, [128, 8])
            A2 = kb.sb("A2", [128, 8, 2], F32); b_A2 = kb.buf("A2")
            kb.op("vector", lambda e: e.scalar_tensor_tensor(out=A2[:], in0=self.modT[:, 24:32, :], scalar=1.0,
                                                            in1=n2[:].unsqueeze(2).to_broadcast([128, 8, 2]), op0=ALU.add, op1=ALU.mult),
                  reads=[self.b_modT, b_n2], writes=[b_A2])
            wr = kb.sb("wrouter", [128, 8, NE], F32); b_wr = kb.buf("wrouter")
            kb.dma("sync", wr[:], self.w_router.rearrange("(kc p) n -> p kc n", p=128), writes=[b_wr])
            br_, b_br = kb.sb("brouter", [128, NE], F32), kb.buf("brouter")
            kb.dma("sync", br_[:], self.b_router.partition_broadcast(128), writes=[b_br])
            xnp = Pool(kb, "m_xn", [128, D], F32, 2)
            junk = Pool(kb, "m_junk", [128, D], F32, 1)
            stat = Pool(kb, "m_stat", [128, 4], F32, 4)
            hfp = Pool(kb, "m_hf", [128, 8, 128], F32, 2)
            lgp = Pool(kb, "m_lg", [128, NE], F32, 2)
            m8p = Pool(kb, "m_m8", [128, 16], F32, 2)
            for t in range(NTL):
                hf, b_hf = hfp.get()
                self._norm_tile2(X[:, t, :], b_X[t], t, A2, b_A2, 16, xnp, junk, stat, h2T, b_h2[t], hf, b_hf)
                pl, b_pl = self.PS.get()
                for kc in range(8):
                    kb.op("tensor", lambda e, kc=kc, pl=pl, hf=hf: e.matmul(pl[:, 0:NE], hf[:, kc, :], wr[:, kc, :], start=(kc == 0), stop=(kc == 7)),
                          reads=[b_hf, b_wr], writes=[b_pl], sig=(kc == 7))
                lg, b_lg = lgp.get()
                m8, b_m8 = m8p.get()
                kb.op("vector", lambda e, pl=pl, lg=lg: e.tensor_tensor(out=lg[:], in0=pl[:, 0:NE], in1=br_[:], op=ALU.add), reads=[b_pl, b_br], writes=[b_lg])
                if t == 0 and hp == 0:
                    self.dump("logits", lg[:], b_lg, [128, NE])
                kb.op("vector", lambda e, lg=lg, m8=m8: e.max(out=m8[:, 0:8], in_=lg[:]), reads=[b_lg], writes=[b_m8])
                kb.op("vector", lambda e, m8=m8: e.tensor_scalar(out=m8[:, 8:9], in0=m8[:, 0:1], scalar1=-1.0, scalar2=0.0, op0=ALU.mult, op1=ALU.add),
                      reads=[b_m8], writes=[b_m8])
                ex, b_ex = lgp.get()
                kb.op("scalar", lambda e, lg=lg, ex=ex, m8=m8: e.activation(out=ex[:], in_=lg[:], func=AF.Exp, bias=m8[:, 8:9]), reads=[b_lg, b_m8], writes=[b_ex])
                kb.op("vector", lambda e, lg=lg, ex=ex, m8=m8: e.scalar_tensor_tensor(out=ex[:], in0=lg[:], scalar=m8[:, 3:4], in1=ex[:], op0=ALU.is_ge, op1=ALU.mult),
                      reads=[b_lg, b_ex, b_m8], writes=[b_ex])
                kb.op("vector", lambda e, ex=ex, m8=m8: e.tensor_reduce(out=m8[:, 9:10], in_=ex[:], axis=AX.X, op=ALU.add), reads=[b_ex], writes=[b_m8])
                kb.op("vector", lambda e, m8=m8: e.reciprocal(out=m8[:, 10:11], in_=m8[:, 9:10]), reads=[b_m8], writes=[b_m8])
                kb.op("vector", lambda e, ex=ex, m8=m8, t=t: e.tensor_scalar(out=comb[:, t, :], in0=ex[:], scalar1=m8[:, 10:11], scalar2=0.0, op0=ALU.mult, op1=ALU.add),
                      reads=[b_ex, b_m8], writes=[b_comb])
                kb.op("vector", lambda e, t=t: e.tensor_scalar(out=combs[:, t, :], in0=comb[:, t, :], scalar1=float(1.0 / 1.702), scalar2=0.0, op0=ALU.mult, op1=ALU.add),
                      reads=[b_comb], writes=[b_comb])
                pT, b_pT = self.PS.get()
                kb.op("tensor", lambda e, pT=pT, t=t: e.transpose(pT[0:NE, 0:128], comb[:, t, :], self.identf[:]), reads=[b_comb, self.b_ident], writes=[b_pT])
                kb.op("scalar", lambda e, pT=pT, t=t: e.activation(out=combT[:, t * 128:(t + 1) * 128], in_=pT[0:NE, 0:128], func=AF.Identity),
                      reads=[b_pT], writes=[b_combT])
            if hp == 0:
                self.dump("comb", comb[:, 0, :], b_comb, [128, NE])
                if "h2T" in self.dbg:
                    t_ = kb.sb("dbg_h2T_t", [128, 8, 256], F32)
                    bt = kb.buf("dbg_h2T")
                    kb.op("vector", lambda e: e.tensor_copy(out=t_[:], in_=h2T[:, :, 0:256]), reads=b_h2[0:2], writes=[bt])
                    self.dump("h2T", t_[:], bt, [128, 8, 256])
        if self.stop == "router":
            return
        with kb.scope():
            bdn, b_bdn = self._ld("bdn", self.b_dn, [NE, D])
            bgu, b_bgu = self._ld("bgu", self.b_gu_t, [128, NE, 16])
            tp = Pool(kb, "moe_t", [128, 512], F32, 3)
            for t in range(NTL):
                for cb in range(2):
                    cs_ = slice(cb * 512, (cb + 1) * 512)
                    pb, b_pb = self.PS.get()
                    kb.op("tensor", lambda e, pb=pb, t=t, cs_=cs_: e.matmul(pb[:], combT[:, t * 128:(t + 1) * 128], bdn[:, cs_], start=True, stop=True),
                          reads=[b_combT, b_bdn], writes=[b_pb])
                    tt_, b_tt = tp.get()
                    kb.op("vector", lambda e, pb=pb, tt_=tt_, cs_=cs_: e.tensor_tensor(out=tt_[:], in0=pb[:], in1=self.gt2_bc[:, cs_], op=ALU.mult),
                          reads=[b_pb, self.b_gt2], writes=[b_tt])
                    kb.op("vector", lambda e, tt_=tt_, t=t, cs_=cs_: e.tensor_tensor(out=X[:, t, cs_], in0=tt_[:], in1=X[:, t, cs_], op=ALU.add),
                          reads=[b_tt], writes=[b_X[t]])
            wgup = Pool(kb, "wgu", [128, 8, 2 * D], BF16, 2)
            wdnp = Pool(kb, "wdn", [128, 8, D], BF16, 2)
            actp = Pool(kb, "actT", [128, 8, 512], BF16, 2)
            gp = Pool(kb, "moe_g", [128, 512], F32, 3)
            sp = Pool(kb, "moe_s", [128, 512], BF16, 3)
            up = Pool(kb, "moe_u", [128, 512], BF16, 3)
            bgu1 = kb.sb("bgu1", [128, NE, 8], F32)
            kb.op("vector", lambda e: e.tensor_scalar(out=bgu1[:], in0=bgu[:, :, 8:16], scalar1=1.0, scalar2=0.0, op0=ALU.add, op1=ALU.add),
                  reads=[b_bgu], writes=[b_bgu])
            NB = NTL * 128 // 512
            nexp = NE if self.stop != "moe1" else 1
            W = {}

            def load_w(ex_):
                wgu, b_wgu = wgup.get()
                wdn, b_wdn = wdnp.get()
                vg = self.w_gu[ex_].rearrange("(kc p) n -> p kc n", p=128)
                for kc in range(8):
                    kb.dma("gpsimd", wgu[:, kc, :], vg[:, kc, :], writes=[b_wgu])
                vd = self.w_dn[ex_].rearrange("(kc p) n -> p kc n", p=128)
                for kc in range(8):
                    kb.dma("gpsimd", wdn[:, kc, :], vd[:, kc, :], writes=[b_wdn])
                kb.op("vector", lambda e, wdn=wdn: e.tensor_tensor(out=wdn[:], in0=wdn[:], in1=self.gt2_bc[:].unsqueeze(1).to_broadcast([128, 8, D]), op=ALU.mult),
                      reads=[b_wdn, self.b_gt2], writes=[b_wdn])
                W[ex_] = (wgu, b_wgu, wdn, b_wdn)

            ACT = {}

            def gu(ex_, n):
                wgu, b_wgu, wdn, b_wdn = W[ex_]
                actT, b_act = actp.get()
                ACT[(ex_, n)] = (actT, b_act)
                toks = slice(n * 512, (n + 1) * 512)
                tl = [b_h2[4 * n + j] for j in range(4)]
                for j in range(8):
                    pg, b_pg = self.PS.get()
                    pu, b_pu = self.PS.get()
                    for kc in range(8):
                        kb.op("tensor", lambda e, kc=kc: e.matmul(pg[:], wgu[:, kc, j * 128:(j + 1) * 128], h2T[:, kc, toks],
                                                                  start=(kc == 0), stop=(kc == 7)), reads=[b_wgu] + tl, writes=[b_pg], sig=(kc == 7))
                    for kc in range(8):
                        kb.op("tensor", lambda e, kc=kc: e.matmul(pu[:], wgu[:, kc, D + j * 128:D + (j + 1) * 128], h2T[:, kc, toks],
                                                                  start=(kc == 0), stop=(kc == 7)), reads=[b_wgu] + tl, writes=[b_pu], sig=(kc == 7))
                    g, b_g = gp.get(); sg, b_sg = sp.get(); u, b_u = up.get()
                    kb.op("vector", lambda e: e.tensor_scalar(out=g[:], in0=pg[:], scalar1=bgu[:, ex_, j:j + 1], scalar2=7.0, op0=ALU.add, op1=ALU.min),
                          reads=[b_pg, b_bgu], writes=[b_g])
                    kb.op("scalar", lambda e: e.activation(out=sg[:], in_=g[:], func=AF.Silu, scale=1.702), reads=[b_g], writes=[b_sg])
                    kb.op("vector", lambda e: e.tensor_scalar(out=u[:], in0=pu[:], scalar1=bgu1[:, ex_, j:j + 1], scalar2=8.0, op0=ALU.add, op1=ALU.min),
                          reads=[b_pu, b_bgu], writes=[b_u])
                    kb.op("vector", lambda e: e.scalar_tensor_tensor(out=actT[:, j, :], in0=u[:], scalar=-6.0, in1=sg[:], op0=ALU.max, op1=ALU.mult),
                          reads=[b_u, b_sg], writes=[b_act])

            def dn(ex_, n):
                wgu, b_wgu, wdn, b_wdn = W[ex_]
                actT, b_act = ACT.pop((ex_, n))
                for tq in range(4):
                    t = n * 4 + tq
                    for cb in range(2):
                        cs_ = slice(cb * 512, (cb + 1) * 512)
                        pd_, b_pd = self.PS.get()
                        for kc in range(8):
                            kb.op("tensor", lambda e, kc=kc: e.matmul(pd_[:], actT[:, kc, tq * 128:(tq + 1) * 128], wdn[:, kc, cs_],
                                                                      start=(kc == 0), stop=(kc == 7)), reads=[b_act, b_wdn], writes=[b_pd], sig=(kc == 7))
                        kb.op("vector", lambda e: e.scalar_tensor_tensor(out=X[:, t, cs_], in0=pd_[:], scalar=combs[:, t, ex_:ex_ + 1], in1=X[:, t, cs_],
                                                                        op0=ALU.mult, op1=ALU.add), reads=[b_pd, b_comb], writes=[b_X[t]])

            items = [(e_, n) for e_ in range(nexp) for n in range(NB)]
            load_w(0)
            if nexp > 1:
                load_w(1)
            gu(*items[0])
            for k, (e_, n) in enumerate(items):
                if k + 1 < len(items):
                    gu(*items[k + 1])
                dn(e_, n)
                if n == NB - 1 and e_ + 2 < nexp:
                    load_w(e_ + 2)
        if hp == 0:
            self.dump("x2", X[:, 0, :], b_X[0], [128, D])
        with kb.scope():
            nf = kb.sb("normf", [128, D], F32); b_nf = kb.buf("normf")
            kb.dma("sync", nf[:], self.norm_f.partition_broadcast(128), writes=[b_nf])
            junk = Pool(kb, "f_junk", [128, D], F32, 1)
            stat = Pool(kb, "f_stat", [128, 4], F32, 4)
            op_ = Pool(kb, "f_o", [128, D], F32, 2)
            for t in range(NTL):
                gt = hp * NTL + t
                jt, b_jt = junk.get(); st, b_st = stat.get()
                kb.op("scalar", lambda e, jt=jt, st=st, t=t: e.activation(out=jt[:], in_=X[:, t, :], func=AF.Square, accum_out=st[:, 0:1]), reads=[b_X[t]], writes=[b_jt, b_st])
                kb.op("scalar", lambda e, st=st: e.activation(out=st[:, 1:2], in_=st[:, 0:1], func=AF.Sqrt, bias=EPS, scale=1.0 / D), reads=[b_st], writes=[b_st])
                kb.op("vector", lambda e, st=st: e.reciprocal(out=st[:, 2:3], in_=st[:, 1:2]), reads=[b_st], writes=[b_st])
                ot, b_ot = op_.get()
                kb.op("vector", lambda e, ot=ot, st=st, t=t: e.scalar_tensor_tensor(out=ot[:], in0=X[:, t, :], scalar=st[:, 2:3], in1=nf[:], op0=ALU.mult, op1=ALU.mult),
                      reads=[b_X[t], b_st, b_nf], writes=[b_ot])
                bo = kb.buf("out%d" % gt)
                kb.dma("sync", self.out[gt * 128:(gt + 1) * 128, :], ot[:], reads=[b_ot], writes=[bo])
                self.fin.append(bo)

    def _norm_tile2(self, xt_ap, b_xt, tt, A, b_A, shift_chunk0, xnp, junk, stat, dstT, b_dst, hf, b_hf):
        kb = self.kb
        jt, b_jt = junk.get()
        st, b_st = stat.get()
        kb.op("scalar", lambda e: e.activation(out=jt[:], in_=xt_ap, func=AF.Square, accum_out=st[:, 0:1]), reads=[b_xt], writes=[b_jt, b_st])
        kb.op("scalar", lambda e: e.activation(out=st[:, 1:2], in_=st[:, 0:1], func=AF.Sqrt, bias=EPS, scale=1.0 / D), reads=[b_st], writes=[b_st])
        kb.op("vector", lambda e: e.reciprocal(out=st[:, 2:3], in_=st[:, 1:2]), reads=[b_st], writes=[b_st])
        xn, b_xn = xnp.get()
        kb.op("scalar", lambda e: e.activation(out=xn[:], in_=xt_ap, func=AF.Identity, scale=st[:, 2:3]), reads=[b_xt, b_st], writes=[b_xn])
        for half in range(2):
            pt, b_pt = self.PS.get()
            for q in range(4):
                kc = half * 4 + q
                kb.op("tensor", lambda e, kc=kc, q=q, pt=pt: e.transpose(pt[:, q * 128:(q + 1) * 128], xn[:, kc * 128:(kc + 1) * 128], self.identf[:]),
                      reads=[b_xn, self.b_ident], writes=[b_pt])
            for q in range(4):
                kc = half * 4 + q
                kb.op("vector", lambda e, kc=kc, q=q, pt=pt: e.tensor_scalar(
                    out=hf[:, kc, :], in0=pt[:, q * 128:(q + 1) * 128], scalar1=A[:, kc, 0:1], scalar2=self.modT[:, shift_chunk0 + kc, 0:1],
                    op0=ALU.mult, op1=ALU.add), reads=[b_pt, b_A, self.b_modT], writes=[b_hf])
        kb.op("scalar", lambda e: e.activation(out=dstT[:, :, tt * 128:(tt + 1) * 128], in_=hf[:], func=AF.Identity), reads=[b_hf], writes=[b_dst])

    def build(self):
        kb = self.kb
        self.declare()
        self.common()
        with kb.scope():
            self.hT = kb.sb("hT", [128, 8, TOK], BF16)
            self.b_hT = [kb.buf("hT%d" % t) for t in range(NT)]
            self.yaT = kb.sb("yaT", [128, 4, L], BF16)
            self.b_yaT = kb.buf("yaT")
            with kb.scope():
                self.phase_s5_setup()
                if self.stop == "s5setup":
                    return self.finish()
                with kb.scope():
                    self.phase_mod()
                if self.stop == "mod":
                    return self.finish()
                with kb.scope():
                    self.phase_norm1()
                if self.stop == "norm1":
                    return self.finish()
                self.uT = kb.sb("uT", [128, 4, TOK], BF16)
                self.b_uT = kb.buf("uT")
                self.yT = kb.sb("yT", [128, 4, L], F32)
                self.b_yT = [kb.buf("yT%d" % q) for q in range(4)]
                with kb.scope():
                    self.phase_u()
                if self.stop == "u":
                    return self.finish()
                with kb.scope():
                    self.phase_s5()
                if self.stop == "s5":
                    return self.finish()
                with kb.scope():
                    self.phase_glu()
                if self.stop == "glu":
                    return self.finish()
            self.ybT = kb.sb("ybT", [128, 4, L], BF16)
            self.b_ybT = kb.buf("ybT")
            with kb.scope():
                self.phase_gdn_setup()
                if self.stop in ("gdnsetup",):
                    return self.finish()
                self.phase_gdn()
                if self.stop in ("gdn", "gdn_h0"):
                    return self.finish()
                with kb.scope():
                    self.phase_gdn_out()
                if self.stop == "gdnout":
                    return self.finish()
            with kb.scope():
                self.phase_merge()
            if self.stop == "merge":
                return self.finish()
        for hp in range(2):
            with kb.scope():
                self.phase_moe_half(hp)
            if self.stop in ("router", "moe1", "half"):
                return self.finish()
        return self.finish()

    def finish(self):
        self.kb.finish(self.fin)
        self.kb.finished = True
        for es in reversed(getattr(self.kb, "scopes", [])):
            es.close()
        self.kb.root.close()
        return self.nc


def _fm(v, nch):
    return np.ascontiguousarray(np.asarray(v, np.float32).reshape(nch, 128).T)


def host_inputs(inputs, b):
    f = lambda a: np.ascontiguousarray(np.asarray(a, np.float32))
    m = {}
    m["x"] = f(inputs["x"][b])
    m["ctx"] = f(inputs["ctx"][b])
    cs = np.stack([_fm(inputs["c"][b], 8), _fm(inputs["c_ctx"], 8)], axis=-1)
    m["cs"] = f(cs)
    m["w_mod"] = f(inputs["w_mod"][0])
    bm = np.asarray(inputs["b_mod"][0], np.float32)
    bmt = _fm(bm, 48)
    order = list(range(0, 16)) + list(range(24, 40)) + list(range(16, 24)) + list(range(40, 48))
    m["b_mod_t"] = f(bmt[:, order])
    m["b_mod"] = f(bm)
    m["norm1_t"] = _fm(inputs["norm1"][0], 8)
    m["norm2_t"] = _fm(inputs["norm2"][0], 8)
    m["w_in"] = f(inputs["w_in"][0])
    m["ident"] = np.eye(128, dtype=np.float32)
    m["s5_d_t"] = _fm(inputs["s5_d"][0], 4)
    def pdl(a):
        a = np.asarray(a, np.float32).reshape(2, 16, 2, 64)
        return f(a.transpose(2, 3, 0, 1).reshape(128, 32))
    m["lam_re_t"] = pdl(inputs["s5_lam_re"][0])
    m["lam_im_t"] = pdl(inputs["s5_lam_im"][0])
    m["logstep_t"] = pdl(np.broadcast_to(np.asarray(inputs["s5_log_step"][0], np.float32)[:, :, None], (2, 32, 64)))
    def blk(re, im, cn):
        out = np.zeros((2, 64, 2, 2, 16, 2, 16), np.float32)
        for ri, arr in enumerate((re, im)):
            arr = np.asarray(arr, np.float32)
            arr = arr if cn else arr.transpose(0, 1, 3, 2)
            arr = arr.reshape(2, 16, 2, 64, 16)
            for g2 in range(2):
                out[g2, :, ri, :, :, g2, :] = arr[:, :, g2].transpose(2, 0, 1, 3)
        return f(out.reshape(128, 2, 32, 32))
    m["Bblk"] = blk(inputs["s5_b_re"][0], inputs["s5_b_im"][0], True)
    m["Cblk"] = blk(inputs["s5_c_re"][0], inputs["s5_c_im"][0], False)
    r_ = np.arange(128)[:, None]; c_ = np.arange(128)[None, :]
    same = (r_ // 64) == (c_ // 64)
    NEG = -30000.0
    Mf = (same & (r_ <= c_)).astype(np.float32); Mb = (same & (r_ >= c_)).astype(np.float32)
    gmk = [Mf, Mb, np.where(same & (r_ > c_), 0.0, NEG), np.where(same & (r_ < c_), 0.0, NEG), np.where(same & (r_ <= c_), 0.0, NEG),
           np.tile((r_ < 64), (1, 128)).astype(np.float32), np.tile((r_ >= 64), (1, 128)).astype(np.float32), same.astype(np.float32),
           -Mf, -Mb, np.where(same & (r_ >= c_), 0.0, NEG)]
    m["gmask"] = f(np.stack([np.asarray(a, np.float32) for a in gmk], axis=1))
    cw = np.asarray(inputs["gdn_conv"][0], np.float32)
    m["conv_t"] = f(cw.reshape(5, 12, 128).transpose(2, 1, 0))
    m["alog_dtb"] = f(np.concatenate([np.asarray(inputs["gdn_a_log"][0]).reshape(8), np.asarray(inputs["gdn_dt_bias"][0]).reshape(8)]))
    m["gdn_norm"] = f(inputs["gdn_norm"][0])
    m["w_glu"] = f(inputs["s5_w_glu"][0])
    m["b_glu_t"] = _fm(inputs["s5_b_glu"][0], 4)
    m["w_ba"] = f(inputs["w_branch_a"][0])
    m["w_bb"] = f(inputs["w_branch_b"][0])
    m["w_out"] = f(inputs["w_out"][0])
    m["w_router"] = f(inputs["w_router"][0])
    m["b_router"] = f(inputs["b_router"][0])
    m["w_gu"] = f(inputs["w_gate_up"][0])
    bgu = np.asarray(inputs["b_gate_up"][0], np.float32)
    m["b_gu_t"] = f(bgu.reshape(NE, 16, 128).transpose(2, 0, 1))
    m["w_dn"] = f(inputs["w_down"][0])
    m["b_dn"] = f(inputs["b_down"][0])
    m["norm_f"] = f(inputs["norm_f"])
    m["iota1"] = f(np.tile(np.arange(1, TS5 + 1, dtype=np.float32)[None], (128, 1)))
    return m


_CACHE = {}


def kernel(**inputs):
    nb = 8
    if "nc" not in _CACHE:
        _CACHE["nc"] = Builder().build()
    nc = _CACHE["nc"]
    shared = host_inputs(inputs, 0)
    in_maps = []
    for b in range(nb):
        m = dict(shared)
        if b > 0:
            f = lambda a: np.ascontiguousarray(np.asarray(a, np.float32))
            m["x"] = f(inputs["x"][b])
            m["ctx"] = f(inputs["ctx"][b])
            m["cs"] = f(np.stack([_fm(inputs["c"][b], 8), _fm(inputs["c_ctx"], 8)], axis=-1))
        in_maps.append(m)
    res = run_bass_kernel_spmd(nc, in_maps, core_ids=list(range(nb)))
    out = np.stack([np.asarray(res.results[b]["out"], np.float32) for b in range(nb)], axis=0)
    return out
```

```python
import os
import numpy as np
from contextlib import ExitStack
import concourse.bass as bass
import concourse.mybir as mybir
from concourse.bass_utils import run_bass_kernel_spmd

F32 = mybir.dt.float32
BF16 = mybir.dt.bfloat16
I32 = mybir.dt.int32
AF = mybir.ActivationFunctionType
ALU = mybir.AluOpType
AX = mybir.AxisListType

D = 1024
L = 2048
CTX = 256
TOK = L + CTX
NT = TOK // 128
EPS = 1e-6
NE = 32
TS5 = 128
POST_ENG = os.environ.get('KPOST', 'gpsimd')
IN_COLS = 4624
OFF_U, OFF_QKV, OFF_GATE, OFF_B, OFF_A, OFF_BR = 0, 512, 2048, 2560, 2568, 2576
BLOCKS = [(0, 256)] + [(256 + 512 * i, 512) for i in range(4)]


class Buf:
    __slots__ = ("name", "w", "r")

    def __init__(self, name):
        self.name = name
        self.w = None
        self.r = {}


class KB:
    def __init__(self, nc, same_engine_sync=True):
        self.nc = nc
        self.es = ExitStack()
        self.root = self.es
        self.same = same_engine_sync
        self.engs = {}
        self.sems = {}
        for name in ("tensor", "vector", "scalar", "gpsimd", "sync"):
            eng = getattr(nc, name)
            sem = self.root.enter_context(nc.semaphore("s_" + name))
            self.sems["E" + name] = sem
            self.engs[name] = dict(eng=eng, key="E" + name, count=0, waited={})
        self.nbuf = 0
        self.dmasems = {}
        self.nalloc = 0

    def sb(self, name, shape, dt=F32):
        nb = int(np.prod(shape[1:])) * (2 if dt == BF16 else 4)
        self.nalloc += (nb + 31) // 32 * 32
        if os.environ.get("KALLOC"):
            print("alloc", name, shape, nb, "total", self.nalloc)
        self.nnames = getattr(self, "nnames", 0) + 1
        return self.es.enter_context(self.nc.sbuf_tensor("sb%d_%s" % (self.nnames, name), list(shape), dt))

    def ps(self, name, shape, dt=F32):
        return self.es.enter_context(self.nc.psum_tensor("pp_" + name, list(shape), dt))

    def buf(self, name=None):
        self.nbuf += 1
        return Buf((name or "b") + "_%d" % self.nbuf)

    def _wait(self, en, key, val):
        e = self.engs[en]
        if key == e["key"] and ((not self.same) or en == "tensor"):
            return
        if e["waited"].get(key, 0) >= val:
            return
        if key in self.dmasems:
            val = self.dmasems[key]
        e["eng"].wait_ge(self.sems[key], val)
        e["waited"][key] = val

    def _deps(self, en, reads, writes):
        need = {}
        for b in reads:
            if b.w is not None:
                k, v = b.w
                need[k] = max(need.get(k, 0), v)
        for b in writes:
            if b.w is not None:
                k, v = b.w
                need[k] = max(need.get(k, 0), v)
            for k, v in b.r.items():
                need[k] = max(need.get(k, 0), v)
        for k, v in need.items():
            self._wait(en, k, v)

    def _post(self, sig, reads, writes):
        k, v = sig
        for b in reads:
            b.r[k] = max(b.r.get(k, 0), v)
        for b in writes:
            b.w = sig
            b.r = {}

    def op(self, en, fn, reads=(), writes=(), sig=True):
        e = self.engs[en]
        self._deps(en, reads, writes)
        inst = fn(e["eng"])
        if sig:
            e["count"] += 1
            inst.then_inc(self.sems[e["key"]], 1)
            e["pending"] = False
            self._post((e["key"], e["count"]), reads, writes)
        else:
            assert en == "tensor"
            e["pending"] = True
            self._post((e["key"], e["count"] + 1), reads, writes)
        return inst

    NDSEM = 64

    def dma(self, en, out, in_, reads=(), writes=(), **kw):
        e = self.engs[en]
        self._deps(en, reads, writes)
        tgt = writes[0] if writes else reads[0]
        if not hasattr(self, "bufsem"):
            self.bufsem = {}
            self.dsem_list = []
        semkey = self.bufsem.get(tgt.name)
        if semkey is None:
            idx = len(self.bufsem) % self.NDSEM
            semkey = "DS%d" % idx
            self.bufsem[tgt.name] = semkey
            if semkey not in self.sems:
                self.sems[semkey] = self.root.enter_context(self.nc.semaphore("d%d" % idx))
                self.dmasems[semkey] = 0
        self.dmasems[semkey] += 16
        inst = e["eng"].dma_start(out=out, in_=in_, **kw)
        inst.then_inc(self.sems[semkey], 16)
        self._post((semkey, self.dmasems[semkey]), reads, writes)
        return inst

    def barrier(self):
        for en, e in self.engs.items():
            for en2, e2 in self.engs.items():
                if en2 != en and e2["count"] > 0:
                    self._wait(en, e2["key"], e2["count"])
            for k, v in self.dmasems.items():
                self._wait(en, k, v)

    def scope(self):
        kb = self

        class _S:
            def __enter__(s):
                s.old = kb.es
                s.base = kb.nalloc
                kb.es = ExitStack()
                kb.scopes = getattr(kb, "scopes", [])
                kb.scopes.append(kb.es)

            def __exit__(s, *a):
                if getattr(kb, "finished", False):
                    return
                kb.barrier()
                kb.es.close()
                kb.scopes.pop()
                kb.es = s.old
                kb.nalloc = s.base
        return _S()

    def finish(self, bufs):
        for b in bufs:
            if b.w is not None:
                self._wait("sync", b.w[0], b.w[1])
            for k, v in b.r.items():
                self._wait("sync", k, v)


class Pool:
    def __init__(self, kb, name, shape, dt, n, psum=False):
        self.tiles = []
        for i in range(n):
            t = (kb.ps if psum else kb.sb)("%s%d" % (name, i), shape, dt)
            self.tiles.append((t, kb.buf("%s%d" % (name, i))))
        self.i = 0

    def get(self):
        t = self.tiles[self.i % len(self.tiles)]
        self.i += 1
        return t


def _rev(ap2):
    return ap2[:, ::-1]


class Builder:
    def __init__(self, dbg=(), stop=None, same=True):
        self.dbg = set(dbg)
        self.stop = stop
        self.nc = bass.Bass("TRN2", target_bir_lowering=False)
        self.kb = KB(self.nc, same_engine_sync=same)
        self.ins = {}
        self.outs = {}
        self.fin = []

    def inp(self, name, shape):
        t = self.nc.dram_tensor(name, list(shape), F32, kind="ExternalInput").ap()
        self.ins[name] = t
        return t

    def outp(self, name, shape):
        t = self.nc.dram_tensor(name, list(shape), F32, kind="ExternalOutput").ap()
        self.outs[name] = t
        return t

    def dump(self, name, tile_ap, b, shape):
        if name not in self.dbg:
            return
        o = self.outp("dbg_" + name, shape)
        bo = self.kb.buf("dbg_" + name)
        self.kb.dma("sync", o, tile_ap, reads=[b], writes=[bo])
        self.fin.append(bo)

    def dump_bf(self, name, tile_ap, b, shape):
        if name not in self.dbg:
            return
        kb = self.kb
        t = kb.sb("dbgt_" + name, shape, F32)
        bt = kb.buf("dbgt_" + name)
        kb.op("vector", lambda e: e.tensor_copy(out=t[:], in_=tile_ap), reads=[b], writes=[bt])
        self.dump(name, t[:], bt, shape)

    def declare(self):
        i = self.inp
        self.x = i("x", [L, D])
        self.ctx = i("ctx", [CTX, D])
        self.cs_in = i("cs", [128, 8, 2])
        self.w_mod = i("w_mod", [D, 6 * D])
        self.b_mod_t = i("b_mod_t", [128, 48])
        self.b_mod = i("b_mod", [6 * D])
        self.norm1_t = i("norm1_t", [128, 8])
        self.norm2_t = i("norm2_t", [128, 8])
        self.w_in = i("w_in", [D, IN_COLS])
        self.ident = i("ident", [128, 128])
        self.s5_d_t = i("s5_d_t", [128, 4])
        self.lam_re_t = i("lam_re_t", [128, 32])
        self.lam_im_t = i("lam_im_t", [128, 32])
        self.logstep_t = i("logstep_t", [128, 32])
        self.Bblk = i("Bblk", [128, 2, 32, 32])
        self.Cblk = i("Cblk", [128, 2, 32, 32])
        self.iota1 = i("iota1", [128, TS5])
        self.gmask = i("gmask", [128, 11, 128])
        self.conv_t = i("conv_t", [128, 12, 5])
        self.alog_dtb = i("alog_dtb", [16])
        self.gdn_norm = i("gdn_norm", [128])
        self.w_glu = i("w_glu", [512, 512])
        self.b_glu_t = i("b_glu_t", [128, 4])
        self.w_ba = i("w_ba", [512, D])
        self.w_bb = i("w_bb", [512, D])
        self.w_out = i("w_out", [D, D])
        self.w_router = i("w_router", [D, NE])
        self.b_router = i("b_router", [NE])
        self.w_gu = i("w_gu", [NE, D, 2 * D])
        self.b_gu_t = i("b_gu_t", [128, NE, 16])
        self.w_dn = i("w_dn", [NE, D, D])
        self.b_dn = i("b_dn", [NE, D])
        self.norm_f = i("norm_f", [D])
        self.out = self.outp("out", [L, D])
        self.x1s = self.nc.dram_tensor("x1_scratch", [L, D], F32, kind="Internal").ap()

    def common(self):
        kb = self.kb
        self.PS = Pool(kb, "ps", [128, 512], F32, 8, psum=True)
        self.modT = kb.sb("modT", [128, 32, 2], F32)
        self.b_modT = kb.buf("modT")
        self.gt1_bc = kb.sb("gt1_bc", [128, D], F32)
        self.gt2_bc = kb.sb("gt2_bc", [128, D], F32)
        self.b_gt1 = kb.buf("gt1")
        self.b_gt2 = kb.buf("gt2")
        self.A1 = kb.sb("A1", [128, 8, 2], F32)
        self.b_A1 = kb.buf("A1")
        self.identf = kb.sb("identf", [128, 128], F32)
        self.b_ident = kb.buf("ident")
        kb.dma("sync", self.identf[:], self.ident, writes=[self.b_ident])

    def phase_mod(self):
        kb, nc = self.kb, self.nc
        cs = kb.sb("cs", [128, 8, 2], F32)
        b_cs = kb.buf("cs")
        kb.dma("sync", cs[:], self.cs_in, writes=[b_cs])
        sg = kb.sb("cs_sg", [128, 8, 2], F32)
        b_sg = kb.buf("cs_sg")
        kb.op("scalar", lambda e: e.activation(out=sg[:], in_=cs[:], func=AF.Sigmoid), reads=[b_cs], writes=[b_sg])
        css = kb.sb("css", [128, 8, 2], F32)
        b_css = kb.buf("css")
        kb.op("vector", lambda e: e.tensor_tensor(out=css[:], in0=cs[:], in1=sg[:], op=ALU.mult), reads=[b_cs, b_sg], writes=[b_css])
        csb = kb.sb("csb", [128, 8, 128], F32)
        b_csb = kb.buf("csb")
        kb.op("vector", lambda e: e.tensor_copy(out=csb[:], in_=css[:, :, 0:1].to_broadcast([128, 8, 128])), reads=[b_css], writes=[b_csb])
        bmt = kb.sb("bmt", [128, 48], F32)
        b_bmt = kb.buf("bmt")
        kb.dma("sync", bmt[:], self.b_mod_t, writes=[b_bmt])
        wpool = Pool(kb, "wmod", [128, 8, 512], F32, 2)
        wv = self.w_mod.rearrange("(kc p) n -> p kc n", p=128)
        pm, b_pm = self.PS.get()
        fm_groups = [0, 1, 2, 3, 6, 7, 8, 9]
        for gi, g in enumerate(fm_groups):
            wt, b_wt = wpool.get()
            kb.dma("sync" if gi % 2 == 0 else "scalar", wt[:], wv[:, :, 512 * g:512 * (g + 1)], writes=[b_wt])
            for cc in range(4):
                j = gi * 4 + cc
                for kc in range(8):
                    kb.op("tensor", lambda e, j=j, kc=kc, cc=cc, wt=wt: e.matmul(
                        pm[:, 2 * j:2 * j + 2], wt[:, kc, cc * 128:(cc + 1) * 128], css[:, kc, :],
                        start=(kc == 0), stop=(kc == 7)), reads=[b_wt, b_css], writes=[b_pm], sig=(kc == 7))
        kb.op("vector", lambda e: e.tensor_tensor(
            out=self.modT[:], in0=pm[:, 0:64].rearrange("p (j t) -> p j t", t=2),
            in1=self._bmt_sel(bmt), op=ALU.add), reads=[b_pm, b_bmt], writes=[self.b_modT])
        for which, (g0, dst, bdst) in enumerate([(4, self.gt1_bc, self.b_gt1), (10, self.gt2_bc, self.b_gt2)]):
            bb = kb.sb("bmodbc%d" % which, [128, D], F32)
            b_bb = kb.buf("bmodbc")
            kb.dma("sync", bb[:], self.b_mod[512 * g0:512 * g0 + D].partition_broadcast(128), writes=[b_bb])
            for half in range(2):
                g = g0 + half
                wt, b_wt = wpool.get()
                kb.dma("sync" if half == 0 else "scalar", wt[:], wv[:, :, 512 * g:512 * (g + 1)], writes=[b_wt])
                pg, b_pg = self.PS.get()
                for kc in range(8):
                    kb.op("tensor", lambda e, kc=kc, wt=wt, pg=pg: e.matmul(
                        pg[:], csb[:, kc, :], wt[:, kc, :], start=(kc == 0), stop=(kc == 7)),
                        reads=[b_wt, b_csb], writes=[b_pg], sig=(kc == 7))
                kb.op("vector", lambda e, pg=pg, half=half, dst=dst, bb=bb: e.tensor_tensor(
                    out=dst[:, 512 * half:512 * (half + 1)], in0=pg[:], in1=bb[:, 512 * half:512 * (half + 1)], op=ALU.add),
                    reads=[b_pg, b_bb], writes=[bdst])
        self.dump("modT", self.modT[:], self.b_modT, [128, 32, 2])
        self.dump("gt1", self.gt1_bc[:], self.b_gt1, [128, D])

    def _bmt_sel(self, bmt):
        return bmt[:, 0:32].unsqueeze(2).to_broadcast([128, 32, 2])

    def norm_to_T(self, src_tiles, dstT, b_dstT_blocks, scale_ap_fn, shift_ap_fn, tile_base, tag):
        raise NotImplementedError

    def phase_norm1(self):
        kb = self.kb
        n1 = kb.sb("norm1", [128, 8], F32)
        b_n1 = kb.buf("n1")
        kb.dma("sync", n1[:], self.norm1_t, writes=[b_n1])
        kb.op("vector", lambda e: e.scalar_tensor_tensor(
            out=self.A1[:], in0=self.modT[:, 8:16, :], scalar=1.0, in1=n1[:].unsqueeze(2).to_broadcast([128, 8, 2]),
            op0=ALU.add, op1=ALU.mult), reads=[self.b_modT, b_n1], writes=[self.b_A1])
        xin = Pool(kb, "xin", [128, D], F32, 3)
        xnp = Pool(kb, "xn", [128, D], F32, 2)
        junk = Pool(kb, "junk", [128, D], F32, 1)
        stat = Pool(kb, "stat", [128, 4], F32, 4)
        for tt in range(NT):
            which = 1 if tt < 2 else 0
            src = self.ctx[tt * 128:(tt + 1) * 128, :] if tt < 2 else self.x[(tt - 2) * 128:(tt - 1) * 128, :]
            xt, b_xt = xin.get()
            kb.dma("sync" if tt % 2 == 0 else "scalar", xt[:], src, writes=[b_xt])
            self._norm_tile(xt, b_xt, tt, which, self.A1, self.b_A1, 0, xnp, junk, stat, self.hT, self.b_hT[tt])
        self.dump_bf("hT", self.hT[:, :, 0:512], self.b_hT[3], [128, 8, 512]) if False else None
        if "hT" in self.dbg:
            allb = kb.buf("hTall")
            t = kb.sb("dbg_hT_t", [128, 8, 384], F32)
            kb.op("vector", lambda e: e.tensor_copy(out=t[:], in_=self.hT[:, :, 128:512]), reads=self.b_hT[1:4], writes=[allb])
            self.dump("hT", t[:], allb, [128, 8, 384])

    def _norm_tile(self, xt, b_xt, tt, which, A, b_A, shift_chunk0, xnp, junk, stat, dstT, b_dst):
        kb = self.kb
        jt, b_jt = junk.get()
        st, b_st = stat.get()
        kb.op("scalar", lambda e: e.activation(out=jt[:], in_=xt[:], func=AF.Square, accum_out=st[:, 0:1]),
              reads=[b_xt], writes=[b_jt, b_st])
        kb.op("scalar", lambda e: e.activation(out=st[:, 1:2], in_=st[:, 0:1], func=AF.Sqrt, bias=EPS, scale=1.0 / D),
              reads=[b_st], writes=[b_st])
        kb.op("vector", lambda e: e.reciprocal(out=st[:, 2:3], in_=st[:, 1:2]), reads=[b_st], writes=[b_st])
        xn, b_xn = xnp.get()
        kb.op("scalar", lambda e: e.activation(out=xn[:], in_=xt[:], func=AF.Identity, scale=st[:, 2:3]),
              reads=[b_xt, b_st], writes=[b_xn])
        for half in range(2):
            pt, b_pt = self.PS.get()
            for q in range(4):
                kc = half * 4 + q
                kb.op("tensor", lambda e, kc=kc, q=q, pt=pt: e.transpose(
                    pt[:, q * 128:(q + 1) * 128], xn[:, kc * 128:(kc + 1) * 128], self.identf[:]),
                    reads=[b_xn, self.b_ident], writes=[b_pt])
            for q in range(4):
                kc = half * 4 + q
                kb.op("vector", lambda e, kc=kc, q=q, pt=pt: e.tensor_scalar(
                    out=dstT[:, kc, tt * 128:(tt + 1) * 128], in0=pt[:, q * 128:(q + 1) * 128],
                    scalar1=A[:, kc, which:which + 1], scalar2=self.modT[:, shift_chunk0 + kc, which:which + 1],
                    op0=ALU.mult, op1=ALU.add), reads=[b_pt, b_A, self.b_modT], writes=[b_dst])

    def load_w_bf16(self, dst, b_dst, src_rows_ap, ncols, col0=0):
        kb = self.kb
        kcs = dst.shape[1]
        v = src_rows_ap.rearrange("(kc p) n -> p kc n", p=128)
        for kc in range(kcs):
            kb.dma("gpsimd", dst[:, kc, :], v[:, kc, col0:col0 + ncols], writes=[b_dst])

    def phase_u(self):
        kb = self.kb
        wu = kb.sb("wu", [128, 8, 512], BF16)
        b_wu = kb.buf("wu")
        self.load_w_bf16(wu, b_wu, self.w_in, 512, OFF_U)
        dsk = kb.sb("s5d", [128, 4], F32)
        b_dsk = kb.buf("s5d")
        kb.dma("sync", dsk[:], self.s5_d_t, writes=[b_dsk])
        for oc in range(4):
            for (s0, n) in BLOCKS:
                pt, b_pt = self.PS.get()
                tiles = range(s0 // 128, (s0 + n) // 128)
                for kc in range(8):
                    kb.op("tensor", lambda e, kc=kc, pt=pt, oc=oc, s0=s0, n=n: e.matmul(
                        pt[:, 0:n], wu[:, kc, oc * 128:(oc + 1) * 128], self.hT[:, kc, s0:s0 + n],
                        start=(kc == 0), stop=(kc == 7)), reads=[b_wu] + [self.b_hT[t] for t in tiles], writes=[b_pt], sig=(kc == 7))
                kb.op("scalar", lambda e, pt=pt, oc=oc, s0=s0, n=n: e.activation(
                    out=self.uT[:, oc, s0:s0 + n], in_=pt[:, 0:n], func=AF.Identity), reads=[b_pt], writes=[self.b_uT])
                if s0 >= CTX:
                    kb.op("scalar", lambda e, pt=pt, oc=oc, s0=s0, n=n: e.activation(
                        out=self.yT[:, oc, s0 - CTX:s0 - CTX + n], in_=pt[:, 0:n], func=AF.Identity, scale=dsk[:, oc:oc + 1]),
                        reads=[b_pt, b_dsk], writes=[self.b_yT[oc]])
        if "uT" in self.dbg:
            t = kb.sb("dbg_uT_t", [128, 4, 512], F32)
            bt = kb.buf("dbg_uT")
            kb.op("vector", lambda e: e.tensor_copy(out=t[:], in_=self.uT[:, :, 0:512]), reads=[self.b_uT], writes=[bt])
            self.dump("uT", t[:], bt, [128, 4, 512])


    def sincos(self, ang, b_ang, n, want_cos, out_ap, b_out, scale=1.0):
        kb = self.kb
        with kb.scope():
            y = kb.sb("sc_y", [128, n], F32)
            ki = kb.sb("sc_k", [128, n], I32)
            kf = kb.sb("sc_kf", [128, n], F32)
            b = kb.buf("sc")
            off = 0.75 if want_cos else 0.5
            kb.op("vector", lambda e: e.tensor_scalar(out=y[:], in0=ang, scalar1=1.0 / (2 * np.pi), scalar2=off + 64.0,
                                                     op0=ALU.mult, op1=ALU.add), reads=[b_ang], writes=[b])
            kb.op("vector", lambda e: e.tensor_copy(out=ki[:], in_=y[:]), reads=[b], writes=[b])
            kb.op("vector", lambda e: e.tensor_copy(out=kf[:], in_=ki[:]), reads=[b], writes=[b])
            kb.op("vector", lambda e: e.tensor_tensor(out=y[:], in0=y[:], in1=kf[:], op=ALU.subtract), reads=[b], writes=[b])
            kb.op("vector", lambda e: e.scalar_tensor_tensor(out=kf[:], in0=y[:], scalar=0.0, in1=y[:], op0=ALU.is_lt, op1=ALU.add),
                  reads=[b], writes=[b])
            kb.op("scalar", lambda e: e.activation(out=y[:], in_=kf[:], func=AF.Sin, scale=6.2831, bias=-3.14155),
                  reads=[b], writes=[b])
            yv = y[:] if len(out_ap.shape) == 2 else y[:].rearrange("p (a t) -> p a t", t=out_ap.shape[-1])
            kb.op("scalar", lambda e: e.activation(out=out_ap, in_=yv, func=AF.Identity, scale=float(scale)),
                  reads=[b], writes=[b_out])

    def phase_s5_setup(self):
        kb = self.kb
        T = TS5
        self.rho = kb.sb("s5_rho", [128, 32], F32)
        self.Bdrv = kb.sb("Bdrv", [128, 2, 4, 2, 128], BF16)
        self.b_Bdrv = kb.buf("Bdrv")
        self.Crd = kb.sb("Crd", [128, 32, 2, 32], BF16)
        self.b_Crd = kb.buf("Crd")
        self.Tc = kb.sb("Tc", [128, 32 * T], BF16)
        self.b_Tc = kb.buf("Tc")
        self.Tsn = kb.sb("Tsn", [128, 32, 2, T], BF16)
        self.b_Tsn = kb.buf("Tsn")
        self.b_s5c = kb.buf("s5setup")
        with kb.scope():
            self._s5_setup_body()

    def _s5_setup_body(self):
        kb = self.kb
        T = TS5
        ld = lambda name, src, shape: self._ld(name, src, shape)
        lre, b_lre = ld("lre", self.lam_re_t, [128, 32])
        lim, b_lim = ld("lim", self.lam_im_t, [128, 32])
        lst, b_lst = ld("lst", self.logstep_t, [128, 32])
        Bb, b_Bb = ld("Bblk", self.Bblk, [128, 2, 32, 32])
        Cb, b_Cb = ld("Cblk", self.Cblk, [128, 2, 32, 32])
        io, b_io = ld("iota1", self.iota1, [128, T])
        V = lambda name: (kb.sb("s5v_" + name, [128, 32], F32))
        b = self.b_s5c
        deps = [b_lre, b_lim, b_lst, b]
        mag = self.rho
        lr, dt, ang, ar, ai, den, am1, fr, fi, t1, t2 = [V(n) for n in
            ("lr", "dt", "ang", "ar", "ai", "den", "am1", "fr", "fi", "t1", "t2")]
        v = lambda fn: kb.op("vector", fn, reads=deps, writes=[b])
        a = lambda fn: kb.op("scalar", fn, reads=deps, writes=[b])
        v(lambda e: e.tensor_scalar(out=lr[:], in0=lre[:], scalar1=-1e-4, scalar2=0.0, op0=ALU.min, op1=ALU.add))
        a(lambda e: e.activation(out=dt[:], in_=lst[:], func=AF.Exp))
        v(lambda e: e.tensor_tensor(out=t1[:], in0=lr[:], in1=dt[:], op=ALU.mult))
        a(lambda e: e.activation(out=mag[:], in_=t1[:], func=AF.Exp))
        v(lambda e: e.tensor_tensor(out=ang[:], in0=lim[:], in1=dt[:], op=ALU.mult))
        sn = V("sn0"); cs = V("cs0")
        self.sincos(ang[:], b, 32, False, sn[:], b)
        self.sincos(ang[:], b, 32, True, cs[:], b)
        v(lambda e: e.tensor_tensor(out=ar[:], in0=mag[:], in1=cs[:], op=ALU.mult))
        v(lambda e: e.tensor_tensor(out=ai[:], in0=mag[:], in1=sn[:], op=ALU.mult))
        v(lambda e: e.tensor_tensor(out=t1[:], in0=lr[:], in1=lr[:], op=ALU.mult))
        v(lambda e: e.tensor_tensor(out=t2[:], in0=lim[:], in1=lim[:], op=ALU.mult))
        v(lambda e: e.tensor_tensor(out=den[:], in0=t1[:], in1=t2[:], op=ALU.add))
        v(lambda e: e.reciprocal(out=den[:], in_=den[:]))
        v(lambda e: e.tensor_scalar(out=am1[:], in0=ar[:], scalar1=-1.0, scalar2=0.0, op0=ALU.add, op1=ALU.add))
        v(lambda e: e.tensor_tensor(out=t1[:], in0=am1[:], in1=lr[:], op=ALU.mult))
        v(lambda e: e.tensor_tensor(out=t2[:], in0=ai[:], in1=lim[:], op=ALU.mult))
        v(lambda e: e.tensor_tensor(out=fr[:], in0=t1[:], in1=t2[:], op=ALU.add))
        v(lambda e: e.tensor_tensor(out=fr[:], in0=fr[:], in1=den[:], op=ALU.mult))
        v(lambda e: e.tensor_tensor(out=t1[:], in0=ai[:], in1=lr[:], op=ALU.mult))
        v(lambda e: e.tensor_tensor(out=t2[:], in0=am1[:], in1=lim[:], op=ALU.mult))
        v(lambda e: e.tensor_tensor(out=fi[:], in0=t1[:], in1=t2[:], op=ALU.subtract))
        v(lambda e: e.tensor_tensor(out=fi[:], in0=fi[:], in1=den[:], op=ALU.mult))
        Bbar = kb.sb("Bbar", [128, 2, 32, 32], F32)
        tb = kb.sb("Bbar_t", [128, 32, 32], F32)
        b_Bbar = kb.buf("Bbar")
        frb = fr[:].unsqueeze(2).to_broadcast([128, 32, 32])
        fib = fi[:].unsqueeze(2).to_broadcast([128, 32, 32])
        vb = lambda fn: kb.op("vector", fn, reads=[b, b_Bb], writes=[b_Bbar])
        vb(lambda e: e.tensor_tensor(out=Bbar[:, 0], in0=Bb[:, 0], in1=frb, op=ALU.mult))
        vb(lambda e: e.tensor_tensor(out=tb[:], in0=Bb[:, 1], in1=fib, op=ALU.mult))
        vb(lambda e: e.tensor_tensor(out=Bbar[:, 0], in0=Bbar[:, 0], in1=tb[:], op=ALU.subtract))
        vb(lambda e: e.tensor_tensor(out=Bbar[:, 1], in0=Bb[:, 1], in1=frb, op=ALU.mult))
        vb(lambda e: e.tensor_tensor(out=tb[:], in0=Bb[:, 0], in1=fib, op=ALU.mult))
        vb(lambda e: e.tensor_tensor(out=Bbar[:, 1], in0=Bbar[:, 1], in1=tb[:], op=ALU.add))
        for d in range(2):
            for qd in range(4):
                pt, b_pt = self.PS.get()
                for ri in range(2):
                    blk = (d * 4 + qd) * 4
                    kb.op("tensor", lambda e, ri=ri, blk=blk, pt=pt: e.transpose(
                        pt[:, ri * 128:(ri + 1) * 128], Bbar[:, ri, blk:blk + 4, :], self.identf[:]),
                        reads=[b_Bbar, self.b_ident], writes=[b_pt])
                kb.op("scalar", lambda e, d=d, qd=qd, pt=pt: e.activation(
                    out=self.Bdrv[:, d, qd, :, :], in_=pt[:, 0:256].rearrange("p (r n) -> p r n", r=2), func=AF.Identity),
                    reads=[b_pt], writes=[self.b_Bdrv])
        kb.op("scalar", lambda e: e.activation(out=self.Crd[:, :, 0, :], in_=Cb[:, 0], func=AF.Identity),
              reads=[b_Cb], writes=[self.b_Crd])
        kb.op("scalar", lambda e: e.activation(out=self.Crd[:, :, 1, :], in_=Cb[:, 1], func=AF.Identity, scale=-1.0),
              reads=[b_Cb], writes=[self.b_Crd])
        ph = kb.sb("s5_ph", [128, 32, T], F32)
        b_ph = kb.buf("s5ph")
        kb.op("vector", lambda e: e.tensor_tensor(out=ph[:], in0=ang[:].unsqueeze(2).to_broadcast([128, 32, T]),
                                                 in1=io[:].unsqueeze(1).to_broadcast([128, 32, T]), op=ALU.mult),
              reads=[b, b_io], writes=[b_ph])
        for a0 in range(0, 32, 8):
            pha = ph[:, a0:a0 + 8, :].rearrange("p a t -> p (a t)")
            self.sincos(pha, b_ph, 8 * T, True, self.Tc[:, a0 * T:(a0 + 8) * T], self.b_Tc)
            self.sincos(pha, b_ph, 8 * T, False, self.Tsn[:, a0:a0 + 8, 0, :], self.b_Tsn)
            self.sincos(pha, b_ph, 8 * T, False, self.Tsn[:, a0:a0 + 8, 1, :], self.b_Tsn, scale=-1.0)
        self.dump("rho", self.rho[:], b, [128, 32])
        self.dump("fr", fr[:], b, [128, 32])
        self.dump("fi", fi[:], b, [128, 32])
        self.dump("Tc", self.Tc[:, 0:4 * T], self.b_Tc, [128, 4 * T])

    def _ld(self, name, src, shape, dt=F32, q="sync"):
        t = self.kb.sb(name, shape, dt)
        b = self.kb.buf(name)
        self.kb.dma(q, t[:], src, writes=[b])
        return t, b

    def phase_s5(self):
        kb = self.kb
        T = TS5
        NCK = TOK // T
        Tc3 = self.Tc[:].rearrange("p (a t) -> p a t", t=T)
        G = kb.sb("s5_G", [128, 32, 2, T], F32)
        b_G = [kb.buf("s5G%d" % i) for i in range(32)]
        carry = kb.sb("s5_carry", [128, 32, 2], F32)
        b_carry0 = kb.buf("s5carry")
        kb.op("vector", lambda e: e.memset(carry[:], 0.0), writes=[b_carry0])
        b_carryg = {}
        P1p = Pool(kb, "s5P1", [128, 2, T], F32, 4)
        P2p = Pool(kb, "s5P2", [128, 2, T], F32, 4)
        Vp = Pool(kb, "s5V", [128, 2, T], F32, 8)
        Q1p = Pool(kb, "s5Q1", [128, 2, T], F32, 4)
        Q2p = Pool(kb, "s5Q2", [128, 2, T], F32, 4)
        Hp = Pool(kb, "s5H", [128, 2, T], BF16, 8)
        ctmp = kb.sb("s5_ctmp", [128, 32, 2], F32)
        ctmp2 = kb.sb("s5_ctmp2", [128, 32, 2], F32)
        tabs = [self.b_Tc, self.b_Tsn]
        for ck in range(NCK):
            is_lat = ck >= CTX // T
            for qd in range(4):
                for d in range(2):
                    if d == 0:
                        s0 = ck * T
                    elif not is_lat:
                        s0 = CTX - (ck + 1) * T
                    else:
                        s0 = TOK - (ck - CTX // T + 1) * T
                    if is_lat:
                        py, b_py = self.PS.get()
                    gkey = (qd, d)
                    if gkey not in b_carryg:
                        b_carryg[gkey] = kb.buf("s5carry%d%d" % gkey)
                        b_carryg[gkey].w = b_carry0.w
                    b_carry = b_carryg[gkey]
                    st = []
                    for ppq in range(4):
                        pd = d * 16 + qd * 4 + ppq
                        rhs = self.uT[ppq * 32:(ppq + 1) * 32, qd, s0:s0 + T]
                        if d == 1:
                            rhs = rhs[:, ::-1]
                        pdv, b_pdv = self.PS.get()
                        for ri in range(2):
                            kb.op("tensor", lambda e, ri=ri, pdv=pdv, rhs=rhs, ppq=ppq: e.matmul(
                                pdv[:, ri * T:(ri + 1) * T], self.Bdrv[ppq * 32:(ppq + 1) * 32, d, qd, ri, :], rhs,
                                start=True, stop=True, tile_position=(ppq * 32, 0)),
                                reads=[self.b_Bdrv, self.b_uT], writes=[b_pdv], sig=(ri == 1))
                        Dv = pdv[:, 0:2 * T].rearrange("p (r t) -> p r t", r=2)
                        cosb = Tc3[:, pd:pd + 1, :].to_broadcast([128, 2, T])
                        st.append(dict(pd=pd, ppq=ppq, Dv=Dv, b_pdv=b_pdv, cosb=cosb, P1=P1p.get(), P2=P2p.get(), V=Vp.get()))
                    for x in st:
                        kb.op("vector", lambda e, x=x: e.tensor_tensor(out=x["P1"][0][:], in0=x["Dv"], in1=x["cosb"], op=ALU.mult),
                              reads=[x["b_pdv"]] + tabs, writes=[x["P1"][1]])
                    for x in st:
                        kb.op("vector", lambda e, x=x: e.tensor_tensor(out=x["P2"][0][:], in0=x["Dv"][:, ::-1, :], in1=self.Tsn[:, x["pd"]], op=ALU.mult),
                              reads=[x["b_pdv"]] + tabs, writes=[x["P2"][1]])
                    for x in st:
                        kb.op("vector", lambda e, x=x: e.tensor_tensor(out=x["V"][0][:], in0=x["P1"][0][:], in1=x["P2"][0][:], op=ALU.add),
                              reads=[x["P1"][1], x["P2"][1]], writes=[x["V"][1]])
                    for ri in range(2):
                        for x in st:
                            pd = x["pd"]
                            kb.op("vector", lambda e, ri=ri, x=x, pd=pd: e.tensor_tensor_scan(
                                out=G[:, pd, ri, :], data0=self.rho[:, pd:pd + 1].to_broadcast([128, T]), data1=x["V"][0][:, ri, :],
                                initial=carry[:, pd, ri:ri + 1], op0=ALU.mult, op1=ALU.add),
                                reads=[x["V"][1], b_carry, self.b_s5c], writes=[b_G[pd]])
                    if ck < NCK - 1:
                        pd0 = d * 16 + qd * 4
                        Gl = G[:, pd0:pd0 + 4, :, T - 1]
                        cl = Tc3[:, pd0:pd0 + 4, T - 1:T].to_broadcast([128, 4, 2])
                        sl = self.Tsn[:, pd0:pd0 + 4, :, T - 1]
                        gb_ = [b_G[pd0 + q] for q in range(4)]
                        c1 = ctmp[:, pd0:pd0 + 4, :]
                        c2 = ctmp2[:, pd0:pd0 + 4, :]
                        kb.op("vector", lambda e, Gl=Gl, cl=cl, c1=c1: e.tensor_tensor(out=c1, in0=Gl, in1=cl, op=ALU.mult),
                              reads=gb_ + tabs, writes=[b_carry])
                        kb.op("vector", lambda e, Gl=Gl, sl=sl, c2=c2: e.tensor_tensor(out=c2, in0=Gl[:, :, ::-1], in1=sl, op=ALU.mult),
                              reads=gb_ + tabs, writes=[b_carry])
                        kb.op("vector", lambda e, c1=c1, c2=c2, pd0=pd0: e.tensor_tensor(out=carry[:, pd0:pd0 + 4, :], in0=c1, in1=c2, op=ALU.subtract),
                              reads=[b_carry], writes=[b_carry])
                    if is_lat:
                        for x in st:
                            x["Q1"] = Q1p.get(); x["Q2"] = Q2p.get(); x["H"] = Hp.get()
                        for x in st:
                            kb.op(POST_ENG, lambda e, x=x: e.tensor_tensor(out=x["Q1"][0][:], in0=G[:, x["pd"]], in1=x["cosb"], op=ALU.mult),
                                  reads=[b_G[x["pd"]]] + tabs, writes=[x["Q1"][1]])
                        for x in st:
                            kb.op(POST_ENG, lambda e, x=x: e.tensor_tensor(out=x["Q2"][0][:], in0=G[:, x["pd"], ::-1, :], in1=self.Tsn[:, x["pd"]], op=ALU.mult),
                                  reads=[b_G[x["pd"]]] + tabs, writes=[x["Q2"][1]])
                        for x in st:
                            kb.op(POST_ENG, lambda e, x=x: e.tensor_tensor(out=x["H"][0][:], in0=x["Q1"][0][:], in1=x["Q2"][0][:], op=ALU.subtract),
                                  reads=[x["Q1"][1], x["Q2"][1]], writes=[x["H"][1]])
                        for x in st:
                            for ri in range(2):
                                kb.op("tensor", lambda e, ri=ri, x=x: e.matmul(
                                    py[x["ppq"] * 32:(x["ppq"] + 1) * 32, 0:T], self.Crd[:, x["pd"], ri, :], x["H"][0][:, ri, :],
                                    start=(ri == 0), stop=(ri == 1), tile_position=(0, x["ppq"] * 32)),
                                    reads=[self.b_Crd, x["H"][1]], writes=[b_py], sig=(ri == 1))
                        l0 = s0 - CTX
                        src = py[:, 0:T] if d == 0 else py[:, 0:T][:, ::-1]
                        kb.op("vector", lambda e, src=src, qd=qd, l0=l0: e.tensor_tensor(
                            out=self.yT[:, qd, l0:l0 + T], in0=src, in1=self.yT[:, qd, l0:l0 + T], op=ALU.add),
                            reads=[b_py], writes=[self.b_yT[qd]])
        if "yT" in self.dbg:
            allb = kb.buf("yTall")
            t = kb.sb("dbg_yT_t", [128, 4, 512], F32)
            kb.op("vector", lambda e: e.tensor_copy(out=t[:], in_=self.yT[:, :, 0:512]), reads=self.b_yT, writes=[allb])
            self.dump("yT", t[:], allb, [128, 4, 512])
            t2 = kb.sb("dbg_yT_t2", [128, 4, 512], F32)
            allb2 = kb.buf("yTall2")
            kb.op("vector", lambda e: e.tensor_copy(out=t2[:], in_=self.yT[:, :, 1536:2048]), reads=self.b_yT, writes=[allb2])
            self.dbg.add("yT2")
            self.dump("yT2", t2[:], allb2, [128, 4, 512])


    def phase_gdn_setup(self):
        kb = self.kb
        self.gm, self.b_gm = self._ld("gmask", self.gmask, [128, 11, 128])
        self.cw, self.b_cw = self._ld("convw", self.conv_t, [128, 12, 5])
        self.beta = kb.sb("g_beta", [128, NT, 8], F32)
        self.gg = kb.sb("g_g", [128, NT, 8], F32)
        self.E1 = kb.sb("g_E1", [128, NT, 8], F32)
        self.E2 = kb.sb("g_E2", [128, NT, 8], F32)
        self.DL = kb.sb("g_DL", [128, 2, NT, 8], F32)
        self.b_gs = kb.buf("gscal")
        b = self.b_gs
        with kb.scope():
            wab = kb.sb("wab", [128, 8, 16], BF16)
            b_wab = kb.buf("wab")
            self.load_w_bf16(wab, b_wab, self.w_in, 16, OFF_B)
            ab = kb.sb("alogdtb", [128, 16], F32)
            b_ab = kb.buf("alogdtb")
            kb.dma("sync", ab[:], self.alog_dtb.partition_broadcast(128), writes=[b_ab])
            pz, b_pz = self.PS.get()
            for tt in range(NT):
                for kc in range(8):
                    kb.op("tensor", lambda e, tt=tt, kc=kc: e.matmul(
                        pz[:, tt * 16:(tt + 1) * 16], self.hT[:, kc, tt * 128:(tt + 1) * 128], wab[:, kc, :],
                        start=(kc == 0), stop=(kc == 7)), reads=[b_wab, self.b_hT[tt]], writes=[b_pz], sig=(kc == 7))
            zab = pz[:, 0:NT * 16].rearrange("p (t c) -> p t c", c=16)
            kb.op("scalar", lambda e: e.activation(out=self.beta[:], in_=zab[:, :, 0:8], func=AF.Sigmoid), reads=[b_pz], writes=[b])
            if self.stop == "gs1":
                return
            t1 = kb.sb("g_t1", [128, NT, 8], F32)
            ea = kb.sb("g_ea", [128, 8], F32)
            kb.op("vector", lambda e: e.tensor_tensor(out=t1[:], in0=zab[:, :, 8:16], in1=ab[:, 8:16].unsqueeze(1).to_broadcast([128, NT, 8]),
                                                     op=ALU.add), reads=[b_pz, b_ab], writes=[b])
            kb.op("scalar", lambda e: e.activation(out=t1[:], in_=t1[:], func=AF.Exp), reads=[b], writes=[b])
            kb.op("scalar", lambda e: e.activation(out=t1[:], in_=t1[:], func=AF.Ln, bias=1.0), reads=[b], writes=[b])
            kb.op("scalar", lambda e: e.activation(out=ea[:], in_=ab[:, 0:8], func=AF.Exp), reads=[b_ab], writes=[b])
            kb.op("vector", lambda e: e.scalar_tensor_tensor(out=self.gg[:], in0=t1[:], scalar=-1.0,
                                                            in1=ea[:].unsqueeze(1).to_broadcast([128, NT, 8]), op0=ALU.mult, op1=ALU.mult),
                  reads=[b], writes=[b])
            if self.stop == "gs2":
                return
            gflat = self.gg[:].rearrange("p t x -> p (t x)")
            gc = kb.sb("g_gcum", [128, 2, NT, 8], F32)
            for d in range(2):
                pc, b_pc = self.PS.get()
                kb.op("tensor", lambda e, d=d, pc=pc: e.matmul(pc[:, 0:NT * 8], self.gm[:, d, :], gflat, start=True, stop=True),
                      reads=[b, self.b_gm], writes=[b_pc])
                kb.op("vector", lambda e, d=d, pc=pc: e.tensor_copy(out=gc[:, d].rearrange("p t x -> p (t x)"), in_=pc[:, 0:NT * 8]),
                      reads=[b_pc], writes=[b])
            if self.stop == "gs3":
                return
            pl, b_pl = self.PS.get()
            kb.op("tensor", lambda e: e.matmul(pl[:, 0:NT * 8], self.gm[:, 7, :], gflat, start=True, stop=True),
                  reads=[b, self.b_gm], writes=[b_pl])
            glv = pl[:, 0:NT * 8].rearrange("p (t x) -> p t x", x=8)
            for d in range(2):
                xs = slice(d * 4, d * 4 + 4)
                kb.op("scalar", lambda e, d=d, xs=xs: e.activation(out=self.E1[:, :, xs], in_=gc[:, d, :, xs], func=AF.Exp),
                      reads=[b], writes=[b])
                kb.op("vector", lambda e, d=d, xs=xs: e.tensor_tensor(out=t1[:, :, xs], in0=glv[:, :, xs], in1=gc[:, d, :, xs], op=ALU.subtract),
                      reads=[b, b_pl], writes=[b])
                kb.op("scalar", lambda e, xs=xs: e.activation(out=self.E2[:, :, xs], in_=t1[:, :, xs], func=AF.Exp), reads=[b], writes=[b])
            if self.stop == "gs4":
                return
            for c2 in ([1] if self.stop == "gs7" else range(2)):
                ph, b_ph = self.PS.get()
                kb.op("tensor", lambda e, c2=c2, ph=ph: e.matmul(ph[:, 0:NT * 8], self.gm[:, 5 + c2, :], gflat, start=True, stop=True),
                      reads=[b, self.b_gm], writes=[b_ph])
                if self.stop == "gs5":
                    return
                kb.op("scalar", lambda e, c2=c2, ph=ph: e.activation(out=self.DL[:, c2].rearrange("p t x -> p (t x)"), in_=ph[:, 0:NT * 8],
                                                                      func=AF.Exp), reads=[b_ph], writes=[b])
                if self.stop == "gs6":
                    return
        self.dump("g_g", self.gg[:], b, [128, NT, 8])
        self.dump("g_E1", self.E1[:], b, [128, NT, 8])
        self.dump("g_E2", self.E2[:], b, [128, NT, 8])
        self.dump("g_DL", self.DL[:], b, [128, 2, NT, 8])

    def gdn_head_front(self, h):
        kb = self.kb
        self.qh = kb.sb("g_qh%d" % h, [128, NT, 128], F32)
        self.kh = kb.sb("g_kh%d" % h, [128, NT, 128], F32)
        self.vh = kb.sb("g_vh%d" % h, [128, NT, 128], F32)
        self.kT = kb.sb("g_kT%d" % h, [128, TOK], F32)
        self.qT = kb.sb("g_qT%d" % h, [128, TOK], F32)
        self.b_qh, self.b_kh, self.b_vh, self.b_kT, self.b_qT = [kb.buf("g_%s%d" % (n, h)) for n in ("qh", "kh", "vh", "kT", "qT")]
        with kb.scope():
            wq = kb.sb("g_wqkv", [128, 8, 3, 128], BF16)
            b_wq = kb.buf("g_wqkv")
            v = self.w_in.rearrange("(kc p) n -> p kc n", p=128)
            for c in range(3):
                for kc in range(8):
                    col = OFF_QKV + c * 512 + h * 128
                    kb.dma("gpsimd", wq[:, kc, c, :], v[:, kc, col:col + 128], writes=[b_wq])
            zb = kb.sb("g_z", [128, TOK], F32)
            acc = kb.sb("g_acc", [128, TOK], F32)
            b_z = kb.buf("g_z")
            b_acc = kb.buf("g_acc")
            dsts = [(self.qh, self.b_qh), (self.kh, self.b_kh), (self.vh, self.b_vh)]
            for c in range(3):
                ch = c * 4 + h
                for (s0, n) in BLOCKS:
                    pt, b_pt = self.PS.get()
                    tiles = range(s0 // 128, (s0 + n) // 128)
                    for kc in range(8):
                        kb.op("tensor", lambda e, kc=kc, pt=pt, c=c, s0=s0, n=n: e.matmul(
                            pt[:, 0:n], wq[:, kc, c, :], self.hT[:, kc, s0:s0 + n], start=(kc == 0), stop=(kc == 7)),
                            reads=[b_wq] + [self.b_hT[t] for t in tiles], writes=[b_pt], sig=(kc == 7))
                    kb.op("scalar", lambda e, pt=pt, s0=s0, n=n: e.activation(out=zb[:, s0:s0 + n], in_=pt[:, 0:n], func=AF.Identity),
                          reads=[b_pt], writes=[b_z])
                kb.op("vector", lambda e, ch=ch: e.tensor_scalar(out=acc[:], in0=zb[:], scalar1=self.cw[:, ch, 2:3], scalar2=0.0,
                                                                 op0=ALU.mult, op1=ALU.add), reads=[b_z, self.b_cw], writes=[b_acc])
                segs = [(zb[:, 0:CTX].rearrange("p (r w) -> p r w", w=CTX), acc[:, 0:CTX].rearrange("p (r w) -> p r w", w=CTX), CTX),
                        (zb[:, CTX:TOK].rearrange("p (r w) -> p r w", w=64), acc[:, CTX:TOK].rearrange("p (r w) -> p r w", w=64), 64)]
                for k in (0, 1, 3, 4):
                    sh = k - 2
                    for (zv, av, w) in segs:
                        lo, hi = max(0, -sh), w - max(0, sh)
                        kb.op("vector", lambda e, zv=zv, av=av, lo=lo, hi=hi, sh=sh, k=k, ch=ch: e.scalar_tensor_tensor(
                            out=av[:, :, lo:hi], in0=zv[:, :, lo + sh:hi + sh], scalar=self.cw[:, ch, k:k + 1], in1=av[:, :, lo:hi],
                            op0=ALU.mult, op1=ALU.add), reads=[b_z, b_acc, self.b_cw], writes=[b_acc])
                kb.op("scalar", lambda e: e.activation(out=zb[:], in_=acc[:], func=AF.Silu), reads=[b_acc], writes=[b_z])
                if c == 0 and h == 0:
                    self.dump("g_cq", zb[:, 0:512], b_z, [128, 512])
                dst, b_dst = dsts[c]
                for t0 in range(0, NT, 4):
                    nt = min(4, NT - t0)
                    pt, b_pt = self.PS.get()
                    for q in range(nt):
                        kb.op("tensor", lambda e, q=q, t0=t0, pt=pt: e.transpose(
                            pt[:, q * 128:(q + 1) * 128], zb[:, (t0 + q) * 128:(t0 + q + 1) * 128], self.identf[:]),
                            reads=[b_z, self.b_ident], writes=[b_pt])
                    kb.op("scalar" if (t0 // 4) % 2 == 0 else "vector", lambda e, t0=t0, nt=nt, pt=pt, dst=dst: (
                        e.activation(out=dst[:, t0:t0 + nt, :], in_=pt[:, 0:nt * 128].rearrange("p (t c) -> p t c", c=128), func=AF.Identity)
                        if e is self.nc.scalar else
                        e.tensor_copy(out=dst[:, t0:t0 + nt, :], in_=pt[:, 0:nt * 128].rearrange("p (t c) -> p t c", c=128))),
                        reads=[b_pt], writes=[b_dst])
            sq = acc[:, 0:NT * 128].rearrange("p (t c) -> p t c", c=128)
            ss = kb.sb("g_ss", [128, 2, NT], F32)
            b_ss = kb.buf("g_ss")
            for qi, (src, b_src) in enumerate([(self.qh, self.b_qh), (self.kh, self.b_kh)]):
                kb.op("vector", lambda e, src=src: e.tensor_tensor(out=sq, in0=src[:], in1=src[:], op=ALU.mult), reads=[b_src], writes=[b_acc])
                kb.op("vector", lambda e, qi=qi: e.tensor_reduce(out=ss[:, qi, :], in_=sq, axis=AX.X, op=ALU.add), reads=[b_acc], writes=[b_ss])
            kb.op("scalar", lambda e: e.activation(out=ss[:], in_=ss[:], func=AF.Sqrt, bias=EPS, scale=1.0), reads=[b_ss], writes=[b_ss])
            kb.op("vector", lambda e: e.reciprocal(out=ss[:], in_=ss[:]), reads=[b_ss], writes=[b_ss])
            kb.op("vector", lambda e: e.tensor_scalar(out=ss[:, 0, :], in0=ss[:, 0, :], scalar1=float(128 ** -0.5), scalar2=0.0,
                                                     op0=ALU.mult, op1=ALU.add), reads=[b_ss], writes=[b_ss])
            for qi, (src, b_src) in enumerate([(self.qh, self.b_qh), (self.kh, self.b_kh)]):
                kb.op("vector", lambda e, src=src, qi=qi: e.tensor_tensor(
                    out=src[:], in0=src[:], in1=ss[:, qi, :].unsqueeze(2).to_broadcast([128, NT, 128]), op=ALU.mult),
                    reads=[b_src, b_ss], writes=[b_src])
            for (src, b_src, dstT, b_dT) in [(self.kh, self.b_kh, self.kT, self.b_kT), (self.qh, self.b_qh, self.qT, self.b_qT)]:
                for t0 in range(0, NT, 4):
                    nt = min(4, NT - t0)
                    pt, b_pt = self.PS.get()
                    for q in range(nt):
                        kb.op("tensor", lambda e, q=q, t0=t0, pt=pt, src=src: e.transpose(
                            pt[:, q * 128:(q + 1) * 128], src[:, t0 + q, :], self.identf[:]), reads=[b_src, self.b_ident], writes=[b_pt])
                    kb.op("scalar", lambda e, t0=t0, nt=nt, pt=pt, dstT=dstT: e.activation(
                        out=dstT[:, t0 * 128:(t0 + nt) * 128].bitcast(mybir.dt.float32r), in_=pt[:, 0:nt * 128], func=AF.Identity), reads=[b_pt], writes=[b_dT])
        if h == 0:
            self.dump("g_qh", self.qh[:, 0:4, :], self.b_qh, [128, 4, 128])
            self.dump("g_kh", self.kh[:, 0:4, :], self.b_kh, [128, 4, 128])
            self.dump("g_vh", self.vh[:, 0:4, :], self.b_vh, [128, 4, 128])


    def gdn_head_core(self, h):
        kb = self.kb
        gm = self.gm
        T_ = lambda name, n=1: Pool(kb, "gc_%s_%d" % (name, h), [128, 128], F32, n)
        R = 4
        rings = {n: [T_("%s%d" % (n, d), R) for d in range(2)] for n in ("wT", "ub", "qkT", "qdT", "kd")}
        tmp = {n: [T_("%s%d" % (n, d), 1) for d in range(2)] for n in
               ("kbt", "kw", "vb", "qd", "kbT", "Dls", "DTs", "DTi", "A", "N", "X0", "X1", "P0", "P1", "Q0", "Q1")}
        vnp = [T_("vn%d" % d, 2) for d in range(2)]
        S = [kb.sb("g_S%d_%d" % (h, d), [128, 128], F32) for d in range(2)]
        b_S = [kb.buf("g_S%d" % d) for d in range(2)]
        for d in range(2):
            kb.op("vector", lambda e, d=d: e.memset(S[d][:], 0.0), writes=[b_S[d]])
        order = {0: list(range(NT)), 1: [1, 0] + list(range(NT - 1, 1, -1))}
        ringent = {}
        gs = self.b_gs
        LS, US, UI, LI = 2, 3, 4, 10

        kb.barrier()
        pst = [t for (t, _) in self.PS.tiles]

        class Reg:
            def __init__(r, ap, b_):
                r.ap = ap
                r.b = b_
        regs = {}
        pb = [b_ for (_, b_) in self.PS.tiles]
        for d in range(2):
            regs[("pD", d)] = Reg(pst[4 * d][:, 0:256], pb[4 * d])
            regs[("pt", d)] = Reg(pst[4 * d + 1][:, 0:256], pb[4 * d + 1])
            regs[("pK", d)] = Reg(pst[4 * d + 2][:, 0:384], pb[4 * d + 2])
            regs[("pw", d)] = Reg(pst[4 * d + 3][:, 0:128], pb[4 * d + 3])
            regs[("po", d)] = Reg(pst[4 * d + 3][:, 128:256], pb[4 * d + 3])
            regs[("ps", d)] = Reg(pst[4 * d + 3][:, 256:384], pb[4 * d + 3])
        R32 = (lambda ap: ap.bitcast(mybir.dt.float32r)) if os.environ.get('KF32R', '1') == '1' else (lambda ap: ap)
        prog = {"prep": [0, 0], "chain": [0, 0]}

        def prep_gen(d):
            for i in range(NT):
                while i - prog["chain"][d] >= R - 1:
                    yield
                tt = order[d][i]
                x = d * 4 + h
                ts_ = slice(tt * 128, (tt + 1) * 128)
                bsc = self.beta[:, tt, x:x + 1]
                e1 = self.E1[:, tt, x:x + 1]
                e2 = self.E2[:, tt, x:x + 1]
                g = lambda n: tmp[n][d].get()
                kbt, b_kbt = g("kbt"); kw, b_kw = g("kw"); vb, b_vb = g("vb"); qd, b_qd = g("qd"); kbT, b_kbT = g("kbT")
                kd, b_kd = rings["kd"][d].get(); qdT, b_qdT = rings["qdT"][d].get()
                ts1 = lambda e, o, i_, sc: e.tensor_scalar(out=R32(o[:]), in0=i_, scalar1=sc, scalar2=0.0, op0=ALU.mult, op1=ALU.add)
                kb.op("vector", lambda e: ts1(e, kbt, self.kh[:, tt, :], bsc), reads=[self.b_kh, gs], writes=[b_kbt])
                kb.op("vector", lambda e: ts1(e, qd, self.qh[:, tt, :], e1), reads=[self.b_qh, gs], writes=[b_qd])
                gb = self.gg[:, tt, x:x + 1].to_broadcast([128, 128])
                M, nM = gm[:, d, :], gm[:, 8 + d, :]
                pD, b_pD = regs[("pD", d)].ap, regs[("pD", d)].b
                kb.op("tensor", lambda e: e.matmul(pD[:, 0:128], M, gb, start=True, stop=False), reads=[gs, self.b_gm], writes=[b_pD], sig=False)
                kb.op("tensor", lambda e: e.matmul(pD[:, 0:128], gb, nM, start=False, stop=True), reads=[gs, self.b_gm], writes=[b_pD], sig=False)
                kb.op("tensor", lambda e: e.matmul(pD[:, 128:256], nM, gb, start=True, stop=False), reads=[gs, self.b_gm], writes=[b_pD], sig=False)
                kb.op("tensor", lambda e: e.matmul(pD[:, 128:256], gb, M, start=False, stop=True), reads=[gs, self.b_gm], writes=[b_pD])
                yield
                kb.op("vector", lambda e: ts1(e, kw, kbt[:], e1), reads=[b_kbt, gs], writes=[b_kw])
                kb.op("vector", lambda e: ts1(e, vb, self.vh[:, tt, :], bsc), reads=[self.b_vh, gs], writes=[b_vb])
                kb.op("vector", lambda e: ts1(e, kd, self.kh[:, tt, :], e2), reads=[self.b_kh, gs], writes=[b_kd])
                pt, b_pt = regs[("pt", d)].ap, regs[("pt", d)].b
                kb.op("tensor", lambda e: e.transpose(pt[:, 0:128], kbt[:], self.identf[:]), reads=[b_kbt, self.b_ident], writes=[b_pt], sig=False)
                kb.op("tensor", lambda e: e.transpose(pt[:, 128:256], qd[:], self.identf[:]), reads=[b_qd, self.b_ident], writes=[b_pt])
                yield
                kb.op("scalar", lambda e: e.activation(out=R32(kbT[:]), in_=pt[:, 0:128], func=AF.Identity), reads=[b_pt], writes=[b_kbT])
                kb.op("scalar", lambda e: e.activation(out=qdT[:], in_=pt[:, 128:256], func=AF.Identity), reads=[b_pt], writes=[b_qdT])
                Dls, b_Dls = g("Dls"); DTs, b_DTs = g("DTs"); DTi, b_DTi = g("DTi")
                mD, mDTs, mDTi = (LS, US, UI) if d == 0 else (US, LS, LI)
                trip = [(Dls, b_Dls, pD[:, 0:128], mD), (DTs, b_DTs, pD[:, 128:256], mDTs), (DTi, b_DTi, pD[:, 128:256], mDTi)]
                for (dst, b_dst, src, mk) in trip:
                    kb.op("vector", lambda e, dst=dst, src=src, mk=mk: e.scalar_tensor_tensor(
                        out=dst[:], in0=src, scalar=0.0, in1=gm[:, mk, :], op0=ALU.min, op1=ALU.add), reads=[b_pD, self.b_gm], writes=[b_dst])
                yield
                for (dst, b_dst, src, mk) in trip:
                    kb.op("scalar", lambda e, dst=dst: e.activation(out=dst[:], in_=dst[:], func=AF.Exp), reads=[b_dst], writes=[b_dst])
                pK, b_pK = regs[("pK", d)].ap, regs[("pK", d)].b
                kTt, qTt = self.kT[:, ts_], self.qT[:, ts_]
                kb.op("tensor", lambda e: e.matmul(pK[:, 0:128], R32(kbT[:]), R32(kTt), start=True, stop=True), reads=[b_kbT, self.b_kT], writes=[b_pK], sig=False)
                kb.op("tensor", lambda e: e.matmul(pK[:, 128:256], R32(kTt), R32(kbT[:]), start=True, stop=True), reads=[b_kbT, self.b_kT], writes=[b_pK], sig=False)
                kb.op("tensor", lambda e: e.matmul(pK[:, 256:384], R32(kTt), R32(qTt), start=True, stop=True), reads=[self.b_qT, self.b_kT], writes=[b_pK])
                yield
                A, b_A = g("A"); N, b_N = g("N"); qkT, b_qkT = rings["qkT"][d].get()
                kb.op("vector", lambda e: e.tensor_tensor(out=R32(A[:]), in0=pK[:, 0:128], in1=Dls[:], op=ALU.mult), reads=[b_pK, b_Dls], writes=[b_A])
                kb.op("vector", lambda e: e.tensor_tensor(out=R32(N[:]), in0=pK[:, 128:256], in1=DTs[:], op=ALU.mult), reads=[b_pK, b_DTs], writes=[b_N])
                kb.op("vector", lambda e: e.tensor_tensor(out=qkT[:], in0=pK[:, 256:384], in1=DTi[:], op=ALU.mult), reads=[b_pK, b_DTi], writes=[b_qkT])
                X, b_X = g("X0")
                yield
                kb.op("vector", lambda e: e.tensor_tensor(out=R32(X[:]), in0=self.identf[:], in1=N[:], op=ALU.subtract), reads=[b_N, self.b_ident], writes=[b_X])
                P, b_P, PT, b_PT = N, b_N, A, b_A
                for sidx in range(1, 6):
                    pp, b_pp = regs[("pK", d)].ap, regs[("pK", d)].b
                    if sidx < 5:
                        kb.op("tensor", lambda e: e.matmul(pp[:, 0:128], R32(PT[:]), R32(P[:]), start=True, stop=True), reads=[b_P, b_PT], writes=[b_pp], sig=False)
                    kb.op("tensor", lambda e: e.matmul(pp[:, 128:256], R32(P[:]), R32(PT[:]), start=True, stop=True), reads=[b_P, b_PT], writes=[b_pp])
                    yield
                    nP, b_nP = tmp["P%d" % (sidx % 2)][d].get()
                    nPT, b_nPT = tmp["Q%d" % (sidx % 2)][d].get()
                    kb.op("scalar", lambda e: e.activation(out=R32(nPT[:]), in_=pp[:, 128:256], func=AF.Identity), reads=[b_pp], writes=[b_nPT])
                    if sidx < 5:
                        kb.op("scalar", lambda e: e.activation(out=R32(nP[:]), in_=pp[:, 0:128], func=AF.Identity), reads=[b_pp], writes=[b_nP])
                    yield
                    kb.op("tensor", lambda e: e.matmul(pp[:, 256:384], R32(nPT[:]), R32(X[:]), start=True, stop=True), reads=[b_nPT, b_X], writes=[b_pp])
                    yield
                    nX, b_nX = tmp["X%d" % (sidx % 2)][d].get()
                    kb.op("vector", lambda e: e.tensor_tensor(out=R32(nX[:]), in0=pp[:, 256:384], in1=X[:], op=ALU.add), reads=[b_pp, b_X], writes=[b_nX])
                    P, b_P, PT, b_PT, X, b_X = nP, b_nP, nPT, b_nPT, nX, b_nX
                    yield
                pu, b_pu = regs[("pK", d)].ap, regs[("pK", d)].b
                kb.op("tensor", lambda e: e.matmul(pu[:, 0:128], R32(X[:]), R32(vb[:]), start=True, stop=True), reads=[b_X, b_vb], writes=[b_pu], sig=False)
                kb.op("tensor", lambda e: e.matmul(pu[:, 128:256], R32(kw[:]), R32(X[:]), start=True, stop=True), reads=[b_X, b_kw], writes=[b_pu])
                yield
                ub, b_ub = rings["ub"][d].get(); wT, b_wT = rings["wT"][d].get()
                kb.op("scalar", lambda e: e.activation(out=ub[:], in_=pu[:, 0:128], func=AF.Identity), reads=[b_pu], writes=[b_ub])
                kb.op("scalar", lambda e: e.activation(out=wT[:], in_=pu[:, 128:256], func=AF.Identity), reads=[b_pu], writes=[b_wT])
                ringent[(d, i)] = dict(d=d, tt=tt, x=x, kd=(kd, b_kd), qdT=(qdT, b_qdT), qkT=(qkT, b_qkT), ub=(ub, b_ub), wT=(wT, b_wT))
                prog["prep"][d] = i + 1
                yield

        def chain_gen(d):
            for i in range(NT):
                while (d, i) not in ringent:
                    yield
                en = ringent[(d, i)]
                tt, x = en["tt"], en["x"]
                wT, b_wT = en["wT"]; ub, b_ub = en["ub"]; qdT, b_qdT = en["qdT"]; qkT, b_qkT = en["qkT"]; kd, b_kd = en["kd"]
                for hi in range(2):
                    c2 = hi if d == 0 else 1 - hi
                    r = slice(c2 * 64, c2 * 64 + 64)
                    pw, b_pw = regs[("pw", d)].ap, regs[("pw", d)].b
                    kb.op("tensor", lambda e: e.matmul(pw[r, :], wT[:, r], S[d][:], start=True, stop=True), reads=[b_wT, b_S[d]], writes=[b_pw])
                    if tt >= 2:
                        po, b_po = regs[("po", d)].ap, regs[("po", d)].b
                        kb.op("tensor", lambda e: e.matmul(po[r, :], qdT[:, r], S[d][:], start=True, stop=False),
                              reads=[b_qdT, b_S[d]], writes=[b_po], sig=False)
                    yield
                    vn, b_vn = vnp[d].get()
                    kb.op("vector", lambda e: e.tensor_tensor(out=vn[r, :], in0=ub[r, :], in1=pw[r, :], op=ALU.subtract),
                          reads=[b_ub, b_pw], writes=[b_vn])
                    yield
                    ps_, b_ps = regs[("ps", d)].ap, regs[("ps", d)].b
                    if tt >= 2:
                        kb.op("tensor", lambda e: e.matmul(po[r, :], qkT[r, r], vn[r, :], start=False, stop=True),
                              reads=[b_qkT, b_vn], writes=[b_po])
                    kb.op("tensor", lambda e: e.matmul(ps_[:, :], kd[r, :], vn[r, :], start=True, stop=True), reads=[b_kd, b_vn], writes=[b_ps])
                    yield
                    kb.op("vector", lambda e: e.scalar_tensor_tensor(out=S[d][:], in0=S[d][:], scalar=self.DL[:, c2, tt, x:x + 1], in1=ps_[:, :],
                                                                    op0=ALU.mult, op1=ALU.add), reads=[b_ps, b_S[d], gs], writes=[b_S[d]])
                    if tt >= 2:
                        od = self.osum[r, tt - 2, h * 128:(h + 1) * 128]
                        kb.op("gpsimd" if False else "vector", lambda e: e.tensor_tensor(out=od, in0=po[r, :], in1=od, op=ALU.add),
                              reads=[b_po], writes=[self.b_osum[tt - 2]])
                    yield
                prog["chain"][d] = i + 1

        gens = [prep_gen(0), prep_gen(1), chain_gen(0), chain_gen(1)]
        while gens:
            for g_ in list(gens):
                try:
                    next(g_)
                except StopIteration:
                    gens.remove(g_)

    def phase_gdn(self):
        kb = self.kb
        self.osum = kb.sb("g_osum", [128, 16, 512], F32)
        self.b_osum = [kb.buf("g_osum%d" % t) for t in range(16)]
        for t in range(16):
            kb.op("vector", lambda e, t=t: e.memset(self.osum[:, t, :], 0.0), writes=[self.b_osum[t]])
        for h in range(4):
            with kb.scope():
                self.gdn_head_front(h)
                self.gdn_head_core(h)
            if self.stop == "gdn_h0":
                break
        if "g_osum" in self.dbg:
            self.dump("g_osum", self.osum[:, 0:4, 0:128], self.b_osum[3], [128, 4, 128])
        if "g_osum2" in self.dbg:
            self.dump("g_osum2", self.osum[:, 12:16, 0:128], self.b_osum[15], [128, 4, 128])


    def phase_glu(self):
        kb = self.kb
        wg = kb.sb("wglu", [128, 4, 512], BF16)
        b_wg = kb.buf("wglu")
        self.load_w_bf16(wg, b_wg, self.w_glu, 512, 0)
        bg, b_bg = self._ld("bglu", self.b_glu_t, [128, 4])
        zT = kb.sb("glu_z", [128, 4, L], BF16)
        b_zT = [kb.buf("glu_z%d" % q) for q in range(4)]
        t1 = kb.sb("glu_t1", [128, L], F32)
        t2 = kb.sb("glu_t2", [128, L], F32)
        b_t = kb.buf("glu_t")
        for q in range(4):
            y = self.yT[:, q, :]
            kb.op("vector", lambda e, y=y: e.tensor_tensor(out=t1[:], in0=y, in1=y, op=ALU.mult), reads=[self.b_yT[q]], writes=[b_t])
            kb.op("vector", lambda e: e.tensor_scalar(out=t1[:], in0=t1[:], scalar1=0.044715, scalar2=1.0, op0=ALU.mult, op1=ALU.add), reads=[b_t], writes=[b_t])
            kb.op("vector", lambda e, y=y: e.tensor_tensor(out=t1[:], in0=t1[:], in1=y, op=ALU.mult), reads=[b_t, self.b_yT[q]], writes=[b_t])
            kb.op("scalar", lambda e: e.activation(out=t2[:], in_=t1[:], func=AF.Sigmoid, scale=1.5957691216057308), reads=[b_t], writes=[b_t])
            kb.op("vector", lambda e, y=y, q=q: e.tensor_tensor(out=zT[:, q, :], in0=t2[:], in1=y, op=ALU.mult), reads=[b_t, self.b_yT[q]], writes=[b_zT[q]])
        glp = Pool(kb, "glu_g", [128, 512], F32, 2)
        for oc in range(4):
            for n in range(4):
                pt, b_pt = self.PS.get()
                for kc in range(4):
                    kb.op("tensor", lambda e, kc=kc, pt=pt, oc=oc, n=n: e.matmul(
                        pt[:], wg[:, kc, oc * 128:(oc + 1) * 128], zT[:, kc, n * 512:(n + 1) * 512], start=(kc == 0), stop=(kc == 3)),
                        reads=[b_wg] + b_zT, writes=[b_pt], sig=(kc == 3))
                gl, b_gl = glp.get()
                kb.op("scalar", lambda e, pt=pt, gl=gl, oc=oc: e.activation(out=gl[:], in_=pt[:], func=AF.Sigmoid, bias=bg[:, oc:oc + 1]),
                      reads=[b_pt, b_bg], writes=[b_gl])
                kb.op("vector", lambda e, gl=gl, oc=oc, n=n: e.tensor_tensor(
                    out=self.yaT[:, oc, n * 512:(n + 1) * 512], in0=zT[:, oc, n * 512:(n + 1) * 512], in1=gl[:], op=ALU.mult),
                    reads=[b_gl, b_zT[oc]], writes=[self.b_yaT])
        if "yaT" in self.dbg:
            t = kb.sb("dbg_yaT_t", [128, 4, 512], F32)
            bt = kb.buf("dbg_yaT")
            kb.op("vector", lambda e: e.tensor_copy(out=t[:], in_=self.yaT[:, :, 0:512]), reads=[self.b_yaT], writes=[bt])
            self.dump("yaT", t[:], bt, [128, 4, 512])

    def phase_gdn_out(self):
        kb = self.kb
        wgt = kb.sb("wgate", [128, 8, 512], BF16)
        b_wgt = kb.buf("wgate")
        self.load_w_bf16(wgt, b_wgt, self.w_in, 512, OFF_GATE)
        nw = kb.sb("gnw", [128, 128], F32)
        b_nw = kb.buf("gnw")
        kb.dma("sync", nw[:], self.gdn_norm.partition_broadcast(128), writes=[b_nw])
        ss = kb.sb("go_ss", [128, 16, 4], F32)
        b_ss = kb.buf("go_ss")
        sqp = Pool(kb, "go_sq", [128, 512], F32, 2)
        for t in range(16):
            sq, b_sq = sqp.get()
            kb.op("vector", lambda e, t=t, sq=sq: e.tensor_tensor(out=sq[:], in0=self.osum[:, t, :], in1=self.osum[:, t, :], op=ALU.mult),
                  reads=[self.b_osum[t]], writes=[b_sq])
            kb.op("vector", lambda e, t=t, sq=sq: e.tensor_reduce(out=ss[:, t, :], in_=sq[:].rearrange("p (h c) -> p h c", c=128), axis=AX.X, op=ALU.add),
                  reads=[b_sq], writes=[b_ss])
        ssf = ss[:].rearrange("p t h -> p (t h)")
        kb.op("scalar", lambda e: e.activation(out=ssf, in_=ssf, func=AF.Sqrt, bias=EPS, scale=1.0 / 128), reads=[b_ss], writes=[b_ss])
        kb.op("vector", lambda e: e.reciprocal(out=ssf, in_=ssf), reads=[b_ss], writes=[b_ss])
        sgp = Pool(kb, "go_sg", [128, 512], F32, 2)
        onp = Pool(kb, "go_on", [128, 512], F32, 2)
        for t in range(16):
            pg, b_pg = self.PS.get()
            for kc in range(8):
                kb.op("tensor", lambda e, kc=kc, t=t, pg=pg: e.matmul(pg[:], self.hT[:, kc, (t + 2) * 128:(t + 3) * 128], wgt[:, kc, :],
                                                                     start=(kc == 0), stop=(kc == 7)), reads=[b_wgt, self.b_hT[t + 2]], writes=[b_pg], sig=(kc == 7))
            sg, b_sg = sgp.get()
            kb.op("scalar", lambda e, pg=pg, sg=sg: e.activation(out=sg[:], in_=pg[:], func=AF.Silu), reads=[b_pg], writes=[b_sg])
            on, b_on = onp.get()
            on3 = on[:].rearrange("p (h c) -> p h c", c=128)
            kb.op("vector", lambda e, t=t, on3=on3: e.tensor_tensor(out=on3, in0=self.osum[:, t, :].rearrange("p (h c) -> p h c", c=128),
                                                                   in1=ss[:, t, :].unsqueeze(2).to_broadcast([128, 4, 128]), op=ALU.mult),
                  reads=[self.b_osum[t], b_ss], writes=[b_on])
            kb.op("vector", lambda e, on3=on3: e.tensor_tensor(out=on3, in0=on3, in1=nw[:].unsqueeze(1).to_broadcast([128, 4, 128]), op=ALU.mult),
                  reads=[b_on, b_nw], writes=[b_on])
            kb.op("vector", lambda e, on=on, sg=sg: e.tensor_tensor(out=on[:], in0=on[:], in1=sg[:], op=ALU.mult), reads=[b_on, b_sg], writes=[b_on])
            pt, b_pt = self.PS.get()
            for c in range(4):
                kb.op("tensor", lambda e, c=c, pt=pt, on=on: e.transpose(pt[:, c * 128:(c + 1) * 128], on[:, c * 128:(c + 1) * 128], self.identf[:]),
                      reads=[b_on, self.b_ident], writes=[b_pt])
            kb.op("scalar", lambda e, pt=pt, t=t: e.activation(out=self.ybT[:, :, t * 128:(t + 1) * 128], in_=pt[:].rearrange("p (c k) -> p c k", k=128),
                                                              func=AF.Identity), reads=[b_pt], writes=[self.b_ybT])
        if "ybT" in self.dbg:
            t_ = kb.sb("dbg_ybT_t", [128, 4, 512], F32)
            bt = kb.buf("dbg_ybT")
            kb.op("vector", lambda e: e.tensor_copy(out=t_[:], in_=self.ybT[:, :, 0:512]), reads=[self.b_ybT], writes=[bt])
            self.dump("ybT", t_[:], bt, [128, 4, 512])

    def phase_merge(self):
        kb = self.kb
        mT = kb.sb("mergedT", [128, 8, L], BF16)
        b_mT = [kb.buf("mergedT%d" % n) for n in range(4)]
        with kb.scope():
            wba = kb.sb("wba", [128, 4, D], BF16); b_wba = kb.buf("wba")
            wbb = kb.sb("wbb", [128, 4, D], BF16); b_wbb = kb.buf("wbb")
            self.load_w_bf16(wba, b_wba, self.w_ba, D, 0)
            self.load_w_bf16(wbb, b_wbb, self.w_bb, D, 0)
            wbrp = Pool(kb, "wbr", [128, 8, 2, 128], BF16, 2)
            gp = Pool(kb, "mg_g", [128, 2, 512], F32, 2)
            m1p = Pool(kb, "mg_m", [128, 2, 512], F32, 2)
            v = self.w_in.rearrange("(kc p) n -> p kc n", p=128)
            for oc in range(8):
                wbr, b_wbr = wbrp.get()
                for ab in range(2):
                    for kc in range(8):
                        col = OFF_BR + ab * D + oc * 128
                        kb.dma("gpsimd", wbr[:, kc, ab, :], v[:, kc, col:col + 128], writes=[b_wbr])
                for n in range(4):
                    tiles = [self.b_hT[2 + 4 * n + j] for j in range(4)]
                    tok = slice(n * 512, (n + 1) * 512)
                    stok = slice(CTX + n * 512, CTX + (n + 1) * 512)
                    g, b_g = gp.get()
                    m1, b_m1 = m1p.get()
                    for ab, (wb, b_wb, yT_, b_y) in enumerate([(wba, b_wba, self.yaT, self.b_yaT), (wbb, b_wbb, self.ybT, self.b_ybT)]):
                        pbr, b_pbr = self.PS.get()
                        for kc in range(8):
                            kb.op("tensor", lambda e, kc=kc, pbr=pbr, ab=ab, wbr=wbr: e.matmul(
                                pbr[:], wbr[:, kc, ab, :], self.hT[:, kc, stok], start=(kc == 0), stop=(kc == 7)),
                                reads=[b_wbr] + tiles, writes=[b_pbr], sig=(kc == 7))
                        kb.op("scalar", lambda e, pbr=pbr, g=g, ab=ab: e.activation(out=g[:, ab, :], in_=pbr[:], func=AF.Sigmoid),
                              reads=[b_pbr], writes=[b_g])
                        pp, b_pp = self.PS.get()
                        for kc in range(4):
                            kb.op("tensor", lambda e, kc=kc, pp=pp, wb=wb, yT_=yT_: e.matmul(
                                pp[:], wb[:, kc, oc * 128:(oc + 1) * 128], yT_[:, kc, tok], start=(kc == 0), stop=(kc == 3)),
                                reads=[b_wb, b_y], writes=[b_pp], sig=(kc == 3))
                        kb.op("vector", lambda e, pp=pp, g=g, m1=m1, ab=ab: e.tensor_tensor(out=m1[:, ab, :], in0=pp[:], in1=g[:, ab, :], op=ALU.mult),
                              reads=[b_pp, b_g], writes=[b_m1])
                    kb.op("vector", lambda e, m1=m1, oc=oc: e.tensor_tensor(out=mT[:, oc, tok], in0=m1[:, 0, :], in1=m1[:, 1, :], op=ALU.add),
                          reads=[b_m1], writes=[b_mT[n]])
        if "mergedT" in self.dbg:
            t_ = kb.sb("dbg_mT_t", [128, 8, 256], F32)
            bt = kb.buf("dbg_mT")
            kb.op("vector", lambda e: e.tensor_copy(out=t_[:], in_=mT[:, :, 0:256]), reads=[b_mT[0]], writes=[bt])
            self.dump("mergedT", t_[:], bt, [128, 8, 256])
        with kb.scope():
            wo = kb.sb("wout", [128, 8, D], BF16); b_wo = kb.buf("wout")
            self.load_w_bf16(wo, b_wo, self.w_out, D, 0)
            xp = Pool(kb, "mg_x", [128, D], F32, 3)
            tp = Pool(kb, "mg_t", [128, D], F32, 2)
            self.b_x1s = [kb.buf("x1s%d" % t) for t in range(16)]
            for t in range(16):
                xt, b_xt = xp.get()
                kb.dma("sync" if t % 2 == 0 else "scalar", xt[:], self.x[t * 128:(t + 1) * 128, :], writes=[b_xt])
                tm_, b_tm = tp.get()
                for cb in range(2):
                    pm, b_pm = self.PS.get()
                    for kc in range(8):
                        kb.op("tensor", lambda e, kc=kc, pm=pm, cb=cb, t=t: e.matmul(
                            pm[:], mT[:, kc, t * 128:(t + 1) * 128], wo[:, kc, cb * 512:(cb + 1) * 512], start=(kc == 0), stop=(kc == 7)),
                            reads=[b_wo, b_mT[t // 4]], writes=[b_pm], sig=(kc == 7))
                    cs_ = slice(cb * 512, (cb + 1) * 512)
                    kb.op("vector", lambda e, pm=pm, tm_=tm_, cs_=cs_: e.tensor_tensor(out=tm_[:, cs_], in0=pm[:], in1=self.gt1_bc[:, cs_], op=ALU.mult),
                          reads=[b_pm, self.b_gt1], writes=[b_tm])
                kb.op("vector", lambda e, tm_=tm_, xt=xt: e.tensor_tensor(out=xt[:], in0=tm_[:], in1=xt[:], op=ALU.add), reads=[b_tm, b_xt], writes=[b_xt])
                kb.dma("sync", self.x1s[t * 128:(t + 1) * 128, :], xt[:], reads=[b_xt], writes=[self.b_x1s[t]])
                if t == 0:
                    self.dump("x1", xt[:], b_xt, [128, D])

    def phase_moe_half(self, hp):
        kb = self.kb
        NTL = 8
        X = kb.sb("X%d" % hp, [128, NTL, D], F32)
        b_X = [kb.buf("X%d_%d" % (hp, t)) for t in range(NTL)]
        h2T = kb.sb("h2T%d" % hp, [128, 8, NTL * 128], BF16)
        b_h2 = [kb.buf("h2T%d_%d" % (hp, t)) for t in range(NTL)]
        comb = kb.sb("comb%d" % hp, [128, NTL, NE], F32)
        b_comb = kb.buf("comb%d" % hp)
        combs = kb.sb("combs%d" % hp, [128, NTL, NE], F32)
        combT = kb.sb("combT%d" % hp, [NE, NTL * 128], F32)
        b_combT = kb.buf("combT%d" % hp)
        for t in range(NTL):
            gt = hp * NTL + t
            kb.dma("sync" if t % 2 == 0 else "scalar", X[:, t, :], self.x1s[gt * 128:(gt + 1) * 128, :], reads=[self.b_x1s[gt]], writes=[b_X[t]])
        with kb.scope():
            n2, b_n2 = self._ld("norm2", self.norm2_t, [128, 8])
            A2 = kb.sb("A2", [128, 8, 2], F32); b_A2 = kb.buf("A2")
            kb.op("vector", lambda e: e.scalar_tensor_tensor(out=A2[:], in0=self.modT[:, 24:32, :], scalar=1.0,
                                                            in1=n2[:].unsqueeze(2).to_broadcast([128, 8, 2]), op0=ALU.add, op1=ALU.mult),
                  reads=[self.b_modT, b_n2], writes=[b_A2])
            wr = kb.sb("wrouter", [128, 8, NE], F32); b_wr = kb.buf("wrouter")
            kb.dma("sync", wr[:], self.w_router.rearrange("(kc p) n -> p kc n", p=128), writes=[b_wr])
            br_, b_br = kb.sb("brouter", [128, NE], F32), kb.buf("brouter")
            kb.dma("sync", br_[:], self.b_router.partition_broadcast(128), writes=[b_br])
            xnp = Pool(kb, "m_xn", [128, D], F32, 2)
            junk = Pool(kb, "m_junk", [128, D], F32, 1)
            stat = Pool(kb, "m_stat", [128, 4], F32, 4)
            hfp = Pool(kb, "m_hf", [128, 8, 128], F32, 2)
            lgp = Pool(kb, "m_lg", [128, NE], F32, 2)
            m8p = Pool(kb, "m_m8", [128, 16], F32, 2)
            for t in range(NTL):
                hf, b_hf = hfp.get()
                self._norm_tile2(X[:, t, :], b_X[t], t, A2, b_A2, 16, xnp, junk, stat, h2T, b_h2[t], hf, b_hf)
                pl, b_pl = self.PS.get()
                for kc in range(8):
                    kb.op("tensor", lambda e, kc=kc, pl=pl, hf=hf: e.matmul(pl[:, 0:NE], hf[:, kc, :], wr[:, kc, :], start=(kc == 0), stop=(kc == 7)),
                          reads=[b_hf, b_wr], writes=[b_pl], sig=(kc == 7))
                lg, b_lg = lgp.get()
                m8, b_m8 = m8p.get()
                kb.op("vector", lambda e, pl=pl, lg=lg: e.tensor_tensor(out=lg[:], in0=pl[:, 0:NE], in1=br_[:], op=ALU.add), reads=[b_pl, b_br], writes=[b_lg])
                if t == 0 and hp == 0:
                    self.dump("logits", lg[:], b_lg, [128, NE])
                kb.op("vector", lambda e, lg=lg, m8=m8: e.max(out=m8[:, 0:8], in_=lg[:]), reads=[b_lg], writes=[b_m8])
                kb.op("vector", lambda e, m8=m8: e.tensor_scalar(out=m8[:, 8:9], in0=m8[:, 0:1], scalar1=-1.0, scalar2=0.0, op0=ALU.mult, op1=ALU.add),
                      reads=[b_m8], writes=[b_m8])
                ex, b_ex = lgp.get()
                kb.op("scalar", lambda e, lg=lg, ex=ex, m8=m8: e.activation(out=ex[:], in_=lg[:], func=AF.Exp, bias=m8[:, 8:9]), reads=[b_lg, b_m8], writes=[b_ex])
                kb.op("vector", lambda e, lg=lg, ex=ex, m8=m8: e.scalar_tensor_tensor(out=ex[:], in0=lg[:], scalar=m8[:, 3:4], in1=ex[:], op0=ALU.is_ge, op1=ALU.mult),
                      reads=[b_lg, b_ex, b_m8], writes=[b_ex])
                kb.op("vector", lambda e, ex=ex, m8=m8: e.tensor_reduce(out=m8[:, 9:10], in_=ex[:], axis=AX.X, op=ALU.add), reads=[b_ex], writes=[b_m8])
                kb.op("vector", lambda e, m8=m8: e.reciprocal(out=m8[:, 10:11], in_=m8[:, 9:10]), reads=[b_m8], writes=[b_m8])
                kb.op("vector", lambda e, ex=ex, m8=m8, t=t: e.tensor_scalar(out=comb[:, t, :], in0=ex[:], scalar1=m8[:, 10:11], scalar2=0.0, op0=ALU.mult, op1=ALU.add),
                      reads=[b_ex, b_m8], writes=[b_comb])
                kb.op("vector", lambda e, t=t: e.tensor_scalar(out=combs[:, t, :], in0=comb[:, t, :], scalar1=float(1.0 / 1.702), scalar2=0.0, op0=ALU.mult, op1=ALU.add),
                      reads=[b_comb], writes=[b_comb])
                pT, b_pT = self.PS.get()
                kb.op("tensor", lambda e, pT=pT, t=t: e.transpose(pT[0:NE, 0:128], comb[:, t, :], self.identf[:]), reads=[b_comb, self.b_ident], writes=[b_pT])
                kb.op("scalar", lambda e, pT=pT, t=t: e.activation(out=combT[:, t * 128:(t + 1) * 128], in_=pT[0:NE, 0:128], func=AF.Identity),
                      reads=[b_pT], writes=[b_combT])
            if hp == 0:
                self.dump("comb", comb[:, 0, :], b_comb, [128, NE])
                if "h2T" in self.dbg:
                    t_ = kb.sb("dbg_h2T_t", [128, 8, 256], F32)
                    bt = kb.buf("dbg_h2T")
                    kb.op("vector", lambda e: e.tensor_copy(out=t_[:], in_=h2T[:, :, 0:256]), reads=b_h2[0:2], writes=[bt])
                    self.dump("h2T", t_[:], bt, [128, 8, 256])
        if self.stop == "router":
            return
        with kb.scope():
            bdn, b_bdn = self._ld("bdn", self.b_dn, [NE, D])
            bgu, b_bgu = self._ld("bgu", self.b_gu_t, [128, NE, 16])
            tp = Pool(kb, "moe_t", [128, 512], F32, 3)
            for t in range(NTL):
                for cb in range(2):
                    cs_ = slice(cb * 512, (cb + 1) * 512)
                    pb, b_pb = self.PS.get()
                    kb.op("tensor", lambda e, pb=pb, t=t, cs_=cs_: e.matmul(pb[:], combT[:, t * 128:(t + 1) * 128], bdn[:, cs_], start=True, stop=True),
                          reads=[b_combT, b_bdn], writes=[b_pb])
                    tt_, b_tt = tp.get()
                    kb.op("vector", lambda e, pb=pb, tt_=tt_, cs_=cs_: e.tensor_tensor(out=tt_[:], in0=pb[:], in1=self.gt2_bc[:, cs_], op=ALU.mult),
                          reads=[b_pb, self.b_gt2], writes=[b_tt])
                    kb.op("vector", lambda e, tt_=tt_, t=t, cs_=cs_: e.tensor_tensor(out=X[:, t, cs_], in0=tt_[:], in1=X[:, t, cs_], op=ALU.add),
                          reads=[b_tt], writes=[b_X[t]])
            wgup = Pool(kb, "wgu", [128, 8, 2 * D], BF16, 2)
            wdnp = Pool(kb, "wdn", [128, 8, D], BF16, 2)
            actp = Pool(kb, "actT", [128, 8, 512], BF16, 2)
            gp = Pool(kb, "moe_g", [128, 512], F32, 3)
            sp = Pool(kb, "moe_s", [128, 512], BF16, 3)
            up = Pool(kb, "moe_u", [128, 512], BF16, 3)
            bgu1 = kb.sb("bgu1", [128, NE, 8], F32)
            kb.op("vector", lambda e: e.tensor_scalar(out=bgu1[:], in0=bgu[:, :, 8:16], scalar1=1.0, scalar2=0.0, op0=ALU.add, op1=ALU.add),
                  reads=[b_bgu], writes=[b_bgu])
            NB = NTL * 128 // 512
            nexp = NE if self.stop != "moe1" else 1
            W = {}

            def load_w(ex_):
                wgu, b_wgu = wgup.get()
                wdn, b_wdn = wdnp.get()
                vg = self.w_gu[ex_].rearrange("(kc p) n -> p kc n", p=128)
                for kc in range(8):
                    kb.dma("gpsimd", wgu[:, kc, :], vg[:, kc, :], writes=[b_wgu])
                vd = self.w_dn[ex_].rearrange("(kc p) n -> p kc n", p=128)
                for kc in range(8):
                    kb.dma("gpsimd", wdn[:, kc, :], vd[:, kc, :], writes=[b_wdn])
                kb.op("vector", lambda e, wdn=wdn: e.tensor_tensor(out=wdn[:], in0=wdn[:], in1=self.gt2_bc[:].unsqueeze(1).to_broadcast([128, 8, D]), op=ALU.mult),
                      reads=[b_wdn, self.b_gt2], writes=[b_wdn])
                W[ex_] = (wgu, b_wgu, wdn, b_wdn)

            ACT = {}

            def gu(ex_, n):
                wgu, b_wgu, wdn, b_wdn = W[ex_]
                actT, b_act = actp.get()
                ACT[(ex_, n)] = (actT, b_act)
                toks = slice(n * 512, (n + 1) * 512)
                tl = [b_h2[4 * n + j] for j in range(4)]
                for j in range(8):
                    pg, b_pg = self.PS.get()
                    pu, b_pu = self.PS.get()
                    for kc in range(8):
                        kb.op("tensor", lambda e, kc=kc: e.matmul(pg[:], wgu[:, kc, j * 128:(j + 1) * 128], h2T[:, kc, toks],
                                                                  start=(kc == 0), stop=(kc == 7)), reads=[b_wgu] + tl, writes=[b_pg], sig=(kc == 7))
                    for kc in range(8):
                        kb.op("tensor", lambda e, kc=kc: e.matmul(pu[:], wgu[:, kc, D + j * 128:D + (j + 1) * 128], h2T[:, kc, toks],
                                                                  start=(kc == 0), stop=(kc == 7)), reads=[b_wgu] + tl, writes=[b_pu], sig=(kc == 7))
                    g, b_g = gp.get(); sg, b_sg = sp.get(); u, b_u = up.get()
                    kb.op("vector", lambda e: e.tensor_scalar(out=g[:], in0=pg[:], scalar1=bgu[:, ex_, j:j + 1], scalar2=7.0, op0=ALU.add, op1=ALU.min),
                          reads=[b_pg, b_bgu], writes=[b_g])
                    kb.op("scalar", lambda e: e.activation(out=sg[:], in_=g[:], func=AF.Silu, scale=1.702), reads=[b_g], writes=[b_sg])
                    kb.op("vector", lambda e: e.tensor_scalar(out=u[:], in0=pu[:], scalar1=bgu1[:, ex_, j:j + 1], scalar2=8.0, op0=ALU.add, op1=ALU.min),
                          reads=[b_pu, b_bgu], writes=[b_u])
                    kb.op("vector", lambda e: e.scalar_tensor_tensor(out=actT[:, j, :], in0=u[:], scalar=-6.0, in1=sg[:], op0=ALU.max, op1=ALU.mult),
                          reads=[b_u, b_sg], writes=[b_act])

            def dn(ex_, n):
                wgu, b_wgu, wdn, b_wdn = W[ex_]
                actT, b_act = ACT.pop((ex_, n))
                for tq in range(4):
                    t = n * 4 + tq
                    for cb in range(2):
                        cs_ = slice(cb * 512, (cb + 1) * 512)
                        pd_, b_pd = self.PS.get()
                        for kc in range(8):
                            kb.op("tensor", lambda e, kc=kc: e.matmul(pd_[:], actT[:, kc, tq * 128:(tq + 1) * 128], wdn[:, kc, cs_],
                                                                      start=(kc == 0), stop=(kc == 7)), reads=[b_act, b_wdn], writes=[b_pd], sig=(kc == 7))
                        kb.op("vector", lambda e: e.scalar_tensor_tensor(out=X[:, t, cs_], in0=pd_[:], scalar=combs[:, t, ex_:ex_ + 1], in1=X[:, t, cs_],
                                                                        op0=ALU.mult, op1=ALU.add), reads=[b_pd, b_comb], writes=[b_X[t]])

            items = [(e_, n) for e_ in range(nexp) for n in range(NB)]
            load_w(0)
            if nexp > 1:
                load_w(1)
            gu(*items[0])
            for k, (e_, n) in enumerate(items):
                if k + 1 < len(items):
                    gu(*items[k + 1])
                dn(e_, n)
                if n == NB - 1 and e_ + 2 < nexp:
                    load_w(e_ + 2)
        if hp == 0:
            self.dump("x2", X[:, 0, :], b_X[0], [128, D])
        with kb.scope():
            nf = kb.sb("normf", [128, D], F32); b_nf = kb.buf("normf")
            kb.dma("sync", nf[:], self.norm_f.partition_broadcast(128), writes=[b_nf])
            junk = Pool(kb, "f_junk", [128, D], F32, 1)
            stat = Pool(kb, "f_stat", [128, 4], F32, 4)
            op_ = Pool(kb, "f_o", [128, D], F32, 2)
            for t in range(NTL):
                gt = hp * NTL + t
                jt, b_jt = junk.get(); st, b_st = stat.get()
                kb.op("scalar", lambda e, jt=jt, st=st, t=t: e.activation(out=jt[:], in_=X[:, t, :], func=AF.Square, accum_out=st[:, 0:1]), reads=[b_X[t]], writes=[b_jt, b_st])
                kb.op("scalar", lambda e, st=st: e.activation(out=st[:, 1:2], in_=st[:, 0:1], func=AF.Sqrt, bias=EPS, scale=1.0 / D), reads=[b_st], writes=[b_st])
                kb.op("vector", lambda e, st=st: e.reciprocal(out=st[:, 2:3], in_=st[:, 1:2]), reads=[b_st], writes=[b_st])
                ot, b_ot = op_.get()
                kb.op("vector", lambda e, ot=ot, st=st, t=t: e.scalar_tensor_tensor(out=ot[:], in0=X[:, t, :], scalar=st[:, 2:3], in1=nf[:], op0=ALU.mult, op1=ALU.mult),
                      reads=[b_X[t], b_st, b_nf], writes=[b_ot])
                bo = kb.buf("out%d" % gt)
                kb.dma("sync", self.out[gt * 128:(gt + 1) * 128, :], ot[:], reads=[b_ot], writes=[bo])
                self.fin.append(bo)

    def _norm_tile2(self, xt_ap, b_xt, tt, A, b_A, shift_chunk0, xnp, junk, stat, dstT, b_dst, hf, b_hf):
        kb = self.kb
        jt, b_jt = junk.get()
        st, b_st = stat.get()
        kb.op("scalar", lambda e: e.activation(out=jt[:], in_=xt_ap, func=AF.Square, accum_out=st[:, 0:1]), reads=[b_xt], writes=[b_jt, b_st])
        kb.op("scalar", lambda e: e.activation(out=st[:, 1:2], in_=st[:, 0:1], func=AF.Sqrt, bias=EPS, scale=1.0 / D), reads=[b_st], writes=[b_st])
        kb.op("vector", lambda e: e.reciprocal(out=st[:, 2:3], in_=st[:, 1:2]), reads=[b_st], writes=[b_st])
        xn, b_xn = xnp.get()
        kb.op("scalar", lambda e: e.activation(out=xn[:], in_=xt_ap, func=AF.Identity, scale=st[:, 2:3]), reads=[b_xt, b_st], writes=[b_xn])
        for half in range(2):
            pt, b_pt = self.PS.get()
            for q in range(4):
                kc = half * 4 + q
                kb.op("tensor", lambda e, kc=kc, q=q, pt=pt: e.transpose(pt[:, q * 128:(q + 1) * 128], xn[:, kc * 128:(kc + 1) * 128], self.identf[:]),
                      reads=[b_xn, self.b_ident], writes=[b_pt])
            for q in range(4):
                kc = half * 4 + q
                kb.op("vector", lambda e, kc=kc, q=q, pt=pt: e.tensor_scalar(
                    out=hf[:, kc, :], in0=pt[:, q * 128:(q + 1) * 128], scalar1=A[:, kc, 0:1], scalar2=self.modT[:, shift_chunk0 + kc, 0:1],
                    op0=ALU.mult, op1=ALU.add), reads=[b_pt, b_A, self.b_modT], writes=[b_hf])
        kb.op("scalar", lambda e: e.activation(out=dstT[:, :, tt * 128:(tt + 1) * 128], in_=hf[:], func=AF.Identity), reads=[b_hf], writes=[b_dst])

    def build(self):
        kb = self.kb
        self.declare()
        self.common()
        with kb.scope():
            self.hT = kb.sb("hT", [128, 8, TOK], BF16)
            self.b_hT = [kb.buf("hT%d" % t) for t in range(NT)]
            self.yaT = kb.sb("yaT", [128, 4, L], BF16)
            self.b_yaT = kb.buf("yaT")
            with kb.scope():
                self.phase_s5_setup()
                if self.stop == "s5setup":
                    return self.finish()
                with kb.scope():
                    self.phase_mod()
                if self.stop == "mod":
                    return self.finish()
                with kb.scope():
                    self.phase_norm1()
                if self.stop == "norm1":
                    return self.finish()
                self.uT = kb.sb("uT", [128, 4, TOK], BF16)
                self.b_uT = kb.buf("uT")
                self.yT = kb.sb("yT", [128, 4, L], F32)
                self.b_yT = [kb.buf("yT%d" % q) for q in range(4)]
                with kb.scope():
                    self.phase_u()
                if self.stop == "u":
                    return self.finish()
                with kb.scope():
                    self.phase_s5()
                if self.stop == "s5":
                    return self.finish()
                with kb.scope():
                    self.phase_glu()
                if self.stop == "glu":
                    return self.finish()
            self.ybT = kb.sb("ybT", [128, 4, L], BF16)
            self.b_ybT = kb.buf("ybT")
            with kb.scope():
                self.phase_gdn_setup()
                if self.stop in ("gdnsetup",):
                    return self.finish()
                self.phase_gdn()
                if self.stop in ("gdn", "gdn_h0"):
                    return self.finish()
                with kb.scope():
                    self.phase_gdn_out()
                if self.stop == "gdnout":
                    return self.finish()
            with kb.scope():
                self.phase_merge()
            if self.stop == "merge":
                return self.finish()
        for hp in range(2):
            with kb.scope():
                self.phase_moe_half(hp)
            if self.stop in ("router", "moe1", "half"):
                return self.finish()
        return self.finish()

    def finish(self):
        self.kb.finish(self.fin)
        self.kb.finished = True
        for es in reversed(getattr(self.kb, "scopes", [])):
            es.close()
        self.kb.root.close()
        return self.nc


def _fm(v, nch):
    return np.ascontiguousarray(np.asarray(v, np.float32).reshape(nch, 128).T)


def host_inputs(inputs, b):
    f = lambda a: np.ascontiguousarray(np.asarray(a, np.float32))
    m = {}
    m["x"] = f(inputs["x"][b])
    m["ctx"] = f(inputs["ctx"][b])
    cs = np.stack([_fm(inputs["c"][b], 8), _fm(inputs["c_ctx"], 8)], axis=-1)
    m["cs"] = f(cs)
    m["w_mod"] = f(inputs["w_mod"][0])
    bm = np.asarray(inputs["b_mod"][0], np.float32)
    bmt = _fm(bm, 48)
    order = list(range(0, 16)) + list(range(24, 40)) + list(range(16, 24)) + list(range(40, 48))
    m["b_mod_t"] = f(bmt[:, order])
    m["b_mod"] = f(bm)
    m["norm1_t"] = _fm(inputs["norm1"][0], 8)
    m["norm2_t"] = _fm(inputs["norm2"][0], 8)
    m["w_in"] = f(inputs["w_in"][0])
    m["ident"] = np.eye(128, dtype=np.float32)
    m["s5_d_t"] = _fm(inputs["s5_d"][0], 4)
    def pdl(a):
        a = np.asarray(a, np.float32).reshape(2, 16, 2, 64)
        return f(a.transpose(2, 3, 0, 1).reshape(128, 32))
    m["lam_re_t"] = pdl(inputs["s5_lam_re"][0])
    m["lam_im_t"] = pdl(inputs["s5_lam_im"][0])
    m["logstep_t"] = pdl(np.broadcast_to(np.asarray(inputs["s5_log_step"][0], np.float32)[:, :, None], (2, 32, 64)))
    def blk(re, im, cn):
        out = np.zeros((2, 64, 2, 2, 16, 2, 16), np.float32)
        for ri, arr in enumerate((re, im)):
            arr = np.asarray(arr, np.float32)
            arr = arr if cn else arr.transpose(0, 1, 3, 2)
            arr = arr.reshape(2, 16, 2, 64, 16)
            for g2 in range(2):
                out[g2, :, ri, :, :, g2, :] = arr[:, :, g2].transpose(2, 0, 1, 3)
        return f(out.reshape(128, 2, 32, 32))
    m["Bblk"] = blk(inputs["s5_b_re"][0], inputs["s5_b_im"][0], True)
    m["Cblk"] = blk(inputs["s5_c_re"][0], inputs["s5_c_im"][0], False)
    r_ = np.arange(128)[:, None]; c_ = np.arange(128)[None, :]
    same = (r_ // 64) == (c_ // 64)
    NEG = -30000.0
    Mf = (same & (r_ <= c_)).astype(np.float32); Mb = (same & (r_ >= c_)).astype(np.float32)
    gmk = [Mf, Mb, np.where(same & (r_ > c_), 0.0, NEG), np.where(same & (r_ < c_), 0.0, NEG), np.where(same & (r_ <= c_), 0.0, NEG),
           np.tile((r_ < 64), (1, 128)).astype(np.float32), np.tile((r_ >= 64), (1, 128)).astype(np.float32), same.astype(np.float32),
           -Mf, -Mb, np.where(same & (r_ >= c_), 0.0, NEG)]
    m["gmask"] = f(np.stack([np.asarray(a, np.float32) for a in gmk], axis=1))
    cw = np.asarray(inputs["gdn_conv"][0], np.float32)
    m["conv_t"] = f(cw.reshape(5, 12, 128).transpose(2, 1, 0))
    m["alog_dtb"] = f(np.concatenate([np.asarray(inputs["gdn_a_log"][0]).reshape(8), np.asarray(inputs["gdn_dt_bias"][0]).reshape(8)]))
    m["gdn_norm"] = f(inputs["gdn_norm"][0])
    m["w_glu"] = f(inputs["s5_w_glu"][0])
    m["b_glu_t"] = _fm(inputs["s5_b_glu"][0], 4)
    m["w_ba"] = f(inputs["w_branch_a"][0])
    m["w_bb"] = f(inputs["w_branch_b"][0])
    m["w_out"] = f(inputs["w_out"][0])
    m["w_router"] = f(inputs["w_router"][0])
    m["b_router"] = f(inputs["b_router"][0])
    m["w_gu"] = f(inputs["w_gate_up"][0])
    bgu = np.asarray(inputs["b_gate_up"][0], np.float32)
    m["b_gu_t"] = f(bgu.reshape(NE, 16, 128).transpose(2, 0, 1))
    m["w_dn"] = f(inputs["w_down"][0])
    m["b_dn"] = f(inputs["b_down"][0])
    m["norm_f"] = f(inputs["norm_f"])
    m["iota1"] = f(np.tile(np.arange(1, TS5 + 1, dtype=np.float32)[None], (128, 1)))
    return m


_CACHE = {}


def kernel(**inputs):
    nb = 8
    if "nc" not in _CACHE:
        _CACHE["nc"] = Builder().build()
    nc = _CACHE["nc"]
    shared = host_inputs(inputs, 0)
    in_maps = []
    for b in range(nb):
        m = dict(shared)
        if b > 0:
            f = lambda a: np.ascontiguousarray(np.asarray(a, np.float32))
            m["x"] = f(inputs["x"][b])
            m["ctx"] = f(inputs["ctx"][b])
            m["cs"] = f(np.stack([_fm(inputs["c"][b], 8), _fm(inputs["c_ctx"], 8)], axis=-1))
        in_maps.append(m)
    res = run_bass_kernel_spmd(nc, in_maps, core_ids=list(range(nb)))
    out = np.stack([np.asarray(res.results[b]["out"], np.float32) for b in range(nb)], axis=0)
    return out
```

```python
import os
import numpy as np
from contextlib import ExitStack
import concourse.bass as bass
import concourse.mybir as mybir
from concourse.bass_utils import run_bass_kernel_spmd

F32 = mybir.dt.float32
BF16 = mybir.dt.bfloat16
I32 = mybir.dt.int32
AF = mybir.ActivationFunctionType
ALU = mybir.AluOpType
AX = mybir.AxisListType

D = 1024
L = 2048
CTX = 256
TOK = L + CTX
NT = TOK // 128
EPS = 1e-6
NE = 32
TS5 = 128
POST_ENG = os.environ.get('KPOST', 'gpsimd')
IN_COLS = 4624
OFF_U, OFF_QKV, OFF_GATE, OFF_B, OFF_A, OFF_BR = 0, 512, 2048, 2560, 2568, 2576
BLOCKS = [(0, 256)] + [(256 + 512 * i, 512) for i in range(4)]


class Buf:
    __slots__ = ("name", "w", "r")

    def __init__(self, name):
        self.name = name
        self.w = None
        self.r = {}


class KB:
    def __init__(self, nc, same_engine_sync=True):
        self.nc = nc
        self.es = ExitStack()
        self.root = self.es
        self.same = same_engine_sync
        self.engs = {}
        self.sems = {}
        for name in ("tensor", "vector", "scalar", "gpsimd", "sync"):
            eng = getattr(nc, name)
            sem = self.root.enter_context(nc.semaphore("s_" + name))
            self.sems["E" + name] = sem
            self.engs[name] = dict(eng=eng, key="E" + name, count=0, waited={})
        self.nbuf = 0
        self.dmasems = {}
        self.nalloc = 0

    def sb(self, name, shape, dt=F32):
        nb = int(np.prod(shape[1:])) * (2 if dt == BF16 else 4)
        self.nalloc += (nb + 31) // 32 * 32
        if os.environ.get("KALLOC"):
            print("alloc", name, shape, nb, "total", self.nalloc)
        self.nnames = getattr(self, "nnames", 0) + 1
        return self.es.enter_context(self.nc.sbuf_tensor("sb%d_%s" % (self.nnames, name), list(shape), dt))

    def ps(self, name, shape, dt=F32):
        return self.es.enter_context(self.nc.psum_tensor("pp_" + name, list(shape), dt))

    def buf(self, name=None):
        self.nbuf += 1
        return Buf((name or "b") + "_%d" % self.nbuf)

    def _wait(self, en, key, val):
        e = self.engs[en]
        if key == e["key"] and ((not self.same) or en == "tensor"):
            return
        if e["waited"].get(key, 0) >= val:
            return
        if key in self.dmasems:
            val = self.dmasems[key]
        e["eng"].wait_ge(self.sems[key], val)
        e["waited"][key] = val

    def _deps(self, en, reads, writes):
        need = {}
        for b in reads:
            if b.w is not None:
                k, v = b.w
                need[k] = max(need.get(k, 0), v)
        for b in writes:
            if b.w is not None:
                k, v = b.w
                need[k] = max(need.get(k, 0), v)
            for k, v in b.r.items():
                need[k] = max(need.get(k, 0), v)
        for k, v in need.items():
            self._wait(en, k, v)

    def _post(self, sig, reads, writes):
        k, v = sig
        for b in reads:
            b.r[k] = max(b.r.get(k, 0), v)
        for b in writes:
            b.w = sig
            b.r = {}

    def op(self, en, fn, reads=(), writes=(), sig=True):
        e = self.engs[en]
        self._deps(en, reads, writes)
        inst = fn(e["eng"])
        if sig:
            e["count"] += 1
            inst.then_inc(self.sems[e["key"]], 1)
            e["pending"] = False
            self._post((e["key"], e["count"]), reads, writes)
        else:
            assert en == "tensor"
            e["pending"] = True
            self._post((e["key"], e["count"] + 1), reads, writes)
        return inst

    NDSEM = 64

    def dma(self, en, out, in_, reads=(), writes=(), **kw):
        e = self.engs[en]
        self._deps(en, reads, writes)
        tgt = writes[0] if writes else reads[0]
        if not hasattr(self, "bufsem"):
            self.bufsem = {}
            self.dsem_list = []
        semkey = self.bufsem.get(tgt.name)
        if semkey is None:
            idx = len(self.bufsem) % self.NDSEM
            semkey = "DS%d" % idx
            self.bufsem[tgt.name] = semkey
            if semkey not in self.sems:
                self.sems[semkey] = self.root.enter_context(self.nc.semaphore("d%d" % idx))
                self.dmasems[semkey] = 0
        self.dmasems[semkey] += 16
        inst = e["eng"].dma_start(out=out, in_=in_, **kw)
        inst.then_inc(self.sems[semkey], 16)
        self._post((semkey, self.dmasems[semkey]), reads, writes)
        return inst

    def barrier(self):
        for en, e in self.engs.items():
            for en2, e2 in self.engs.items():
                if en2 != en and e2["count"] > 0:
                    self._wait(en, e2["key"], e2["count"])
            for k, v in self.dmasems.items():
                self._wait(en, k, v)

    def scope(self):
        kb = self

        class _S:
            def __enter__(s):
                s.old = kb.es
                s.base = kb.nalloc
                kb.es = ExitStack()
                kb.scopes = getattr(kb, "scopes", [])
                kb.scopes.append(kb.es)

            def __exit__(s, *a):
                if getattr(kb, "finished", False):
                    return
                kb.barrier()
                kb.es.close()
                kb.scopes.pop()
                kb.es = s.old
                kb.nalloc = s.base
        return _S()

    def finish(self, bufs):
        for b in bufs:
            if b.w is not None:
                self._wait("sync", b.w[0], b.w[1])
            for k, v in b.r.items():
                self._wait("sync", k, v)


class Pool:
    def __init__(self, kb, name, shape, dt, n, psum=False):
        self.tiles = []
        for i in range(n):
            t = (kb.ps if psum else kb.sb)("%s%d" % (name, i), shape, dt)
            self.tiles.append((t, kb.buf("%s%d" % (name, i))))
        self.i = 0

    def get(self):
        t = self.tiles[self.i % len(self.tiles)]
        self.i += 1
        return t


def _rev(ap2):
    return ap2[:, ::-1]


class Builder:
    def __init__(self, dbg=(), stop=None, same=True):
        self.dbg = set(dbg)
        self.stop = stop
        self.nc = bass.Bass("TRN2", target_bir_lowering=False)
        self.kb = KB(self.nc, same_engine_sync=same)
        self.ins = {}
        self.outs = {}
        self.fin = []

    def inp(self, name, shape):
        t = self.nc.dram_tensor(name, list(shape), F32, kind="ExternalInput").ap()
        self.ins[name] = t
        return t

    def outp(self, name, shape):
        t = self.nc.dram_tensor(name, list(shape), F32, kind="ExternalOutput").ap()
        self.outs[name] = t
        return t

    def dump(self, name, tile_ap, b, shape):
        if name not in self.dbg:
            return
        o = self.outp("dbg_" + name, shape)
        bo = self.kb.buf("dbg_" + name)
        self.kb.dma("sync", o, tile_ap, reads=[b], writes=[bo])
        self.fin.append(bo)

    def dump_bf(self, name, tile_ap, b, shape):
        if name not in self.dbg:
            return
        kb = self.kb
        t = kb.sb("dbgt_" + name, shape, F32)
        bt = kb.buf("dbgt_" + name)
        kb.op("vector", lambda e: e.tensor_copy(out=t[:], in_=tile_ap), reads=[b], writes=[bt])
        self.dump(name, t[:], bt, shape)

    def declare(self):
        i = self.inp
        self.x = i("x", [L, D])
        self.ctx = i("ctx", [CTX, D])
        self.cs_in = i("cs", [128, 8, 2])
        self.w_mod = i("w_mod", [D, 6 * D])
        self.b_mod_t = i("b_mod_t", [128, 48])
        self.b_mod = i("b_mod", [6 * D])
        self.norm1_t = i("norm1_t", [128, 8])
        self.norm2_t = i("norm2_t", [128, 8])
        self.w_in = i("w_in", [D, IN_COLS])
        self.ident = i("ident", [128, 128])
        self.s5_d_t = i("s5_d_t", [128, 4])
        self.lam_re_t = i("lam_re_t", [128, 32])
        self.lam_im_t = i("lam_im_t", [128, 32])
        self.logstep_t = i("logstep_t", [128, 32])
        self.Bblk = i("Bblk", [128, 2, 32, 32])
        self.Cblk = i("Cblk", [128, 2, 32, 32])
        self.iota1 = i("iota1", [128, TS5])
        self.gmask = i("gmask", [128, 11, 128])
        self.conv_t = i("conv_t", [128, 12, 5])
        self.alog_dtb = i("alog_dtb", [16])
        self.gdn_norm = i("gdn_norm", [128])
        self.w_glu = i("w_glu", [512, 512])
        self.b_glu_t = i("b_glu_t", [128, 4])
        self.w_ba = i("w_ba", [512, D])
        self.w_bb = i("w_bb", [512, D])
        self.w_out = i("w_out", [D, D])
        self.w_router = i("w_router", [D, NE])
        self.b_router = i("b_router", [NE])
        self.w_gu = i("w_gu", [NE, D, 2 * D])
        self.b_gu_t = i("b_gu_t", [128, NE, 16])
        self.w_dn = i("w_dn", [NE, D, D])
        self.b_dn = i("b_dn", [NE, D])
        self.norm_f = i("norm_f", [D])
        self.out = self.outp("out", [L, D])
        self.x1s = self.nc.dram_tensor("x1_scratch", [L, D], F32, kind="Internal").ap()

    def common(self):
        kb = self.kb
        self.PS = Pool(kb, "ps", [128, 512], F32, 8, psum=True)
        self.modT = kb.sb("modT", [128, 32, 2], F32)
        self.b_modT = kb.buf("modT")
        self.gt1_bc = kb.sb("gt1_bc", [128, D], F32)
        self.gt2_bc = kb.sb("gt2_bc", [128, D], F32)
        self.b_gt1 = kb.buf("gt1")
        self.b_gt2 = kb.buf("gt2")
        self.A1 = kb.sb("A1", [128, 8, 2], F32)
        self.b_A1 = kb.buf("A1")
        self.identf = kb.sb("identf", [128, 128], F32)
        self.b_ident = kb.buf("ident")
        kb.dma("sync", self.identf[:], self.ident, writes=[self.b_ident])

    def phase_mod(self):
        kb, nc = self.kb, self.nc
        cs = kb.sb("cs", [128, 8, 2], F32)
        b_cs = kb.buf("cs")
        kb.dma("sync", cs[:], self.cs_in, writes=[b_cs])
        sg = kb.sb("cs_sg", [128, 8, 2], F32)
        b_sg = kb.buf("cs_sg")
        kb.op("scalar", lambda e: e.activation(out=sg[:], in_=cs[:], func=AF.Sigmoid), reads=[b_cs], writes=[b_sg])
        css = kb.sb("css", [128, 8, 2], F32)
        b_css = kb.buf("css")
        kb.op("vector", lambda e: e.tensor_tensor(out=css[:], in0=cs[:], in1=sg[:], op=ALU.mult), reads=[b_cs, b_sg], writes=[b_css])
        csb = kb.sb("csb", [128, 8, 128], F32)
        b_csb = kb.buf("csb")
        kb.op("vector", lambda e: e.tensor_copy(out=csb[:], in_=css[:, :, 0:1].to_broadcast([128, 8, 128])), reads=[b_css], writes=[b_csb])
        bmt = kb.sb("bmt", [128, 48], F32)
        b_bmt = kb.buf("bmt")
        kb.dma("sync", bmt[:], self.b_mod_t, writes=[b_bmt])
        wpool = Pool(kb, "wmod", [128, 8, 512], F32, 2)
        wv = self.w_mod.rearrange("(kc p) n -> p kc n", p=128)
        pm, b_pm = self.PS.get()
        fm_groups = [0, 1, 2, 3, 6, 7, 8, 9]
        for gi, g in enumerate(fm_groups):
            wt, b_wt = wpool.get()
            kb.dma("sync" if gi % 2 == 0 else "scalar", wt[:], wv[:, :, 512 * g:512 * (g + 1)], writes=[b_wt])
            for cc in range(4):
                j = gi * 4 + cc
                for kc in range(8):
                    kb.op("tensor", lambda e, j=j, kc=kc, cc=cc, wt=wt: e.matmul(
                        pm[:, 2 * j:2 * j + 2], wt[:, kc, cc * 128:(cc + 1) * 128], css[:, kc, :],
                        start=(kc == 0), stop=(kc == 7)), reads=[b_wt, b_css], writes=[b_pm], sig=(kc == 7))
        kb.op("vector", lambda e: e.tensor_tensor(
            out=self.modT[:], in0=pm[:, 0:64].rearrange("p (j t) -> p j t", t=2),
            in1=self._bmt_sel(bmt), op=ALU.add), reads=[b_pm, b_bmt], writes=[self.b_modT])
        for which, (g0, dst, bdst) in enumerate([(4, self.gt1_bc, self.b_gt1), (10, self.gt2_bc, self.b_gt2)]):
            bb = kb.sb("bmodbc%d" % which, [128, D], F32)
            b_bb = kb.buf("bmodbc")
            kb.dma("sync", bb[:], self.b_mod[512 * g0:512 * g0 + D].partition_broadcast(128), writes=[b_bb])
            for half in range(2):
                g = g0 + half
                wt, b_wt = wpool.get()
                kb.dma("sync" if half == 0 else "scalar", wt[:], wv[:, :, 512 * g:512 * (g + 1)], writes=[b_wt])
                pg, b_pg = self.PS.get()
                for kc in range(8):
                    kb.op("tensor", lambda e, kc=kc, wt=wt, pg=pg: e.matmul(
                        pg[:], csb[:, kc, :], wt[:, kc, :], start=(kc == 0), stop=(kc == 7)),
                        reads=[b_wt, b_csb], writes=[b_pg], sig=(kc == 7))
                kb.op("vector", lambda e, pg=pg, half=half, dst=dst, bb=bb: e.tensor_tensor(
                    out=dst[:, 512 * half:512 * (half + 1)], in0=pg[:], in1=bb[:, 512 * half:512 * (half + 1)], op=ALU.add),
                    reads=[b_pg, b_bb], writes=[bdst])
        self.dump("modT", self.modT[:], self.b_modT, [128, 32, 2])
        self.dump("gt1", self.gt1_bc[:], self.b_gt1, [128, D])

    def _bmt_sel(self, bmt):
        return bmt[:, 0:32].unsqueeze(2).to_broadcast([128, 32, 2])

    def norm_to_T(self, src_tiles, dstT, b_dstT_blocks, scale_ap_fn, shift_ap_fn, tile_base, tag):
        raise NotImplementedError

    def phase_norm1(self):
        kb = self.kb
        n1 = kb.sb("norm1", [128, 8], F32)
        b_n1 = kb.buf("n1")
        kb.dma("sync", n1[:], self.norm1_t, writes=[b_n1])
        kb.op("vector", lambda e: e.scalar_tensor_tensor(
            out=self.A1[:], in0=self.modT[:, 8:16, :], scalar=1.0, in1=n1[:].unsqueeze(2).to_broadcast([128, 8, 2]),
            op0=ALU.add, op1=ALU.mult), reads=[self.b_modT, b_n1], writes=[self.b_A1])
        xin = Pool(kb, "xin", [128, D], F32, 3)
        xnp = Pool(kb, "xn", [128, D], F32, 2)
        junk = Pool(kb, "junk", [128, D], F32, 1)
        stat = Pool(kb, "stat", [128, 4], F32, 4)
        for tt in range(NT):
            which = 1 if tt < 2 else 0
            src = self.ctx[tt * 128:(tt + 1) * 128, :] if tt < 2 else self.x[(tt - 2) * 128:(tt - 1) * 128, :]
            xt, b_xt = xin.get()
            kb.dma("sync" if tt % 2 == 0 else "scalar", xt[:], src, writes=[b_xt])
            self._norm_tile(xt, b_xt, tt, which, self.A1, self.b_A1, 0, xnp, junk, stat, self.hT, self.b_hT[tt])
        self.dump_bf("hT", self.hT[:, :, 0:512], self.b_hT[3], [128, 8, 512]) if False else None
        if "hT" in self.dbg:
            allb = kb.buf("hTall")
            t = kb.sb("dbg_hT_t", [128, 8, 384], F32)
            kb.op("vector", lambda e: e.tensor_copy(out=t[:], in_=self.hT[:, :, 128:512]), reads=self.b_hT[1:4], writes=[allb])
            self.dump("hT", t[:], allb, [128, 8, 384])

    def _norm_tile(self, xt, b_xt, tt, which, A, b_A, shift_chunk0, xnp, junk, stat, dstT, b_dst):
        kb = self.kb
        jt, b_jt = junk.get()
        st, b_st = stat.get()
        kb.op("scalar", lambda e: e.activation(out=jt[:], in_=xt[:], func=AF.Square, accum_out=st[:, 0:1]),
              reads=[b_xt], writes=[b_jt, b_st])
        kb.op("scalar", lambda e: e.activation(out=st[:, 1:2], in_=st[:, 0:1], func=AF.Sqrt, bias=EPS, scale=1.0 / D),
              reads=[b_st], writes=[b_st])
        kb.op("vector", lambda e: e.reciprocal(out=st[:, 2:3], in_=st[:, 1:2]), reads=[b_st], writes=[b_st])
        xn, b_xn = xnp.get()
        kb.op("scalar", lambda e: e.activation(out=xn[:], in_=xt[:], func=AF.Identity, scale=st[:, 2:3]),
              reads=[b_xt, b_st], writes=[b_xn])
        for half in range(2):
            pt, b_pt = self.PS.get()
            for q in range(4):
                kc = half * 4 + q
                kb.op("tensor", lambda e, kc=kc, q=q, pt=pt: e.transpose(
                    pt[:, q * 128:(q + 1) * 128], xn[:, kc * 128:(kc + 1) * 128], self.identf[:]),
                    reads=[b_xn, self.b_ident], writes=[b_pt])
            for q in range(4):
                kc = half * 4 + q
                kb.op("vector", lambda e, kc=kc, q=q, pt=pt: e.tensor_scalar(
                    out=dstT[:, kc, tt * 128:(tt + 1) * 128], in0=pt[:, q * 128:(q + 1) * 128],
                    scalar1=A[:, kc, which:which + 1], scalar2=self.modT[:, shift_chunk0 + kc, which:which + 1],
                    op0=ALU.mult, op1=ALU.add), reads=[b_pt, b_A, self.b_modT], writes=[b_dst])

    def load_w_bf16(self, dst, b_dst, src_rows_ap, ncols, col0=0):
        kb = self.kb
        kcs = dst.shape[1]
        v = src_rows_ap.rearrange("(kc p) n -> p kc n", p=128)
        for kc in range(kcs):
            kb.dma("gpsimd", dst[:, kc, :], v[:, kc, col0:col0 + ncols], writes=[b_dst])

    def phase_u(self):
        kb = self.kb
        wu = kb.sb("wu", [128, 8, 512], BF16)
        b_wu = kb.buf("wu")
        self.load_w_bf16(wu, b_wu, self.w_in, 512, OFF_U)
        dsk = kb.sb("s5d", [128, 4], F32)
        b_dsk = kb.buf("s5d")
        kb.dma("sync", dsk[:], self.s5_d_t, writes=[b_dsk])
        for oc in range(4):
            for (s0, n) in BLOCKS:
                pt, b_pt = self.PS.get()
                tiles = range(s0 // 128, (s0 + n) // 128)
                for kc in range(8):
                    kb.op("tensor", lambda e, kc=kc, pt=pt, oc=oc, s0=s0, n=n: e.matmul(
                        pt[:, 0:n], wu[:, kc, oc * 128:(oc + 1) * 128], self.hT[:, kc, s0:s0 + n],
                        start=(kc == 0), stop=(kc == 7)), reads=[b_wu] + [self.b_hT[t] for t in tiles], writes=[b_pt], sig=(kc == 7))
                kb.op("scalar", lambda e, pt=pt, oc=oc, s0=s0, n=n: e.activation(
                    out=self.uT[:, oc, s0:s0 + n], in_=pt[:, 0:n], func=AF.Identity), reads=[b_pt], writes=[self.b_uT])
                if s0 >= CTX:
                    kb.op("scalar", lambda e, pt=pt, oc=oc, s0=s0, n=n: e.activation(
                        out=self.yT[:, oc, s0 - CTX:s0 - CTX + n], in_=pt[:, 0:n], func=AF.Identity, scale=dsk[:, oc:oc + 1]),
                        reads=[b_pt, b_dsk], writes=[self.b_yT[oc]])
        if "uT" in self.dbg:
            t = kb.sb("dbg_uT_t", [128, 4, 512], F32)
            bt = kb.buf("dbg_uT")
            kb.op("vector", lambda e: e.tensor_copy(out=t[:], in_=self.uT[:, :, 0:512]), reads=[self.b_uT], writes=[bt])
            self.dump("uT", t[:], bt, [128, 4, 512])


    def sincos(self, ang, b_ang, n, want_cos, out_ap, b_out, scale=1.0):
        kb = self.kb
        with kb.scope():
            y = kb.sb("sc_y", [128, n], F32)
            ki = kb.sb("sc_k", [128, n], I32)
            kf = kb.sb("sc_kf", [128, n], F32)
            b = kb.buf("sc")
            off = 0.75 if want_cos else 0.5
            kb.op("vector", lambda e: e.tensor_scalar(out=y[:], in0=ang, scalar1=1.0 / (2 * np.pi), scalar2=off + 64.0,
                                                     op0=ALU.mult, op1=ALU.add), reads=[b_ang], writes=[b])
            kb.op("vector", lambda e: e.tensor_copy(out=ki[:], in_=y[:]), reads=[b], writes=[b])
            kb.op("vector", lambda e: e.tensor_copy(out=kf[:], in_=ki[:]), reads=[b], writes=[b])
            kb.op("vector", lambda e: e.tensor_tensor(out=y[:], in0=y[:], in1=kf[:], op=ALU.subtract), reads=[b], writes=[b])
            kb.op("vector", lambda e: e.scalar_tensor_tensor(out=kf[:], in0=y[:], scalar=0.0, in1=y[:], op0=ALU.is_lt, op1=ALU.add),
                  reads=[b], writes=[b])
            kb.op("scalar", lambda e: e.activation(out=y[:], in_=kf[:], func=AF.Sin, scale=6.2831, bias=-3.14155),
                  reads=[b], writes=[b])
            yv = y[:] if len(out_ap.shape) == 2 else y[:].rearrange("p (a t) -> p a t", t=out_ap.shape[-1])
            kb.op("scalar", lambda e: e.activation(out=out_ap, in_=yv, func=AF.Identity, scale=float(scale)),
                  reads=[b], writes=[b_out])

    def phase_s5_setup(self):
        kb = self.kb
        T = TS5
        self.rho = kb.sb("s5_rho", [128, 32], F32)
        self.Bdrv = kb.sb("Bdrv", [128, 2, 4, 2, 128], BF16)
        self.b_Bdrv = kb.buf("Bdrv")
        self.Crd = kb.sb("Crd", [128, 32, 2, 32], BF16)
        self.b_Crd = kb.buf("Crd")
        self.Tc = kb.sb("Tc", [128, 32 * T], BF16)
        self.b_Tc = kb.buf("Tc")
        self.Tsn = kb.sb("Tsn", [128, 32, 2, T], BF16)
        self.b_Tsn = kb.buf("Tsn")
        self.b_s5c = kb.buf("s5setup")
        with kb.scope():
            self._s5_setup_body()

    def _s5_setup_body(self):
        kb = self.kb
        T = TS5
        ld = lambda name, src, shape: self._ld(name, src, shape)
        lre, b_lre = ld("lre", self.lam_re_t, [128, 32])
        lim, b_lim = ld("lim", self.lam_im_t, [128, 32])
        lst, b_lst = ld("lst", self.logstep_t, [128, 32])
        Bb, b_Bb = ld("Bblk", self.Bblk, [128, 2, 32, 32])
        Cb, b_Cb = ld("Cblk", self.Cblk, [128, 2, 32, 32])
        io, b_io = ld("iota1", self.iota1, [128, T])
        V = lambda name: (kb.sb("s5v_" + name, [128, 32], F32))
        b = self.b_s5c
        deps = [b_lre, b_lim, b_lst, b]
        mag = self.rho
        lr, dt, ang, ar, ai, den, am1, fr, fi, t1, t2 = [V(n) for n in
            ("lr", "dt", "ang", "ar", "ai", "den", "am1", "fr", "fi", "t1", "t2")]
        v = lambda fn: kb.op("vector", fn, reads=deps, writes=[b])
        a = lambda fn: kb.op("scalar", fn, reads=deps, writes=[b])
        v(lambda e: e.tensor_scalar(out=lr[:], in0=lre[:], scalar1=-1e-4, scalar2=0.0, op0=ALU.min, op1=ALU.add))
        a(lambda e: e.activation(out=dt[:], in_=lst[:], func=AF.Exp))
        v(lambda e: e.tensor_tensor(out=t1[:], in0=lr[:], in1=dt[:], op=ALU.mult))
        a(lambda e: e.activation(out=mag[:], in_=t1[:], func=AF.Exp))
        v(lambda e: e.tensor_tensor(out=ang[:], in0=lim[:], in1=dt[:], op=ALU.mult))
        sn = V("sn0"); cs = V("cs0")
        self.sincos(ang[:], b, 32, False, sn[:], b)
        self.sincos(ang[:], b, 32, True, cs[:], b)
        v(lambda e: e.tensor_tensor(out=ar[:], in0=mag[:], in1=cs[:], op=ALU.mult))
        v(lambda e: e.tensor_tensor(out=ai[:], in0=mag[:], in1=sn[:], op=ALU.mult))
        v(lambda e: e.tensor_tensor(out=t1[:], in0=lr[:], in1=lr[:], op=ALU.mult))
        v(lambda e: e.tensor_tensor(out=t2[:], in0=lim[:], in1=lim[:], op=ALU.mult))
        v(lambda e: e.tensor_tensor(out=den[:], in0=t1[:], in1=t2[:], op=ALU.add))
        v(lambda e: e.reciprocal(out=den[:], in_=den[:]))
        v(lambda e: e.tensor_scalar(out=am1[:], in0=ar[:], scalar1=-1.0, scalar2=0.0, op0=ALU.add, op1=ALU.add))
        v(lambda e: e.tensor_tensor(out=t1[:], in0=am1[:], in1=lr[:], op=ALU.mult))
        v(lambda e: e.tensor_tensor(out=t2[:], in0=ai[:], in1=lim[:], op=ALU.mult))
        v(lambda e: e.tensor_tensor(out=fr[:], in0=t1[:], in1=t2[:], op=ALU.add))
        v(lambda e: e.tensor_tensor(out=fr[:], in0=fr[:], in1=den[:], op=ALU.mult))
        v(lambda e: e.tensor_tensor(out=t1[:], in0=ai[:], in1=lr[:], op=ALU.mult))
        v(lambda e: e.tensor_tensor(out=t2[:], in0=am1[:], in1=lim[:], op=ALU.mult))
        v(lambda e: e.tensor_tensor(out=fi[:], in0=t1[:], in1=t2[:], op=ALU.subtract))
        v(lambda e: e.tensor_tensor(out=fi[:], in0=fi[:], in1=den[:], op=ALU.mult))
        Bbar = kb.sb("Bbar", [128, 2, 32, 32], F32)
        tb = kb.sb("Bbar_t", [128, 32, 32], F32)
        b_Bbar = kb.buf("Bbar")
        frb = fr[:].unsqueeze(2).to_broadcast([128, 32, 32])
        fib = fi[:].unsqueeze(2).to_broadcast([128, 32, 32])
        vb = lambda fn: kb.op("vector", fn, reads=[b, b_Bb], writes=[b_Bbar])
        vb(lambda e: e.tensor_tensor(out=Bbar[:, 0], in0=Bb[:, 0], in1=frb, op=ALU.mult))
        vb(lambda e: e.tensor_tensor(out=tb[:], in0=Bb[:, 1], in1=fib, op=ALU.mult))
        vb(lambda e: e.tensor_tensor(out=Bbar[:, 0], in0=Bbar[:, 0], in1=tb[:], op=ALU.subtract))
        vb(lambda e: e.tensor_tensor(out=Bbar[:, 1], in0=Bb[:, 1], in1=frb, op=ALU.mult))
        vb(lambda e: e.tensor_tensor(out=tb[:], in0=Bb[:, 0], in1=fib, op=ALU.mult))
        vb(lambda e: e.tensor_tensor(out=Bbar[:, 1], in0=Bbar[:, 1], in1=tb[:], op=ALU.add))
        for d in range(2):
            for qd in range(4):
                pt, b_pt = self.PS.get()
                for ri in range(2):
                    blk = (d * 4 + qd) * 4
                    kb.op("tensor", lambda e, ri=ri, blk=blk, pt=pt: e.transpose(
                        pt[:, ri * 128:(ri + 1) * 128], Bbar[:, ri, blk:blk + 4, :], self.identf[:]),
                        reads=[b_Bbar, self.b_ident], writes=[b_pt])
                kb.op("scalar", lambda e, d=d, qd=qd, pt=pt: e.activation(
                    out=self.Bdrv[:, d, qd, :, :], in_=pt[:, 0:256].rearrange("p (r n) -> p r n", r=2), func=AF.Identity),
                    reads=[b_pt], writes=[self.b_Bdrv])
        kb.op("scalar", lambda e: e.activation(out=self.Crd[:, :, 0, :], in_=Cb[:, 0], func=AF.Identity),
              reads=[b_Cb], writes=[self.b_Crd])
        kb.op("scalar", lambda e: e.activation(out=self.Crd[:, :, 1, :], in_=Cb[:, 1], func=AF.Identity, scale=-1.0),
              reads=[b_Cb], writes=[self.b_Crd])
        ph = kb.sb("s5_ph", [128, 32, T], F32)
        b_ph = kb.buf("s5ph")
        kb.op("vector", lambda e: e.tensor_tensor(out=ph[:], in0=ang[:].unsqueeze(2).to_broadcast([128, 32, T]),
                                                 in1=io[:].unsqueeze(1).to_broadcast([128, 32, T]), op=ALU.mult),
              reads=[b, b_io], writes=[b_ph])
        for a0 in range(0, 32, 8):
            pha = ph[:, a0:a0 + 8, :].rearrange("p a t -> p (a t)")
            self.sincos(pha, b_ph, 8 * T, True, self.Tc[:, a0 * T:(a0 + 8) * T], self.b_Tc)
            self.sincos(pha, b_ph, 8 * T, False, self.Tsn[:, a0:a0 + 8, 0, :], self.b_Tsn)
            self.sincos(pha, b_ph, 8 * T, False, self.Tsn[:, a0:a0 + 8, 1, :], self.b_Tsn, scale=-1.0)
        self.dump("rho", self.rho[:], b, [128, 32])
        self.dump("fr", fr[:], b, [128, 32])
        self.dump("fi", fi[:], b, [128, 32])
        self.dump("Tc", self.Tc[:, 0:4 * T], self.b_Tc, [128, 4 * T])

    def _ld(self, name, src, shape, dt=F32, q="sync"):
        t = self.kb.sb(name, shape, dt)
        b = self.kb.buf(name)
        self.kb.dma(q, t[:], src, writes=[b])
        return t, b

    def phase_s5(self):
        kb = self.kb
        T = TS5
        NCK = TOK // T
        Tc3 = self.Tc[:].rearrange("p (a t) -> p a t", t=T)
        G = kb.sb("s5_G", [128, 32, 2, T], F32)
        b_G = [kb.buf("s5G%d" % i) for i in range(32)]
        carry = kb.sb("s5_carry", [128, 32, 2], F32)
        b_carry0 = kb.buf("s5carry")
        kb.op("vector", lambda e: e.memset(carry[:], 0.0), writes=[b_carry0])
        b_carryg = {}
        P1p = Pool(kb, "s5P1", [128, 2, T], F32, 4)
        P2p = Pool(kb, "s5P2", [128, 2, T], F32, 4)
        Vp = Pool(kb, "s5V", [128, 2, T], F32, 8)
        Q1p = Pool(kb, "s5Q1", [128, 2, T], F32, 4)
        Q2p = Pool(kb, "s5Q2", [128, 2, T], F32, 4)
        Hp = Pool(kb, "s5H", [128, 2, T], BF16, 8)
        ctmp = kb.sb("s5_ctmp", [128, 32, 2], F32)
        ctmp2 = kb.sb("s5_ctmp2", [128, 32, 2], F32)
        tabs = [self.b_Tc, self.b_Tsn]
        for ck in range(NCK):
            is_lat = ck >= CTX // T
            for qd in range(4):
                for d in range(2):
                    if d == 0:
                        s0 = ck * T
                    elif not is_lat:
                        s0 = CTX - (ck + 1) * T
                    else:
                        s0 = TOK - (ck - CTX // T + 1) * T
                    if is_lat:
                        py, b_py = self.PS.get()
                    gkey = (qd, d)
                    if gkey not in b_carryg:
                        b_carryg[gkey] = kb.buf("s5carry%d%d" % gkey)
                        b_carryg[gkey].w = b_carry0.w
                    b_carry = b_carryg[gkey]
                    st = []
                    for ppq in range(4):
                        pd = d * 16 + qd * 4 + ppq
                        rhs = self.uT[ppq * 32:(ppq + 1) * 32, qd, s0:s0 + T]
                        if d == 1:
                            rhs = rhs[:, ::-1]
                        pdv, b_pdv = self.PS.get()
                        for ri in range(2):
                            kb.op("tensor", lambda e, ri=ri, pdv=pdv, rhs=rhs, ppq=ppq: e.matmul(
                                pdv[:, ri * T:(ri + 1) * T], self.Bdrv[ppq * 32:(ppq + 1) * 32, d, qd, ri, :], rhs,
                                start=True, stop=True, tile_position=(ppq * 32, 0)),
                                reads=[self.b_Bdrv, self.b_uT], writes=[b_pdv], sig=(ri == 1))
                        Dv = pdv[:, 0:2 * T].rearrange("p (r t) -> p r t", r=2)
                        cosb = Tc3[:, pd:pd + 1, :].to_broadcast([128, 2, T])
                        st.append(dict(pd=pd, ppq=ppq, Dv=Dv, b_pdv=b_pdv, cosb=cosb, P1=P1p.get(), P2=P2p.get(), V=Vp.get()))
                    for x in st:
                        kb.op("vector", lambda e, x=x: e.tensor_tensor(out=x["P1"][0][:], in0=x["Dv"], in1=x["cosb"], op=ALU.mult),
                              reads=[x["b_pdv"]] + tabs, writes=[x["P1"][1]])
                    for x in st:
                        kb.op("vector", lambda e, x=x: e.tensor_tensor(out=x["P2"][0][:], in0=x["Dv"][:, ::-1, :], in1=self.Tsn[:, x["pd"]], op=ALU.mult),
                              reads=[x["b_pdv"]] + tabs, writes=[x["P2"][1]])
                    for x in st:
                        kb.op("vector", lambda e, x=x: e.tensor_tensor(out=x["V"][0][:], in0=x["P1"][0][:], in1=x["P2"][0][:], op=ALU.add),
                              reads=[x["P1"][1], x["P2"][1]], writes=[x["V"][1]])
                    for ri in range(2):
                        for x in st:
                            pd = x["pd"]
                            kb.op("vector", lambda e, ri=ri, x=x, pd=pd: e.tensor_tensor_scan(
                                out=G[:, pd, ri, :], data0=self.rho[:, pd:pd + 1].to_broadcast([128, T]), data1=x["V"][0][:, ri, :],
                                initial=carry[:, pd, ri:ri + 1], op0=ALU.mult, op1=ALU.add),
                                reads=[x["V"][1], b_carry, self.b_s5c], writes=[b_G[pd]])
                    if ck < NCK - 1:
                        pd0 = d * 16 + qd * 4
                        Gl = G[:, pd0:pd0 + 4, :, T - 1]
                        cl = Tc3[:, pd0:pd0 + 4, T - 1:T].to_broadcast([128, 4, 2])
                        sl = self.Tsn[:, pd0:pd0 + 4, :, T - 1]
                        gb_ = [b_G[pd0 + q] for q in range(4)]
                        c1 = ctmp[:, pd0:pd0 + 4, :]
                        c2 = ctmp2[:, pd0:pd0 + 4, :]
                        kb.op("vector", lambda e, Gl=Gl, cl=cl, c1=c1: e.tensor_tensor(out=c1, in0=Gl, in1=cl, op=ALU.mult),
                              reads=gb_ + tabs, writes=[b_carry])
                        kb.op("vector", lambda e, Gl=Gl, sl=sl, c2=c2: e.tensor_tensor(out=c2, in0=Gl[:, :, ::-1], in1=sl, op=ALU.mult),
                              reads=gb_ + tabs, writes=[b_carry])
                        kb.op("vector", lambda e, c1=c1, c2=c2, pd0=pd0: e.tensor_tensor(out=carry[:, pd0:pd0 + 4, :], in0=c1, in1=c2, op=ALU.subtract),
                              reads=[b_carry], writes=[b_carry])
                    if is_lat:
                        for x in st:
                            x["Q1"] = Q1p.get(); x["Q2"] = Q2p.get(); x["H"] = Hp.get()
                        for x in st:
                            kb.op(POST_ENG, lambda e, x=x: e.tensor_tensor(out=x["Q1"][0][:], in0=G[:, x["pd"]], in1=x["cosb"], op=ALU.mult),
                                  reads=[b_G[x["pd"]]] + tabs, writes=[x["Q1"][1]])
                        for x in st:
                            kb.op(POST_ENG, lambda e, x=x: e.tensor_tensor(out=x["Q2"][0][:], in0=G[:, x["pd"], ::-1, :], in1=self.Tsn[:, x["pd"]], op=ALU.mult),
                                  reads=[b_G[x["pd"]]] + tabs, writes=[x["Q2"][1]])
                        for x in st:
                            kb.op(POST_ENG, lambda e, x=x: e.tensor_tensor(out=x["H"][0][:], in0=x["Q1"][0][:], in1=x["Q2"][0][:], op=ALU.subtract),
                                  reads=[x["Q1"][1], x["Q2"][1]], writes=[x["H"][1]])
                        for x in st:
                            for ri in range(2):
                                kb.op("tensor", lambda e, ri=ri, x=x: e.matmul(
                                    py[x["ppq"] * 32:(x["ppq"] + 1) * 32, 0:T], self.Crd[:, x["pd"], ri, :], x["H"][0][:, ri, :],
                                    start=(ri == 0), stop=(ri == 1), tile_position=(0, x["ppq"] * 32)),
                                    reads=[self.b_Crd, x["H"][1]], writes=[b_py], sig=(ri == 1))
                        l0 = s0 - CTX
                        src = py[:, 0:T] if d == 0 else py[:, 0:T][:, ::-1]
                        kb.op("vector", lambda e, src=src, qd=qd, l0=l0: e.tensor_tensor(
                            out=self.yT[:, qd, l0:l0 + T], in0=src, in1=self.yT[:, qd, l0:l0 + T], op=ALU.add),
                            reads=[b_py], writes=[self.b_yT[qd]])
        if "yT" in self.dbg:
            allb = kb.buf("yTall")
            t = kb.sb("dbg_yT_t", [128, 4, 512], F32)
            kb.op("vector", lambda e: e.tensor_copy(out=t[:], in_=self.yT[:, :, 0:512]), reads=self.b_yT, writes=[allb])
            self.dump("yT", t[:], allb, [128, 4, 512])
            t2 = kb.sb("dbg_yT_t2", [128, 4, 512], F32)
            allb2 = kb.buf("yTall2")
            kb.op("vector", lambda e: e.tensor_copy(out=t2[:], in_=self.yT[:, :, 1536:2048]), reads=self.b_yT, writes=[allb2])
            self.dbg.add("yT2")
            self.dump("yT2", t2[:], allb2, [128, 4, 512])


    def phase_gdn_setup(self):
        kb = self.kb
        self.gm, self.b_gm = self._ld("gmask", self.gmask, [128, 11, 128])
        self.cw, self.b_cw = self._ld("convw", self.conv_t, [128, 12, 5])
        self.beta = kb.sb("g_beta", [128, NT, 8], F32)
        self.gg = kb.sb("g_g", [128, NT, 8], F32)
        self.E1 = kb.sb("g_E1", [128, NT, 8], F32)
        self.E2 = kb.sb("g_E2", [128, NT, 8], F32)
        self.DL = kb.sb("g_DL", [128, 2, NT, 8], F32)
        self.b_gs = kb.buf("gscal")
        b = self.b_gs
        with kb.scope():
            wab = kb.sb("wab", [128, 8, 16], BF16)
            b_wab = kb.buf("wab")
            self.load_w_bf16(wab, b_wab, self.w_in, 16, OFF_B)
            ab = kb.sb("alogdtb", [128, 16], F32)
            b_ab = kb.buf("alogdtb")
            kb.dma("sync", ab[:], self.alog_dtb.partition_broadcast(128), writes=[b_ab])
            pz, b_pz = self.PS.get()
            for tt in range(NT):
                for kc in range(8):
                    kb.op("tensor", lambda e, tt=tt, kc=kc: e.matmul(
                        pz[:, tt * 16:(tt + 1) * 16], self.hT[:, kc, tt * 128:(tt + 1) * 128], wab[:, kc, :],
                        start=(kc == 0), stop=(kc == 7)), reads=[b_wab, self.b_hT[tt]], writes=[b_pz], sig=(kc == 7))
            zab = pz[:, 0:NT * 16].rearrange("p (t c) -> p t c", c=16)
            kb.op("scalar", lambda e: e.activation(out=self.beta[:], in_=zab[:, :, 0:8], func=AF.Sigmoid), reads=[b_pz], writes=[b])
            if self.stop == "gs1":
                return
            t1 = kb.sb("g_t1", [128, NT, 8], F32)
            ea = kb.sb("g_ea", [128, 8], F32)
            kb.op("vector", lambda e: e.tensor_tensor(out=t1[:], in0=zab[:, :, 8:16], in1=ab[:, 8:16].unsqueeze(1).to_broadcast([128, NT, 8]),
                                                     op=ALU.add), reads=[b_pz, b_ab], writes=[b])
            kb.op("scalar", lambda e: e.activation(out=t1[:], in_=t1[:], func=AF.Exp), reads=[b], writes=[b])
            kb.op("scalar", lambda e: e.activation(out=t1[:], in_=t1[:], func=AF.Ln, bias=1.0), reads=[b], writes=[b])
            kb.op("scalar", lambda e: e.activation(out=ea[:], in_=ab[:, 0:8], func=AF.Exp), reads=[b_ab], writes=[b])
            kb.op("vector", lambda e: e.scalar_tensor_tensor(out=self.gg[:], in0=t1[:], scalar=-1.0,
                                                            in1=ea[:].unsqueeze(1).to_broadcast([128, NT, 8]), op0=ALU.mult, op1=ALU.mult),
                  reads=[b], writes=[b])
            if self.stop == "gs2":
                return
            gflat = self.gg[:].rearrange("p t x -> p (t x)")
            gc = kb.sb("g_gcum", [128, 2, NT, 8], F32)
            for d in range(2):
                pc, b_pc = self.PS.get()
                kb.op("tensor", lambda e, d=d, pc=pc: e.matmul(pc[:, 0:NT * 8], self.gm[:, d, :], gflat, start=True, stop=True),
                      reads=[b, self.b_gm], writes=[b_pc])
                kb.op("vector", lambda e, d=d, pc=pc: e.tensor_copy(out=gc[:, d].rearrange("p t x -> p (t x)"), in_=pc[:, 0:NT * 8]),
                      reads=[b_pc], writes=[b])
            if self.stop == "gs3":
                return
            pl, b_pl = self.PS.get()
            kb.op("tensor", lambda e: e.matmul(pl[:, 0:NT * 8], self.gm[:, 7, :], gflat, start=True, stop=True),
                  reads=[b, self.b_gm], writes=[b_pl])
            glv = pl[:, 0:NT * 8].rearrange("p (t x) -> p t x", x=8)
            for d in range(2):
                xs = slice(d * 4, d * 4 + 4)
                kb.op("scalar", lambda e, d=d, xs=xs: e.activation(out=self.E1[:, :, xs], in_=gc[:, d, :, xs], func=AF.Exp),
                      reads=[b], writes=[b])
                kb.op("vector", lambda e, d=d, xs=xs: e.tensor_tensor(out=t1[:, :, xs], in0=glv[:, :, xs], in1=gc[:, d, :, xs], op=ALU.subtract),
                      reads=[b, b_pl], writes=[b])
                kb.op("scalar", lambda e, xs=xs: e.activation(out=self.E2[:, :, xs], in_=t1[:, :, xs], func=AF.Exp), reads=[b], writes=[b])
            if self.stop == "gs4":
                return
            for c2 in ([1] if self.stop == "gs7" else range(2)):
                ph, b_ph = self.PS.get()
                kb.op("tensor", lambda e, c2=c2, ph=ph: e.matmul(ph[:, 0:NT * 8], self.gm[:, 5 + c2, :], gflat, start=True, stop=True),
                      reads=[b, self.b_gm], writes=[b_ph])
                if self.stop == "gs5":
                    return
                kb.op("scalar", lambda e, c2=c2, ph=ph: e.activation(out=self.DL[:, c2].rearrange("p t x -> p (t x)"), in_=ph[:, 0:NT * 8],
                                                                      func=AF.Exp), reads=[b_ph], writes=[b])
                if self.stop == "gs6":
                    return
        self.dump("g_g", self.gg[:], b, [128, NT, 8])
        self.dump("g_E1", self.E1[:], b, [128, NT, 8])
        self.dump("g_E2", self.E2[:], b, [128, NT, 8])
        self.dump("g_DL", self.DL[:], b, [128, 2, NT, 8])

    def gdn_head_front(self, h):
        kb = self.kb
        self.qh = kb.sb("g_qh%d" % h, [128, NT, 128], F32)
        self.kh = kb.sb("g_kh%d" % h, [128, NT, 128], F32)
        self.vh = kb.sb("g_vh%d" % h, [128, NT, 128], F32)
        self.kT = kb.sb("g_kT%d" % h, [128, TOK], F32)
        self.qT = kb.sb("g_qT%d" % h, [128, TOK], F32)
        self.b_qh, self.b_kh, self.b_vh, self.b_kT, self.b_qT = [kb.buf("g_%s%d" % (n, h)) for n in ("qh", "kh", "vh", "kT", "qT")]
        with kb.scope():
            wq = kb.sb("g_wqkv", [128, 8, 3, 128], BF16)
            b_wq = kb.buf("g_wqkv")
            v = self.w_in.rearrange("(kc p) n -> p kc n", p=128)
            for c in range(3):
                for kc in range(8):
                    col = OFF_QKV + c * 512 + h * 128
                    kb.dma("gpsimd", wq[:, kc, c, :], v[:, kc, col:col + 128], writes=[b_wq])
            zb = kb.sb("g_z", [128, TOK], F32)
            acc = kb.sb("g_acc", [128, TOK], F32)
            b_z = kb.buf("g_z")
            b_acc = kb.buf("g_acc")
            dsts = [(self.qh, self.b_qh), (self.kh, self.b_kh), (self.vh, self.b_vh)]
            for c in range(3):
                ch = c * 4 + h
                for (s0, n) in BLOCKS:
                    pt, b_pt = self.PS.get()
                    tiles = range(s0 // 128, (s0 + n) // 128)
                    for kc in range(8):
                        kb.op("tensor", lambda e, kc=kc, pt=pt, c=c, s0=s0, n=n: e.matmul(
                            pt[:, 0:n], wq[:, kc, c, :], self.hT[:, kc, s0:s0 + n], start=(kc == 0), stop=(kc == 7)),
                            reads=[b_wq] + [self.b_hT[t] for t in tiles], writes=[b_pt], sig=(kc == 7))
                    kb.op("scalar", lambda e, pt=pt, s0=s0, n=n: e.activation(out=zb[:, s0:s0 + n], in_=pt[:, 0:n], func=AF.Identity),
                          reads=[b_pt], writes=[b_z])
                kb.op("vector", lambda e, ch=ch: e.tensor_scalar(out=acc[:], in0=zb[:], scalar1=self.cw[:, ch, 2:3], scalar2=0.0,
                                                                 op0=ALU.mult, op1=ALU.add), reads=[b_z, self.b_cw], writes=[b_acc])
                segs = [(zb[:, 0:CTX].rearrange("p (r w) -> p r w", w=CTX), acc[:, 0:CTX].rearrange("p (r w) -> p r w", w=CTX), CTX),
                        (zb[:, CTX:TOK].rearrange("p (r w) -> p r w", w=64), acc[:, CTX:TOK].rearrange("p (r w) -> p r w", w=64), 64)]
                for k in (0, 1, 3, 4):
                    sh = k - 2
                    for (zv, av, w) in segs:
                        lo, hi = max(0, -sh), w - max(0, sh)
                        kb.op("vector", lambda e, zv=zv, av=av, lo=lo, hi=hi, sh=sh, k=k, ch=ch: e.scalar_tensor_tensor(
                            out=av[:, :, lo:hi], in0=zv[:, :, lo + sh:hi + sh], scalar=self.cw[:, ch, k:k + 1], in1=av[:, :, lo:hi],
                            op0=ALU.mult, op1=ALU.add), reads=[b_z, b_acc, self.b_cw], writes=[b_acc])
                kb.op("scalar", lambda e: e.activation(out=zb[:], in_=acc[:], func=AF.Silu), reads=[b_acc], writes=[b_z])
                if c == 0 and h == 0:
                    self.dump("g_cq", zb[:, 0:512], b_z, [128, 512])
                dst, b_dst = dsts[c]
                for t0 in range(0, NT, 4):
                    nt = min(4, NT - t0)
                    pt, b_pt = self.PS.get()
                    for q in range(nt):
                        kb.op("tensor", lambda e, q=q, t0=t0, pt=pt: e.transpose(
                            pt[:, q * 128:(q + 1) * 128], zb[:, (t0 + q) * 128:(t0 + q + 1) * 128], self.identf[:]),
                            reads=[b_z, self.b_ident], writes=[b_pt])
                    kb.op("scalar" if (t0 // 4) % 2 == 0 else "vector", lambda e, t0=t0, nt=nt, pt=pt, dst=dst: (
                        e.activation(out=dst[:, t0:t0 + nt, :], in_=pt[:, 0:nt * 128].rearrange("p (t c) -> p t c", c=128), func=AF.Identity)
                        if e is self.nc.scalar else
                        e.tensor_copy(out=dst[:, t0:t0 + nt, :], in_=pt[:, 0:nt * 128].rearrange("p (t c) -> p t c", c=128))),
                        reads=[b_pt], writes=[b_dst])
            sq = acc[:, 0:NT * 128].rearrange("p (t c) -> p t c", c=128)
            ss = kb.sb("g_ss", [128, 2, NT], F32)
            b_ss = kb.buf("g_ss")
            for qi, (src, b_src) in enumerate([(self.qh, self.b_qh), (self.kh, self.b_kh)]):
                kb.op("vector", lambda e, src=src: e.tensor_tensor(out=sq, in0=src[:], in1=src[:], op=ALU.mult), reads=[b_src], writes=[b_acc])
                kb.op("vector", lambda e, qi=qi: e.tensor_reduce(out=ss[:, qi, :], in_=sq, axis=AX.X, op=ALU.add), reads=[b_acc], writes=[b_ss])
            kb.op("scalar", lambda e: e.activation(out=ss[:], in_=ss[:], func=AF.Sqrt, bias=EPS, scale=1.0), reads=[b_ss], writes=[b_ss])
            kb.op("vector", lambda e: e.reciprocal(out=ss[:], in_=ss[:]), reads=[b_ss], writes=[b_ss])
            kb.op("vector", lambda e: e.tensor_scalar(out=ss[:, 0, :], in0=ss[:, 0, :], scalar1=float(128 ** -0.5), scalar2=0.0,
                                                     op0=ALU.mult, op1=ALU.add), reads=[b_ss], writes=[b_ss])
            for qi, (src, b_src) in enumerate([(self.qh, self.b_qh), (self.kh, self.b_kh)]):
                kb.op("vector", lambda e, src=src, qi=qi: e.tensor_tensor(
                    out=src[:], in0=src[:], in1=ss[:, qi, :].unsqueeze(2).to_broadcast([128, NT, 128]), op=ALU.mult),
                    reads=[b_src, b_ss], writes=[b_src])
            for (src, b_src, dstT, b_dT) in [(self.kh, self.b_kh, self.kT, self.b_kT), (self.qh, self.b_qh, self.qT, self.b_qT)]:
                for t0 in range(0, NT, 4):
                    nt = min(4, NT - t0)
                    pt, b_pt = self.PS.get()
                    for q in range(nt):
                        kb.op("tensor", lambda e, q=q, t0=t0, pt=pt, src=src: e.transpose(
                            pt[:, q * 128:(q + 1) * 128], src[:, t0 + q, :], self.identf[:]), reads=[b_src, self.b_ident], writes=[b_pt])
                    kb.op("scalar", lambda e, t0=t0, nt=nt, pt=pt, dstT=dstT: e.activation(
                        out=dstT[:, t0 * 128:(t0 + nt) * 128].bitcast(mybir.dt.float32r), in_=pt[:, 0:nt * 128], func=AF.Identity), reads=[b_pt], writes=[b_dT])
        if h == 0:
            self.dump("g_qh", self.qh[:, 0:4, :], self.b_qh, [128, 4, 128])
            self.dump("g_kh", self.kh[:, 0:4, :], self.b_kh, [128, 4, 128])
            self.dump("g_vh", self.vh[:, 0:4, :], self.b_vh, [128, 4, 128])


    def gdn_head_core(self, h):
        kb = self.kb
        gm = self.gm
        T_ = lambda name, n=1: Pool(kb, "gc_%s_%d" % (name, h), [128, 128], F32, n)
        R = 4
        rings = {n: [T_("%s%d" % (n, d), R) for d in range(2)] for n in ("wT", "ub", "qkT", "qdT", "kd")}
        tmp = {n: [T_("%s%d" % (n, d), 1) for d in range(2)] for n in
               ("kbt", "kw", "vb", "qd", "kbT", "Dls", "DTs", "DTi", "A", "N", "X0", "X1", "P0", "P1", "Q0", "Q1")}
        vnp = [T_("vn%d" % d, 2) for d in range(2)]
        S = [kb.sb("g_S%d_%d" % (h, d), [128, 128], F32) for d in range(2)]
        b_S = [kb.buf("g_S%d" % d) for d in range(2)]
        for d in range(2):
            kb.op("vector", lambda e, d=d: e.memset(S[d][:], 0.0), writes=[b_S[d]])
        order = {0: list(range(NT)), 1: [1, 0] + list(range(NT - 1, 1, -1))}
        ringent = {}
        gs = self.b_gs
        LS, US, UI, LI = 2, 3, 4, 10

        kb.barrier()
        pst = [t for (t, _) in self.PS.tiles]

        class Reg:
            def __init__(r, ap, b_):
                r.ap = ap
                r.b = b_
        regs = {}
        pb = [b_ for (_, b_) in self.PS.tiles]
        for d in range(2):
            regs[("pD", d)] = Reg(pst[4 * d][:, 0:256], pb[4 * d])
            regs[("pt", d)] = Reg(pst[4 * d + 1][:, 0:256], pb[4 * d + 1])
            regs[("pK", d)] = Reg(pst[4 * d + 2][:, 0:384], pb[4 * d + 2])
            regs[("pw", d)] = Reg(pst[4 * d + 3][:, 0:128], pb[4 * d + 3])
            regs[("po", d)] = Reg(pst[4 * d + 3][:, 128:256], pb[4 * d + 3])
            regs[("ps", d)] = Reg(pst[4 * d + 3][:, 256:384], pb[4 * d + 3])
        R32 = (lambda ap: ap.bitcast(mybir.dt.float32r)) if os.environ.get('KF32R', '1') == '1' else (lambda ap: ap)
        prog = {"prep": [0, 0], "chain": [0, 0]}

        def prep_gen(d):
            for i in range(NT):
                while i - prog["chain"][d] >= R - 1:
                    yield
                tt = order[d][i]
                x = d * 4 + h
                ts_ = slice(tt * 128, (tt + 1) * 128)
                bsc = self.beta[:, tt, x:x + 1]
                e1 = self.E1[:, tt, x:x + 1]
                e2 = self.E2[:, tt, x:x + 1]
                g = lambda n: tmp[n][d].get()
                kbt, b_kbt = g("kbt"); kw, b_kw = g("kw"); vb, b_vb = g("vb"); qd, b_qd = g("qd"); kbT, b_kbT = g("kbT")
                kd, b_kd = rings["kd"][d].get(); qdT, b_qdT = rings["qdT"][d].get()
                ts1 = lambda e, o, i_, sc: e.tensor_scalar(out=R32(o[:]), in0=i_, scalar1=sc, scalar2=0.0, op0=ALU.mult, op1=ALU.add)
                kb.op("vector", lambda e: ts1(e, kbt, self.kh[:, tt, :], bsc), reads=[self.b_kh, gs], writes=[b_kbt])
                kb.op("vector", lambda e: ts1(e, qd, self.qh[:, tt, :], e1), reads=[self.b_qh, gs], writes=[b_qd])
                gb = self.gg[:, tt, x:x + 1].to_broadcast([128, 128])
                M, nM = gm[:, d, :], gm[:, 8 + d, :]
                pD, b_pD = regs[("pD", d)].ap, regs[("pD", d)].b
                kb.op("tensor", lambda e: e.matmul(pD[:, 0:128], M, gb, start=True, stop=False), reads=[gs, self.b_gm], writes=[b_pD], sig=False)
                kb.op("tensor", lambda e: e.matmul(pD[:, 0:128], gb, nM, start=False, stop=True), reads=[gs, self.b_gm], writes=[b_pD], sig=False)
                kb.op("tensor", lambda e: e.matmul(pD[:, 128:256], nM, gb, start=True, stop=False), reads=[gs, self.b_gm], writes=[b_pD], sig=False)
                kb.op("tensor", lambda e: e.matmul(pD[:, 128:256], gb, M, start=False, stop=True), reads=[gs, self.b_gm], writes=[b_pD])
                yield
                kb.op("vector", lambda e: ts1(e, kw, kbt[:], e1), reads=[b_kbt, gs], writes=[b_kw])
                kb.op("vector", lambda e: ts1(e, vb, self.vh[:, tt, :], bsc), reads=[self.b_vh, gs], writes=[b_vb])
                kb.op("vector", lambda e: ts1(e, kd, self.kh[:, tt, :], e2), reads=[self.b_kh, gs], writes=[b_kd])
                pt, b_pt = regs[("pt", d)].ap, regs[("pt", d)].b
                kb.op("tensor", lambda e: e.transpose(pt[:, 0:128], kbt[:], self.identf[:]), reads=[b_kbt, self.b_ident], writes=[b_pt], sig=False)
                kb.op("tensor", lambda e: e.transpose(pt[:, 128:256], qd[:], self.identf[:]), reads=[b_qd, self.b_ident], writes=[b_pt])
                yield
                kb.op("scalar", lambda e: e.activation(out=R32(kbT[:]), in_=pt[:, 0:128], func=AF.Identity), reads=[b_pt], writes=[b_kbT])
                kb.op("scalar", lambda e: e.activation(out=qdT[:], in_=pt[:, 128:256], func=AF.Identity), reads=[b_pt], writes=[b_qdT])
                Dls, b_Dls = g("Dls"); DTs, b_DTs = g("DTs"); DTi, b_DTi = g("DTi")
                mD, mDTs, mDTi = (LS, US, UI) if d == 0 else (US, LS, LI)
                trip = [(Dls, b_Dls, pD[:, 0:128], mD), (DTs, b_DTs, pD[:, 128:256], mDTs), (DTi, b_DTi, pD[:, 128:256], mDTi)]
                for (dst, b_dst, src, mk) in trip:
                    kb.op("vector", lambda e, dst=dst, src=src, mk=mk: e.scalar_tensor_tensor(
                        out=dst[:], in0=src, scalar=0.0, in1=gm[:, mk, :], op0=ALU.min, op1=ALU.add), reads=[b_pD, self.b_gm], writes=[b_dst])
                yield
                for (dst, b_dst, src, mk) in trip:
                    kb.op("scalar", lambda e, dst=dst: e.activation(out=dst[:], in_=dst[:], func=AF.Exp), reads=[b_dst], writes=[b_dst])
                pK, b_pK = regs[("pK", d)].ap, regs[("pK", d)].b
                kTt, qTt = self.kT[:, ts_], self.qT[:, ts_]
                kb.op("tensor", lambda e: e.matmul(pK[:, 0:128], R32(kbT[:]), R32(kTt), start=True, stop=True), reads=[b_kbT, self.b_kT], writes=[b_pK], sig=False)
                kb.op("tensor", lambda e: e.matmul(pK[:, 128:256], R32(kTt), R32(kbT[:]), start=True, stop=True), reads=[b_kbT, self.b_kT], writes=[b_pK], sig=False)
                kb.op("tensor", lambda e: e.matmul(pK[:, 256:384], R32(kTt), R32(qTt), start=True, stop=True), reads=[self.b_qT, self.b_kT], writes=[b_pK])
                yield
                A, b_A = g("A"); N, b_N = g("N"); qkT, b_qkT = rings["qkT"][d].get()
                kb.op("vector", lambda e: e.tensor_tensor(out=R32(A[:]), in0=pK[:, 0:128], in1=Dls[:], op=ALU.mult), reads=[b_pK, b_Dls], writes=[b_A])
                kb.op("vector", lambda e: e.tensor_tensor(out=R32(N[:]), in0=pK[:, 128:256], in1=DTs[:], op=ALU.mult), reads=[b_pK, b_DTs], writes=[b_N])
                kb.op("vector", lambda e: e.tensor_tensor(out=qkT[:], in0=pK[:, 256:384], in1=DTi[:], op=ALU.mult), reads=[b_pK, b_DTi], writes=[b_qkT])
                X, b_X = g("X0")
                yield
                kb.op("vector", lambda e: e.tensor_tensor(out=R32(X[:]), in0=self.identf[:], in1=N[:], op=ALU.subtract), reads=[b_N, self.b_ident], writes=[b_X])
                P, b_P, PT, b_PT = N, b_N, A, b_A
                for sidx in range(1, 6):
                    pp, b_pp = regs[("pK", d)].ap, regs[("pK", d)].b
                    if sidx < 5:
                        kb.op("tensor", lambda e: e.matmul(pp[:, 0:128], R32(PT[:]), R32(P[:]), start=True, stop=True), reads=[b_P, b_PT], writes=[b_pp], sig=False)
                    kb.op("tensor", lambda e: e.matmul(pp[:, 128:256], R32(P[:]), R32(PT[:]), start=True, stop=True), reads=[b_P, b_PT], writes=[b_pp])
                    yield
                    nP, b_nP = tmp["P%d" % (sidx % 2)][d].get()
                    nPT, b_nPT = tmp["Q%d" % (sidx % 2)][d].get()
                    kb.op("scalar", lambda e: e.activation(out=R32(nPT[:]), in_=pp[:, 128:256], func=AF.Identity), reads=[b_pp], writes=[b_nPT])
                    if sidx < 5:
                        kb.op("scalar", lambda e: e.activation(out=R32(nP[:]), in_=pp[:, 0:128], func=AF.Identity), reads=[b_pp], writes=[b_nP])
                    yield
                    kb.op("tensor", lambda e: e.matmul(pp[:, 256:384], R32(nPT[:]), R32(X[:]), start=True, stop=True), reads=[b_nPT, b_X], writes=[b_pp])
                    yield
                    nX, b_nX = tmp["X%d" % (sidx % 2)][d].get()
                    kb.op("vector", lambda e: e.tensor_tensor(out=R32(nX[:]), in0=pp[:, 256:384], in1=X[:], op=ALU.add), reads=[b_pp, b_X], writes=[b_nX])
                    P, b_P, PT, b_PT, X, b_X = nP, b_nP, nPT, b_nPT, nX, b_nX
                    yield
                pu, b_pu = regs[("pK", d)].ap, regs[("pK", d)].b
                kb.op("tensor", lambda e: e.matmul(pu[:, 0:128], R32(X[:]), R32(vb[:]), start=True, stop=True), reads=[b_X, b_vb], writes=[b_pu], sig=False)
                kb.op("tensor", lambda e: e.matmul(pu[:, 128:256], R32(kw[:]), R32(X[:]), start=True, stop=True), reads=[b_X, b_kw], writes=[b_pu])
                yield
                ub, b_ub = rings["ub"][d].get(); wT, b_wT = rings["wT"][d].get()
                kb.op("scalar", lambda e: e.activation(out=ub[:], in_=pu[:, 0:128], func=AF.Identity), reads=[b_pu], writes=[b_ub])
                kb.op("scalar", lambda e: e.activation(out=wT[:], in_=pu[:, 128:256], func=AF.Identity), reads=[b_pu], writes=[b_wT])
                ringent[(d, i)] = dict(d=d, tt=tt, x=x, kd=(kd, b_kd), qdT=(qdT, b_qdT), qkT=(qkT, b_qkT), ub=(ub, b_ub), wT=(wT, b_wT))
                prog["prep"][d] = i + 1
                yield

        def chain_gen(d):
            for i in range(NT):
                while (d, i) not in ringent:
                    yield
                en = ringent[(d, i)]
                tt, x = en["tt"], en["x"]
                wT, b_wT = en["wT"]; ub, b_ub = en["ub"]; qdT, b_qdT = en["qdT"]; qkT, b_qkT = en["qkT"]; kd, b_kd = en["kd"]
                for hi in range(2):
                    c2 = hi if d == 0 else 1 - hi
                    r = slice(c2 * 64, c2 * 64 + 64)
                    pw, b_pw = regs[("pw", d)].ap, regs[("pw", d)].b
                    kb.op("tensor", lambda e: e.matmul(pw[r, :], wT[:, r], S[d][:], start=True, stop=True), reads=[b_wT, b_S[d]], writes=[b_pw])
                    if tt >= 2:
                        po, b_po = regs[("po", d)].ap, regs[("po", d)].b
                        kb.op("tensor", lambda e: e.matmul(po[r, :], qdT[:, r], S[d][:], start=True, stop=False),
                              reads=[b_qdT, b_S[d]], writes=[b_po], sig=False)
                    yield
                    vn, b_vn = vnp[d].get()
                    kb.op("vector", lambda e: e.tensor_tensor(out=vn[r, :], in0=ub[r, :], in1=pw[r, :], op=ALU.subtract),
                          reads=[b_ub, b_pw], writes=[b_vn])
                    yield
                    ps_, b_ps = regs[("ps", d)].ap, regs[("ps", d)].b
                    if tt >= 2:
                        kb.op("tensor", lambda e: e.matmul(po[r, :], qkT[r, r], vn[r, :], start=False, stop=True),
                              reads=[b_qkT, b_vn], writes=[b_po])
                    kb.op("tensor", lambda e: e.matmul(ps_[:, :], kd[r, :], vn[r, :], start=True, stop=True), reads=[b_kd, b_vn], writes=[b_ps])
                    yield
                    kb.op("vector", lambda e: e.scalar_tensor_tensor(out=S[d][:], in0=S[d][:], scalar=self.DL[:, c2, tt, x:x + 1], in1=ps_[:, :],
                                                                    op0=ALU.mult, op1=ALU.add), reads=[b_ps, b_S[d], gs], writes=[b_S[d]])
                    if tt >= 2:
                        od = self.osum[r, tt - 2, h * 128:(h + 1) * 128]
                        kb.op("gpsimd" if False else "vector", lambda e: e.tensor_tensor(out=od, in0=po[r, :], in1=od, op=ALU.add),
                              reads=[b_po], writes=[self.b_osum[tt - 2]])
                    yield
                prog["chain"][d] = i + 1

        gens = [prep_gen(0), prep_gen(1), chain_gen(0), chain_gen(1)]
        while gens:
            for g_ in list(gens):
                try:
                    next(g_)
                except StopIteration:
                    gens.remove(g_)

    def phase_gdn(self):
        kb = self.kb
        self.osum = kb.sb("g_osum", [128, 16, 512], F32)
        self.b_osum = [kb.buf("g_osum%d" % t) for t in range(16)]
        for t in range(16):
            kb.op("vector", lambda e, t=t: e.memset(self.osum[:, t, :], 0.0), writes=[self.b_osum[t]])
        for h in range(4):
            with kb.scope():
                self.gdn_head_front(h)
                self.gdn_head_core(h)
            if self.stop == "gdn_h0":
                break
        if "g_osum" in self.dbg:
            self.dump("g_osum", self.osum[:, 0:4, 0:128], self.b_osum[3], [128, 4, 128])
        if "g_osum2" in self.dbg:
            self.dump("g_osum2", self.osum[:, 12:16, 0:128], self.b_osum[15], [128, 4, 128])


    def phase_glu(self):
        kb = self.kb
        wg = kb.sb("wglu", [128, 4, 512], BF16)
        b_wg = kb.buf("wglu")
        self.load_w_bf16(wg, b_wg, self.w_glu, 512, 0)
        bg, b_bg = self._ld("bglu", self.b_glu_t, [128, 4])
        zT = kb.sb("glu_z", [128, 4, L], BF16)
        b_zT = [kb.buf("glu_z%d" % q) for q in range(4)]
        t1 = kb.sb("glu_t1", [128, L], F32)
        t2 = kb.sb("glu_t2", [128, L], F32)
        b_t = kb.buf("glu_t")
        for q in range(4):
            y = self.yT[:, q, :]
            kb.op("vector", lambda e, y=y: e.tensor_tensor(out=t1[:], in0=y, in1=y, op=ALU.mult), reads=[self.b_yT[q]], writes=[b_t])
            kb.op("vector", lambda e: e.tensor_scalar(out=t1[:], in0=t1[:], scalar1=0.044715, scalar2=1.0, op0=ALU.mult, op1=ALU.add), reads=[b_t], writes=[b_t])
            kb.op("vector", lambda e, y=y: e.tensor_tensor(out=t1[:], in0=t1[:], in1=y, op=ALU.mult), reads=[b_t, self.b_yT[q]], writes=[b_t])
            kb.op("scalar", lambda e: e.activation(out=t2[:], in_=t1[:], func=AF.Sigmoid, scale=1.5957691216057308), reads=[b_t], writes=[b_t])
            kb.op("vector", lambda e, y=y, q=q: e.tensor_tensor(out=zT[:, q, :], in0=t2[:], in1=y, op=ALU.mult), reads=[b_t, self.b_yT[q]], writes=[b_zT[q]])
        glp = Pool(kb, "glu_g", [128, 512], F32, 2)
        for oc in range(4):
            for n in range(4):
                pt, b_pt = self.PS.get()
                for kc in range(4):
                    kb.op("tensor", lambda e, kc=kc, pt=pt, oc=oc, n=n: e.matmul(
                        pt[:], wg[:, kc, oc * 128:(oc + 1) * 128], zT[:, kc, n * 512:(n + 1) * 512], start=(kc == 0), stop=(kc == 3)),
                        reads=[b_wg] + b_zT, writes=[b_pt], sig=(kc == 3))
                gl, b_gl = glp.get()
                kb.op("scalar", lambda e, pt=pt, gl=gl, oc=oc: e.activation(out=gl[:], in_=pt[:], func=AF.Sigmoid, bias=bg[:, oc:oc + 1]),
                      reads=[b_pt, b_bg], writes=[b_gl])
                kb.op("vector", lambda e, gl=gl, oc=oc, n=n: e.tensor_tensor(
                    out=self.yaT[:, oc, n * 512:(n + 1) * 512], in0=zT[:, oc, n * 512:(n + 1) * 512], in1=gl[:], op=ALU.mult),
                    reads=[b_gl, b_zT[oc]], writes=[self.b_yaT])
        if "yaT" in self.dbg:
            t = kb.sb("dbg_yaT_t", [128, 4, 512], F32)
            bt = kb.buf("dbg_yaT")
            kb.op("vector", lambda e: e.tensor_copy(out=t[:], in_=self.yaT[:, :, 0:512]), reads=[self.b_yaT], writes=[bt])
            self.dump("yaT", t[:], bt, [128, 4, 512])

    def phase_gdn_out(self):
        kb = self.kb
        wgt = kb.sb("wgate", [128, 8, 512], BF16)
        b_wgt = kb.buf("wgate")
        self.load_w_bf16(wgt, b_wgt, self.w_in, 512, OFF_GATE)
        nw = kb.sb("gnw", [128, 128], F32)
        b_nw = kb.buf("gnw")
        kb.dma("sync", nw[:], self.gdn_norm.partition_broadcast(128), writes=[b_nw])
        ss = kb.sb("go_ss", [128, 16, 4], F32)
        b_ss = kb.buf("go_ss")
        sqp = Pool(kb, "go_sq", [128, 512], F32, 2)
        for t in range(16):
            sq, b_sq = sqp.get()
            kb.op("vector", lambda e, t=t, sq=sq: e.tensor_tensor(out=sq[:], in0=self.osum[:, t, :], in1=self.osum[:, t, :], op=ALU.mult),
                  reads=[self.b_osum[t]], writes=[b_sq])
            kb.op("vector", lambda e, t=t, sq=sq: e.tensor_reduce(out=ss[:, t, :], in_=sq[:].rearrange("p (h c) -> p h c", c=128), axis=AX.X, op=ALU.add),
                  reads=[b_sq], writes=[b_ss])
        ssf = ss[:].rearrange("p t h -> p (t h)")
        kb.op("scalar", lambda e: e.activation(out=ssf, in_=ssf, func=AF.Sqrt, bias=EPS, scale=1.0 / 128), reads=[b_ss], writes=[b_ss])
        kb.op("vector", lambda e: e.reciprocal(out=ssf, in_=ssf), reads=[b_ss], writes=[b_ss])
        sgp = Pool(kb, "go_sg", [128, 512], F32, 2)
        onp = Pool(kb, "go_on", [128, 512], F32, 2)
        for t in range(16):
            pg, b_pg = self.PS.get()
            for kc in range(8):
                kb.op("tensor", lambda e, kc=kc, t=t, pg=pg: e.matmul(pg[:], self.hT[:, kc, (t + 2) * 128:(t + 3) * 128], wgt[:, kc, :],
                                                                     start=(kc == 0), stop=(kc == 7)), reads=[b_wgt, self.b_hT[t + 2]], writes=[b_pg], sig=(kc == 7))
            sg, b_sg = sgp.get()
            kb.op("scalar", lambda e, pg=pg, sg=sg: e.activation(out=sg[:], in_=pg[:], func=AF.Silu), reads=[b_pg], writes=[b_sg])
            on, b_on = onp.get()
            on3 = on[:].rearrange("p (h c) -> p h c", c=128)
            kb.op("vector", lambda e, t=t, on3=on3: e.tensor_tensor(out=on3, in0=self.osum[:, t, :].rearrange("p (h c) -> p h c", c=128),
                                                                   in1=ss[:, t, :].unsqueeze(2).to_broadcast([128, 4, 128]), op=ALU.mult),
                  reads=[self.b_osum[t], b_ss], writes=[b_on])
            kb.op("vector", lambda e, on3=on3: e.tensor_tensor(out=on3, in0=on3, in1=nw[:].unsqueeze(1).to_broadcast([128, 4, 128]), op=ALU.mult),
                  reads=[b_on, b_nw], writes=[b_on])
            kb.op("vector", lambda e, on=on, sg=sg: e.tensor_tensor(out=on[:], in0=on[:], in1=sg[:], op=ALU.mult), reads=[b_on, b_sg], writes=[b_on])
            pt, b_pt = self.PS.get()
            for c in range(4):
                kb.op("tensor", lambda e, c=c, pt=pt, on=on: e.transpose(pt[:, c * 128:(c + 1) * 128], on[:, c * 128:(c + 1) * 128], self.identf[:]),
                      reads=[b_on, self.b_ident], writes=[b_pt])
            kb.op("scalar", lambda e, pt=pt, t=t: e.activation(out=self.ybT[:, :, t * 128:(t + 1) * 128], in_=pt[:].rearrange("p (c k) -> p c k", k=128),
                                                              func=AF.Identity), reads=[b_pt], writes=[self.b_ybT])
        if "ybT" in self.dbg:
            t_ = kb.sb("dbg_ybT_t", [128, 4, 512], F32)
            bt = kb.buf("dbg_ybT")
            kb.op("vector", lambda e: e.tensor_copy(out=t_[:], in_=self.ybT[:, :, 0:512]), reads=[self.b_ybT], writes=[bt])
            self.dump("ybT", t_[:], bt, [128, 4, 512])

    def phase_merge(self):
        kb = self.kb
        mT = kb.sb("mergedT", [128, 8, L], BF16)
        b_mT = [kb.buf("mergedT%d" % n) for n in range(4)]
        with kb.scope():
            wba = kb.sb("wba", [128, 4, D], BF16); b_wba = kb.buf("wba")
            wbb = kb.sb("wbb", [128, 4, D], BF16); b_wbb = kb.buf("wbb")
            self.load_w_bf16(wba, b_wba, self.w_ba, D, 0)
            self.load_w_bf16(wbb, b_wbb, self.w_bb, D, 0)
            wbrp = Pool(kb, "wbr", [128, 8, 2, 128], BF16, 2)
            gp = Pool(kb, "mg_g", [128, 2, 512], F32, 2)
            m1p = Pool(kb, "mg_m", [128, 2, 512], F32, 2)
            v = self.w_in.rearrange("(kc p) n -> p kc n", p=128)
            for oc in range(8):
                wbr, b_wbr = wbrp.get()
                for ab in range(2):
                    for kc in range(8):
                        col = OFF_BR + ab * D + oc * 128
                        kb.dma("gpsimd", wbr[:, kc, ab, :], v[:, kc, col:col + 128], writes=[b_wbr])
                for n in range(4):
                    tiles = [self.b_hT[2 + 4 * n + j] for j in range(4)]
                    tok = slice(n * 512, (n + 1) * 512)
                    stok = slice(CTX + n * 512, CTX + (n + 1) * 512)
                    g, b_g = gp.get()
                    m1, b_m1 = m1p.get()
                    for ab, (wb, b_wb, yT_, b_y) in enumerate([(wba, b_wba, self.yaT, self.b_yaT), (wbb, b_wbb, self.ybT, self.b_ybT)]):
                        pbr, b_pbr = self.PS.get()
                        for kc in range(8):
                            kb.op("tensor", lambda e, kc=kc, pbr=pbr, ab=ab, wbr=wbr: e.matmul(
                                pbr[:], wbr[:, kc, ab, :], self.hT[:, kc, stok], start=(kc == 0), stop=(kc == 7)),
                                reads=[b_wbr] + tiles, writes=[b_pbr], sig=(kc == 7))
                        kb.op("scalar", lambda e, pbr=pbr, g=g, ab=ab: e.activation(out=g[:, ab, :], in_=pbr[:], func=AF.Sigmoid),
                              reads=[b_pbr], writes=[b_g])
                        pp, b_pp = self.PS.get()
                        for kc in range(4):
                            kb.op("tensor", lambda e, kc=kc, pp=pp, wb=wb, yT_=yT_: e.matmul(
                                pp[:], wb[:, kc, oc * 128:(oc + 1) * 128], yT_[:, kc, tok], start=(kc == 0), stop=(kc == 3)),
                                reads=[b_wb, b_y], writes=[b_pp], sig=(kc == 3))
                        kb.op("vector", lambda e, pp=pp, g=g, m1=m1, ab=ab: e.tensor_tensor(out=m1[:, ab, :], in0=pp[:], in1=g[:, ab, :], op=ALU.mult),
                              reads=[b_pp, b_g], writes=[b_m1])
                    kb.op("vector", lambda e, m1=m1, oc=oc: e.tensor_tensor(out=mT[:, oc, tok], in0=m1[:, 0, :], in1=m1[:, 1, :], op=ALU.add),
                          reads=[b_m1], writes=[b_mT[n]])
        if "mergedT" in self.dbg:
            t_ = kb.sb("dbg_mT_t", [128, 8, 256], F32)
            bt = kb.buf("dbg_mT")
            kb.op("vector", lambda e: e.tensor_copy(out=t_[:], in_=mT[:, :, 0:256]), reads=[b_mT[0]], writes=[bt])
            self.dump("mergedT", t_[:], bt, [128, 8, 256])
        with kb.scope():
            wo = kb.sb("wout", [128, 8, D], BF16); b_wo = kb.buf("wout")
            self.load_w_bf16(wo, b_wo, self.w_out, D, 0)
            xp = Pool(kb, "mg_x", [128, D], F32, 3)
            tp = Pool(kb, "mg_t", [128, D], F32, 2)
            self.b_x1s = [kb.buf("x1s%d" % t) for t in range(16)]
            for t in range(16):
                xt, b_xt = xp.get()
                kb.dma("sync" if t % 2 == 0 else "scalar", xt[:], self.x[t * 128:(t + 1) * 128, :], writes=[b_xt])
                tm_, b_tm = tp.get()
                for cb in range(2):
                    pm, b_pm = self.PS.get()
                    for kc in range(8):
                        kb.op("tensor", lambda e, kc=kc, pm=pm, cb=cb, t=t: e.matmul(
                            pm[:], mT[:, kc, t * 128:(t + 1) * 128], wo[:, kc, cb * 512:(cb + 1) * 512], start=(kc == 0), stop=(kc == 7)),
                            reads=[b_wo, b_mT[t // 4]], writes=[b_pm], sig=(kc == 7))
                    cs_ = slice(cb * 512, (cb + 1) * 512)
                    kb.op("vector", lambda e, pm=pm, tm_=tm_, cs_=cs_: e.tensor_tensor(out=tm_[:, cs_], in0=pm[:], in1=self.gt1_bc[:, cs_], op=ALU.mult),
                          reads=[b_pm, self.b_gt1], writes=[b_tm])
                kb.op("vector", lambda e, tm_=tm_, xt=xt: e.tensor_tensor(out=xt[:], in0=tm_[:], in1=xt[:], op=ALU.add), reads=[b_tm, b_xt], writes=[b_xt])
                kb.dma("sync", self.x1s[t * 128:(t + 1) * 128, :], xt[:], reads=[b_xt], writes=[self.b_x1s[t]])
                if t == 0:
                    self.dump("x1", xt[:], b_xt, [128, D])

    def phase_moe_half(self, hp):
        kb = self.kb
        NTL = 8
        X = kb.sb("X%d" % hp, [128, NTL, D], F32)
        b_X = [kb.buf("X%d_%d" % (hp, t)) for t in range(NTL)]
        h2T = kb.sb("h2T%d" % hp, [128, 8, NTL * 128], BF16)
        b_h2 = [kb.buf("h2T%d_%d" % (hp, t)) for t in range(NTL)]
        comb = kb.sb("comb%d" % hp, [128, NTL, NE], F32)
        b_comb = kb.buf("comb%d" % hp)
        combs = kb.sb("combs%d" % hp, [128, NTL, NE], F32)
        combT = kb.sb("combT%d" % hp, [NE, NTL * 128], F32)
        b_combT = kb.buf("combT%d" % hp)
        for t in range(NTL):
            gt = hp * NTL + t
            kb.dma("sync" if t % 2 == 0 else "scalar", X[:, t, :], self.x1s[gt * 128:(gt + 1) * 128, :], reads=[self.b_x1s[gt]], writes=[b_X[t]])
        with kb.scope():
            n2, b_n2 = self._ld("norm2", self.norm2_t, [128, 8])
            A2 = kb.sb("A2", [128, 8, 2], F32); b_A2 = kb.buf("A2")
            kb.op("vector", lambda e: e.scalar_tensor_tensor(out=A2[:], in0=self.modT[:, 24:32, :], scalar=1.0,
                                                            in1=n2[:].unsqueeze(2).to_broadcast([128, 8, 2]), op0=ALU.add, op1=ALU.mult),
                  reads=[self.b_modT, b_n2], writes=[b_A2])
            wr = kb.sb("wrouter", [128, 8, NE], F32); b_wr = kb.buf("wrouter")
            kb.dma("sync", wr[:], self.w_router.rearrange("(kc p) n -> p kc n", p=128), writes=[b_wr])
            br_, b_br = kb.sb("brouter", [128, NE], F32), kb.buf("brouter")
            kb.dma("sync", br_[:], self.b_router.partition_broadcast(128), writes=[b_br])
            xnp = Pool(kb, "m_xn", [128, D], F32, 2)
            junk = Pool(kb, "m_junk", [128, D], F32, 1)
            stat = Pool(kb, "m_stat", [128, 4], F32, 4)
            hfp = Pool(kb, "m_hf", [128, 8, 128], F32, 2)
            lgp = Pool(kb, "m_lg", [128, NE], F32, 2)
            m8p = Pool(kb, "m_m8", [128, 16], F32, 2)
            for t in range(NTL):
                hf, b_hf = hfp.get()
                self._norm_tile2(X[:, t, :], b_X[t], t, A2, b_A2, 16, xnp, junk, stat, h2T, b_h2[t], hf, b_hf)
                pl, b_pl = self.PS.get()
                for kc in range(8):
                    kb.op("tensor", lambda e, kc=kc, pl=pl, hf=hf: e.matmul(pl[:, 0:NE], hf[:, kc, :], wr[:, kc, :], start=(kc == 0), stop=(kc == 7)),
                          reads=[b_hf, b_wr], writes=[b_pl], sig=(kc == 7))
                lg, b_lg = lgp.get()
                m8, b_m8 = m8p.get()
                kb.op("vector", lambda e, pl=pl, lg=lg: e.tensor_tensor(out=lg[:], in0=pl[:, 0:NE], in1=br_[:], op=ALU.add), reads=[b_pl, b_br], writes=[b_lg])
                if t == 0 and hp == 0:
                    self.dump("logits", lg[:], b_lg, [128, NE])
                kb.op("vector", lambda e, lg=lg, m8=m8: e.max(out=m8[:, 0:8], in_=lg[:]), reads=[b_lg], writes=[b_m8])
                kb.op("vector", lambda e, m8=m8: e.tensor_scalar(out=m8[:, 8:9], in0=m8[:, 0:1], scalar1=-1.0, scalar2=0.0, op0=ALU.mult, op1=ALU.add),
                      reads=[b_m8], writes=[b_m8])
                ex, b_ex = lgp.get()
                kb.op("scalar", lambda e, lg=lg, ex=ex, m8=m8: e.activation(out=ex[:], in_=lg[:], func=AF.Exp, bias=m8[:, 8:9]), reads=[b_lg, b_m8], writes=[b_ex])
                kb.op("vector", lambda e, lg=lg, ex=ex, m8=m8: e.scalar_tensor_tensor(out=ex[:], in0=lg[:], scalar=m8[:, 3:4], in1=ex[:], op0=ALU.is_ge, op1=ALU.mult),
                      reads=[b_lg, b_ex, b_m8], writes=[b_ex])
                kb.op("vector", lambda e, ex=ex, m8=m8: e.tensor_reduce(out=m8[:, 9:10], in_=ex[:], axis=AX.X, op=ALU.add), reads=[b_ex], writes=[b_m8])
                kb.op("vector", lambda e, m8=m8: e.reciprocal(out=m8[:, 10:11], in_=m8[:, 9:10]), reads=[b_m8], writes=[b_m8])
                kb.op("vector", lambda e, ex=ex, m8=m8, t=t: e.tensor_scalar(out=comb[:, t, :], in0=ex[:], scalar1=m8[:, 10:11], scalar2=0.0, op0=ALU.mult, op1=ALU.add),
                      reads=[b_ex, b_m8], writes=[b_comb])
                kb.op("vector", lambda e, t=t: e.tensor_scalar(out=combs[:, t, :], in0=comb[:, t, :], scalar1=float(1.0 / 1.702), scalar2=0.0, op0=ALU.mult, op1=ALU.add),
                      reads=[b_comb], writes=[b_comb])
                pT, b_pT = self.PS.get()
                kb.op("tensor", lambda e, pT=pT, t=t: e.transpose(pT[0:NE, 0:128], comb[:, t, :], self.identf[:]), reads=[b_comb, self.b_ident], writes=[b_pT])
                kb.op("scalar", lambda e, pT=pT, t=t: e.activation(out=combT[:, t * 128:(t + 1) * 128], in_=pT[0:NE, 0:128], func=AF.Identity),
                      reads=[b_pT], writes=[b_combT])
            if hp == 0:
                self.dump("comb", comb[:, 0, :], b_comb, [128, NE])
                if "h2T" in self.dbg:
                    t_ = kb.sb("dbg_h2T_t", [128, 8, 256], F32)
                    bt = kb.buf("dbg_h2T")
                    kb.op("vector", lambda e: e.tensor_copy(out=t_[:], in_=h2T[:, :, 0:256]), reads=b_h2[0:2], writes=[bt])
                    self.dump("h2T", t_[:], bt, [128, 8, 256])
        if self.stop == "router":
            return
        with kb.scope():
            bdn, b_bdn = self._ld("bdn", self.b_dn, [NE, D])
            bgu, b_bgu = self._ld("bgu", self.b_gu_t, [128, NE, 16])
            tp = Pool(kb, "moe_t", [128, 512], F32, 3)
            for t in range(NTL):
                for cb in range(2):
                    cs_ = slice(cb * 512, (cb + 1) * 512)
                    pb, b_pb = self.PS.get()
                    kb.op("tensor", lambda e, pb=pb, t=t, cs_=cs_: e.matmul(pb[:], combT[:, t * 128:(t + 1) * 128], bdn[:, cs_], start=True, stop=True),
                          reads=[b_combT, b_bdn], writes=[b_pb])
                    tt_, b_tt = tp.get()
                    kb.op("vector", lambda e, pb=pb, tt_=tt_, cs_=cs_: e.tensor_tensor(out=tt_[:], in0=pb[:], in1=self.gt2_bc[:, cs_], op=ALU.mult),
                          reads=[b_pb, self.b_gt2], writes=[b_tt])
                    kb.op("vector", lambda e, tt_=tt_, t=t, cs_=cs_: e.tensor_tensor(out=X[:, t, cs_], in0=tt_[:], in1=X[:, t, cs_], op=ALU.add),
                          reads=[b_tt], writes=[b_X[t]])
            wgup = Pool(kb, "wgu", [128, 8, 2 * D], BF16, 2)
            wdnp = Pool(kb, "wdn", [128, 8, D], BF16, 2)
            actp = Pool(kb, "actT", [128, 8, 512], BF16, 2)
            gp = Pool(kb, "moe_g", [128, 512], F32, 3)
            sp = Pool(kb, "moe_s", [128, 512], BF16, 3)
            up = Pool(kb, "moe_u", [128, 512], BF16, 3)
            bgu1 = kb.sb("bgu1", [128, NE, 8], F32)
            kb.op("vector", lambda e: e.tensor_scalar(out=bgu1[:], in0=bgu[:, :, 8:16], scalar1=1.0, scalar2=0.0, op0=ALU.add, op1=ALU.add),
                  reads=[b_bgu], writes=[b_bgu])
            NB = NTL * 128 // 512
            nexp = NE if self.stop != "moe1" else 1
            W = {}

            def load_w(ex_):
                wgu, b_wgu = wgup.get()
                wdn, b_wdn = wdnp.get()
                vg = self.w_gu[ex_].rearrange("(kc p) n -> p kc n", p=128)
                for kc in range(8):
                    kb.dma("gpsimd", wgu[:, kc, :], vg[:, kc, :], writes=[b_wgu])
                vd = self.w_dn[ex_].rearrange("(kc p) n -> p kc n", p=128)
                for kc in range(8):
                    kb.dma("gpsimd", wdn[:, kc, :], vd[:, kc, :], writes=[b_wdn])
                W[ex_] = (wgu, b_wgu, wdn, b_wdn)

            def fold(ex_):
                wgu, b_wgu, wdn, b_wdn = W[ex_]
                for q in range(2):
                    kb.op("vector", lambda e, q=q: e.tensor_tensor(out=wdn[:, q::2, :], in0=wdn[:, q::2, :],
                                                                  in1=self.gt2_bc[:].unsqueeze(1).to_broadcast([128, 4, D]), op=ALU.mult),
                          reads=[b_wdn, self.b_gt2], writes=[b_wdn])

            ACT = {}

            def gu(ex_, n):
                wgu, b_wgu, wdn, b_wdn = W[ex_]
                actT, b_act = actp.get()
                ACT[(ex_, n)] = (actT, b_act)
                toks = slice(n * 512, (n + 1) * 512)
                tl = [b_h2[4 * n + j] for j in range(4)]
                for j in range(8):
                    pg, b_pg = self.PS.get()
                    pu, b_pu = self.PS.get()
                    for kc in range(8):
                        kb.op("tensor", lambda e, kc=kc: e.matmul(pg[:], wgu[:, kc, j * 128:(j + 1) * 128], h2T[:, kc, toks],
                                                                  start=(kc == 0), stop=(kc == 7)), reads=[b_wgu] + tl, writes=[b_pg], sig=(kc == 7))
                    for kc in range(8):
                        kb.op("tensor", lambda e, kc=kc: e.matmul(pu[:], wgu[:, kc, D + j * 128:D + (j + 1) * 128], h2T[:, kc, toks],
                                                                  start=(kc == 0), stop=(kc == 7)), reads=[b_wgu] + tl, writes=[b_pu], sig=(kc == 7))
                    g, b_g = gp.get(); sg, b_sg = sp.get(); u, b_u = up.get()
                    kb.op("vector", lambda e: e.tensor_scalar(out=g[:], in0=pg[:], scalar1=bgu[:, ex_, j:j + 1], scalar2=7.0, op0=ALU.add, op1=ALU.min),
                          reads=[b_pg, b_bgu], writes=[b_g])
                    kb.op("scalar", lambda e: e.activation(out=sg[:], in_=g[:], func=AF.Silu, scale=1.702), reads=[b_g], writes=[b_sg])
                    kb.op("vector", lambda e: e.tensor_scalar(out=u[:], in0=pu[:], scalar1=bgu1[:, ex_, j:j + 1], scalar2=8.0, op0=ALU.add, op1=ALU.min),
                          reads=[b_pu, b_bgu], writes=[b_u])
                    kb.op("vector", lambda e: e.scalar_tensor_tensor(out=actT[:, j, :], in0=u[:], scalar=-6.0, in1=sg[:], op0=ALU.max, op1=ALU.mult),
                          reads=[b_u, b_sg], writes=[b_act])
                    if n == 0 and j == 3:
                        fold(ex_)

            def dn(ex_, n):
                wgu, b_wgu, wdn, b_wdn = W[ex_]
                actT, b_act = ACT.pop((ex_, n))
                for tq in range(4):
                    t = n * 4 + tq
                    for cb in range(2):
                        cs_ = slice(cb * 512, (cb + 1) * 512)
                        pd_, b_pd = self.PS.get()
                        for kc in range(8):
                            kb.op("tensor", lambda e, kc=kc: e.matmul(pd_[:], actT[:, kc, tq * 128:(tq + 1) * 128], wdn[:, kc, cs_],
                                                                      start=(kc == 0), stop=(kc == 7)), reads=[b_act, b_wdn], writes=[b_pd], sig=(kc == 7))
                        kb.op("vector", lambda e: e.scalar_tensor_tensor(out=X[:, t, cs_], in0=pd_[:], scalar=combs[:, t, ex_:ex_ + 1], in1=X[:, t, cs_],
                                                                        op0=ALU.mult, op1=ALU.add), reads=[b_pd, b_comb], writes=[b_X[t]])

            items = [(e_, n) for e_ in range(nexp) for n in range(NB)]
            load_w(0)
            if nexp > 1:
                load_w(1)
            gu(*items[0])
            for k, (e_, n) in enumerate(items):
                if k + 1 < len(items):
                    gu(*items[k + 1])
                dn(e_, n)
                if n == NB - 1 and e_ + 2 < nexp:
                    load_w(e_ + 2)
        if hp == 0:
            self.dump("x2", X[:, 0, :], b_X[0], [128, D])
        with kb.scope():
            nf = kb.sb("normf", [128, D], F32); b_nf = kb.buf("normf")
            kb.dma("sync", nf[:], self.norm_f.partition_broadcast(128), writes=[b_nf])
            junk = Pool(kb, "f_junk", [128, D], F32, 1)
            stat = Pool(kb, "f_stat", [128, 4], F32, 4)
            op_ = Pool(kb, "f_o", [128, D], F32, 2)
            for t in range(NTL):
                gt = hp * NTL + t
                jt, b_jt = junk.get(); st, b_st = stat.get()
                kb.op("scalar", lambda e, jt=jt, st=st, t=t: e.activation(out=jt[:], in_=X[:, t, :], func=AF.Square, accum_out=st[:, 0:1]), reads=[b_X[t]], writes=[b_jt, b_st])
                kb.op("scalar", lambda e, st=st: e.activation(out=st[:, 1:2], in_=st[:, 0:1], func=AF.Sqrt, bias=EPS, scale=1.0 / D), reads=[b_st], writes=[b_st])
                kb.op("vector", lambda e, st=st: e.reciprocal(out=st[:, 2:3], in_=st[:, 1:2]), reads=[b_st], writes=[b_st])
                ot, b_ot = op_.get()
                kb.op("vector", lambda e, ot=ot, st=st, t=t: e.scalar_tensor_tensor(out=ot[:], in0=X[:, t, :], scalar=st[:, 2:3], in1=nf[:], op0=ALU.mult, op1=ALU.mult),
                      reads=[b_X[t], b_st, b_nf], writes=[b_ot])
                bo = kb.buf("out%d" % gt)
                kb.dma("sync", self.out[gt * 128:(gt + 1) * 128, :], ot[:], reads=[b_ot], writes=[bo])
                self.fin.append(bo)

    def _norm_tile2(self, xt_ap, b_xt, tt, A, b_A, shift_chunk0, xnp, junk, stat, dstT, b_dst, hf, b_hf):
        kb = self.kb
        jt, b_jt = junk.get()
        st, b_st = stat.get()
        kb.op("scalar", lambda e: e.activation(out=jt[:], in_=xt_ap, func=AF.Square, accum_out=st[:, 0:1]), reads=[b_xt], writes=[b_jt, b_st])
        kb.op("scalar", lambda e: e.activation(out=st[:, 1:2], in_=st[:, 0:1], func=AF.Sqrt, bias=EPS, scale=1.0 / D), reads=[b_st], writes=[b_st])
        kb.op("vector", lambda e: e.reciprocal(out=st[:, 2:3], in_=st[:, 1:2]), reads=[b_st], writes=[b_st])
        xn, b_xn = xnp.get()
        kb.op("scalar", lambda e: e.activation(out=xn[:], in_=xt_ap, func=AF.Identity, scale=st[:, 2:3]), reads=[b_xt, b_st], writes=[b_xn])
        for half in range(2):
            pt, b_pt = self.PS.get()
            for q in range(4):
                kc = half * 4 + q
                kb.op("tensor", lambda e, kc=kc, q=q, pt=pt: e.transpose(pt[:, q * 128:(q + 1) * 128], xn[:, kc * 128:(kc + 1) * 128], self.identf[:]),
                      reads=[b_xn, self.b_ident], writes=[b_pt])
            for q in range(4):
                kc = half * 4 + q
                kb.op("vector", lambda e, kc=kc, q=q, pt=pt: e.tensor_scalar(
                    out=hf[:, kc, :], in0=pt[:, q * 128:(q + 1) * 128], scalar1=A[:, kc, 0:1], scalar2=self.modT[:, shift_chunk0 + kc, 0:1],
                    op0=ALU.mult, op1=ALU.add), reads=[b_pt, b_A, self.b_modT], writes=[b_hf])
        kb.op("scalar", lambda e: e.activation(out=dstT[:, :, tt * 128:(tt + 1) * 128], in_=hf[:], func=AF.Identity), reads=[b_hf], writes=[b_dst])

    def build(self):
        kb = self.kb
        self.declare()
        self.common()
        with kb.scope():
            self.hT = kb.sb("hT", [128, 8, TOK], BF16)
            self.b_hT = [kb.buf("hT%d" % t) for t in range(NT)]
            self.yaT = kb.sb("yaT", [128, 4, L], BF16)
            self.b_yaT = kb.buf("yaT")
            with kb.scope():
                self.phase_s5_setup()
                if self.stop == "s5setup":
                    return self.finish()
                with kb.scope():
                    self.phase_mod()
                if self.stop == "mod":
                    return self.finish()
                with kb.scope():
                    self.phase_norm1()
                if self.stop == "norm1":
                    return self.finish()
                self.uT = kb.sb("uT", [128, 4, TOK], BF16)
                self.b_uT = kb.buf("uT")
                self.yT = kb.sb("yT", [128, 4, L], F32)
                self.b_yT = [kb.buf("yT%d" % q) for q in range(4)]
                with kb.scope():
                    self.phase_u()
                if self.stop == "u":
                    return self.finish()
                with kb.scope():
                    self.phase_s5()
                if self.stop == "s5":
                    return self.finish()
                with kb.scope():
                    self.phase_glu()
                if self.stop == "glu":
                    return self.finish()
            self.ybT = kb.sb("ybT", [128, 4, L], BF16)
            self.b_ybT = kb.buf("ybT")
            with kb.scope():
                self.phase_gdn_setup()
                if self.stop in ("gdnsetup",):
                    return self.finish()
                self.phase_gdn()
                if self.stop in ("gdn", "gdn_h0"):
                    return self.finish()
                with kb.scope():
                    self.phase_gdn_out()
                if self.stop == "gdnout":
                    return self.finish()
            with kb.scope():
                self.phase_merge()
            if self.stop == "merge":
                return self.finish()
        for hp in range(2):
            with kb.scope():
                self.phase_moe_half(hp)
            if self.stop in ("router", "moe1", "half"):
                return self.finish()
        return self.finish()

    def finish(self):
        self.kb.finish(self.fin)
        self.kb.finished = True
        for es in reversed(getattr(self.kb, "scopes", [])):
            es.close()
        self.kb.root.close()
        return self.nc


def _fm(v, nch):
    return np.ascontiguousarray(np.asarray(v, np.float32).reshape(nch, 128).T)


def host_inputs(inputs, b):
    f = lambda a: np.ascontiguousarray(np.asarray(a, np.float32))
    m = {}
    m["x"] = f(inputs["x"][b])
    m["ctx"] = f(inputs["ctx"][b])
    cs = np.stack([_fm(inputs["c"][b], 8), _fm(inputs["c_ctx"], 8)], axis=-1)
    m["cs"] = f(cs)
    m["w_mod"] = f(inputs["w_mod"][0])
    bm = np.asarray(inputs["b_mod"][0], np.float32)
    bmt = _fm(bm, 48)
    order = list(range(0, 16)) + list(range(24, 40)) + list(range(16, 24)) + list(range(40, 48))
    m["b_mod_t"] = f(bmt[:, order])
    m["b_mod"] = f(bm)
    m["norm1_t"] = _fm(inputs["norm1"][0], 8)
    m["norm2_t"] = _fm(inputs["norm2"][0], 8)
    m["w_in"] = f(inputs["w_in"][0])
    m["ident"] = np.eye(128, dtype=np.float32)
    m["s5_d_t"] = _fm(inputs["s5_d"][0], 4)
    def pdl(a):
        a = np.asarray(a, np.float32).reshape(2, 16, 2, 64)
        return f(a.transpose(2, 3, 0, 1).reshape(128, 32))
    m["lam_re_t"] = pdl(inputs["s5_lam_re"][0])
    m["lam_im_t"] = pdl(inputs["s5_lam_im"][0])
    m["logstep_t"] = pdl(np.broadcast_to(np.asarray(inputs["s5_log_step"][0], np.float32)[:, :, None], (2, 32, 64)))
    def blk(re, im, cn):
        out = np.zeros((2, 64, 2, 2, 16, 2, 16), np.float32)
        for ri, arr in enumerate((re, im)):
            arr = np.asarray(arr, np.float32)
            arr = arr if cn else arr.transpose(0, 1, 3, 2)
            arr = arr.reshape(2, 16, 2, 64, 16)
            for g2 in range(2):
                out[g2, :, ri, :, :, g2, :] = arr[:, :, g2].transpose(2, 0, 1, 3)
        return f(out.reshape(128, 2, 32, 32))
    m["Bblk"] = blk(inputs["s5_b_re"][0], inputs["s5_b_im"][0], True)
    m["Cblk"] = blk(inputs["s5_c_re"][0], inputs["s5_c_im"][0], False)
    r_ = np.arange(128)[:, None]; c_ = np.arange(128)[None, :]
    same = (r_ // 64) == (c_ // 64)
    NEG = -30000.0
    Mf = (same & (r_ <= c_)).astype(np.float32); Mb = (same & (r_ >= c_)).astype(np.float32)
    gmk = [Mf, Mb, np.where(same & (r_ > c_), 0.0, NEG), np.where(same & (r_ < c_), 0.0, NEG), np.where(same & (r_ <= c_), 0.0, NEG),
           np.tile((r_ < 64), (1, 128)).astype(np.float32), np.tile((r_ >= 64), (1, 128)).astype(np.float32), same.astype(np.float32),
           -Mf, -Mb, np.where(same & (r_ >= c_), 0.0, NEG)]
    m["gmask"] = f(np.stack([np.asarray(a, np.float32) for a in gmk], axis=1))
    cw = np.asarray(inputs["gdn_conv"][0], np.float32)
    m["conv_t"] = f(cw.reshape(5, 12, 128).transpose(2, 1, 0))
    m["alog_dtb"] = f(np.concatenate([np.asarray(inputs["gdn_a_log"][0]).reshape(8), np.asarray(inputs["gdn_dt_bias"][0]).reshape(8)]))
    m["gdn_norm"] = f(inputs["gdn_norm"][0])
    m["w_glu"] = f(inputs["s5_w_glu"][0])
    m["b_glu_t"] = _fm(inputs["s5_b_glu"][0], 4)
    m["w_ba"] = f(inputs["w_branch_a"][0])
    m["w_bb"] = f(inputs["w_branch_b"][0])
    m["w_out"] = f(inputs["w_out"][0])
    m["w_router"] = f(inputs["w_router"][0])
    m["b_router"] = f(inputs["b_router"][0])
    m["w_gu"] = f(inputs["w_gate_up"][0])
    bgu = np.asarray(inputs["b_gate_up"][0], np.float32)
    m["b_gu_t"] = f(bgu.reshape(NE, 16, 128).transpose(2, 0, 1))
    m["w_dn"] = f(inputs["w_down"][0])
    m["b_dn"] = f(inputs["b_down"][0])
    m["norm_f"] = f(inputs["norm_f"])
    m["iota1"] = f(np.tile(np.arange(1, TS5 + 1, dtype=np.float32)[None], (128, 1)))
    return m


_CACHE = {}


def kernel(**inputs):
    nb = 8
    if "nc" not in _CACHE:
        _CACHE["nc"] = Builder().build()
    nc = _CACHE["nc"]
    shared = host_inputs(inputs, 0)
    in_maps = []
    for b in range(nb):
        m = dict(shared)
        if b > 0:
            f = lambda a: np.ascontiguousarray(np.asarray(a, np.float32))
            m["x"] = f(inputs["x"][b])
            m["ctx"] = f(inputs["ctx"][b])
            m["cs"] = f(np.stack([_fm(inputs["c"][b], 8), _fm(inputs["c_ctx"], 8)], axis=-1))
        in_maps.append(m)
    res = run_bass_kernel_spmd(nc, in_maps, core_ids=list(range(nb)))
    out = np.stack([np.asarray(res.results[b]["out"], np.float32) for b in range(nb)], axis=0)
    return out
```

```python
import os
import numpy as np
from contextlib import ExitStack
import concourse.bass as bass
import concourse.mybir as mybir
from concourse.bass_utils import run_bass_kernel_spmd

F32 = mybir.dt.float32
BF16 = mybir.dt.bfloat16
I32 = mybir.dt.int32
AF = mybir.ActivationFunctionType
ALU = mybir.AluOpType
AX = mybir.AxisListType

D = 1024
L = 2048
CTX = 256
TOK = L + CTX
NT = TOK // 128
EPS = 1e-6
NE = 32
TS5 = 128
POST_ENG = os.environ.get('KPOST', 'gpsimd')
IN_COLS = 4624
OFF_U, OFF_QKV, OFF_GATE, OFF_B, OFF_A, OFF_BR = 0, 512, 2048, 2560, 2568, 2576
BLOCKS = [(0, 256)] + [(256 + 512 * i, 512) for i in range(4)]


class Buf:
    __slots__ = ("name", "w", "r")

    def __init__(self, name):
        self.name = name
        self.w = None
        self.r = {}


class KB:
    def __init__(self, nc, same_engine_sync=True):
        self.nc = nc
        self.es = ExitStack()
        self.root = self.es
        self.same = same_engine_sync
        self.engs = {}
        self.sems = {}
        for name in ("tensor", "vector", "scalar", "gpsimd", "sync"):
            eng = getattr(nc, name)
            sem = self.root.enter_context(nc.semaphore("s_" + name))
            self.sems["E" + name] = sem
            self.engs[name] = dict(eng=eng, key="E" + name, count=0, waited={})
        self.nbuf = 0
        self.dmasems = {}
        self.nalloc = 0

    def sb(self, name, shape, dt=F32):
        nb = int(np.prod(shape[1:])) * (2 if dt == BF16 else 4)
        self.nalloc += (nb + 31) // 32 * 32
        if os.environ.get("KALLOC"):
            print("alloc", name, shape, nb, "total", self.nalloc)
        self.nnames = getattr(self, "nnames", 0) + 1
        return self.es.enter_context(self.nc.sbuf_tensor("sb%d_%s" % (self.nnames, name), list(shape), dt))

    def ps(self, name, shape, dt=F32):
        return self.es.enter_context(self.nc.psum_tensor("pp_" + name, list(shape), dt))

    def buf(self, name=None):
        self.nbuf += 1
        return Buf((name or "b") + "_%d" % self.nbuf)

    def _wait(self, en, key, val):
        e = self.engs[en]
        if key == e["key"] and ((not self.same) or en == "tensor"):
            return
        if e["waited"].get(key, 0) >= val:
            return
        if key in self.dmasems:
            val = self.dmasems[key]
        e["eng"].wait_ge(self.sems[key], val)
        e["waited"][key] = val

    def _deps(self, en, reads, writes):
        need = {}
        for b in reads:
            if b.w is not None:
                k, v = b.w
                need[k] = max(need.get(k, 0), v)
        own = self.engs[en]["key"]
        relax = os.environ.get("KRELAX", "1") == "1"
        for b in writes:
            if b.w is not None:
                k, v = b.w
                if not (relax and k == own):
                    need[k] = max(need.get(k, 0), v)
            for k, v in b.r.items():
                if not (relax and k == own):
                    need[k] = max(need.get(k, 0), v)
        for k, v in need.items():
            self._wait(en, k, v)

    def _post(self, sig, reads, writes):
        k, v = sig
        for b in reads:
            b.r[k] = max(b.r.get(k, 0), v)
        for b in writes:
            b.w = sig
            b.r = {}

    def op(self, en, fn, reads=(), writes=(), sig=True):
        e = self.engs[en]
        self._deps(en, reads, writes)
        inst = fn(e["eng"])
        if sig:
            e["count"] += 1
            inst.then_inc(self.sems[e["key"]], 1)
            e["pending"] = False
            self._post((e["key"], e["count"]), reads, writes)
        else:
            assert en == "tensor"
            e["pending"] = True
            self._post((e["key"], e["count"] + 1), reads, writes)
        return inst

    NDSEM = 64

    def dma(self, en, out, in_, reads=(), writes=(), **kw):
        e = self.engs[en]
        self._deps(en, reads, writes)
        tgt = writes[0] if writes else reads[0]
        if not hasattr(self, "bufsem"):
            self.bufsem = {}
            self.dsem_list = []
        semkey = self.bufsem.get(tgt.name)
        if semkey is None:
            idx = len(self.bufsem) % self.NDSEM
            semkey = "DS%d" % idx
            self.bufsem[tgt.name] = semkey
            if semkey not in self.sems:
                self.sems[semkey] = self.root.enter_context(self.nc.semaphore("d%d" % idx))
                self.dmasems[semkey] = 0
        self.dmasems[semkey] += 16
        inst = e["eng"].dma_start(out=out, in_=in_, **kw)
        inst.then_inc(self.sems[semkey], 16)
        self._post((semkey, self.dmasems[semkey]), reads, writes)
        return inst

    def barrier(self):
        for en, e in self.engs.items():
            for en2, e2 in self.engs.items():
                if en2 != en and e2["count"] > 0:
                    self._wait(en, e2["key"], e2["count"])
            for k, v in self.dmasems.items():
                self._wait(en, k, v)

    def scope(self):
        kb = self

        class _S:
            def __enter__(s):
                s.old = kb.es
                s.base = kb.nalloc
                kb.es = ExitStack()
                kb.scopes = getattr(kb, "scopes", [])
                kb.scopes.append(kb.es)

            def __exit__(s, *a):
                if getattr(kb, "finished", False):
                    return
                kb.barrier()
                kb.es.close()
                kb.scopes.pop()
                kb.es = s.old
                kb.nalloc = s.base
        return _S()

    def finish(self, bufs):
        for b in bufs:
            if b.w is not None:
                self._wait("sync", b.w[0], b.w[1])
            for k, v in b.r.items():
                self._wait("sync", k, v)


class Pool:
    def __init__(self, kb, name, shape, dt, n, psum=False):
        self.tiles = []
        for i in range(n):
            t = (kb.ps if psum else kb.sb)("%s%d" % (name, i), shape, dt)
            self.tiles.append((t, kb.buf("%s%d" % (name, i))))
        self.i = 0

    def get(self):
        t = self.tiles[self.i % len(self.tiles)]
        self.i += 1
        return t


def _rev(ap2):
    return ap2[:, ::-1]


class Builder:
    def __init__(self, dbg=(), stop=None, same=True):
        self.dbg = set(dbg)
        self.stop = stop
        self.nc = bass.Bass("TRN2", target_bir_lowering=False)
        self.kb = KB(self.nc, same_engine_sync=same)
        self.ins = {}
        self.outs = {}
        self.fin = []

    def inp(self, name, shape):
        t = self.nc.dram_tensor(name, list(shape), F32, kind="ExternalInput").ap()
        self.ins[name] = t
        return t

    def outp(self, name, shape):
        t = self.nc.dram_tensor(name, list(shape), F32, kind="ExternalOutput").ap()
        self.outs[name] = t
        return t

    def dump(self, name, tile_ap, b, shape):
        if name not in self.dbg:
            return
        o = self.outp("dbg_" + name, shape)
        bo = self.kb.buf("dbg_" + name)
        self.kb.dma("sync", o, tile_ap, reads=[b], writes=[bo])
        self.fin.append(bo)

    def dump_bf(self, name, tile_ap, b, shape):
        if name not in self.dbg:
            return
        kb = self.kb
        t = kb.sb("dbgt_" + name, shape, F32)
        bt = kb.buf("dbgt_" + name)
        kb.op("vector", lambda e: e.tensor_copy(out=t[:], in_=tile_ap), reads=[b], writes=[bt])
        self.dump(name, t[:], bt, shape)

    def declare(self):
        i = self.inp
        self.x = i("x", [L, D])
        self.ctx = i("ctx", [CTX, D])
        self.cs_in = i("cs", [128, 8, 2])
        self.w_mod = i("w_mod", [D, 6 * D])
        self.b_mod_t = i("b_mod_t", [128, 48])
        self.b_mod = i("b_mod", [6 * D])
        self.norm1_t = i("norm1_t", [128, 8])
        self.norm2_t = i("norm2_t", [128, 8])
        self.w_in = i("w_in", [D, IN_COLS])
        self.ident = i("ident", [128, 128])
        self.s5_d_t = i("s5_d_t", [128, 4])
        self.lam_re_t = i("lam_re_t", [128, 32])
        self.lam_im_t = i("lam_im_t", [128, 32])
        self.logstep_t = i("logstep_t", [128, 32])
        self.Bblk = i("Bblk", [128, 2, 32, 32])
        self.Cblk = i("Cblk", [128, 2, 32, 32])
        self.iota1 = i("iota1", [128, TS5])
        self.gmask = i("gmask", [128, 11, 128])
        self.conv_t = i("conv_t", [128, 12, 5])
        self.alog_dtb = i("alog_dtb", [16])
        self.gdn_norm = i("gdn_norm", [128])
        self.w_glu = i("w_glu", [512, 512])
        self.b_glu_t = i("b_glu_t", [128, 4])
        self.w_ba = i("w_ba", [512, D])
        self.w_bb = i("w_bb", [512, D])
        self.w_out = i("w_out", [D, D])
        self.w_router = i("w_router", [D, NE])
        self.b_router = i("b_router", [NE])
        self.w_gu = i("w_gu", [NE, D, 2 * D])
        self.b_gu_t = i("b_gu_t", [128, NE, 16])
        self.w_dn = i("w_dn", [NE, D, D])
        self.b_dn = i("b_dn", [NE, D])
        self.norm_f = i("norm_f", [D])
        self.out = self.outp("out", [L, D])
        self.x1s = self.nc.dram_tensor("x1_scratch", [L, D], F32, kind="Internal").ap()

    def common(self):
        kb = self.kb
        self.PS = Pool(kb, "ps", [128, 512], F32, 8, psum=True)
        self.modT = kb.sb("modT", [128, 32, 2], F32)
        self.b_modT = kb.buf("modT")
        self.gt1_bc = kb.sb("gt1_bc", [128, D], F32)
        self.gt2_bc = kb.sb("gt2_bc", [128, D], F32)
        self.b_gt1 = kb.buf("gt1")
        self.b_gt2 = kb.buf("gt2")
        self.A1 = kb.sb("A1", [128, 8, 2], F32)
        self.b_A1 = kb.buf("A1")
        self.identf = kb.sb("identf", [128, 128], F32)
        self.b_ident = kb.buf("ident")
        kb.dma("sync", self.identf[:], self.ident, writes=[self.b_ident])

    def phase_mod(self):
        kb, nc = self.kb, self.nc
        cs = kb.sb("cs", [128, 8, 2], F32)
        b_cs = kb.buf("cs")
        kb.dma("sync", cs[:], self.cs_in, writes=[b_cs])
        sg = kb.sb("cs_sg", [128, 8, 2], F32)
        b_sg = kb.buf("cs_sg")
        kb.op("scalar", lambda e: e.activation(out=sg[:], in_=cs[:], func=AF.Sigmoid), reads=[b_cs], writes=[b_sg])
        css = kb.sb("css", [128, 8, 2], F32)
        b_css = kb.buf("css")
        kb.op("vector", lambda e: e.tensor_tensor(out=css[:], in0=cs[:], in1=sg[:], op=ALU.mult), reads=[b_cs, b_sg], writes=[b_css])
        csb = kb.sb("csb", [128, 8, 128], F32)
        b_csb = kb.buf("csb")
        kb.op("vector", lambda e: e.tensor_copy(out=csb[:], in_=css[:, :, 0:1].to_broadcast([128, 8, 128])), reads=[b_css], writes=[b_csb])
        bmt = kb.sb("bmt", [128, 48], F32)
        b_bmt = kb.buf("bmt")
        kb.dma("sync", bmt[:], self.b_mod_t, writes=[b_bmt])
        wpool = Pool(kb, "wmod", [128, 8, 512], F32, 2)
        wv = self.w_mod.rearrange("(kc p) n -> p kc n", p=128)
        pm, b_pm = self.PS.get()
        fm_groups = [0, 1, 2, 3, 6, 7, 8, 9]
        for gi, g in enumerate(fm_groups):
            wt, b_wt = wpool.get()
            kb.dma("sync" if gi % 2 == 0 else "scalar", wt[:], wv[:, :, 512 * g:512 * (g + 1)], writes=[b_wt])
            for cc in range(4):
                j = gi * 4 + cc
                for kc in range(8):
                    kb.op("tensor", lambda e, j=j, kc=kc, cc=cc, wt=wt: e.matmul(
                        pm[:, 2 * j:2 * j + 2], wt[:, kc, cc * 128:(cc + 1) * 128], css[:, kc, :],
                        start=(kc == 0), stop=(kc == 7)), reads=[b_wt, b_css], writes=[b_pm], sig=(kc == 7))
        kb.op("vector", lambda e: e.tensor_tensor(
            out=self.modT[:], in0=pm[:, 0:64].rearrange("p (j t) -> p j t", t=2),
            in1=self._bmt_sel(bmt), op=ALU.add), reads=[b_pm, b_bmt], writes=[self.b_modT])
        for which, (g0, dst, bdst) in enumerate([(4, self.gt1_bc, self.b_gt1), (10, self.gt2_bc, self.b_gt2)]):
            bb = kb.sb("bmodbc%d" % which, [128, D], F32)
            b_bb = kb.buf("bmodbc")
            kb.dma("sync", bb[:], self.b_mod[512 * g0:512 * g0 + D].partition_broadcast(128), writes=[b_bb])
            for half in range(2):
                g = g0 + half
                wt, b_wt = wpool.get()
                kb.dma("sync" if half == 0 else "scalar", wt[:], wv[:, :, 512 * g:512 * (g + 1)], writes=[b_wt])
                pg, b_pg = self.PS.get()
                for kc in range(8):
                    kb.op("tensor", lambda e, kc=kc, wt=wt, pg=pg: e.matmul(
                        pg[:], csb[:, kc, :], wt[:, kc, :], start=(kc == 0), stop=(kc == 7)),
                        reads=[b_wt, b_csb], writes=[b_pg], sig=(kc == 7))
                kb.op("vector", lambda e, pg=pg, half=half, dst=dst, bb=bb: e.tensor_tensor(
                    out=dst[:, 512 * half:512 * (half + 1)], in0=pg[:], in1=bb[:, 512 * half:512 * (half + 1)], op=ALU.add),
                    reads=[b_pg, b_bb], writes=[bdst])
        self.dump("modT", self.modT[:], self.b_modT, [128, 32, 2])
        self.dump("gt1", self.gt1_bc[:], self.b_gt1, [128, D])

    def _bmt_sel(self, bmt):
        return bmt[:, 0:32].unsqueeze(2).to_broadcast([128, 32, 2])

    def norm_to_T(self, src_tiles, dstT, b_dstT_blocks, scale_ap_fn, shift_ap_fn, tile_base, tag):
        raise NotImplementedError

    def phase_norm1(self):
        kb = self.kb
        n1 = kb.sb("norm1", [128, 8], F32)
        b_n1 = kb.buf("n1")
        kb.dma("sync", n1[:], self.norm1_t, writes=[b_n1])
        kb.op("vector", lambda e: e.scalar_tensor_tensor(
            out=self.A1[:], in0=self.modT[:, 8:16, :], scalar=1.0, in1=n1[:].unsqueeze(2).to_broadcast([128, 8, 2]),
            op0=ALU.add, op1=ALU.mult), reads=[self.b_modT, b_n1], writes=[self.b_A1])
        xin = Pool(kb, "xin", [128, D], F32, 3)
        xnp = Pool(kb, "xn", [128, D], F32, 2)
        junk = Pool(kb, "junk", [128, D], F32, 1)
        stat = Pool(kb, "stat", [128, 4], F32, 4)
        for tt in range(NT):
            which = 1 if tt < 2 else 0
            src = self.ctx[tt * 128:(tt + 1) * 128, :] if tt < 2 else self.x[(tt - 2) * 128:(tt - 1) * 128, :]
            xt, b_xt = xin.get()
            kb.dma("sync" if tt % 2 == 0 else "scalar", xt[:], src, writes=[b_xt])
            self._norm_tile(xt, b_xt, tt, which, self.A1, self.b_A1, 0, xnp, junk, stat, self.hT, self.b_hT[tt])
        self.dump_bf("hT", self.hT[:, :, 0:512], self.b_hT[3], [128, 8, 512]) if False else None
        if "hT" in self.dbg:
            allb = kb.buf("hTall")
            t = kb.sb("dbg_hT_t", [128, 8, 384], F32)
            kb.op("vector", lambda e: e.tensor_copy(out=t[:], in_=self.hT[:, :, 128:512]), reads=self.b_hT[1:4], writes=[allb])
            self.dump("hT", t[:], allb, [128, 8, 384])

    def _norm_tile(self, xt, b_xt, tt, which, A, b_A, shift_chunk0, xnp, junk, stat, dstT, b_dst):
        kb = self.kb
        jt, b_jt = junk.get()
        st, b_st = stat.get()
        kb.op("scalar", lambda e: e.activation(out=jt[:], in_=xt[:], func=AF.Square, accum_out=st[:, 0:1]),
              reads=[b_xt], writes=[b_jt, b_st])
        kb.op("scalar", lambda e: e.activation(out=st[:, 1:2], in_=st[:, 0:1], func=AF.Sqrt, bias=EPS, scale=1.0 / D),
              reads=[b_st], writes=[b_st])
        kb.op("vector", lambda e: e.reciprocal(out=st[:, 2:3], in_=st[:, 1:2]), reads=[b_st], writes=[b_st])
        xn, b_xn = xnp.get()
        kb.op("scalar", lambda e: e.activation(out=xn[:], in_=xt[:], func=AF.Identity, scale=st[:, 2:3]),
              reads=[b_xt, b_st], writes=[b_xn])
        for half in range(2):
            pt, b_pt = self.PS.get()
            for q in range(4):
                kc = half * 4 + q
                kb.op("tensor", lambda e, kc=kc, q=q, pt=pt: e.transpose(
                    pt[:, q * 128:(q + 1) * 128], xn[:, kc * 128:(kc + 1) * 128], self.identf[:]),
                    reads=[b_xn, self.b_ident], writes=[b_pt])
            for q in range(4):
                kc = half * 4 + q
                kb.op("vector", lambda e, kc=kc, q=q, pt=pt: e.tensor_scalar(
                    out=dstT[:, kc, tt * 128:(tt + 1) * 128], in0=pt[:, q * 128:(q + 1) * 128],
                    scalar1=A[:, kc, which:which + 1], scalar2=self.modT[:, shift_chunk0 + kc, which:which + 1],
                    op0=ALU.mult, op1=ALU.add), reads=[b_pt, b_A, self.b_modT], writes=[b_dst])

    def load_w_bf16(self, dst, b_dst, src_rows_ap, ncols, col0=0):
        kb = self.kb
        kcs = dst.shape[1]
        v = src_rows_ap.rearrange("(kc p) n -> p kc n", p=128)
        for kc in range(kcs):
            kb.dma("gpsimd", dst[:, kc, :], v[:, kc, col0:col0 + ncols], writes=[b_dst])

    def phase_u(self):
        kb = self.kb
        wu = kb.sb("wu", [128, 8, 512], BF16)
        b_wu = kb.buf("wu")
        self.load_w_bf16(wu, b_wu, self.w_in, 512, OFF_U)
        dsk = kb.sb("s5d", [128, 4], F32)
        b_dsk = kb.buf("s5d")
        kb.dma("sync", dsk[:], self.s5_d_t, writes=[b_dsk])
        for oc in range(4):
            for (s0, n) in BLOCKS:
                pt, b_pt = self.PS.get()
                tiles = range(s0 // 128, (s0 + n) // 128)
                for kc in range(8):
                    kb.op("tensor", lambda e, kc=kc, pt=pt, oc=oc, s0=s0, n=n: e.matmul(
                        pt[:, 0:n], wu[:, kc, oc * 128:(oc + 1) * 128], self.hT[:, kc, s0:s0 + n],
                        start=(kc == 0), stop=(kc == 7)), reads=[b_wu] + [self.b_hT[t] for t in tiles], writes=[b_pt], sig=(kc == 7))
                kb.op("scalar", lambda e, pt=pt, oc=oc, s0=s0, n=n: e.activation(
                    out=self.uT[:, oc, s0:s0 + n], in_=pt[:, 0:n], func=AF.Identity), reads=[b_pt], writes=[self.b_uT])
                if s0 >= CTX:
                    kb.op("scalar", lambda e, pt=pt, oc=oc, s0=s0, n=n: e.activation(
                        out=self.yT[:, oc, s0 - CTX:s0 - CTX + n], in_=pt[:, 0:n], func=AF.Identity, scale=dsk[:, oc:oc + 1]),
                        reads=[b_pt, b_dsk], writes=[self.b_yT[oc]])
        if "uT" in self.dbg:
            t = kb.sb("dbg_uT_t", [128, 4, 512], F32)
            bt = kb.buf("dbg_uT")
            kb.op("vector", lambda e: e.tensor_copy(out=t[:], in_=self.uT[:, :, 0:512]), reads=[self.b_uT], writes=[bt])
            self.dump("uT", t[:], bt, [128, 4, 512])


    def sincos(self, ang, b_ang, n, want_cos, out_ap, b_out, scale=1.0):
        kb = self.kb
        with kb.scope():
            y = kb.sb("sc_y", [128, n], F32)
            ki = kb.sb("sc_k", [128, n], I32)
            kf = kb.sb("sc_kf", [128, n], F32)
            b = kb.buf("sc")
            off = 0.75 if want_cos else 0.5
            kb.op("vector", lambda e: e.tensor_scalar(out=y[:], in0=ang, scalar1=1.0 / (2 * np.pi), scalar2=off + 64.0,
                                                     op0=ALU.mult, op1=ALU.add), reads=[b_ang], writes=[b])
            kb.op("vector", lambda e: e.tensor_copy(out=ki[:], in_=y[:]), reads=[b], writes=[b])
            kb.op("vector", lambda e: e.tensor_copy(out=kf[:], in_=ki[:]), reads=[b], writes=[b])
            kb.op("vector", lambda e: e.tensor_tensor(out=y[:], in0=y[:], in1=kf[:], op=ALU.subtract), reads=[b], writes=[b])
            kb.op("vector", lambda e: e.scalar_tensor_tensor(out=kf[:], in0=y[:], scalar=0.0, in1=y[:], op0=ALU.is_lt, op1=ALU.add),
                  reads=[b], writes=[b])
            kb.op("scalar", lambda e: e.activation(out=y[:], in_=kf[:], func=AF.Sin, scale=6.2831, bias=-3.14155),
                  reads=[b], writes=[b])
            yv = y[:] if len(out_ap.shape) == 2 else y[:].rearrange("p (a t) -> p a t", t=out_ap.shape[-1])
            kb.op("scalar", lambda e: e.activation(out=out_ap, in_=yv, func=AF.Identity, scale=float(scale)),
                  reads=[b], writes=[b_out])

    def phase_s5_setup(self):
        kb = self.kb
        T = TS5
        self.rho = kb.sb("s5_rho", [128, 32], F32)
        self.Bdrv = kb.sb("Bdrv", [128, 2, 4, 2, 128], BF16)
        self.b_Bdrv = kb.buf("Bdrv")
        self.Crd = kb.sb("Crd", [128, 32, 2, 32], BF16)
        self.b_Crd = kb.buf("Crd")
        self.Tc = kb.sb("Tc", [128, 32 * T], BF16)
        self.b_Tc = kb.buf("Tc")
        self.Tsn = kb.sb("Tsn", [128, 32, 2, T], BF16)
        self.b_Tsn = kb.buf("Tsn")
        self.b_s5c = kb.buf("s5setup")
        if getattr(self, "defer_s5_body", False):
            return
        with kb.scope():
            self._s5_setup_body()

    def _s5_setup_body(self):
        kb = self.kb
        T = TS5
        ld = lambda name, src, shape: self._ld(name, src, shape)
        lre, b_lre = ld("lre", self.lam_re_t, [128, 32])
        lim, b_lim = ld("lim", self.lam_im_t, [128, 32])
        lst, b_lst = ld("lst", self.logstep_t, [128, 32])
        Bb, b_Bb = ld("Bblk", self.Bblk, [128, 2, 32, 32])
        Cb, b_Cb = ld("Cblk", self.Cblk, [128, 2, 32, 32])
        io, b_io = ld("iota1", self.iota1, [128, T])
        V = lambda name: (kb.sb("s5v_" + name, [128, 32], F32))
        b = self.b_s5c
        deps = [b_lre, b_lim, b_lst, b]
        mag = self.rho
        lr, dt, ang, ar, ai, den, am1, fr, fi, t1, t2 = [V(n) for n in
            ("lr", "dt", "ang", "ar", "ai", "den", "am1", "fr", "fi", "t1", "t2")]
        v = lambda fn: kb.op("vector", fn, reads=deps, writes=[b])
        a = lambda fn: kb.op("scalar", fn, reads=deps, writes=[b])
        v(lambda e: e.tensor_scalar(out=lr[:], in0=lre[:], scalar1=-1e-4, scalar2=0.0, op0=ALU.min, op1=ALU.add))
        a(lambda e: e.activation(out=dt[:], in_=lst[:], func=AF.Exp))
        v(lambda e: e.tensor_tensor(out=t1[:], in0=lr[:], in1=dt[:], op=ALU.mult))
        a(lambda e: e.activation(out=mag[:], in_=t1[:], func=AF.Exp))
        v(lambda e: e.tensor_tensor(out=ang[:], in0=lim[:], in1=dt[:], op=ALU.mult))
        sn = V("sn0"); cs = V("cs0")
        self.sincos(ang[:], b, 32, False, sn[:], b)
        self.sincos(ang[:], b, 32, True, cs[:], b)
        v(lambda e: e.tensor_tensor(out=ar[:], in0=mag[:], in1=cs[:], op=ALU.mult))
        v(lambda e: e.tensor_tensor(out=ai[:], in0=mag[:], in1=sn[:], op=ALU.mult))
        v(lambda e: e.tensor_tensor(out=t1[:], in0=lr[:], in1=lr[:], op=ALU.mult))
        v(lambda e: e.tensor_tensor(out=t2[:], in0=lim[:], in1=lim[:], op=ALU.mult))
        v(lambda e: e.tensor_tensor(out=den[:], in0=t1[:], in1=t2[:], op=ALU.add))
        v(lambda e: e.reciprocal(out=den[:], in_=den[:]))
        v(lambda e: e.tensor_scalar(out=am1[:], in0=ar[:], scalar1=-1.0, scalar2=0.0, op0=ALU.add, op1=ALU.add))
        v(lambda e: e.tensor_tensor(out=t1[:], in0=am1[:], in1=lr[:], op=ALU.mult))
        v(lambda e: e.tensor_tensor(out=t2[:], in0=ai[:], in1=lim[:], op=ALU.mult))
        v(lambda e: e.tensor_tensor(out=fr[:], in0=t1[:], in1=t2[:], op=ALU.add))
        v(lambda e: e.tensor_tensor(out=fr[:], in0=fr[:], in1=den[:], op=ALU.mult))
        v(lambda e: e.tensor_tensor(out=t1[:], in0=ai[:], in1=lr[:], op=ALU.mult))
        v(lambda e: e.tensor_tensor(out=t2[:], in0=am1[:], in1=lim[:], op=ALU.mult))
        v(lambda e: e.tensor_tensor(out=fi[:], in0=t1[:], in1=t2[:], op=ALU.subtract))
        v(lambda e: e.tensor_tensor(out=fi[:], in0=fi[:], in1=den[:], op=ALU.mult))
        Bbar = kb.sb("Bbar", [128, 2, 32, 32], F32)
        tb = kb.sb("Bbar_t", [128, 32, 32], F32)
        b_Bbar = kb.buf("Bbar")
        frb = fr[:].unsqueeze(2).to_broadcast([128, 32, 32])
        fib = fi[:].unsqueeze(2).to_broadcast([128, 32, 32])
        vb = lambda fn: kb.op("vector", fn, reads=[b, b_Bb], writes=[b_Bbar])
        vb(lambda e: e.tensor_tensor(out=Bbar[:, 0], in0=Bb[:, 0], in1=frb, op=ALU.mult))
        vb(lambda e: e.tensor_tensor(out=tb[:], in0=Bb[:, 1], in1=fib, op=ALU.mult))
        vb(lambda e: e.tensor_tensor(out=Bbar[:, 0], in0=Bbar[:, 0], in1=tb[:], op=ALU.subtract))
        vb(lambda e: e.tensor_tensor(out=Bbar[:, 1], in0=Bb[:, 1], in1=frb, op=ALU.mult))
        vb(lambda e: e.tensor_tensor(out=tb[:], in0=Bb[:, 0], in1=fib, op=ALU.mult))
        vb(lambda e: e.tensor_tensor(out=Bbar[:, 1], in0=Bbar[:, 1], in1=tb[:], op=ALU.add))
        for d in range(2):
            for qd in range(4):
                pt, b_pt = self.PS.get()
                for ri in range(2):
                    blk = (d * 4 + qd) * 4
                    kb.op("tensor", lambda e, ri=ri, blk=blk, pt=pt: e.transpose(
                        pt[:, ri * 128:(ri + 1) * 128], Bbar[:, ri, blk:blk + 4, :], self.identf[:]),
                        reads=[b_Bbar, self.b_ident], writes=[b_pt])
                kb.op("scalar", lambda e, d=d, qd=qd, pt=pt: e.activation(
                    out=self.Bdrv[:, d, qd, :, :], in_=pt[:, 0:256].rearrange("p (r n) -> p r n", r=2), func=AF.Identity),
                    reads=[b_pt], writes=[self.b_Bdrv])
        kb.op("scalar", lambda e: e.activation(out=self.Crd[:, :, 0, :], in_=Cb[:, 0], func=AF.Identity),
              reads=[b_Cb], writes=[self.b_Crd])
        kb.op("scalar", lambda e: e.activation(out=self.Crd[:, :, 1, :], in_=Cb[:, 1], func=AF.Identity, scale=-1.0),
              reads=[b_Cb], writes=[self.b_Crd])
        ph = kb.sb("s5_ph", [128, 32, T], F32)
        b_ph = kb.buf("s5ph")
        kb.op("vector", lambda e: e.tensor_tensor(out=ph[:], in0=ang[:].unsqueeze(2).to_broadcast([128, 32, T]),
                                                 in1=io[:].unsqueeze(1).to_broadcast([128, 32, T]), op=ALU.mult),
              reads=[b, b_io], writes=[b_ph])
        for a0 in range(0, 32, 8):
            pha = ph[:, a0:a0 + 8, :].rearrange("p a t -> p (a t)")
            self.sincos(pha, b_ph, 8 * T, True, self.Tc[:, a0 * T:(a0 + 8) * T], self.b_Tc)
            self.sincos(pha, b_ph, 8 * T, False, self.Tsn[:, a0:a0 + 8, 0, :], self.b_Tsn)
            self.sincos(pha, b_ph, 8 * T, False, self.Tsn[:, a0:a0 + 8, 1, :], self.b_Tsn, scale=-1.0)
        self.dump("rho", self.rho[:], b, [128, 32])
        self.dump("fr", fr[:], b, [128, 32])
        self.dump("fi", fi[:], b, [128, 32])
        self.dump("Tc", self.Tc[:, 0:4 * T], self.b_Tc, [128, 4 * T])

    def _ld(self, name, src, shape, dt=F32, q="sync"):
        t = self.kb.sb(name, shape, dt)
        b = self.kb.buf(name)
        self.kb.dma(q, t[:], src, writes=[b])
        return t, b

    def phase_s5(self):
        kb = self.kb
        T = TS5
        NCK = TOK // T
        Tc3 = self.Tc[:].rearrange("p (a t) -> p a t", t=T)
        G = kb.sb("s5_G", [128, 32, 2, T], F32)
        b_G = [kb.buf("s5G%d" % i) for i in range(32)]
        carry = kb.sb("s5_carry", [128, 32, 2], F32)
        b_carry0 = kb.buf("s5carry")
        kb.op("vector", lambda e: e.memset(carry[:], 0.0), writes=[b_carry0])
        b_carryg = {}
        P1p = Pool(kb, "s5P1", [128, 2, T], F32, 4)
        P2p = Pool(kb, "s5P2", [128, 2, T], F32, 4)
        Vp = Pool(kb, "s5V", [128, 2, T], F32, 8)
        Q1p = Pool(kb, "s5Q1", [128, 2, T], F32, 4)
        Q2p = Pool(kb, "s5Q2", [128, 2, T], F32, 4)
        Hp = Pool(kb, "s5H", [128, 2, T], BF16, 8)
        ctmp = kb.sb("s5_ctmp", [128, 32, 2], F32)
        ctmp2 = kb.sb("s5_ctmp2", [128, 32, 2], F32)
        tabs = [self.b_Tc, self.b_Tsn]
        drv_tiles = self.PS.tiles[0:5]
        py_tiles = self.PS.tiles[5:8]
        cnt = {"drv": 0, "py": 0}
        pending = []

        def flush(keep):
            while len(pending) > keep:
                (src, qd_, l0_, b_py_) = pending.pop(0)
                kb.op("vector", lambda e, src=src, qd_=qd_, l0_=l0_: e.tensor_tensor(
                    out=self.yT[:, qd_, l0_:l0_ + T], in0=src, in1=self.yT[:, qd_, l0_:l0_ + T], op=ALU.add),
                    reads=[b_py_], writes=[self.b_yT[qd_]])
        for ck in range(NCK):
            is_lat = ck >= CTX // T
            for qd in range(4):
                for d in range(2):
                    if d == 0:
                        s0 = ck * T
                    elif not is_lat:
                        s0 = CTX - (ck + 1) * T
                    else:
                        s0 = TOK - (ck - CTX // T + 1) * T
                    if is_lat:
                        py, b_py = py_tiles[cnt["py"] % 3]
                        cnt["py"] += 1
                    gkey = (qd, d)
                    if gkey not in b_carryg:
                        b_carryg[gkey] = kb.buf("s5carry%d%d" % gkey)
                        b_carryg[gkey].w = b_carry0.w
                    b_carry = b_carryg[gkey]
                    st = []
                    for ppq in range(4):
                        pd = d * 16 + qd * 4 + ppq
                        rhs = self.uT[ppq * 32:(ppq + 1) * 32, qd, s0:s0 + T]
                        if d == 1:
                            rhs = rhs[:, ::-1]
                        pdv, b_pdv = drv_tiles[cnt["drv"] % 5]
                        cnt["drv"] += 1
                        for ri in range(2):
                            kb.op("tensor", lambda e, ri=ri, pdv=pdv, rhs=rhs, ppq=ppq: e.matmul(
                                pdv[:, ri * T:(ri + 1) * T], self.Bdrv[ppq * 32:(ppq + 1) * 32, d, qd, ri, :], rhs,
                                start=True, stop=True, tile_position=(ppq * 32, 0)),
                                reads=[self.b_Bdrv, self.b_uT], writes=[b_pdv], sig=(ri == 1))
                        Dv = pdv[:, 0:2 * T].rearrange("p (r t) -> p r t", r=2)
                        cosb = Tc3[:, pd:pd + 1, :].to_broadcast([128, 2, T])
                        st.append(dict(pd=pd, ppq=ppq, Dv=Dv, b_pdv=b_pdv, cosb=cosb, P1=P1p.get(), P2=P2p.get(), V=Vp.get()))
                    for x in st:
                        kb.op("vector", lambda e, x=x: e.tensor_tensor(out=x["P1"][0][:], in0=x["Dv"], in1=x["cosb"], op=ALU.mult),
                              reads=[x["b_pdv"]] + tabs, writes=[x["P1"][1]])
                    for x in st:
                        kb.op("vector", lambda e, x=x: e.tensor_tensor(out=x["P2"][0][:], in0=x["Dv"][:, ::-1, :], in1=self.Tsn[:, x["pd"]], op=ALU.mult),
                              reads=[x["b_pdv"]] + tabs, writes=[x["P2"][1]])
                    for x in st:
                        kb.op("vector", lambda e, x=x: e.tensor_tensor(out=x["V"][0][:], in0=x["P1"][0][:], in1=x["P2"][0][:], op=ALU.add),
                              reads=[x["P1"][1], x["P2"][1]], writes=[x["V"][1]])
                    for ri in range(2):
                        for x in st:
                            pd = x["pd"]
                            kb.op("vector", lambda e, ri=ri, x=x, pd=pd: e.tensor_tensor_scan(
                                out=G[:, pd, ri, :], data0=self.rho[:, pd:pd + 1].to_broadcast([128, T]), data1=x["V"][0][:, ri, :],
                                initial=carry[:, pd, ri:ri + 1], op0=ALU.mult, op1=ALU.add),
                                reads=[x["V"][1], b_carry, self.b_s5c], writes=[b_G[pd]])
                    if ck < NCK - 1:
                        pd0 = d * 16 + qd * 4
                        Gl = G[:, pd0:pd0 + 4, :, T - 1]
                        cl = Tc3[:, pd0:pd0 + 4, T - 1:T].to_broadcast([128, 4, 2])
                        sl = self.Tsn[:, pd0:pd0 + 4, :, T - 1]
                        gb_ = [b_G[pd0 + q] for q in range(4)]
                        c1 = ctmp[:, pd0:pd0 + 4, :]
                        c2 = ctmp2[:, pd0:pd0 + 4, :]
                        kb.op("vector", lambda e, Gl=Gl, cl=cl, c1=c1: e.tensor_tensor(out=c1, in0=Gl, in1=cl, op=ALU.mult),
                              reads=gb_ + tabs, writes=[b_carry])
                        kb.op("vector", lambda e, Gl=Gl, sl=sl, c2=c2: e.tensor_tensor(out=c2, in0=Gl[:, :, ::-1], in1=sl, op=ALU.mult),
                              reads=gb_ + tabs, writes=[b_carry])
                        kb.op("vector", lambda e, c1=c1, c2=c2, pd0=pd0: e.tensor_tensor(out=carry[:, pd0:pd0 + 4, :], in0=c1, in1=c2, op=ALU.subtract),
                              reads=[b_carry], writes=[b_carry])
                    if is_lat:
                        for x in st:
                            x["Q1"] = Q1p.get(); x["Q2"] = Q2p.get(); x["H"] = Hp.get()
                        for x in st:
                            kb.op(POST_ENG, lambda e, x=x: e.tensor_tensor(out=x["Q1"][0][:], in0=G[:, x["pd"]], in1=x["cosb"], op=ALU.mult),
                                  reads=[b_G[x["pd"]]] + tabs, writes=[x["Q1"][1]])
                        for x in st:
                            kb.op(POST_ENG, lambda e, x=x: e.tensor_tensor(out=x["Q2"][0][:], in0=G[:, x["pd"], ::-1, :], in1=self.Tsn[:, x["pd"]], op=ALU.mult),
                                  reads=[b_G[x["pd"]]] + tabs, writes=[x["Q2"][1]])
                        for x in st:
                            kb.op(POST_ENG, lambda e, x=x: e.tensor_tensor(out=x["H"][0][:], in0=x["Q1"][0][:], in1=x["Q2"][0][:], op=ALU.subtract),
                                  reads=[x["Q1"][1], x["Q2"][1]], writes=[x["H"][1]])
                        for x in st:
                            for ri in range(2):
                                kb.op("tensor", lambda e, ri=ri, x=x: e.matmul(
                                    py[x["ppq"] * 32:(x["ppq"] + 1) * 32, 0:T], self.Crd[:, x["pd"], ri, :], x["H"][0][:, ri, :],
                                    start=(ri == 0), stop=(ri == 1), tile_position=(0, x["ppq"] * 32)),
                                    reads=[self.b_Crd, x["H"][1]], writes=[b_py], sig=(ri == 1))
                        l0 = s0 - CTX
                        src = py[:, 0:T] if d == 0 else py[:, 0:T][:, ::-1]
                        pending.append((src, qd, l0, b_py))
                        flush(2)
        flush(0)
        if "yT" in self.dbg:
            allb = kb.buf("yTall")
            t = kb.sb("dbg_yT_t", [128, 4, 512], F32)
            kb.op("vector", lambda e: e.tensor_copy(out=t[:], in_=self.yT[:, :, 0:512]), reads=self.b_yT, writes=[allb])
            self.dump("yT", t[:], allb, [128, 4, 512])
            t2 = kb.sb("dbg_yT_t2", [128, 4, 512], F32)
            allb2 = kb.buf("yTall2")
            kb.op("vector", lambda e: e.tensor_copy(out=t2[:], in_=self.yT[:, :, 1536:2048]), reads=self.b_yT, writes=[allb2])
            self.dbg.add("yT2")
            self.dump("yT2", t2[:], allb2, [128, 4, 512])


    def phase_gdn_setup(self):
        kb = self.kb
        self.gm, self.b_gm = self._ld("gmask", self.gmask, [128, 11, 128])
        self.cw, self.b_cw = self._ld("convw", self.conv_t, [128, 12, 5])
        self.beta = kb.sb("g_beta", [128, NT, 8], F32)
        self.gg = kb.sb("g_g", [128, NT, 8], F32)
        self.E1 = kb.sb("g_E1", [128, NT, 8], F32)
        self.E2 = kb.sb("g_E2", [128, NT, 8], F32)
        self.DL = kb.sb("g_DL", [128, 2, NT, 8], F32)
        self.b_gs = kb.buf("gscal")
        b = self.b_gs
        with kb.scope():
            wab = kb.sb("wab", [128, 8, 16], BF16)
            b_wab = kb.buf("wab")
            self.load_w_bf16(wab, b_wab, self.w_in, 16, OFF_B)
            ab = kb.sb("alogdtb", [128, 16], F32)
            b_ab = kb.buf("alogdtb")
            kb.dma("sync", ab[:], self.alog_dtb.partition_broadcast(128), writes=[b_ab])
            pz, b_pz = self.PS.get()
            for tt in range(NT):
                for kc in range(8):
                    kb.op("tensor", lambda e, tt=tt, kc=kc: e.matmul(
                        pz[:, tt * 16:(tt + 1) * 16], self.hT[:, kc, tt * 128:(tt + 1) * 128], wab[:, kc, :],
                        start=(kc == 0), stop=(kc == 7)), reads=[b_wab, self.b_hT[tt]], writes=[b_pz], sig=(kc == 7))
            zab = pz[:, 0:NT * 16].rearrange("p (t c) -> p t c", c=16)
            kb.op("scalar", lambda e: e.activation(out=self.beta[:], in_=zab[:, :, 0:8], func=AF.Sigmoid), reads=[b_pz], writes=[b])
            if self.stop == "gs1":
                return
            t1 = kb.sb("g_t1", [128, NT, 8], F32)
            ea = kb.sb("g_ea", [128, 8], F32)
            kb.op("vector", lambda e: e.tensor_tensor(out=t1[:], in0=zab[:, :, 8:16], in1=ab[:, 8:16].unsqueeze(1).to_broadcast([128, NT, 8]),
                                                     op=ALU.add), reads=[b_pz, b_ab], writes=[b])
            kb.op("scalar", lambda e: e.activation(out=t1[:], in_=t1[:], func=AF.Exp), reads=[b], writes=[b])
            kb.op("scalar", lambda e: e.activation(out=t1[:], in_=t1[:], func=AF.Ln, bias=1.0), reads=[b], writes=[b])
            kb.op("scalar", lambda e: e.activation(out=ea[:], in_=ab[:, 0:8], func=AF.Exp), reads=[b_ab], writes=[b])
            kb.op("vector", lambda e: e.scalar_tensor_tensor(out=self.gg[:], in0=t1[:], scalar=-1.0,
                                                            in1=ea[:].unsqueeze(1).to_broadcast([128, NT, 8]), op0=ALU.mult, op1=ALU.mult),
                  reads=[b], writes=[b])
            if self.stop == "gs2":
                return
            gflat = self.gg[:].rearrange("p t x -> p (t x)")
            gc = kb.sb("g_gcum", [128, 2, NT, 8], F32)
            for d in range(2):
                pc, b_pc = self.PS.get()
                kb.op("tensor", lambda e, d=d, pc=pc: e.matmul(pc[:, 0:NT * 8], self.gm[:, d, :], gflat, start=True, stop=True),
                      reads=[b, self.b_gm], writes=[b_pc])
                kb.op("vector", lambda e, d=d, pc=pc: e.tensor_copy(out=gc[:, d].rearrange("p t x -> p (t x)"), in_=pc[:, 0:NT * 8]),
                      reads=[b_pc], writes=[b])
            if self.stop == "gs3":
                return
            pl, b_pl = self.PS.get()
            kb.op("tensor", lambda e: e.matmul(pl[:, 0:NT * 8], self.gm[:, 7, :], gflat, start=True, stop=True),
                  reads=[b, self.b_gm], writes=[b_pl])
            glv = pl[:, 0:NT * 8].rearrange("p (t x) -> p t x", x=8)
            for d in range(2):
                xs = slice(d * 4, d * 4 + 4)
                kb.op("scalar", lambda e, d=d, xs=xs: e.activation(out=self.E1[:, :, xs], in_=gc[:, d, :, xs], func=AF.Exp),
                      reads=[b], writes=[b])
                kb.op("vector", lambda e, d=d, xs=xs: e.tensor_tensor(out=t1[:, :, xs], in0=glv[:, :, xs], in1=gc[:, d, :, xs], op=ALU.subtract),
                      reads=[b, b_pl], writes=[b])
                kb.op("scalar", lambda e, xs=xs: e.activation(out=self.E2[:, :, xs], in_=t1[:, :, xs], func=AF.Exp), reads=[b], writes=[b])
            if self.stop == "gs4":
                return
            for c2 in ([1] if self.stop == "gs7" else range(2)):
                ph, b_ph = self.PS.get()
                kb.op("tensor", lambda e, c2=c2, ph=ph: e.matmul(ph[:, 0:NT * 8], self.gm[:, 5 + c2, :], gflat, start=True, stop=True),
                      reads=[b, self.b_gm], writes=[b_ph])
                if self.stop == "gs5":
                    return
                kb.op("scalar", lambda e, c2=c2, ph=ph: e.activation(out=self.DL[:, c2].rearrange("p t x -> p (t x)"), in_=ph[:, 0:NT * 8],
                                                                      func=AF.Exp), reads=[b_ph], writes=[b])
                if self.stop == "gs6":
                    return
        self.dump("g_g", self.gg[:], b, [128, NT, 8])
        self.dump("g_E1", self.E1[:], b, [128, NT, 8])
        self.dump("g_E2", self.E2[:], b, [128, NT, 8])
        self.dump("g_DL", self.DL[:], b, [128, 2, NT, 8])

    def gdn_head_front(self, h):
        kb = self.kb
        self.qh = kb.sb("g_qh%d" % h, [128, NT, 128], F32)
        self.kh = kb.sb("g_kh%d" % h, [128, NT, 128], F32)
        self.vh = kb.sb("g_vh%d" % h, [128, NT, 128], F32)
        GB = BF16 if os.environ.get("KGBF", "0") == "1" else F32
        self.kT = kb.sb("g_kT%d" % h, [128, TOK], GB)
        self.qT = kb.sb("g_qT%d" % h, [128, TOK], GB)
        self.b_qh, self.b_kh, self.b_vh, self.b_kT, self.b_qT = [kb.buf("g_%s%d" % (n, h)) for n in ("qh", "kh", "vh", "kT", "qT")]
        with kb.scope():
            wq = kb.sb("g_wqkv", [128, 8, 3, 128], BF16)
            b_wq = kb.buf("g_wqkv")
            v = self.w_in.rearrange("(kc p) n -> p kc n", p=128)
            for c in range(3):
                for kc in range(8):
                    col = OFF_QKV + c * 512 + h * 128
                    kb.dma("gpsimd", wq[:, kc, c, :], v[:, kc, col:col + 128], writes=[b_wq])
            zb = kb.sb("g_z", [128, TOK], F32)
            acc = kb.sb("g_acc", [128, TOK], F32)
            b_z = kb.buf("g_z")
            b_acc = kb.buf("g_acc")
            PADC = CTX + 4
            zp = kb.sb("g_zp", [128, PADC + 32 * 68], BF16)
            b_zp = kb.buf("g_zp")
            kb.op("vector", lambda e: e.memset(zp[:], 0.0), writes=[b_zp])
            zp_lat = zp[:, PADC:PADC + 32 * 68].rearrange("p (r w) -> p r w", w=68)
            dgp = Pool(kb, "g_dg", [128, 5, 128], BF16, 2)
            identb = kb.sb("g_identb", [128, 128], BF16)
            b_idb = kb.buf("g_identb")
            kb.op("vector", lambda e: e.tensor_copy(out=identb[:], in_=self.identf[:]), reads=[self.b_ident], writes=[b_idb])
            dsts = [(self.qh, self.b_qh), (self.kh, self.b_kh), (self.vh, self.b_vh)]
            for c in range(3):
                ch = c * 4 + h
                dg, b_dg = dgp.get()
                for k in range(5):
                    kb.op("vector", lambda e, k=k, dg=dg, ch=ch: e.tensor_scalar(out=dg[:, k, :], in0=identb[:], scalar1=self.cw[:, ch, k:k + 1], scalar2=0.0,
                                                                            op0=ALU.mult, op1=ALU.add), reads=[b_idb, self.b_cw], writes=[b_dg])
                for (s0, n) in BLOCKS:
                    pt, b_pt = self.PS.get()
                    tiles = range(s0 // 128, (s0 + n) // 128)
                    for kc in range(8):
                        kb.op("tensor", lambda e, kc=kc, pt=pt, c=c, s0=s0, n=n: e.matmul(
                            pt[:, 0:n], wq[:, kc, c, :], self.hT[:, kc, s0:s0 + n], start=(kc == 0), stop=(kc == 7)),
                            reads=[b_wq] + [self.b_hT[t] for t in tiles], writes=[b_pt], sig=(kc == 7))
                    if s0 < CTX:
                        kb.op("scalar", lambda e, pt=pt, n=n: e.activation(out=zp[:, 2:2 + CTX], in_=pt[:, 0:n], func=AF.Identity),
                              reads=[b_pt], writes=[b_zp])
                    else:
                        r0 = (s0 - CTX) // 64
                        kb.op("scalar", lambda e, pt=pt, n=n, r0=r0: e.activation(out=zp_lat[:, r0:r0 + 8, 2:66], in_=pt[:, 0:n].rearrange("p (r w) -> p r w", w=64),
                                                                                 func=AF.Identity), reads=[b_pt], writes=[b_zp])
                for (s0, n) in BLOCKS:
                    pc, b_pc = self.PS.get()
                    for k in range(5):
                        if s0 < CTX:
                            mv = zp[:, k:k + CTX]
                        else:
                            r0 = (s0 - CTX) // 64
                            mv = zp_lat[:, r0:r0 + 8, k:k + 64]
                        kb.op("tensor", lambda e, k=k, pc=pc, mv=mv, n=n, dg=dg: e.matmul(pc[:, 0:n], dg[:, k, :], mv, start=(k == 0), stop=(k == 4)),
                              reads=[b_dg, b_zp], writes=[b_pc], sig=(k == 4))
                    kb.op("scalar", lambda e, pc=pc, s0=s0, n=n: e.activation(out=zb[:, s0:s0 + n], in_=pc[:, 0:n], func=AF.Silu), reads=[b_pc], writes=[b_z])
                if c == 0 and h == 0:
                    self.dump("g_cq", zb[:, 0:512], b_z, [128, 512])
                dst, b_dst = dsts[c]
                for t0 in range(0, NT, 4):
                    nt = min(4, NT - t0)
                    pt, b_pt = self.PS.get()
                    for q in range(nt):
                        kb.op("tensor", lambda e, q=q, t0=t0, pt=pt: e.transpose(
                            pt[:, q * 128:(q + 1) * 128], zb[:, (t0 + q) * 128:(t0 + q + 1) * 128], self.identf[:]),
                            reads=[b_z, self.b_ident], writes=[b_pt])
                    kb.op("scalar" if (t0 // 4) % 2 == 0 else "vector", lambda e, t0=t0, nt=nt, pt=pt, dst=dst: (
                        e.activation(out=dst[:, t0:t0 + nt, :], in_=pt[:, 0:nt * 128].rearrange("p (t c) -> p t c", c=128), func=AF.Identity)
                        if e is self.nc.scalar else
                        e.tensor_copy(out=dst[:, t0:t0 + nt, :], in_=pt[:, 0:nt * 128].rearrange("p (t c) -> p t c", c=128))),
                        reads=[b_pt], writes=[b_dst])
            sq = acc[:, 0:NT * 128].rearrange("p (t c) -> p t c", c=128)
            ss = kb.sb("g_ss", [128, 2, NT], F32)
            b_ss = kb.buf("g_ss")
            for qi, (src, b_src) in enumerate([(self.qh, self.b_qh), (self.kh, self.b_kh)]):
                kb.op("vector", lambda e, src=src: e.tensor_tensor(out=sq, in0=src[:], in1=src[:], op=ALU.mult), reads=[b_src], writes=[b_acc])
                kb.op("vector", lambda e, qi=qi: e.tensor_reduce(out=ss[:, qi, :], in_=sq, axis=AX.X, op=ALU.add), reads=[b_acc], writes=[b_ss])
            kb.op("scalar", lambda e: e.activation(out=ss[:], in_=ss[:], func=AF.Sqrt, bias=EPS, scale=1.0), reads=[b_ss], writes=[b_ss])
            kb.op("vector", lambda e: e.reciprocal(out=ss[:], in_=ss[:]), reads=[b_ss], writes=[b_ss])
            kb.op("vector", lambda e: e.tensor_scalar(out=ss[:, 0, :], in0=ss[:, 0, :], scalar1=float(128 ** -0.5), scalar2=0.0,
                                                     op0=ALU.mult, op1=ALU.add), reads=[b_ss], writes=[b_ss])
            for qi, (src, b_src) in enumerate([(self.qh, self.b_qh), (self.kh, self.b_kh)]):
                kb.op("vector", lambda e, src=src, qi=qi: e.tensor_tensor(
                    out=src[:], in0=src[:], in1=ss[:, qi, :].unsqueeze(2).to_broadcast([128, NT, 128]), op=ALU.mult),
                    reads=[b_src, b_ss], writes=[b_src])
            for (src, b_src, dstT, b_dT) in [(self.kh, self.b_kh, self.kT, self.b_kT), (self.qh, self.b_qh, self.qT, self.b_qT)]:
                for t0 in range(0, NT, 4):
                    nt = min(4, NT - t0)
                    pt, b_pt = self.PS.get()
                    for q in range(nt):
                        kb.op("tensor", lambda e, q=q, t0=t0, pt=pt, src=src: e.transpose(
                            pt[:, q * 128:(q + 1) * 128], src[:, t0 + q, :], self.identf[:]), reads=[b_src, self.b_ident], writes=[b_pt])
                    kb.op("scalar", lambda e, t0=t0, nt=nt, pt=pt, dstT=dstT: e.activation(
                        out=dstT[:, t0 * 128:(t0 + nt) * 128], in_=pt[:, 0:nt * 128], func=AF.Identity), reads=[b_pt], writes=[b_dT])
        if h == 0:
            self.dump("g_qh", self.qh[:, 0:4, :], self.b_qh, [128, 4, 128])
            self.dump("g_kh", self.kh[:, 0:4, :], self.b_kh, [128, 4, 128])
            self.dump("g_vh", self.vh[:, 0:4, :], self.b_vh, [128, 4, 128])


    def gdn_head_core(self, h):
        kb = self.kb
        gm = self.gm
        BFN = ("kw", "vb", "kbT", "A", "N", "X0", "X1", "P0", "P1", "Q0", "Q1")
        T_ = lambda name, n=1: Pool(kb, "gc_%s_%d" % (name, h), [128, 128],
                                    BF16 if (os.environ.get("KGBF", "0") == "1" and name[:-1] in BFN) else F32, n)
        R = 4
        rings = {n: [T_("%s%d" % (n, d), R) for d in range(2)] for n in ("wT", "ub", "qkT", "qdT", "kd")}
        tmp = {n: [T_("%s%d" % (n, d), 1) for d in range(2)] for n in
               ("kbt", "kw", "vb", "qd", "kbT", "Dls", "DTs", "DTi", "A", "N", "X0", "X1", "P0", "P1", "Q0", "Q1")}
        vnp = [T_("vn%d" % d, 2) for d in range(2)]
        S = [kb.sb("g_S%d_%d" % (h, d), [128, 128], F32) for d in range(2)]
        b_S = [kb.buf("g_S%d" % d) for d in range(2)]
        for d in range(2):
            kb.op("vector", lambda e, d=d: e.memset(S[d][:], 0.0), writes=[b_S[d]])
        order = {0: list(range(NT)), 1: [1, 0] + list(range(NT - 1, 1, -1))}
        ringent = {}
        gs = self.b_gs
        LS, US, UI, LI = 2, 3, 4, 10

        kb.barrier()
        pst = [t for (t, _) in self.PS.tiles]

        class Reg:
            def __init__(r, ap, b_):
                r.ap = ap
                r.b = b_
        regs = {}
        pb = [b_ for (_, b_) in self.PS.tiles]
        for d in range(2):
            regs[("pD", d)] = Reg(pst[4 * d][:, 0:256], pb[4 * d])
            regs[("pt", d)] = Reg(pst[4 * d + 1][:, 0:256], pb[4 * d + 1])
            regs[("pK", d)] = Reg(pst[4 * d + 2][:, 0:384], pb[4 * d + 2])
            regs[("pw", d)] = Reg(pst[4 * d + 3][:, 0:128], pb[4 * d + 3])
            regs[("po", d)] = Reg(pst[4 * d + 3][:, 128:256], pb[4 * d + 3])
            regs[("ps", d)] = Reg(pst[4 * d + 3][:, 256:384], pb[4 * d + 3])
        R32 = (lambda ap: ap)
        prog = {"prep": [0, 0], "chain": [0, 0]}

        def prep_gen(d):
            for i in range(NT):
                while i - prog["chain"][d] >= R - 1:
                    yield
                tt = order[d][i]
                x = d * 4 + h
                ts_ = slice(tt * 128, (tt + 1) * 128)
                bsc = self.beta[:, tt, x:x + 1]
                e1 = self.E1[:, tt, x:x + 1]
                e2 = self.E2[:, tt, x:x + 1]
                g = lambda n: tmp[n][d].get()
                kbt, b_kbt = g("kbt"); kw, b_kw = g("kw"); vb, b_vb = g("vb"); qd, b_qd = g("qd"); kbT, b_kbT = g("kbT")
                kd, b_kd = rings["kd"][d].get(); qdT, b_qdT = rings["qdT"][d].get()
                ts1 = lambda e, o, i_, sc: e.tensor_scalar(out=R32(o[:]), in0=i_, scalar1=sc, scalar2=0.0, op0=ALU.mult, op1=ALU.add)
                kb.op("vector", lambda e: ts1(e, kbt, self.kh[:, tt, :], bsc), reads=[self.b_kh, gs], writes=[b_kbt])
                kb.op("vector", lambda e: ts1(e, qd, self.qh[:, tt, :], e1), reads=[self.b_qh, gs], writes=[b_qd])
                gb = self.gg[:, tt, x:x + 1].to_broadcast([128, 128])
                M, nM = gm[:, d, :], gm[:, 8 + d, :]
                pD, b_pD = regs[("pD", d)].ap, regs[("pD", d)].b
                kb.op("tensor", lambda e: e.matmul(pD[:, 0:128], M, gb, start=True, stop=False), reads=[gs, self.b_gm], writes=[b_pD], sig=False)
                kb.op("tensor", lambda e: e.matmul(pD[:, 0:128], gb, nM, start=False, stop=True), reads=[gs, self.b_gm], writes=[b_pD], sig=False)
                kb.op("tensor", lambda e: e.matmul(pD[:, 128:256], nM, gb, start=True, stop=False), reads=[gs, self.b_gm], writes=[b_pD], sig=False)
                kb.op("tensor", lambda e: e.matmul(pD[:, 128:256], gb, M, start=False, stop=True), reads=[gs, self.b_gm], writes=[b_pD])
                yield
                kb.op("vector", lambda e: ts1(e, kw, kbt[:], e1), reads=[b_kbt, gs], writes=[b_kw])
                kb.op("vector", lambda e: ts1(e, vb, self.vh[:, tt, :], bsc), reads=[self.b_vh, gs], writes=[b_vb])
                kb.op("vector", lambda e: ts1(e, kd, self.kh[:, tt, :], e2), reads=[self.b_kh, gs], writes=[b_kd])
                pt, b_pt = regs[("pt", d)].ap, regs[("pt", d)].b
                kb.op("tensor", lambda e: e.transpose(pt[:, 0:128], kbt[:], self.identf[:]), reads=[b_kbt, self.b_ident], writes=[b_pt], sig=False)
                kb.op("tensor", lambda e: e.transpose(pt[:, 128:256], qd[:], self.identf[:]), reads=[b_qd, self.b_ident], writes=[b_pt])
                yield
                kb.op("scalar", lambda e: e.activation(out=R32(kbT[:]), in_=pt[:, 0:128], func=AF.Identity), reads=[b_pt], writes=[b_kbT])
                kb.op("scalar", lambda e: e.activation(out=qdT[:], in_=pt[:, 128:256], func=AF.Identity), reads=[b_pt], writes=[b_qdT])
                Dls, b_Dls = g("Dls"); DTs, b_DTs = g("DTs"); DTi, b_DTi = g("DTi")
                mD, mDTs, mDTi = (LS, US, UI) if d == 0 else (US, LS, LI)
                trip = [(Dls, b_Dls, pD[:, 0:128], mD), (DTs, b_DTs, pD[:, 128:256], mDTs), (DTi, b_DTi, pD[:, 128:256], mDTi)]
                for (dst, b_dst, src, mk) in trip:
                    kb.op("vector", lambda e, dst=dst, src=src, mk=mk: e.scalar_tensor_tensor(
                        out=dst[:], in0=src, scalar=0.0, in1=gm[:, mk, :], op0=ALU.min, op1=ALU.add), reads=[b_pD, self.b_gm], writes=[b_dst])
                yield
                for (dst, b_dst, src, mk) in trip:
                    kb.op("scalar", lambda e, dst=dst: e.activation(out=dst[:], in_=dst[:], func=AF.Exp), reads=[b_dst], writes=[b_dst])
                pK, b_pK = regs[("pK", d)].ap, regs[("pK", d)].b
                kTt, qTt = self.kT[:, ts_], self.qT[:, ts_]
                kb.op("tensor", lambda e: e.matmul(pK[:, 0:128], R32(kbT[:]), R32(kTt), start=True, stop=True), reads=[b_kbT, self.b_kT], writes=[b_pK], sig=False)
                kb.op("tensor", lambda e: e.matmul(pK[:, 128:256], R32(kTt), R32(kbT[:]), start=True, stop=True), reads=[b_kbT, self.b_kT], writes=[b_pK], sig=False)
                kb.op("tensor", lambda e: e.matmul(pK[:, 256:384], R32(kTt), R32(qTt), start=True, stop=True), reads=[self.b_qT, self.b_kT], writes=[b_pK])
                yield
                A, b_A = g("A"); N, b_N = g("N"); qkT, b_qkT = rings["qkT"][d].get()
                kb.op("vector", lambda e: e.tensor_tensor(out=R32(A[:]), in0=pK[:, 0:128], in1=Dls[:], op=ALU.mult), reads=[b_pK, b_Dls], writes=[b_A])
                kb.op("vector", lambda e: e.tensor_tensor(out=R32(N[:]), in0=pK[:, 128:256], in1=DTs[:], op=ALU.mult), reads=[b_pK, b_DTs], writes=[b_N])
                kb.op("vector", lambda e: e.tensor_tensor(out=qkT[:], in0=pK[:, 256:384], in1=DTi[:], op=ALU.mult), reads=[b_pK, b_DTi], writes=[b_qkT])
                X, b_X = g("X0")
                yield
                kb.op("vector", lambda e: e.tensor_tensor(out=R32(X[:]), in0=self.identf[:], in1=N[:], op=ALU.subtract), reads=[b_N, self.b_ident], writes=[b_X])
                P, b_P, PT, b_PT = N, b_N, A, b_A
                for sidx in range(1, 6):
                    pp, b_pp = regs[("pK", d)].ap, regs[("pK", d)].b
                    if sidx < 5:
                        kb.op("tensor", lambda e: e.matmul(pp[:, 0:128], R32(PT[:]), R32(P[:]), start=True, stop=True), reads=[b_P, b_PT], writes=[b_pp], sig=False)
                    kb.op("tensor", lambda e: e.matmul(pp[:, 128:256], R32(P[:]), R32(PT[:]), start=True, stop=True), reads=[b_P, b_PT], writes=[b_pp])
                    yield
                    nP, b_nP = tmp["P%d" % (sidx % 2)][d].get()
                    nPT, b_nPT = tmp["Q%d" % (sidx % 2)][d].get()
                    kb.op("scalar", lambda e: e.activation(out=R32(nPT[:]), in_=pp[:, 128:256], func=AF.Identity), reads=[b_pp], writes=[b_nPT])
                    if sidx < 5:
                        kb.op("scalar", lambda e: e.activation(out=R32(nP[:]), in_=pp[:, 0:128], func=AF.Identity), reads=[b_pp], writes=[b_nP])
                    yield
                    kb.op("tensor", lambda e: e.matmul(pp[:, 256:384], R32(nPT[:]), R32(X[:]), start=True, stop=True), reads=[b_nPT, b_X], writes=[b_pp])
                    yield
                    nX, b_nX = tmp["X%d" % (sidx % 2)][d].get()
                    kb.op("vector", lambda e: e.tensor_tensor(out=R32(nX[:]), in0=pp[:, 256:384], in1=X[:], op=ALU.add), reads=[b_pp, b_X], writes=[b_nX])
                    P, b_P, PT, b_PT, X, b_X = nP, b_nP, nPT, b_nPT, nX, b_nX
                    yield
                pu, b_pu = regs[("pK", d)].ap, regs[("pK", d)].b
                kb.op("tensor", lambda e: e.matmul(pu[:, 0:128], R32(X[:]), R32(vb[:]), start=True, stop=True), reads=[b_X, b_vb], writes=[b_pu], sig=False)
                kb.op("tensor", lambda e: e.matmul(pu[:, 128:256], R32(kw[:]), R32(X[:]), start=True, stop=True), reads=[b_X, b_kw], writes=[b_pu])
                yield
                ub, b_ub = rings["ub"][d].get(); wT, b_wT = rings["wT"][d].get()
                kb.op("scalar", lambda e: e.activation(out=ub[:], in_=pu[:, 0:128], func=AF.Identity), reads=[b_pu], writes=[b_ub])
                kb.op("scalar", lambda e: e.activation(out=wT[:], in_=pu[:, 128:256], func=AF.Identity), reads=[b_pu], writes=[b_wT])
                ringent[(d, i)] = dict(d=d, tt=tt, x=x, kd=(kd, b_kd), qdT=(qdT, b_qdT), qkT=(qkT, b_qkT), ub=(ub, b_ub), wT=(wT, b_wT))
                prog["prep"][d] = i + 1
                yield

        def chain_gen(d):
            for i in range(NT):
                while (d, i) not in ringent:
                    yield
                en = ringent[(d, i)]
                tt, x = en["tt"], en["x"]
                wT, b_wT = en["wT"]; ub, b_ub = en["ub"]; qdT, b_qdT = en["qdT"]; qkT, b_qkT = en["qkT"]; kd, b_kd = en["kd"]
                for hi in range(2):
                    c2 = hi if d == 0 else 1 - hi
                    r = slice(c2 * 64, c2 * 64 + 64)
                    pw, b_pw = regs[("pw", d)].ap, regs[("pw", d)].b
                    kb.op("tensor", lambda e: e.matmul(pw[r, :], wT[:, r], S[d][:], start=True, stop=True), reads=[b_wT, b_S[d]], writes=[b_pw])
                    if tt >= 2:
                        po, b_po = regs[("po", d)].ap, regs[("po", d)].b
                        kb.op("tensor", lambda e: e.matmul(po[r, :], qdT[:, r], S[d][:], start=True, stop=False),
                              reads=[b_qdT, b_S[d]], writes=[b_po], sig=False)
                    yield
                    vn, b_vn = vnp[d].get()
                    kb.op("vector", lambda e: e.tensor_tensor(out=vn[r, :], in0=ub[r, :], in1=pw[r, :], op=ALU.subtract),
                          reads=[b_ub, b_pw], writes=[b_vn])
                    yield
                    ps_, b_ps = regs[("ps", d)].ap, regs[("ps", d)].b
                    if tt >= 2:
                        kb.op("tensor", lambda e: e.matmul(po[r, :], qkT[r, r], vn[r, :], start=False, stop=True),
                              reads=[b_qkT, b_vn], writes=[b_po])
                    kb.op("tensor", lambda e: e.matmul(ps_[:, :], kd[r, :], vn[r, :], start=True, stop=True), reads=[b_kd, b_vn], writes=[b_ps])
                    yield
                    kb.op("vector", lambda e: e.scalar_tensor_tensor(out=S[d][:], in0=S[d][:], scalar=self.DL[:, c2, tt, x:x + 1], in1=ps_[:, :],
                                                                    op0=ALU.mult, op1=ALU.add), reads=[b_ps, b_S[d], gs], writes=[b_S[d]])
                    if tt >= 2:
                        od = self.osum[r, tt - 2, h * 128:(h + 1) * 128]
                        kb.op("gpsimd" if False else "vector", lambda e: e.tensor_tensor(out=od, in0=po[r, :], in1=od, op=ALU.add),
                              reads=[b_po], writes=[self.b_osum[tt - 2]])
                    yield
                prog["chain"][d] = i + 1

        gens = [prep_gen(0), prep_gen(1), chain_gen(0), chain_gen(1)]
        while gens:
            for g_ in list(gens):
                try:
                    next(g_)
                except StopIteration:
                    gens.remove(g_)

    def phase_gdn(self):
        kb = self.kb
        self.osum = kb.sb("g_osum", [128, 16, 512], F32)
        self.b_osum = [kb.buf("g_osum%d" % t) for t in range(16)]
        for t in range(16):
            kb.op("vector", lambda e, t=t: e.memset(self.osum[:, t, :], 0.0), writes=[self.b_osum[t]])
        for h in range(4):
            with kb.scope():
                self.gdn_head_front(h)
                self.gdn_head_core(h)
            if self.stop == "gdn_h0":
                break
        if "g_osum" in self.dbg:
            self.dump("g_osum", self.osum[:, 0:4, 0:128], self.b_osum[3], [128, 4, 128])
        if "g_osum2" in self.dbg:
            self.dump("g_osum2", self.osum[:, 12:16, 0:128], self.b_osum[15], [128, 4, 128])


    def phase_glu(self):
        kb = self.kb
        wg = kb.sb("wglu", [128, 4, 512], BF16)
        b_wg = kb.buf("wglu")
        self.load_w_bf16(wg, b_wg, self.w_glu, 512, 0)
        bg, b_bg = self._ld("bglu", self.b_glu_t, [128, 4])
        zT = kb.sb("glu_z", [128, 4, L], BF16)
        b_zT = [kb.buf("glu_z%d" % q) for q in range(4)]
        t1 = kb.sb("glu_t1", [128, L], F32)
        t2 = kb.sb("glu_t2", [128, L], F32)
        b_t = kb.buf("glu_t")
        for q in range(4):
            y = self.yT[:, q, :]
            kb.op("vector", lambda e, y=y: e.tensor_tensor(out=t1[:], in0=y, in1=y, op=ALU.mult), reads=[self.b_yT[q]], writes=[b_t])
            kb.op("vector", lambda e: e.tensor_scalar(out=t1[:], in0=t1[:], scalar1=0.044715, scalar2=1.0, op0=ALU.mult, op1=ALU.add), reads=[b_t], writes=[b_t])
            kb.op("vector", lambda e, y=y: e.tensor_tensor(out=t1[:], in0=t1[:], in1=y, op=ALU.mult), reads=[b_t, self.b_yT[q]], writes=[b_t])
            kb.op("scalar", lambda e: e.activation(out=t2[:], in_=t1[:], func=AF.Sigmoid, scale=1.5957691216057308), reads=[b_t], writes=[b_t])
            kb.op("vector", lambda e, y=y, q=q: e.tensor_tensor(out=zT[:, q, :], in0=t2[:], in1=y, op=ALU.mult), reads=[b_t, self.b_yT[q]], writes=[b_zT[q]])
        glp = Pool(kb, "glu_g", [128, 512], F32, 2)
        for oc in range(4):
            for n in range(4):
                pt, b_pt = self.PS.get()
                for kc in range(4):
                    kb.op("tensor", lambda e, kc=kc, pt=pt, oc=oc, n=n: e.matmul(
                        pt[:], wg[:, kc, oc * 128:(oc + 1) * 128], zT[:, kc, n * 512:(n + 1) * 512], start=(kc == 0), stop=(kc == 3)),
                        reads=[b_wg] + b_zT, writes=[b_pt], sig=(kc == 3))
                gl, b_gl = glp.get()
                kb.op("scalar", lambda e, pt=pt, gl=gl, oc=oc: e.activation(out=gl[:], in_=pt[:], func=AF.Sigmoid, bias=bg[:, oc:oc + 1]),
                      reads=[b_pt, b_bg], writes=[b_gl])
                kb.op("vector", lambda e, gl=gl, oc=oc, n=n: e.tensor_tensor(
                    out=self.yaT[:, oc, n * 512:(n + 1) * 512], in0=zT[:, oc, n * 512:(n + 1) * 512], in1=gl[:], op=ALU.mult),
                    reads=[b_gl, b_zT[oc]], writes=[self.b_yaT])
        if "yaT" in self.dbg:
            t = kb.sb("dbg_yaT_t", [128, 4, 512], F32)
            bt = kb.buf("dbg_yaT")
            kb.op("vector", lambda e: e.tensor_copy(out=t[:], in_=self.yaT[:, :, 0:512]), reads=[self.b_yaT], writes=[bt])
            self.dump("yaT", t[:], bt, [128, 4, 512])

    def phase_gdn_out(self):
        kb = self.kb
        wgt = kb.sb("wgate", [128, 8, 512], BF16)
        b_wgt = kb.buf("wgate")
        self.load_w_bf16(wgt, b_wgt, self.w_in, 512, OFF_GATE)
        nw = kb.sb("gnw", [128, 128], F32)
        b_nw = kb.buf("gnw")
        kb.dma("sync", nw[:], self.gdn_norm.partition_broadcast(128), writes=[b_nw])
        ss = kb.sb("go_ss", [128, 16, 4], F32)
        b_ss = kb.buf("go_ss")
        sqp = Pool(kb, "go_sq", [128, 512], F32, 2)
        for t in range(16):
            sq, b_sq = sqp.get()
            kb.op("vector", lambda e, t=t, sq=sq: e.tensor_tensor(out=sq[:], in0=self.osum[:, t, :], in1=self.osum[:, t, :], op=ALU.mult),
                  reads=[self.b_osum[t]], writes=[b_sq])
            kb.op("vector", lambda e, t=t, sq=sq: e.tensor_reduce(out=ss[:, t, :], in_=sq[:].rearrange("p (h c) -> p h c", c=128), axis=AX.X, op=ALU.add),
                  reads=[b_sq], writes=[b_ss])
        ssf = ss[:].rearrange("p t h -> p (t h)")
        kb.op("scalar", lambda e: e.activation(out=ssf, in_=ssf, func=AF.Sqrt, bias=EPS, scale=1.0 / 128), reads=[b_ss], writes=[b_ss])
        kb.op("vector", lambda e: e.reciprocal(out=ssf, in_=ssf), reads=[b_ss], writes=[b_ss])
        sgp = Pool(kb, "go_sg", [128, 512], F32, 2)
        onp = Pool(kb, "go_on", [128, 512], F32, 2)
        for t in range(16):
            pg, b_pg = self.PS.get()
            for kc in range(8):
                kb.op("tensor", lambda e, kc=kc, t=t, pg=pg: e.matmul(pg[:], self.hT[:, kc, (t + 2) * 128:(t + 3) * 128], wgt[:, kc, :],
                                                                     start=(kc == 0), stop=(kc == 7)), reads=[b_wgt, self.b_hT[t + 2]], writes=[b_pg], sig=(kc == 7))
            sg, b_sg = sgp.get()
            kb.op("scalar", lambda e, pg=pg, sg=sg: e.activation(out=sg[:], in_=pg[:], func=AF.Silu), reads=[b_pg], writes=[b_sg])
            on, b_on = onp.get()
            on3 = on[:].rearrange("p (h c) -> p h c", c=128)
            kb.op("vector", lambda e, t=t, on3=on3: e.tensor_tensor(out=on3, in0=self.osum[:, t, :].rearrange("p (h c) -> p h c", c=128),
                                                                   in1=ss[:, t, :].unsqueeze(2).to_broadcast([128, 4, 128]), op=ALU.mult),
                  reads=[self.b_osum[t], b_ss], writes=[b_on])
            kb.op("vector", lambda e, on3=on3: e.tensor_tensor(out=on3, in0=on3, in1=nw[:].unsqueeze(1).to_broadcast([128, 4, 128]), op=ALU.mult),
                  reads=[b_on, b_nw], writes=[b_on])
            kb.op("vector", lambda e, on=on, sg=sg: e.tensor_tensor(out=on[:], in0=on[:], in1=sg[:], op=ALU.mult), reads=[b_on, b_sg], writes=[b_on])
            pt, b_pt = self.PS.get()
            for c in range(4):
                kb.op("tensor", lambda e, c=c, pt=pt, on=on: e.transpose(pt[:, c * 128:(c + 1) * 128], on[:, c * 128:(c + 1) * 128], self.identf[:]),
                      reads=[b_on, self.b_ident], writes=[b_pt])
            kb.op("scalar", lambda e, pt=pt, t=t: e.activation(out=self.ybT[:, :, t * 128:(t + 1) * 128], in_=pt[:].rearrange("p (c k) -> p c k", k=128),
                                                              func=AF.Identity), reads=[b_pt], writes=[self.b_ybT])
        if "ybT" in self.dbg:
            t_ = kb.sb("dbg_ybT_t", [128, 4, 512], F32)
            bt = kb.buf("dbg_ybT")
            kb.op("vector", lambda e: e.tensor_copy(out=t_[:], in_=self.ybT[:, :, 0:512]), reads=[self.b_ybT], writes=[bt])
            self.dump("ybT", t_[:], bt, [128, 4, 512])

    def phase_merge(self):
        kb = self.kb
        mT = kb.sb("mergedT", [128, 8, L], BF16)
        b_mT = [kb.buf("mergedT%d" % n) for n in range(4)]
        with kb.scope():
            wba = kb.sb("wba", [128, 4, D], BF16); b_wba = kb.buf("wba")
            wbb = kb.sb("wbb", [128, 4, D], BF16); b_wbb = kb.buf("wbb")
            self.load_w_bf16(wba, b_wba, self.w_ba, D, 0)
            self.load_w_bf16(wbb, b_wbb, self.w_bb, D, 0)
            wbrp = Pool(kb, "wbr", [128, 8, 2, 128], BF16, 2)
            gp = Pool(kb, "mg_g", [128, 2, 512], F32, 2)
            m1p = Pool(kb, "mg_m", [128, 2, 512], F32, 2)
            v = self.w_in.rearrange("(kc p) n -> p kc n", p=128)
            for oc in range(8):
                wbr, b_wbr = wbrp.get()
                for ab in range(2):
                    for kc in range(8):
                        col = OFF_BR + ab * D + oc * 128
                        kb.dma("gpsimd", wbr[:, kc, ab, :], v[:, kc, col:col + 128], writes=[b_wbr])
                for n in range(4):
                    tiles = [self.b_hT[2 + 4 * n + j] for j in range(4)]
                    tok = slice(n * 512, (n + 1) * 512)
                    stok = slice(CTX + n * 512, CTX + (n + 1) * 512)
                    g, b_g = gp.get()
                    m1, b_m1 = m1p.get()
                    for ab, (wb, b_wb, yT_, b_y) in enumerate([(wba, b_wba, self.yaT, self.b_yaT), (wbb, b_wbb, self.ybT, self.b_ybT)]):
                        pbr, b_pbr = self.PS.get()
                        for kc in range(8):
                            kb.op("tensor", lambda e, kc=kc, pbr=pbr, ab=ab, wbr=wbr: e.matmul(
                                pbr[:], wbr[:, kc, ab, :], self.hT[:, kc, stok], start=(kc == 0), stop=(kc == 7)),
                                reads=[b_wbr] + tiles, writes=[b_pbr], sig=(kc == 7))
                        kb.op("scalar", lambda e, pbr=pbr, g=g, ab=ab: e.activation(out=g[:, ab, :], in_=pbr[:], func=AF.Sigmoid),
                              reads=[b_pbr], writes=[b_g])
                        pp, b_pp = self.PS.get()
                        for kc in range(4):
                            kb.op("tensor", lambda e, kc=kc, pp=pp, wb=wb, yT_=yT_: e.matmul(
                                pp[:], wb[:, kc, oc * 128:(oc + 1) * 128], yT_[:, kc, tok], start=(kc == 0), stop=(kc == 3)),
                                reads=[b_wb, b_y], writes=[b_pp], sig=(kc == 3))
                        kb.op("vector", lambda e, pp=pp, g=g, m1=m1, ab=ab: e.tensor_tensor(out=m1[:, ab, :], in0=pp[:], in1=g[:, ab, :], op=ALU.mult),
                              reads=[b_pp, b_g], writes=[b_m1])
                    kb.op("vector", lambda e, m1=m1, oc=oc: e.tensor_tensor(out=mT[:, oc, tok], in0=m1[:, 0, :], in1=m1[:, 1, :], op=ALU.add),
                          reads=[b_m1], writes=[b_mT[n]])
        if "mergedT" in self.dbg:
            t_ = kb.sb("dbg_mT_t", [128, 8, 256], F32)
            bt = kb.buf("dbg_mT")
            kb.op("vector", lambda e: e.tensor_copy(out=t_[:], in_=mT[:, :, 0:256]), reads=[b_mT[0]], writes=[bt])
            self.dump("mergedT", t_[:], bt, [128, 8, 256])
        with kb.scope():
            wo = kb.sb("wout", [128, 8, D], BF16); b_wo = kb.buf("wout")
            self.load_w_bf16(wo, b_wo, self.w_out, D, 0)
            xp = Pool(kb, "mg_x", [128, D], F32, 3)
            tp = Pool(kb, "mg_t", [128, D], F32, 2)
            self.b_x1s = [kb.buf("x1s%d" % t) for t in range(16)]
            for t in range(16):
                xt, b_xt = xp.get()
                kb.dma("sync" if t % 2 == 0 else "scalar", xt[:], self.x[t * 128:(t + 1) * 128, :], writes=[b_xt])
                tm_, b_tm = tp.get()
                for cb in range(2):
                    pm, b_pm = self.PS.get()
                    for kc in range(8):
                        kb.op("tensor", lambda e, kc=kc, pm=pm, cb=cb, t=t: e.matmul(
                            pm[:], mT[:, kc, t * 128:(t + 1) * 128], wo[:, kc, cb * 512:(cb + 1) * 512], start=(kc == 0), stop=(kc == 7)),
                            reads=[b_wo, b_mT[t // 4]], writes=[b_pm], sig=(kc == 7))
                    cs_ = slice(cb * 512, (cb + 1) * 512)
                    kb.op("vector", lambda e, pm=pm, tm_=tm_, cs_=cs_: e.tensor_tensor(out=tm_[:, cs_], in0=pm[:], in1=self.gt1_bc[:, cs_], op=ALU.mult),
                          reads=[b_pm, self.b_gt1], writes=[b_tm])
                kb.op("vector", lambda e, tm_=tm_, xt=xt: e.tensor_tensor(out=xt[:], in0=tm_[:], in1=xt[:], op=ALU.add), reads=[b_tm, b_xt], writes=[b_xt])
                kb.dma("sync", self.x1s[t * 128:(t + 1) * 128, :], xt[:], reads=[b_xt], writes=[self.b_x1s[t]])
                if t == 0:
                    self.dump("x1", xt[:], b_xt, [128, D])

    def phase_moe_half(self, hp):
        kb = self.kb
        NTL = 8
        X = kb.sb("X%d" % hp, [128, NTL, D], F32)
        b_X = [kb.buf("X%d_%d" % (hp, t)) for t in range(NTL)]
        h2T = kb.sb("h2T%d" % hp, [128, 8, NTL * 128], BF16)
        b_h2 = [kb.buf("h2T%d_%d" % (hp, t)) for t in range(NTL)]
        comb = kb.sb("comb%d" % hp, [128, NTL, NE], F32)
        b_comb = kb.buf("comb%d" % hp)
        combs = kb.sb("combs%d" % hp, [128, NTL, NE], F32)
        combT = kb.sb("combT%d" % hp, [NE, NTL * 128], F32)
        b_combT = kb.buf("combT%d" % hp)
        for t in range(NTL):
            gt = hp * NTL + t
            kb.dma("sync" if t % 2 == 0 else "scalar", X[:, t, :], self.x1s[gt * 128:(gt + 1) * 128, :], reads=[self.b_x1s[gt]], writes=[b_X[t]])
        with kb.scope():
            n2, b_n2 = self._ld("norm2", self.norm2_t, [128, 8])
            A2 = kb.sb("A2", [128, 8, 2], F32); b_A2 = kb.buf("A2")
            kb.op("vector", lambda e: e.scalar_tensor_tensor(out=A2[:], in0=self.modT[:, 24:32, :], scalar=1.0,
                                                            in1=n2[:].unsqueeze(2).to_broadcast([128, 8, 2]), op0=ALU.add, op1=ALU.mult),
                  reads=[self.b_modT, b_n2], writes=[b_A2])
            wr = kb.sb("wrouter", [128, 8, NE], F32); b_wr = kb.buf("wrouter")
            kb.dma("sync", wr[:], self.w_router.rearrange("(kc p) n -> p kc n", p=128), writes=[b_wr])
            br_, b_br = kb.sb("brouter", [128, NE], F32), kb.buf("brouter")
            kb.dma("sync", br_[:], self.b_router.partition_broadcast(128), writes=[b_br])
            xnp = Pool(kb, "m_xn", [128, D], F32, 2)
            junk = Pool(kb, "m_junk", [128, D], F32, 1)
            stat = Pool(kb, "m_stat", [128, 4], F32, 4)
            hfp = Pool(kb, "m_hf", [128, 8, 128], F32, 2)
            lgp = Pool(kb, "m_lg", [128, NE], F32, 2)
            m8p = Pool(kb, "m_m8", [128, 16], F32, 2)
            for t in range(NTL):
                hf, b_hf = hfp.get()
                self._norm_tile2(X[:, t, :], b_X[t], t, A2, b_A2, 16, xnp, junk, stat, h2T, b_h2[t], hf, b_hf)
                pl, b_pl = self.PS.get()
                for kc in range(8):
                    kb.op("tensor", lambda e, kc=kc, pl=pl, hf=hf: e.matmul(pl[:, 0:NE], hf[:, kc, :], wr[:, kc, :], start=(kc == 0), stop=(kc == 7)),
                          reads=[b_hf, b_wr], writes=[b_pl], sig=(kc == 7))
                lg, b_lg = lgp.get()
                m8, b_m8 = m8p.get()
                kb.op("vector", lambda e, pl=pl, lg=lg: e.tensor_tensor(out=lg[:], in0=pl[:, 0:NE], in1=br_[:], op=ALU.add), reads=[b_pl, b_br], writes=[b_lg])
                if t == 0 and hp == 0:
                    self.dump("logits", lg[:], b_lg, [128, NE])
                kb.op("vector", lambda e, lg=lg, m8=m8: e.max(out=m8[:, 0:8], in_=lg[:]), reads=[b_lg], writes=[b_m8])
                kb.op("vector", lambda e, m8=m8: e.tensor_scalar(out=m8[:, 8:9], in0=m8[:, 0:1], scalar1=-1.0, scalar2=0.0, op0=ALU.mult, op1=ALU.add),
                      reads=[b_m8], writes=[b_m8])
                ex, b_ex = lgp.get()
                kb.op("scalar", lambda e, lg=lg, ex=ex, m8=m8: e.activation(out=ex[:], in_=lg[:], func=AF.Exp, bias=m8[:, 8:9]), reads=[b_lg, b_m8], writes=[b_ex])
                kb.op("vector", lambda e, lg=lg, ex=ex, m8=m8: e.scalar_tensor_tensor(out=ex[:], in0=lg[:], scalar=m8[:, 3:4], in1=ex[:], op0=ALU.is_ge, op1=ALU.mult),
                      reads=[b_lg, b_ex, b_m8], writes=[b_ex])
                kb.op("vector", lambda e, ex=ex, m8=m8: e.tensor_reduce(out=m8[:, 9:10], in_=ex[:], axis=AX.X, op=ALU.add), reads=[b_ex], writes=[b_m8])
                kb.op("vector", lambda e, m8=m8: e.reciprocal(out=m8[:, 10:11], in_=m8[:, 9:10]), reads=[b_m8], writes=[b_m8])
                kb.op("vector", lambda e, ex=ex, m8=m8, t=t: e.tensor_scalar(out=comb[:, t, :], in0=ex[:], scalar1=m8[:, 10:11], scalar2=0.0, op0=ALU.mult, op1=ALU.add),
                      reads=[b_ex, b_m8], writes=[b_comb])
                kb.op("vector", lambda e, t=t: e.tensor_scalar(out=combs[:, t, :], in0=comb[:, t, :], scalar1=float(1.0 / 1.702), scalar2=0.0, op0=ALU.mult, op1=ALU.add),
                      reads=[b_comb], writes=[b_comb])
                pT, b_pT = self.PS.get()
                kb.op("tensor", lambda e, pT=pT, t=t: e.transpose(pT[0:NE, 0:128], comb[:, t, :], self.identf[:]), reads=[b_comb, self.b_ident], writes=[b_pT])
                kb.op("scalar", lambda e, pT=pT, t=t: e.activation(out=combT[:, t * 128:(t + 1) * 128], in_=pT[0:NE, 0:128], func=AF.Identity),
                      reads=[b_pT], writes=[b_combT])
            if hp == 0:
                self.dump("comb", comb[:, 0, :], b_comb, [128, NE])
                if "h2T" in self.dbg:
                    t_ = kb.sb("dbg_h2T_t", [128, 8, 256], F32)
                    bt = kb.buf("dbg_h2T")
                    kb.op("vector", lambda e: e.tensor_copy(out=t_[:], in_=h2T[:, :, 0:256]), reads=b_h2[0:2], writes=[bt])
                    self.dump("h2T", t_[:], bt, [128, 8, 256])
        if self.stop == "router":
            return
        with kb.scope():
            bdn, b_bdn = self._ld("bdn", self.b_dn, [NE, D])
            bgu, b_bgu = self._ld("bgu", self.b_gu_t, [128, NE, 16])
            tp = Pool(kb, "moe_t", [128, 512], F32, 3)
            for t in range(NTL):
                for cb in range(2):
                    cs_ = slice(cb * 512, (cb + 1) * 512)
                    pb, b_pb = self.PS.get()
                    kb.op("tensor", lambda e, pb=pb, t=t, cs_=cs_: e.matmul(pb[:], combT[:, t * 128:(t + 1) * 128], bdn[:, cs_], start=True, stop=True),
                          reads=[b_combT, b_bdn], writes=[b_pb])
                    tt_, b_tt = tp.get()
                    kb.op("vector", lambda e, pb=pb, tt_=tt_, cs_=cs_: e.tensor_tensor(out=tt_[:], in0=pb[:], in1=self.gt2_bc[:, cs_], op=ALU.mult),
                          reads=[b_pb, self.b_gt2], writes=[b_tt])
                    kb.op("vector", lambda e, tt_=tt_, t=t, cs_=cs_: e.tensor_tensor(out=X[:, t, cs_], in0=tt_[:], in1=X[:, t, cs_], op=ALU.add),
                          reads=[b_tt], writes=[b_X[t]])
            wgup = Pool(kb, "wgu", [128, 8, 2 * D], BF16, 2)
            wdnp = Pool(kb, "wdn", [128, 8, D], BF16, 2)
            actp = Pool(kb, "actT", [128, 8, 512], BF16, 2)
            gp = Pool(kb, "moe_g", [128, 512], F32, 3)
            sp = Pool(kb, "moe_s", [128, 512], BF16, 3)
            up = Pool(kb, "moe_u", [128, 512], BF16, 3)
            bgu1 = kb.sb("bgu1", [128, NE, 8], F32)
            kb.op("vector", lambda e: e.tensor_scalar(out=bgu1[:], in0=bgu[:, :, 8:16], scalar1=1.0, scalar2=0.0, op0=ALU.add, op1=ALU.add),
                  reads=[b_bgu], writes=[b_bgu])
            NB = NTL * 128 // 512
            nexp = NE if self.stop != "moe1" else 1
            W = {}

            def load_w(ex_):
                wgu, b_wgu = wgup.get()
                wdn, b_wdn = wdnp.get()
                vg = self.w_gu[ex_].rearrange("(kc p) n -> p kc n", p=128)
                for kc in range(8):
                    kb.dma("gpsimd", wgu[:, kc, :], vg[:, kc, :], writes=[b_wgu])
                vd = self.w_dn[ex_].rearrange("(kc p) n -> p kc n", p=128)
                for kc in range(8):
                    kb.dma("gpsimd", wdn[:, kc, :], vd[:, kc, :], writes=[b_wdn])
                W[ex_] = (wgu, b_wgu, wdn, b_wdn)

            def fold(ex_):
                wgu, b_wgu, wdn, b_wdn = W[ex_]
                for q in range(2):
                    kb.op("vector", lambda e, q=q: e.tensor_tensor(out=wdn[:, q::2, :], in0=wdn[:, q::2, :],
                                                                  in1=self.gt2_bc[:].unsqueeze(1).to_broadcast([128, 4, D]), op=ALU.mult),
                          reads=[b_wdn, self.b_gt2], writes=[b_wdn])

            ACT = {}

            def gu(ex_, n):
                wgu, b_wgu, wdn, b_wdn = W[ex_]
                actT, b_act = actp.get()
                ACT[(ex_, n)] = (actT, b_act)
                toks = slice(n * 512, (n + 1) * 512)
                tl = [b_h2[4 * n + j] for j in range(4)]
                for j in range(8):
                    pg, b_pg = self.PS.get()
                    pu, b_pu = self.PS.get()
                    for kc in range(8):
                        kb.op("tensor", lambda e, kc=kc: e.matmul(pg[:], wgu[:, kc, j * 128:(j + 1) * 128], h2T[:, kc, toks],
                                                                  start=(kc == 0), stop=(kc == 7)), reads=[b_wgu] + tl, writes=[b_pg], sig=(kc == 7))
                    for kc in range(8):
                        kb.op("tensor", lambda e, kc=kc: e.matmul(pu[:], wgu[:, kc, D + j * 128:D + (j + 1) * 128], h2T[:, kc, toks],
                                                                  start=(kc == 0), stop=(kc == 7)), reads=[b_wgu] + tl, writes=[b_pu], sig=(kc == 7))
                    g, b_g = gp.get(); sg, b_sg = sp.get(); u, b_u = up.get()
                    kb.op("vector", lambda e: e.tensor_scalar(out=g[:], in0=pg[:], scalar1=bgu[:, ex_, j:j + 1], scalar2=7.0, op0=ALU.add, op1=ALU.min),
                          reads=[b_pg, b_bgu], writes=[b_g])
                    kb.op("scalar", lambda e: e.activation(out=sg[:], in_=g[:], func=AF.Silu, scale=1.702), reads=[b_g], writes=[b_sg])
                    kb.op("vector", lambda e: e.tensor_scalar(out=u[:], in0=pu[:], scalar1=bgu1[:, ex_, j:j + 1], scalar2=8.0, op0=ALU.add, op1=ALU.min),
                          reads=[b_pu, b_bgu], writes=[b_u])
                    kb.op("vector", lambda e: e.scalar_tensor_tensor(out=actT[:, j, :], in0=u[:], scalar=-6.0, in1=sg[:], op0=ALU.max, op1=ALU.mult),
                          reads=[b_u, b_sg], writes=[b_act])
                    if n == 0 and j == 3:
                        fold(ex_)

            def dn(ex_, n):
                wgu, b_wgu, wdn, b_wdn = W[ex_]
                actT, b_act = ACT.pop((ex_, n))
                for tq in range(4):
                    t = n * 4 + tq
                    for cb in range(2):
                        cs_ = slice(cb * 512, (cb + 1) * 512)
                        pd_, b_pd = self.PS.get()
                        for kc in range(8):
                            kb.op("tensor", lambda e, kc=kc: e.matmul(pd_[:], actT[:, kc, tq * 128:(tq + 1) * 128], wdn[:, kc, cs_],
                                                                      start=(kc == 0), stop=(kc == 7)), reads=[b_act, b_wdn], writes=[b_pd], sig=(kc == 7))
                        kb.op("vector", lambda e: e.scalar_tensor_tensor(out=X[:, t, cs_], in0=pd_[:], scalar=combs[:, t, ex_:ex_ + 1], in1=X[:, t, cs_],
                                                                        op0=ALU.mult, op1=ALU.add), reads=[b_pd, b_comb], writes=[b_X[t]])

            items = [(e_, n) for e_ in range(nexp) for n in range(NB)]
            load_w(0)
            if nexp > 1:
                load_w(1)
            gu(*items[0])
            for k, (e_, n) in enumerate(items):
                if k + 1 < len(items):
                    gu(*items[k + 1])
                dn(e_, n)
                if n == NB - 1 and e_ + 2 < nexp:
                    load_w(e_ + 2)
        if hp == 0:
            self.dump("x2", X[:, 0, :], b_X[0], [128, D])
        with kb.scope():
            nf = kb.sb("normf", [128, D], F32); b_nf = kb.buf("normf")
            kb.dma("sync", nf[:], self.norm_f.partition_broadcast(128), writes=[b_nf])
            junk = Pool(kb, "f_junk", [128, D], F32, 1)
            stat = Pool(kb, "f_stat", [128, 4], F32, 4)
            op_ = Pool(kb, "f_o", [128, D], F32, 2)
            for t in range(NTL):
                gt = hp * NTL + t
                jt, b_jt = junk.get(); st, b_st = stat.get()
                kb.op("scalar", lambda e, jt=jt, st=st, t=t: e.activation(out=jt[:], in_=X[:, t, :], func=AF.Square, accum_out=st[:, 0:1]), reads=[b_X[t]], writes=[b_jt, b_st])
                kb.op("scalar", lambda e, st=st: e.activation(out=st[:, 1:2], in_=st[:, 0:1], func=AF.Sqrt, bias=EPS, scale=1.0 / D), reads=[b_st], writes=[b_st])
                kb.op("vector", lambda e, st=st: e.reciprocal(out=st[:, 2:3], in_=st[:, 1:2]), reads=[b_st], writes=[b_st])
                ot, b_ot = op_.get()
                kb.op("vector", lambda e, ot=ot, st=st, t=t: e.scalar_tensor_tensor(out=ot[:], in0=X[:, t, :], scalar=st[:, 2:3], in1=nf[:], op0=ALU.mult, op1=ALU.mult),
                      reads=[b_X[t], b_st, b_nf], writes=[b_ot])
                bo = kb.buf("out%d" % gt)
                kb.dma("sync", self.out[gt * 128:(gt + 1) * 128, :], ot[:], reads=[b_ot], writes=[bo])
                self.fin.append(bo)

    def _norm_tile2(self, xt_ap, b_xt, tt, A, b_A, shift_chunk0, xnp, junk, stat, dstT, b_dst, hf, b_hf):
        kb = self.kb
        jt, b_jt = junk.get()
        st, b_st = stat.get()
        kb.op("scalar", lambda e: e.activation(out=jt[:], in_=xt_ap, func=AF.Square, accum_out=st[:, 0:1]), reads=[b_xt], writes=[b_jt, b_st])
        kb.op("scalar", lambda e: e.activation(out=st[:, 1:2], in_=st[:, 0:1], func=AF.Sqrt, bias=EPS, scale=1.0 / D), reads=[b_st], writes=[b_st])
        kb.op("vector", lambda e: e.reciprocal(out=st[:, 2:3], in_=st[:, 1:2]), reads=[b_st], writes=[b_st])
        xn, b_xn = xnp.get()
        kb.op("scalar", lambda e: e.activation(out=xn[:], in_=xt_ap, func=AF.Identity, scale=st[:, 2:3]), reads=[b_xt, b_st], writes=[b_xn])
        for half in range(2):
            pt, b_pt = self.PS.get()
            for q in range(4):
                kc = half * 4 + q
                kb.op("tensor", lambda e, kc=kc, q=q, pt=pt: e.transpose(pt[:, q * 128:(q + 1) * 128], xn[:, kc * 128:(kc + 1) * 128], self.identf[:]),
                      reads=[b_xn, self.b_ident], writes=[b_pt])
            for q in range(4):
                kc = half * 4 + q
                kb.op("vector", lambda e, kc=kc, q=q, pt=pt: e.tensor_scalar(
                    out=hf[:, kc, :], in0=pt[:, q * 128:(q + 1) * 128], scalar1=A[:, kc, 0:1], scalar2=self.modT[:, shift_chunk0 + kc, 0:1],
                    op0=ALU.mult, op1=ALU.add), reads=[b_pt, b_A, self.b_modT], writes=[b_hf])
        kb.op("scalar", lambda e: e.activation(out=dstT[:, :, tt * 128:(tt + 1) * 128], in_=hf[:], func=AF.Identity), reads=[b_hf], writes=[b_dst])

    def build(self):
        kb = self.kb
        self.declare()
        self.common()
        with kb.scope():
            self.hT = kb.sb("hT", [128, 8, TOK], BF16)
            self.b_hT = [kb.buf("hT%d" % t) for t in range(NT)]
            self.yaT = kb.sb("yaT", [128, 4, L], BF16)
            self.b_yaT = kb.buf("yaT")
            with kb.scope():
                self.defer_s5_body = True
                self.phase_s5_setup()
                with kb.scope():
                    self.phase_mod()
                    self._s5_setup_body()
                if self.stop in ("mod", "s5setup"):
                    return self.finish()
                with kb.scope():
                    self.phase_norm1()
                if self.stop == "norm1":
                    return self.finish()
                self.uT = kb.sb("uT", [128, 4, TOK], BF16)
                self.b_uT = kb.buf("uT")
                self.yT = kb.sb("yT", [128, 4, L], F32)
                self.b_yT = [kb.buf("yT%d" % q) for q in range(4)]
                with kb.scope():
                    self.phase_u()
                if self.stop == "u":
                    return self.finish()
                with kb.scope():
                    self.phase_s5()
                if self.stop == "s5":
                    return self.finish()
                with kb.scope():
                    self.phase_glu()
                if self.stop == "glu":
                    return self.finish()
            self.ybT = kb.sb("ybT", [128, 4, L], BF16)
            self.b_ybT = kb.buf("ybT")
            with kb.scope():
                self.phase_gdn_setup()
                if self.stop in ("gdnsetup",):
                    return self.finish()
                self.phase_gdn()
                if self.stop in ("gdn", "gdn_h0"):
                    return self.finish()
                with kb.scope():
                    self.phase_gdn_out()
                if self.stop == "gdnout":
                    return self.finish()
            with kb.scope():
                self.phase_merge()
            if self.stop == "merge":
                return self.finish()
        for hp in range(2):
            with kb.scope():
                self.phase_moe_half(hp)
            if self.stop in ("router", "moe1", "half"):
                return self.finish()
        return self.finish()

    def finish(self):
        self.kb.finish(self.fin)
        self.kb.finished = True
        for es in reversed(getattr(self.kb, "scopes", [])):
            es.close()
        self.kb.root.close()
        return self.nc


def _fm(v, nch):
    return np.ascontiguousarray(np.asarray(v, np.float32).reshape(nch, 128).T)


def host_inputs(inputs, b):
    f = lambda a: np.ascontiguousarray(np.asarray(a, np.float32))
    m = {}
    m["x"] = f(inputs["x"][b])
    m["ctx"] = f(inputs["ctx"][b])
    cs = np.stack([_fm(inputs["c"][b], 8), _fm(inputs["c_ctx"], 8)], axis=-1)
    m["cs"] = f(cs)
    m["w_mod"] = f(inputs["w_mod"][0])
    bm = np.asarray(inputs["b_mod"][0], np.float32)
    bmt = _fm(bm, 48)
    order = list(range(0, 16)) + list(range(24, 40)) + list(range(16, 24)) + list(range(40, 48))
    m["b_mod_t"] = f(bmt[:, order])
    m["b_mod"] = f(bm)
    m["norm1_t"] = _fm(inputs["norm1"][0], 8)
    m["norm2_t"] = _fm(inputs["norm2"][0], 8)
    m["w_in"] = f(inputs["w_in"][0])
    m["ident"] = np.eye(128, dtype=np.float32)
    m["s5_d_t"] = _fm(inputs["s5_d"][0], 4)
    def pdl(a):
        a = np.asarray(a, np.float32).reshape(2, 16, 2, 64)
        return f(a.transpose(2, 3, 0, 1).reshape(128, 32))
    m["lam_re_t"] = pdl(inputs["s5_lam_re"][0])
    m["lam_im_t"] = pdl(inputs["s5_lam_im"][0])
    m["logstep_t"] = pdl(np.broadcast_to(np.asarray(inputs["s5_log_step"][0], np.float32)[:, :, None], (2, 32, 64)))
    def blk(re, im, cn):
        out = np.zeros((2, 64, 2, 2, 16, 2, 16), np.float32)
        for ri, arr in enumerate((re, im)):
            arr = np.asarray(arr, np.float32)
            arr = arr if cn else arr.transpose(0, 1, 3, 2)
            arr = arr.reshape(2, 16, 2, 64, 16)
            for g2 in range(2):
                out[g2, :, ri, :, :, g2, :] = arr[:, :, g2].transpose(2, 0, 1, 3)
        return f(out.reshape(128, 2, 32, 32))
    m["Bblk"] = blk(inputs["s5_b_re"][0], inputs["s5_b_im"][0], True)
    m["Cblk"] = blk(inputs["s5_c_re"][0], inputs["s5_c_im"][0], False)
    r_ = np.arange(128)[:, None]; c_ = np.arange(128)[None, :]
    same = (r_ // 64) == (c_ // 64)
    NEG = -30000.0
    Mf = (same & (r_ <= c_)).astype(np.float32); Mb = (same & (r_ >= c_)).astype(np.float32)
    gmk = [Mf, Mb, np.where(same & (r_ > c_), 0.0, NEG), np.where(same & (r_ < c_), 0.0, NEG), np.where(same & (r_ <= c_), 0.0, NEG),
           np.tile((r_ < 64), (1, 128)).astype(np.float32), np.tile((r_ >= 64), (1, 128)).astype(np.float32), same.astype(np.float32),
           -Mf, -Mb, np.where(same & (r_ >= c_), 0.0, NEG)]
    m["gmask"] = f(np.stack([np.asarray(a, np.float32) for a in gmk], axis=1))
    cw = np.asarray(inputs["gdn_conv"][0], np.float32)
    m["conv_t"] = f(cw.reshape(5, 12, 128).transpose(2, 1, 0))
    m["alog_dtb"] = f(np.concatenate([np.asarray(inputs["gdn_a_log"][0]).reshape(8), np.asarray(inputs["gdn_dt_bias"][0]).reshape(8)]))
    m["gdn_norm"] = f(inputs["gdn_norm"][0])
    m["w_glu"] = f(inputs["s5_w_glu"][0])
    m["b_glu_t"] = _fm(inputs["s5_b_glu"][0], 4)
    m["w_ba"] = f(inputs["w_branch_a"][0])
    m["w_bb"] = f(inputs["w_branch_b"][0])
    m["w_out"] = f(inputs["w_out"][0])
    m["w_router"] = f(inputs["w_router"][0])
    m["b_router"] = f(inputs["b_router"][0])
    m["w_gu"] = f(inputs["w_gate_up"][0])
    bgu = np.asarray(inputs["b_gate_up"][0], np.float32)
    m["b_gu_t"] = f(bgu.reshape(NE, 16, 128).transpose(2, 0, 1))
    m["w_dn"] = f(inputs["w_down"][0])
    m["b_dn"] = f(inputs["b_down"][0])
    m["norm_f"] = f(inputs["norm_f"])
    m["iota1"] = f(np.tile(np.arange(1, TS5 + 1, dtype=np.float32)[None], (128, 1)))
    return m


_CACHE = {}


def kernel(**inputs):
    nb = 8
    if "nc" not in _CACHE:
        _CACHE["nc"] = Builder().build()
    nc = _CACHE["nc"]
    shared = host_inputs(inputs, 0)
    in_maps = []
    for b in range(nb):
        m = dict(shared)
        if b > 0:
            f = lambda a: np.ascontiguousarray(np.asarray(a, np.float32))
            m["x"] = f(inputs["x"][b])
            m["ctx"] = f(inputs["ctx"][b])
            m["cs"] = f(np.stack([_fm(inputs["c"][b], 8), _fm(inputs["c_ctx"], 8)], axis=-1))
        in_maps.append(m)
    res = run_bass_kernel_spmd(nc, in_maps, core_ids=list(range(nb)))
    out = np.stack([np.asarray(res.results[b]["out"], np.float32) for b in range(nb)], axis=0)
    return out
```

```python
import os
import numpy as np
from contextlib import ExitStack
import concourse.bass as bass
import concourse.mybir as mybir
from concourse.bass_utils import run_bass_kernel_spmd

F32 = mybir.dt.float32
BF16 = mybir.dt.bfloat16
I32 = mybir.dt.int32
AF = mybir.ActivationFunctionType
ALU = mybir.AluOpType
AX = mybir.AxisListType

D = 1024
L = 2048
CTX = 256
TOK = L + CTX
NT = TOK // 128
EPS = 1e-6
NE = 32
TS5 = 128
POST_ENG = os.environ.get('KPOST', 'gpsimd')
IN_COLS = 4624
OFF_U, OFF_QKV, OFF_GATE, OFF_B, OFF_A, OFF_BR = 0, 512, 2048, 2560, 2568, 2576
BLOCKS = [(0, 256)] + [(256 + 512 * i, 512) for i in range(4)]


class Buf:
    __slots__ = ("name", "w", "r")

    def __init__(self, name):
        self.name = name
        self.w = None
        self.r = {}


class KB:
    def __init__(self, nc, same_engine_sync=True):
        self.nc = nc
        self.es = ExitStack()
        self.root = self.es
        self.same = same_engine_sync
        self.engs = {}
        self.sems = {}
        for name in ("tensor", "vector", "scalar", "gpsimd", "sync"):
            eng = getattr(nc, name)
            sem = self.root.enter_context(nc.semaphore("s_" + name))
            self.sems["E" + name] = sem
            self.engs[name] = dict(eng=eng, key="E" + name, count=0, waited={})
        self.nbuf = 0
        self.dmasems = {}
        self.nalloc = 0

    def sb(self, name, shape, dt=F32):
        nb = int(np.prod(shape[1:])) * (2 if dt == BF16 else 4)
        self.nalloc += (nb + 31) // 32 * 32
        if os.environ.get("KALLOC"):
            print("alloc", name, shape, nb, "total", self.nalloc)
        self.nnames = getattr(self, "nnames", 0) + 1
        return self.es.enter_context(self.nc.sbuf_tensor("sb%d_%s" % (self.nnames, name), list(shape), dt))

    def ps(self, name, shape, dt=F32):
        return self.es.enter_context(self.nc.psum_tensor("pp_" + name, list(shape), dt))

    def buf(self, name=None):
        self.nbuf += 1
        return Buf((name or "b") + "_%d" % self.nbuf)

    def _wait(self, en, key, val):
        e = self.engs[en]
        if key == e["key"] and ((not self.same) or en == "tensor"):
            return
        if e["waited"].get(key, 0) >= val:
            return
        if key in self.dmasems:
            val = self.dmasems[key]
        e["eng"].wait_ge(self.sems[key], val)
        e["waited"][key] = val

    def _deps(self, en, reads, writes):
        need = {}
        for b in reads:
            if b.w is not None:
                k, v = b.w
                need[k] = max(need.get(k, 0), v)
        own = self.engs[en]["key"]
        relax = os.environ.get("KRELAX", "1") == "1"
        for b in writes:
            if b.w is not None:
                k, v = b.w
                if not (relax and k == own):
                    need[k] = max(need.get(k, 0), v)
            for k, v in b.r.items():
                if not (relax and k == own):
                    need[k] = max(need.get(k, 0), v)
        for k, v in need.items():
            self._wait(en, k, v)

    def _post(self, sig, reads, writes):
        k, v = sig
        for b in reads:
            b.r[k] = max(b.r.get(k, 0), v)
        for b in writes:
            b.w = sig
            b.r = {}

    def op(self, en, fn, reads=(), writes=(), sig=True):
        e = self.engs[en]
        self._deps(en, reads, writes)
        inst = fn(e["eng"])
        if sig:
            e["count"] += 1
            inst.then_inc(self.sems[e["key"]], 1)
            e["pending"] = False
            self._post((e["key"], e["count"]), reads, writes)
        else:
            assert en == "tensor"
            e["pending"] = True
            self._post((e["key"], e["count"] + 1), reads, writes)
        return inst

    NDSEM = 64

    def dma(self, en, out, in_, reads=(), writes=(), **kw):
        e = self.engs[en]
        self._deps(en, reads, writes)
        tgt = writes[0] if writes else reads[0]
        if not hasattr(self, "bufsem"):
            self.bufsem = {}
            self.dsem_list = []
        semkey = self.bufsem.get(tgt.name)
        if semkey is None:
            idx = len(self.bufsem) % self.NDSEM
            semkey = "DS%d" % idx
            self.bufsem[tgt.name] = semkey
            if semkey not in self.sems:
                self.sems[semkey] = self.root.enter_context(self.nc.semaphore("d%d" % idx))
                self.dmasems[semkey] = 0
        self.dmasems[semkey] += 16
        inst = e["eng"].dma_start(out=out, in_=in_, **kw)
        inst.then_inc(self.sems[semkey], 16)
        self._post((semkey, self.dmasems[semkey]), reads, writes)
        return inst

    def barrier(self):
        for en, e in self.engs.items():
            for en2, e2 in self.engs.items():
                if en2 != en and e2["count"] > 0:
                    self._wait(en, e2["key"], e2["count"])
            for k, v in self.dmasems.items():
                self._wait(en, k, v)

    def scope(self):
        kb = self

        class _S:
            def __enter__(s):
                s.old = kb.es
                s.base = kb.nalloc
                kb.es = ExitStack()
                kb.scopes = getattr(kb, "scopes", [])
                kb.scopes.append(kb.es)

            def __exit__(s, *a):
                if getattr(kb, "finished", False):
                    return
                kb.barrier()
                kb.es.close()
                kb.scopes.pop()
                kb.es = s.old
                kb.nalloc = s.base
        return _S()

    def finish(self, bufs):
        for b in bufs:
            if b.w is not None:
                self._wait("sync", b.w[0], b.w[1])
            for k, v in b.r.items():
                self._wait("sync", k, v)


class Pool:
    def __init__(self, kb, name, shape, dt, n, psum=False):
        self.tiles = []
        for i in range(n):
            t = (kb.ps if psum else kb.sb)("%s%d" % (name, i), shape, dt)
            self.tiles.append((t, kb.buf("%s%d" % (name, i))))
        self.i = 0

    def get(self):
        t = self.tiles[self.i % len(self.tiles)]
        self.i += 1
        return t


def _rev(ap2):
    return ap2[:, ::-1]


class Builder:
    def __init__(self, dbg=(), stop=None, same=True):
        self.dbg = set(dbg)
        self.stop = stop
        self.nc = bass.Bass("TRN2", target_bir_lowering=False)
        self.kb = KB(self.nc, same_engine_sync=same)
        self.ins = {}
        self.outs = {}
        self.fin = []

    def inp(self, name, shape):
        t = self.nc.dram_tensor(name, list(shape), F32, kind="ExternalInput").ap()
        self.ins[name] = t
        return t

    def outp(self, name, shape):
        t = self.nc.dram_tensor(name, list(shape), F32, kind="ExternalOutput").ap()
        self.outs[name] = t
        return t

    def dump(self, name, tile_ap, b, shape):
        if name not in self.dbg:
            return
        o = self.outp("dbg_" + name, shape)
        bo = self.kb.buf("dbg_" + name)
        self.kb.dma("sync", o, tile_ap, reads=[b], writes=[bo])
        self.fin.append(bo)

    def dump_bf(self, name, tile_ap, b, shape):
        if name not in self.dbg:
            return
        kb = self.kb
        t = kb.sb("dbgt_" + name, shape, F32)
        bt = kb.buf("dbgt_" + name)
        kb.op("vector", lambda e: e.tensor_copy(out=t[:], in_=tile_ap), reads=[b], writes=[bt])
        self.dump(name, t[:], bt, shape)

    def declare(self):
        i = self.inp
        self.x = i("x", [L, D])
        self.ctx = i("ctx", [CTX, D])
        self.cs_in = i("cs", [128, 8, 2])
        self.w_mod = i("w_mod", [D, 6 * D])
        self.b_mod_t = i("b_mod_t", [128, 48])
        self.b_mod = i("b_mod", [6 * D])
        self.norm1_t = i("norm1_t", [128, 8])
        self.norm2_t = i("norm2_t", [128, 8])
        self.w_in = i("w_in", [D, IN_COLS])
        self.ident = i("ident", [128, 128])
        self.s5_d_t = i("s5_d_t", [128, 4])
        self.lam_re_t = i("lam_re_t", [128, 32])
        self.lam_im_t = i("lam_im_t", [128, 32])
        self.logstep_t = i("logstep_t", [128, 32])
        self.Bblk = i("Bblk", [128, 2, 32, 32])
        self.Cblk = i("Cblk", [128, 2, 32, 32])
        self.iota1 = i("iota1", [128, TS5])
        self.gmask = i("gmask", [128, 11, 128])
        self.conv_t = i("conv_t", [128, 12, 5])
        self.alog_dtb = i("alog_dtb", [16])
        self.gdn_norm = i("gdn_norm", [128])
        self.w_glu = i("w_glu", [512, 512])
        self.b_glu_t = i("b_glu_t", [128, 4])
        self.w_ba = i("w_ba", [512, D])
        self.w_bb = i("w_bb", [512, D])
        self.w_out = i("w_out", [D, D])
        self.w_router = i("w_router", [D, NE])
        self.b_router = i("b_router", [NE])
        self.w_gu = i("w_gu", [NE, D, 2 * D])
        self.b_gu_t = i("b_gu_t", [128, NE, 16])
        self.w_dn = i("w_dn", [NE, D, D])
        self.b_dn = i("b_dn", [NE, D])
        self.norm_f = i("norm_f", [D])
        self.out = self.outp("out", [L, D])
        self.x1s = self.nc.dram_tensor("x1_scratch", [L, D], F32, kind="Internal").ap()

    def common(self):
        kb = self.kb
        self.PS = Pool(kb, "ps", [128, 512], F32, 8, psum=True)
        self.modT = kb.sb("modT", [128, 32, 2], F32)
        self.b_modT = kb.buf("modT")
        self.gt1_bc = kb.sb("gt1_bc", [128, D], F32)
        self.gt2_bc = kb.sb("gt2_bc", [128, D], F32)
        self.b_gt1 = kb.buf("gt1")
        self.b_gt2 = kb.buf("gt2")
        self.A1 = kb.sb("A1", [128, 8, 2], F32)
        self.b_A1 = kb.buf("A1")
        self.identf = kb.sb("identf", [128, 128], F32)
        self.b_ident = kb.buf("ident")
        kb.dma("sync", self.identf[:], self.ident, writes=[self.b_ident])

    def phase_mod(self):
        kb, nc = self.kb, self.nc
        cs = kb.sb("cs", [128, 8, 2], F32)
        b_cs = kb.buf("cs")
        kb.dma("sync", cs[:], self.cs_in, writes=[b_cs])
        sg = kb.sb("cs_sg", [128, 8, 2], F32)
        b_sg = kb.buf("cs_sg")
        kb.op("scalar", lambda e: e.activation(out=sg[:], in_=cs[:], func=AF.Sigmoid), reads=[b_cs], writes=[b_sg])
        css = kb.sb("css", [128, 8, 2], F32)
        b_css = kb.buf("css")
        kb.op("vector", lambda e: e.tensor_tensor(out=css[:], in0=cs[:], in1=sg[:], op=ALU.mult), reads=[b_cs, b_sg], writes=[b_css])
        csb = kb.sb("csb", [128, 8, 128], F32)
        b_csb = kb.buf("csb")
        kb.op("vector", lambda e: e.tensor_copy(out=csb[:], in_=css[:, :, 0:1].to_broadcast([128, 8, 128])), reads=[b_css], writes=[b_csb])
        bmt = kb.sb("bmt", [128, 48], F32)
        b_bmt = kb.buf("bmt")
        kb.dma("sync", bmt[:], self.b_mod_t, writes=[b_bmt])
        wpool = Pool(kb, "wmod", [128, 8, 512], F32, 2)
        wv = self.w_mod.rearrange("(kc p) n -> p kc n", p=128)
        pm, b_pm = self.PS.get()
        fm_groups = [0, 1, 2, 3, 6, 7, 8, 9]
        for gi, g in enumerate(fm_groups):
            wt, b_wt = wpool.get()
            kb.dma("sync" if gi % 2 == 0 else "scalar", wt[:], wv[:, :, 512 * g:512 * (g + 1)], writes=[b_wt])
            for cc in range(4):
                j = gi * 4 + cc
                for kc in range(8):
                    kb.op("tensor", lambda e, j=j, kc=kc, cc=cc, wt=wt: e.matmul(
                        pm[:, 2 * j:2 * j + 2], wt[:, kc, cc * 128:(cc + 1) * 128], css[:, kc, :],
                        start=(kc == 0), stop=(kc == 7)), reads=[b_wt, b_css], writes=[b_pm], sig=(kc == 7))
        kb.op("vector", lambda e: e.tensor_tensor(
            out=self.modT[:], in0=pm[:, 0:64].rearrange("p (j t) -> p j t", t=2),
            in1=self._bmt_sel(bmt), op=ALU.add), reads=[b_pm, b_bmt], writes=[self.b_modT])
        for which, (g0, dst, bdst) in enumerate([(4, self.gt1_bc, self.b_gt1), (10, self.gt2_bc, self.b_gt2)]):
            bb = kb.sb("bmodbc%d" % which, [128, D], F32)
            b_bb = kb.buf("bmodbc")
            kb.dma("sync", bb[:], self.b_mod[512 * g0:512 * g0 + D].partition_broadcast(128), writes=[b_bb])
            for half in range(2):
                g = g0 + half
                wt, b_wt = wpool.get()
                kb.dma("sync" if half == 0 else "scalar", wt[:], wv[:, :, 512 * g:512 * (g + 1)], writes=[b_wt])
                pg, b_pg = self.PS.get()
                for kc in range(8):
                    kb.op("tensor", lambda e, kc=kc, wt=wt, pg=pg: e.matmul(
                        pg[:], csb[:, kc, :], wt[:, kc, :], start=(kc == 0), stop=(kc == 7)),
                        reads=[b_wt, b_csb], writes=[b_pg], sig=(kc == 7))
                kb.op("vector", lambda e, pg=pg, half=half, dst=dst, bb=bb: e.tensor_tensor(
                    out=dst[:, 512 * half:512 * (half + 1)], in0=pg[:], in1=bb[:, 512 * half:512 * (half + 1)], op=ALU.add),
                    reads=[b_pg, b_bb], writes=[bdst])
        self.dump("modT", self.modT[:], self.b_modT, [128, 32, 2])
        self.dump("gt1", self.gt1_bc[:], self.b_gt1, [128, D])

    def _bmt_sel(self, bmt):
        return bmt[:, 0:32].unsqueeze(2).to_broadcast([128, 32, 2])

    def norm_to_T(self, src_tiles, dstT, b_dstT_blocks, scale_ap_fn, shift_ap_fn, tile_base, tag):
        raise NotImplementedError

    def phase_norm1(self):
        kb = self.kb
        n1 = kb.sb("norm1", [128, 8], F32)
        b_n1 = kb.buf("n1")
        kb.dma("sync", n1[:], self.norm1_t, writes=[b_n1])
        kb.op("vector", lambda e: e.scalar_tensor_tensor(
            out=self.A1[:], in0=self.modT[:, 8:16, :], scalar=1.0, in1=n1[:].unsqueeze(2).to_broadcast([128, 8, 2]),
            op0=ALU.add, op1=ALU.mult), reads=[self.b_modT, b_n1], writes=[self.b_A1])
        xin = Pool(kb, "xin", [128, D], F32, 3)
        xnp = Pool(kb, "xn", [128, D], F32, 2)
        junk = Pool(kb, "junk", [128, D], F32, 1)
        stat = Pool(kb, "stat", [128, 4], F32, 4)
        for tt in range(NT):
            which = 1 if tt < 2 else 0
            src = self.ctx[tt * 128:(tt + 1) * 128, :] if tt < 2 else self.x[(tt - 2) * 128:(tt - 1) * 128, :]
            xt, b_xt = xin.get()
            kb.dma("sync" if tt % 2 == 0 else "scalar", xt[:], src, writes=[b_xt])
            self._norm_tile(xt, b_xt, tt, which, self.A1, self.b_A1, 0, xnp, junk, stat, self.hT, self.b_hT[tt])
        self.dump_bf("hT", self.hT[:, :, 0:512], self.b_hT[3], [128, 8, 512]) if False else None
        if "hT" in self.dbg:
            allb = kb.buf("hTall")
            t = kb.sb("dbg_hT_t", [128, 8, 384], F32)
            kb.op("vector", lambda e: e.tensor_copy(out=t[:], in_=self.hT[:, :, 128:512]), reads=self.b_hT[1:4], writes=[allb])
            self.dump("hT", t[:], allb, [128, 8, 384])

    def _norm_tile(self, xt, b_xt, tt, which, A, b_A, shift_chunk0, xnp, junk, stat, dstT, b_dst):
        kb = self.kb
        jt, b_jt = junk.get()
        st, b_st = stat.get()
        kb.op("scalar", lambda e: e.activation(out=jt[:], in_=xt[:], func=AF.Square, accum_out=st[:, 0:1]),
              reads=[b_xt], writes=[b_jt, b_st])
        kb.op("scalar", lambda e: e.activation(out=st[:, 1:2], in_=st[:, 0:1], func=AF.Sqrt, bias=EPS, scale=1.0 / D),
              reads=[b_st], writes=[b_st])
        kb.op("vector", lambda e: e.reciprocal(out=st[:, 2:3], in_=st[:, 1:2]), reads=[b_st], writes=[b_st])
        xn, b_xn = xnp.get()
        kb.op("scalar", lambda e: e.activation(out=xn[:], in_=xt[:], func=AF.Identity, scale=st[:, 2:3]),
              reads=[b_xt, b_st], writes=[b_xn])
        for half in range(2):
            pt, b_pt = self.PS.get()
            for q in range(4):
                kc = half * 4 + q
                kb.op("tensor", lambda e, kc=kc, q=q, pt=pt: e.transpose(
                    pt[:, q * 128:(q + 1) * 128], xn[:, kc * 128:(kc + 1) * 128], self.identf[:]),
                    reads=[b_xn, self.b_ident], writes=[b_pt])
            for q in range(4):
                kc = half * 4 + q
                kb.op("vector", lambda e, kc=kc, q=q, pt=pt: e.tensor_scalar(
                    out=dstT[:, kc, tt * 128:(tt + 1) * 128], in0=pt[:, q * 128:(q + 1) * 128],
                    scalar1=A[:, kc, which:which + 1], scalar2=self.modT[:, shift_chunk0 + kc, which:which + 1],
                    op0=ALU.mult, op1=ALU.add), reads=[b_pt, b_A, self.b_modT], writes=[b_dst])

    def load_w_bf16(self, dst, b_dst, src_rows_ap, ncols, col0=0):
        kb = self.kb
        kcs = dst.shape[1]
        v = src_rows_ap.rearrange("(kc p) n -> p kc n", p=128)
        for kc in range(kcs):
            kb.dma("gpsimd", dst[:, kc, :], v[:, kc, col0:col0 + ncols], writes=[b_dst])

    def phase_u(self):
        kb = self.kb
        wu = kb.sb("wu", [128, 8, 512], BF16)
        b_wu = kb.buf("wu")
        self.load_w_bf16(wu, b_wu, self.w_in, 512, OFF_U)
        dsk = kb.sb("s5d", [128, 4], F32)
        b_dsk = kb.buf("s5d")
        kb.dma("sync", dsk[:], self.s5_d_t, writes=[b_dsk])
        for oc in range(4):
            for (s0, n) in BLOCKS:
                pt, b_pt = self.PS.get()
                tiles = range(s0 // 128, (s0 + n) // 128)
                for kc in range(8):
                    kb.op("tensor", lambda e, kc=kc, pt=pt, oc=oc, s0=s0, n=n: e.matmul(
                        pt[:, 0:n], wu[:, kc, oc * 128:(oc + 1) * 128], self.hT[:, kc, s0:s0 + n],
                        start=(kc == 0), stop=(kc == 7)), reads=[b_wu] + [self.b_hT[t] for t in tiles], writes=[b_pt], sig=(kc == 7))
                kb.op("scalar", lambda e, pt=pt, oc=oc, s0=s0, n=n: e.activation(
                    out=self.uT[:, oc, s0:s0 + n], in_=pt[:, 0:n], func=AF.Identity), reads=[b_pt], writes=[self.b_uT])
                if s0 >= CTX:
                    kb.op("scalar", lambda e, pt=pt, oc=oc, s0=s0, n=n: e.activation(
                        out=self.yT[:, oc, s0 - CTX:s0 - CTX + n], in_=pt[:, 0:n], func=AF.Identity, scale=dsk[:, oc:oc + 1]),
                        reads=[b_pt, b_dsk], writes=[self.b_yT[oc]])
        if "uT" in self.dbg:
            t = kb.sb("dbg_uT_t", [128, 4, 512], F32)
            bt = kb.buf("dbg_uT")
            kb.op("vector", lambda e: e.tensor_copy(out=t[:], in_=self.uT[:, :, 0:512]), reads=[self.b_uT], writes=[bt])
            self.dump("uT", t[:], bt, [128, 4, 512])


    def sincos(self, ang, b_ang, n, want_cos, out_ap, b_out, scale=1.0):
        kb = self.kb
        with kb.scope():
            y = kb.sb("sc_y", [128, n], F32)
            ki = kb.sb("sc_k", [128, n], I32)
            kf = kb.sb("sc_kf", [128, n], F32)
            b = kb.buf("sc")
            off = 0.75 if want_cos else 0.5
            kb.op("vector", lambda e: e.tensor_scalar(out=y[:], in0=ang, scalar1=1.0 / (2 * np.pi), scalar2=off + 64.0,
                                                     op0=ALU.mult, op1=ALU.add), reads=[b_ang], writes=[b])
            kb.op("vector", lambda e: e.tensor_copy(out=ki[:], in_=y[:]), reads=[b], writes=[b])
            kb.op("vector", lambda e: e.tensor_copy(out=kf[:], in_=ki[:]), reads=[b], writes=[b])
            kb.op("vector", lambda e: e.tensor_tensor(out=y[:], in0=y[:], in1=kf[:], op=ALU.subtract), reads=[b], writes=[b])
            kb.op("vector", lambda e: e.scalar_tensor_tensor(out=kf[:], in0=y[:], scalar=0.0, in1=y[:], op0=ALU.is_lt, op1=ALU.add),
                  reads=[b], writes=[b])
            kb.op("scalar", lambda e: e.activation(out=y[:], in_=kf[:], func=AF.Sin, scale=6.2831, bias=-3.14155),
                  reads=[b], writes=[b])
            yv = y[:] if len(out_ap.shape) == 2 else y[:].rearrange("p (a t) -> p a t", t=out_ap.shape[-1])
            kb.op("scalar", lambda e: e.activation(out=out_ap, in_=yv, func=AF.Identity, scale=float(scale)),
                  reads=[b], writes=[b_out])

    def phase_s5_setup(self):
        kb = self.kb
        T = TS5
        self.rho = kb.sb("s5_rho", [128, 32], F32)
        self.Bdrv = kb.sb("Bdrv", [128, 2, 4, 2, 128], BF16)
        self.b_Bdrv = kb.buf("Bdrv")
        self.Crd = kb.sb("Crd", [128, 32, 2, 32], BF16)
        self.b_Crd = kb.buf("Crd")
        self.Tc = kb.sb("Tc", [128, 32 * T], BF16)
        self.b_Tc = kb.buf("Tc")
        self.Tsn = kb.sb("Tsn", [128, 32, 2, T], BF16)
        self.b_Tsn = kb.buf("Tsn")
        self.b_s5c = kb.buf("s5setup")
        if getattr(self, "defer_s5_body", False):
            return
        with kb.scope():
            self._s5_setup_body()

    def _s5_setup_body(self):
        kb = self.kb
        T = TS5
        ld = lambda name, src, shape: self._ld(name, src, shape)
        lre, b_lre = ld("lre", self.lam_re_t, [128, 32])
        lim, b_lim = ld("lim", self.lam_im_t, [128, 32])
        lst, b_lst = ld("lst", self.logstep_t, [128, 32])
        Bb, b_Bb = ld("Bblk", self.Bblk, [128, 2, 32, 32])
        Cb, b_Cb = ld("Cblk", self.Cblk, [128, 2, 32, 32])
        io, b_io = ld("iota1", self.iota1, [128, T])
        V = lambda name: (kb.sb("s5v_" + name, [128, 32], F32))
        b = self.b_s5c
        deps = [b_lre, b_lim, b_lst, b]
        mag = self.rho
        lr, dt, ang, ar, ai, den, am1, fr, fi, t1, t2 = [V(n) for n in
            ("lr", "dt", "ang", "ar", "ai", "den", "am1", "fr", "fi", "t1", "t2")]
        v = lambda fn: kb.op("vector", fn, reads=deps, writes=[b])
        a = lambda fn: kb.op("scalar", fn, reads=deps, writes=[b])
        v(lambda e: e.tensor_scalar(out=lr[:], in0=lre[:], scalar1=-1e-4, scalar2=0.0, op0=ALU.min, op1=ALU.add))
        a(lambda e: e.activation(out=dt[:], in_=lst[:], func=AF.Exp))
        v(lambda e: e.tensor_tensor(out=t1[:], in0=lr[:], in1=dt[:], op=ALU.mult))
        a(lambda e: e.activation(out=mag[:], in_=t1[:], func=AF.Exp))
        v(lambda e: e.tensor_tensor(out=ang[:], in0=lim[:], in1=dt[:], op=ALU.mult))
        sn = V("sn0"); cs = V("cs0")
        self.sincos(ang[:], b, 32, False, sn[:], b)
        self.sincos(ang[:], b, 32, True, cs[:], b)
        v(lambda e: e.tensor_tensor(out=ar[:], in0=mag[:], in1=cs[:], op=ALU.mult))
        v(lambda e: e.tensor_tensor(out=ai[:], in0=mag[:], in1=sn[:], op=ALU.mult))
        v(lambda e: e.tensor_tensor(out=t1[:], in0=lr[:], in1=lr[:], op=ALU.mult))
        v(lambda e: e.tensor_tensor(out=t2[:], in0=lim[:], in1=lim[:], op=ALU.mult))
        v(lambda e: e.tensor_tensor(out=den[:], in0=t1[:], in1=t2[:], op=ALU.add))
        v(lambda e: e.reciprocal(out=den[:], in_=den[:]))
        v(lambda e: e.tensor_scalar(out=am1[:], in0=ar[:], scalar1=-1.0, scalar2=0.0, op0=ALU.add, op1=ALU.add))
        v(lambda e: e.tensor_tensor(out=t1[:], in0=am1[:], in1=lr[:], op=ALU.mult))
        v(lambda e: e.tensor_tensor(out=t2[:], in0=ai[:], in1=lim[:], op=ALU.mult))
        v(lambda e: e.tensor_tensor(out=fr[:], in0=t1[:], in1=t2[:], op=ALU.add))
        v(lambda e: e.tensor_tensor(out=fr[:], in0=fr[:], in1=den[:], op=ALU.mult))
        v(lambda e: e.tensor_tensor(out=t1[:], in0=ai[:], in1=lr[:], op=ALU.mult))
        v(lambda e: e.tensor_tensor(out=t2[:], in0=am1[:], in1=lim[:], op=ALU.mult))
        v(lambda e: e.tensor_tensor(out=fi[:], in0=t1[:], in1=t2[:], op=ALU.subtract))
        v(lambda e: e.tensor_tensor(out=fi[:], in0=fi[:], in1=den[:], op=ALU.mult))
        Bbar = kb.sb("Bbar", [128, 2, 32, 32], F32)
        tb = kb.sb("Bbar_t", [128, 32, 32], F32)
        b_Bbar = kb.buf("Bbar")
        frb = fr[:].unsqueeze(2).to_broadcast([128, 32, 32])
        fib = fi[:].unsqueeze(2).to_broadcast([128, 32, 32])
        vb = lambda fn: kb.op("vector", fn, reads=[b, b_Bb], writes=[b_Bbar])
        vb(lambda e: e.tensor_tensor(out=Bbar[:, 0], in0=Bb[:, 0], in1=frb, op=ALU.mult))
        vb(lambda e: e.tensor_tensor(out=tb[:], in0=Bb[:, 1], in1=fib, op=ALU.mult))
        vb(lambda e: e.tensor_tensor(out=Bbar[:, 0], in0=Bbar[:, 0], in1=tb[:], op=ALU.subtract))
        vb(lambda e: e.tensor_tensor(out=Bbar[:, 1], in0=Bb[:, 1], in1=frb, op=ALU.mult))
        vb(lambda e: e.tensor_tensor(out=tb[:], in0=Bb[:, 0], in1=fib, op=ALU.mult))
        vb(lambda e: e.tensor_tensor(out=Bbar[:, 1], in0=Bbar[:, 1], in1=tb[:], op=ALU.add))
        for d in range(2):
            for qd in range(4):
                pt, b_pt = self.PS.get()
                for ri in range(2):
                    blk = (d * 4 + qd) * 4
                    kb.op("tensor", lambda e, ri=ri, blk=blk, pt=pt: e.transpose(
                        pt[:, ri * 128:(ri + 1) * 128], Bbar[:, ri, blk:blk + 4, :], self.identf[:]),
                        reads=[b_Bbar, self.b_ident], writes=[b_pt])
                kb.op("scalar", lambda e, d=d, qd=qd, pt=pt: e.activation(
                    out=self.Bdrv[:, d, qd, :, :], in_=pt[:, 0:256].rearrange("p (r n) -> p r n", r=2), func=AF.Identity),
                    reads=[b_pt], writes=[self.b_Bdrv])
        kb.op("scalar", lambda e: e.activation(out=self.Crd[:, :, 0, :], in_=Cb[:, 0], func=AF.Identity),
              reads=[b_Cb], writes=[self.b_Crd])
        kb.op("scalar", lambda e: e.activation(out=self.Crd[:, :, 1, :], in_=Cb[:, 1], func=AF.Identity, scale=-1.0),
              reads=[b_Cb], writes=[self.b_Crd])
        ph = kb.sb("s5_ph", [128, 32, T], F32)
        b_ph = kb.buf("s5ph")
        kb.op("vector", lambda e: e.tensor_tensor(out=ph[:], in0=ang[:].unsqueeze(2).to_broadcast([128, 32, T]),
                                                 in1=io[:].unsqueeze(1).to_broadcast([128, 32, T]), op=ALU.mult),
              reads=[b, b_io], writes=[b_ph])
        for a0 in range(0, 32, 8):
            pha = ph[:, a0:a0 + 8, :].rearrange("p a t -> p (a t)")
            self.sincos(pha, b_ph, 8 * T, True, self.Tc[:, a0 * T:(a0 + 8) * T], self.b_Tc)
            self.sincos(pha, b_ph, 8 * T, False, self.Tsn[:, a0:a0 + 8, 0, :], self.b_Tsn)
            self.sincos(pha, b_ph, 8 * T, False, self.Tsn[:, a0:a0 + 8, 1, :], self.b_Tsn, scale=-1.0)
        self.dump("rho", self.rho[:], b, [128, 32])
        self.dump("fr", fr[:], b, [128, 32])
        self.dump("fi", fi[:], b, [128, 32])
        self.dump("Tc", self.Tc[:, 0:4 * T], self.b_Tc, [128, 4 * T])

    def _ld(self, name, src, shape, dt=F32, q="sync"):
        t = self.kb.sb(name, shape, dt)
        b = self.kb.buf(name)
        self.kb.dma(q, t[:], src, writes=[b])
        return t, b

    def phase_s5(self):
        kb = self.kb
        T = TS5
        NCK = TOK // T
        Tc3 = self.Tc[:].rearrange("p (a t) -> p a t", t=T)
        G = kb.sb("s5_G", [128, 32, 2, T], BF16 if os.environ.get("KG16", "0") == "1" else F32)
        b_G = [kb.buf("s5G%d" % i) for i in range(32)]
        carry = kb.sb("s5_carry", [128, 32, 2], F32)
        b_carry0 = kb.buf("s5carry")
        kb.op("vector", lambda e: e.memset(carry[:], 0.0), writes=[b_carry0])
        b_carryg = {}
        P1p = Pool(kb, "s5P1", [128, 2, T], F32, 4)
        P2p = Pool(kb, "s5P2", [128, 2, T], F32, 4)
        Vp = Pool(kb, "s5V", [128, 2, T], F32, 8)
        Q1p = Pool(kb, "s5Q1", [128, 2, T], BF16 if os.environ.get("KG16", "0") == "1" else F32, 4)
        Q2p = Pool(kb, "s5Q2", [128, 2, T], BF16 if os.environ.get("KG16", "0") == "1" else F32, 4)
        Hp = Pool(kb, "s5H", [128, 2, T], BF16, 8)
        ctmp = kb.sb("s5_ctmp", [128, 32, 2], F32)
        ctmp2 = kb.sb("s5_ctmp2", [128, 32, 2], F32)
        tabs = [self.b_Tc, self.b_Tsn]
        drv_tiles = self.PS.tiles[0:5]
        py_tiles = self.PS.tiles[5:8]
        cnt = {"drv": 0, "py": 0}
        pending = []

        def flush(keep):
            while len(pending) > keep:
                (src, qd_, l0_, b_py_) = pending.pop(0)
                kb.op("vector", lambda e, src=src, qd_=qd_, l0_=l0_: e.tensor_tensor(
                    out=self.yT[:, qd_, l0_:l0_ + T], in0=src, in1=self.yT[:, qd_, l0_:l0_ + T], op=ALU.add),
                    reads=[b_py_], writes=[self.b_yT[qd_]])
        for ck in range(NCK):
            is_lat = ck >= CTX // T
            for qd in range(4):
                for d in range(2):
                    if d == 0:
                        s0 = ck * T
                    elif not is_lat:
                        s0 = CTX - (ck + 1) * T
                    else:
                        s0 = TOK - (ck - CTX // T + 1) * T
                    if is_lat:
                        py, b_py = py_tiles[cnt["py"] % 3]
                        cnt["py"] += 1
                    gkey = (qd, d)
                    if gkey not in b_carryg:
                        b_carryg[gkey] = kb.buf("s5carry%d%d" % gkey)
                        b_carryg[gkey].w = b_carry0.w
                    b_carry = b_carryg[gkey]
                    st = []
                    for ppq in range(4):
                        pd = d * 16 + qd * 4 + ppq
                        rhs = self.uT[ppq * 32:(ppq + 1) * 32, qd, s0:s0 + T]
                        if d == 1:
                            rhs = rhs[:, ::-1]
                        pdv, b_pdv = drv_tiles[cnt["drv"] % 5]
                        cnt["drv"] += 1
                        for ri in range(2):
                            kb.op("tensor", lambda e, ri=ri, pdv=pdv, rhs=rhs, ppq=ppq: e.matmul(
                                pdv[:, ri * T:(ri + 1) * T], self.Bdrv[ppq * 32:(ppq + 1) * 32, d, qd, ri, :], rhs,
                                start=True, stop=True, tile_position=(ppq * 32, 0)),
                                reads=[self.b_Bdrv, self.b_uT], writes=[b_pdv], sig=(ri == 1))
                        Dv = pdv[:, 0:2 * T].rearrange("p (r t) -> p r t", r=2)
                        cosb = Tc3[:, pd:pd + 1, :].to_broadcast([128, 2, T])
                        st.append(dict(pd=pd, ppq=ppq, Dv=Dv, b_pdv=b_pdv, cosb=cosb, P1=P1p.get(), P2=P2p.get(), V=Vp.get()))
                    for x in st:
                        kb.op("vector", lambda e, x=x: e.tensor_tensor(out=x["P1"][0][:], in0=x["Dv"], in1=x["cosb"], op=ALU.mult),
                              reads=[x["b_pdv"]] + tabs, writes=[x["P1"][1]])
                    for x in st:
                        kb.op("vector", lambda e, x=x: e.tensor_tensor(out=x["P2"][0][:], in0=x["Dv"][:, ::-1, :], in1=self.Tsn[:, x["pd"]], op=ALU.mult),
                              reads=[x["b_pdv"]] + tabs, writes=[x["P2"][1]])
                    for x in st:
                        kb.op("vector", lambda e, x=x: e.tensor_tensor(out=x["V"][0][:], in0=x["P1"][0][:], in1=x["P2"][0][:], op=ALU.add),
                              reads=[x["P1"][1], x["P2"][1]], writes=[x["V"][1]])
                    for ri in range(2):
                        for x in st:
                            pd = x["pd"]
                            kb.op("vector", lambda e, ri=ri, x=x, pd=pd: e.tensor_tensor_scan(
                                out=G[:, pd, ri, :], data0=self.rho[:, pd:pd + 1].to_broadcast([128, T]), data1=x["V"][0][:, ri, :],
                                initial=carry[:, pd, ri:ri + 1], op0=ALU.mult, op1=ALU.add),
                                reads=[x["V"][1], b_carry, self.b_s5c], writes=[b_G[pd]])
                    if ck < NCK - 1:
                        pd0 = d * 16 + qd * 4
                        Gl = G[:, pd0:pd0 + 4, :, T - 1]
                        cl = Tc3[:, pd0:pd0 + 4, T - 1:T].to_broadcast([128, 4, 2])
                        sl = self.Tsn[:, pd0:pd0 + 4, :, T - 1]
                        gb_ = [b_G[pd0 + q] for q in range(4)]
                        c1 = ctmp[:, pd0:pd0 + 4, :]
                        c2 = ctmp2[:, pd0:pd0 + 4, :]
                        kb.op("vector", lambda e, Gl=Gl, cl=cl, c1=c1: e.tensor_tensor(out=c1, in0=Gl, in1=cl, op=ALU.mult),
                              reads=gb_ + tabs, writes=[b_carry])
                        kb.op("vector", lambda e, Gl=Gl, sl=sl, c2=c2: e.tensor_tensor(out=c2, in0=Gl[:, :, ::-1], in1=sl, op=ALU.mult),
                              reads=gb_ + tabs, writes=[b_carry])
                        kb.op("vector", lambda e, c1=c1, c2=c2, pd0=pd0: e.tensor_tensor(out=carry[:, pd0:pd0 + 4, :], in0=c1, in1=c2, op=ALU.subtract),
                              reads=[b_carry], writes=[b_carry])
                    if is_lat:
                        for x in st:
                            x["Q1"] = Q1p.get(); x["Q2"] = Q2p.get(); x["H"] = Hp.get()
                        for x in st:
                            kb.op(POST_ENG, lambda e, x=x: e.tensor_tensor(out=x["Q1"][0][:], in0=G[:, x["pd"]], in1=x["cosb"], op=ALU.mult),
                                  reads=[b_G[x["pd"]]] + tabs, writes=[x["Q1"][1]])
                        for x in st:
                            kb.op(POST_ENG, lambda e, x=x: e.tensor_tensor(out=x["Q2"][0][:], in0=G[:, x["pd"], ::-1, :], in1=self.Tsn[:, x["pd"]], op=ALU.mult),
                                  reads=[b_G[x["pd"]]] + tabs, writes=[x["Q2"][1]])
                        for x in st:
                            kb.op(POST_ENG, lambda e, x=x: e.tensor_tensor(out=x["H"][0][:], in0=x["Q1"][0][:], in1=x["Q2"][0][:], op=ALU.subtract),
                                  reads=[x["Q1"][1], x["Q2"][1]], writes=[x["H"][1]])
                        for x in st:
                            for ri in range(2):
                                kb.op("tensor", lambda e, ri=ri, x=x: e.matmul(
                                    py[x["ppq"] * 32:(x["ppq"] + 1) * 32, 0:T], self.Crd[:, x["pd"], ri, :], x["H"][0][:, ri, :],
                                    start=(ri == 0), stop=(ri == 1), tile_position=(0, x["ppq"] * 32)),
                                    reads=[self.b_Crd, x["H"][1]], writes=[b_py], sig=(ri == 1))
                        l0 = s0 - CTX
                        src = py[:, 0:T] if d == 0 else py[:, 0:T][:, ::-1]
                        pending.append((src, qd, l0, b_py))
                        flush(2)
        flush(0)
        if "yT" in self.dbg:
            allb = kb.buf("yTall")
            t = kb.sb("dbg_yT_t", [128, 4, 512], F32)
            kb.op("vector", lambda e: e.tensor_copy(out=t[:], in_=self.yT[:, :, 0:512]), reads=self.b_yT, writes=[allb])
            self.dump("yT", t[:], allb, [128, 4, 512])
            t2 = kb.sb("dbg_yT_t2", [128, 4, 512], F32)
            allb2 = kb.buf("yTall2")
            kb.op("vector", lambda e: e.tensor_copy(out=t2[:], in_=self.yT[:, :, 1536:2048]), reads=self.b_yT, writes=[allb2])
            self.dbg.add("yT2")
            self.dump("yT2", t2[:], allb2, [128, 4, 512])


    def phase_gdn_setup(self):
        kb = self.kb
        self.gm, self.b_gm = self._ld("gmask", self.gmask, [128, 11, 128])
        self.cw, self.b_cw = self._ld("convw", self.conv_t, [128, 12, 5])
        self.beta = kb.sb("g_beta", [128, NT, 8], F32)
        self.gg = kb.sb("g_g", [128, NT, 8], F32)
        self.E1 = kb.sb("g_E1", [128, NT, 8], F32)
        self.E2 = kb.sb("g_E2", [128, NT, 8], F32)
        self.DL = kb.sb("g_DL", [128, 2, NT, 8], F32)
        self.b_gs = kb.buf("gscal")
        b = self.b_gs
        with kb.scope():
            wab = kb.sb("wab", [128, 8, 16], BF16)
            b_wab = kb.buf("wab")
            self.load_w_bf16(wab, b_wab, self.w_in, 16, OFF_B)
            ab = kb.sb("alogdtb", [128, 16], F32)
            b_ab = kb.buf("alogdtb")
            kb.dma("sync", ab[:], self.alog_dtb.partition_broadcast(128), writes=[b_ab])
            pz, b_pz = self.PS.get()
            for tt in range(NT):
                for kc in range(8):
                    kb.op("tensor", lambda e, tt=tt, kc=kc: e.matmul(
                        pz[:, tt * 16:(tt + 1) * 16], self.hT[:, kc, tt * 128:(tt + 1) * 128], wab[:, kc, :],
                        start=(kc == 0), stop=(kc == 7)), reads=[b_wab, self.b_hT[tt]], writes=[b_pz], sig=(kc == 7))
            zab = pz[:, 0:NT * 16].rearrange("p (t c) -> p t c", c=16)
            kb.op("scalar", lambda e: e.activation(out=self.beta[:], in_=zab[:, :, 0:8], func=AF.Sigmoid), reads=[b_pz], writes=[b])
            if self.stop == "gs1":
                return
            t1 = kb.sb("g_t1", [128, NT, 8], F32)
            ea = kb.sb("g_ea", [128, 8], F32)
            kb.op("vector", lambda e: e.tensor_tensor(out=t1[:], in0=zab[:, :, 8:16], in1=ab[:, 8:16].unsqueeze(1).to_broadcast([128, NT, 8]),
                                                     op=ALU.add), reads=[b_pz, b_ab], writes=[b])
            kb.op("scalar", lambda e: e.activation(out=t1[:], in_=t1[:], func=AF.Exp), reads=[b], writes=[b])
            kb.op("scalar", lambda e: e.activation(out=t1[:], in_=t1[:], func=AF.Ln, bias=1.0), reads=[b], writes=[b])
            kb.op("scalar", lambda e: e.activation(out=ea[:], in_=ab[:, 0:8], func=AF.Exp), reads=[b_ab], writes=[b])
            kb.op("vector", lambda e: e.scalar_tensor_tensor(out=self.gg[:], in0=t1[:], scalar=-1.0,
                                                            in1=ea[:].unsqueeze(1).to_broadcast([128, NT, 8]), op0=ALU.mult, op1=ALU.mult),
                  reads=[b], writes=[b])
            if self.stop == "gs2":
                return
            gflat = self.gg[:].rearrange("p t x -> p (t x)")
            gc = kb.sb("g_gcum", [128, 2, NT, 8], F32)
            for d in range(2):
                pc, b_pc = self.PS.get()
                kb.op("tensor", lambda e, d=d, pc=pc: e.matmul(pc[:, 0:NT * 8], self.gm[:, d, :], gflat, start=True, stop=True),
                      reads=[b, self.b_gm], writes=[b_pc])
                kb.op("vector", lambda e, d=d, pc=pc: e.tensor_copy(out=gc[:, d].rearrange("p t x -> p (t x)"), in_=pc[:, 0:NT * 8]),
                      reads=[b_pc], writes=[b])
            if self.stop == "gs3":
                return
            pl, b_pl = self.PS.get()
            kb.op("tensor", lambda e: e.matmul(pl[:, 0:NT * 8], self.gm[:, 7, :], gflat, start=True, stop=True),
                  reads=[b, self.b_gm], writes=[b_pl])
            glv = pl[:, 0:NT * 8].rearrange("p (t x) -> p t x", x=8)
            for d in range(2):
                xs = slice(d * 4, d * 4 + 4)
                kb.op("scalar", lambda e, d=d, xs=xs: e.activation(out=self.E1[:, :, xs], in_=gc[:, d, :, xs], func=AF.Exp),
                      reads=[b], writes=[b])
                kb.op("vector", lambda e, d=d, xs=xs: e.tensor_tensor(out=t1[:, :, xs], in0=glv[:, :, xs], in1=gc[:, d, :, xs], op=ALU.subtract),
                      reads=[b, b_pl], writes=[b])
                kb.op("scalar", lambda e, xs=xs: e.activation(out=self.E2[:, :, xs], in_=t1[:, :, xs], func=AF.Exp), reads=[b], writes=[b])
            if self.stop == "gs4":
                return
            for c2 in ([1] if self.stop == "gs7" else range(2)):
                ph, b_ph = self.PS.get()
                kb.op("tensor", lambda e, c2=c2, ph=ph: e.matmul(ph[:, 0:NT * 8], self.gm[:, 5 + c2, :], gflat, start=True, stop=True),
                      reads=[b, self.b_gm], writes=[b_ph])
                if self.stop == "gs5":
                    return
                kb.op("scalar", lambda e, c2=c2, ph=ph: e.activation(out=self.DL[:, c2].rearrange("p t x -> p (t x)"), in_=ph[:, 0:NT * 8],
                                                                      func=AF.Exp), reads=[b_ph], writes=[b])
                if self.stop == "gs6":
                    return
        self.dump("g_g", self.gg[:], b, [128, NT, 8])
        self.dump("g_E1", self.E1[:], b, [128, NT, 8])
        self.dump("g_E2", self.E2[:], b, [128, NT, 8])
        self.dump("g_DL", self.DL[:], b, [128, 2, NT, 8])

    def gdn_head_front(self, h):
        kb = self.kb
        self.qh = kb.sb("g_qh%d" % h, [128, NT, 128], F32)
        self.kh = kb.sb("g_kh%d" % h, [128, NT, 128], F32)
        self.vh = kb.sb("g_vh%d" % h, [128, NT, 128], F32)
        GB = BF16 if os.environ.get("KGBF", "0") == "1" else F32
        self.kT = kb.sb("g_kT%d" % h, [128, TOK], GB)
        self.qT = kb.sb("g_qT%d" % h, [128, TOK], GB)
        self.b_qh, self.b_kh, self.b_vh, self.b_kT, self.b_qT = [kb.buf("g_%s%d" % (n, h)) for n in ("qh", "kh", "vh", "kT", "qT")]
        with kb.scope():
            wq = kb.sb("g_wqkv", [128, 8, 3, 128], BF16)
            b_wq = kb.buf("g_wqkv")
            v = self.w_in.rearrange("(kc p) n -> p kc n", p=128)
            for c in range(3):
                for kc in range(8):
                    col = OFF_QKV + c * 512 + h * 128
                    kb.dma("gpsimd", wq[:, kc, c, :], v[:, kc, col:col + 128], writes=[b_wq])
            zb = kb.sb("g_z", [128, TOK], F32)
            acc = kb.sb("g_acc", [128, TOK], F32)
            b_z = kb.buf("g_z")
            b_acc = kb.buf("g_acc")
            PADC = CTX + 4
            zp = kb.sb("g_zp", [128, PADC + 32 * 68], BF16)
            b_zp = kb.buf("g_zp")
            kb.op("vector", lambda e: e.memset(zp[:], 0.0), writes=[b_zp])
            zp_lat = zp[:, PADC:PADC + 32 * 68].rearrange("p (r w) -> p r w", w=68)
            dgp = Pool(kb, "g_dg", [128, 5, 128], BF16, 2)
            identb = kb.sb("g_identb", [128, 128], BF16)
            b_idb = kb.buf("g_identb")
            kb.op("vector", lambda e: e.tensor_copy(out=identb[:], in_=self.identf[:]), reads=[self.b_ident], writes=[b_idb])
            dsts = [(self.qh, self.b_qh), (self.kh, self.b_kh), (self.vh, self.b_vh)]
            for c in range(3):
                ch = c * 4 + h
                dg, b_dg = dgp.get()
                for k in range(5):
                    kb.op("vector", lambda e, k=k, dg=dg, ch=ch: e.tensor_scalar(out=dg[:, k, :], in0=identb[:], scalar1=self.cw[:, ch, k:k + 1], scalar2=0.0,
                                                                            op0=ALU.mult, op1=ALU.add), reads=[b_idb, self.b_cw], writes=[b_dg])
                for (s0, n) in BLOCKS:
                    pt, b_pt = self.PS.get()
                    tiles = range(s0 // 128, (s0 + n) // 128)
                    for kc in range(8):
                        kb.op("tensor", lambda e, kc=kc, pt=pt, c=c, s0=s0, n=n: e.matmul(
                            pt[:, 0:n], wq[:, kc, c, :], self.hT[:, kc, s0:s0 + n], start=(kc == 0), stop=(kc == 7)),
                            reads=[b_wq] + [self.b_hT[t] for t in tiles], writes=[b_pt], sig=(kc == 7))
                    if s0 < CTX:
                        kb.op("scalar", lambda e, pt=pt, n=n: e.activation(out=zp[:, 2:2 + CTX], in_=pt[:, 0:n], func=AF.Identity),
                              reads=[b_pt], writes=[b_zp])
                    else:
                        r0 = (s0 - CTX) // 64
                        kb.op("scalar", lambda e, pt=pt, n=n, r0=r0: e.activation(out=zp_lat[:, r0:r0 + 8, 2:66], in_=pt[:, 0:n].rearrange("p (r w) -> p r w", w=64),
                                                                                 func=AF.Identity), reads=[b_pt], writes=[b_zp])
                for (s0, n) in BLOCKS:
                    pc, b_pc = self.PS.get()
                    for k in range(5):
                        if s0 < CTX:
                            mv = zp[:, k:k + CTX]
                        else:
                            r0 = (s0 - CTX) // 64
                            mv = zp_lat[:, r0:r0 + 8, k:k + 64]
                        kb.op("tensor", lambda e, k=k, pc=pc, mv=mv, n=n, dg=dg: e.matmul(pc[:, 0:n], dg[:, k, :], mv, start=(k == 0), stop=(k == 4)),
                              reads=[b_dg, b_zp], writes=[b_pc], sig=(k == 4))
                    kb.op("scalar", lambda e, pc=pc, s0=s0, n=n: e.activation(out=zb[:, s0:s0 + n], in_=pc[:, 0:n], func=AF.Silu), reads=[b_pc], writes=[b_z])
                if c == 0 and h == 0:
                    self.dump("g_cq", zb[:, 0:512], b_z, [128, 512])
                dst, b_dst = dsts[c]
                for t0 in range(0, NT, 4):
                    nt = min(4, NT - t0)
                    pt, b_pt = self.PS.get()
                    for q in range(nt):
                        kb.op("tensor", lambda e, q=q, t0=t0, pt=pt: e.transpose(
                            pt[:, q * 128:(q + 1) * 128], zb[:, (t0 + q) * 128:(t0 + q + 1) * 128], self.identf[:]),
                            reads=[b_z, self.b_ident], writes=[b_pt])
                    kb.op("scalar" if (t0 // 4) % 2 == 0 else "vector", lambda e, t0=t0, nt=nt, pt=pt, dst=dst: (
                        e.activation(out=dst[:, t0:t0 + nt, :], in_=pt[:, 0:nt * 128].rearrange("p (t c) -> p t c", c=128), func=AF.Identity)
                        if e is self.nc.scalar else
                        e.tensor_copy(out=dst[:, t0:t0 + nt, :], in_=pt[:, 0:nt * 128].rearrange("p (t c) -> p t c", c=128))),
                        reads=[b_pt], writes=[b_dst])
            sq = acc[:, 0:NT * 128].rearrange("p (t c) -> p t c", c=128)
            ss = kb.sb("g_ss", [128, 2, NT], F32)
            b_ss = kb.buf("g_ss")
            for qi, (src, b_src) in enumerate([(self.qh, self.b_qh), (self.kh, self.b_kh)]):
                kb.op("vector", lambda e, src=src: e.tensor_tensor(out=sq, in0=src[:], in1=src[:], op=ALU.mult), reads=[b_src], writes=[b_acc])
                kb.op("vector", lambda e, qi=qi: e.tensor_reduce(out=ss[:, qi, :], in_=sq, axis=AX.X, op=ALU.add), reads=[b_acc], writes=[b_ss])
            kb.op("scalar", lambda e: e.activation(out=ss[:], in_=ss[:], func=AF.Sqrt, bias=EPS, scale=1.0), reads=[b_ss], writes=[b_ss])
            kb.op("vector", lambda e: e.reciprocal(out=ss[:], in_=ss[:]), reads=[b_ss], writes=[b_ss])
            kb.op("vector", lambda e: e.tensor_scalar(out=ss[:, 0, :], in0=ss[:, 0, :], scalar1=float(128 ** -0.5), scalar2=0.0,
                                                     op0=ALU.mult, op1=ALU.add), reads=[b_ss], writes=[b_ss])
            for qi, (src, b_src) in enumerate([(self.qh, self.b_qh), (self.kh, self.b_kh)]):
                kb.op("vector", lambda e, src=src, qi=qi: e.tensor_tensor(
                    out=src[:], in0=src[:], in1=ss[:, qi, :].unsqueeze(2).to_broadcast([128, NT, 128]), op=ALU.mult),
                    reads=[b_src, b_ss], writes=[b_src])
            for (src, b_src, dstT, b_dT) in [(self.kh, self.b_kh, self.kT, self.b_kT), (self.qh, self.b_qh, self.qT, self.b_qT)]:
                for t0 in range(0, NT, 4):
                    nt = min(4, NT - t0)
                    pt, b_pt = self.PS.get()
                    for q in range(nt):
                        kb.op("tensor", lambda e, q=q, t0=t0, pt=pt, src=src: e.transpose(
                            pt[:, q * 128:(q + 1) * 128], src[:, t0 + q, :], self.identf[:]), reads=[b_src, self.b_ident], writes=[b_pt])
                    kb.op("scalar", lambda e, t0=t0, nt=nt, pt=pt, dstT=dstT: e.activation(
                        out=dstT[:, t0 * 128:(t0 + nt) * 128], in_=pt[:, 0:nt * 128], func=AF.Identity), reads=[b_pt], writes=[b_dT])
        if h == 0:
            self.dump("g_qh", self.qh[:, 0:4, :], self.b_qh, [128, 4, 128])
            self.dump("g_kh", self.kh[:, 0:4, :], self.b_kh, [128, 4, 128])
            self.dump("g_vh", self.vh[:, 0:4, :], self.b_vh, [128, 4, 128])


    def gdn_head_core(self, h):
        kb = self.kb
        gm = self.gm
        BFN = ("kw", "vb", "kbT", "A", "N", "X0", "X1", "P0", "P1", "Q0", "Q1")
        T_ = lambda name, n=1: Pool(kb, "gc_%s_%d" % (name, h), [128, 128],
                                    BF16 if (os.environ.get("KGBF", "0") == "1" and name[:-1] in BFN) else F32, n)
        R = 4
        rings = {n: [T_("%s%d" % (n, d), R) for d in range(2)] for n in ("wT", "ub", "qkT", "qdT", "kd")}
        tmp = {n: [T_("%s%d" % (n, d), 1) for d in range(2)] for n in
               ("kbt", "kw", "vb", "qd", "kbT", "Dls", "DTs", "DTi", "A", "N", "X0", "X1", "P0", "P1", "Q0", "Q1")}
        vnp = [T_("vn%d" % d, 2) for d in range(2)]
        S = [kb.sb("g_S%d_%d" % (h, d), [128, 128], F32) for d in range(2)]
        b_S = [kb.buf("g_S%d" % d) for d in range(2)]
        for d in range(2):
            kb.op("vector", lambda e, d=d: e.memset(S[d][:], 0.0), writes=[b_S[d]])
        order = {0: list(range(NT)), 1: [1, 0] + list(range(NT - 1, 1, -1))}
        ringent = {}
        gs = self.b_gs
        LS, US, UI, LI = 2, 3, 4, 10

        kb.barrier()
        pst = [t for (t, _) in self.PS.tiles]

        class Reg:
            def __init__(r, ap, b_):
                r.ap = ap
                r.b = b_
        regs = {}
        pb = [b_ for (_, b_) in self.PS.tiles]
        for d in range(2):
            regs[("pD", d)] = Reg(pst[4 * d][:, 0:256], pb[4 * d])
            regs[("pt", d)] = Reg(pst[4 * d + 1][:, 0:256], pb[4 * d + 1])
            regs[("pK", d)] = Reg(pst[4 * d + 2][:, 0:384], pb[4 * d + 2])
            regs[("pw", d)] = Reg(pst[4 * d + 3][:, 0:128], pb[4 * d + 3])
            regs[("po", d)] = Reg(pst[4 * d + 3][:, 128:256], pb[4 * d + 3])
            regs[("ps", d)] = Reg(pst[4 * d + 3][:, 256:384], pb[4 * d + 3])
        R32 = (lambda ap: ap)
        prog = {"prep": [0, 0], "chain": [0, 0]}

        def prep_gen(d):
            for i in range(NT):
                while i - prog["chain"][d] >= R - 1:
                    yield
                tt = order[d][i]
                x = d * 4 + h
                ts_ = slice(tt * 128, (tt + 1) * 128)
                bsc = self.beta[:, tt, x:x + 1]
                e1 = self.E1[:, tt, x:x + 1]
                e2 = self.E2[:, tt, x:x + 1]
                g = lambda n: tmp[n][d].get()
                kbt, b_kbt = g("kbt"); kw, b_kw = g("kw"); vb, b_vb = g("vb"); qd, b_qd = g("qd"); kbT, b_kbT = g("kbT")
                kd, b_kd = rings["kd"][d].get(); qdT, b_qdT = rings["qdT"][d].get()
                ts1 = lambda e, o, i_, sc: e.tensor_scalar(out=R32(o[:]), in0=i_, scalar1=sc, scalar2=0.0, op0=ALU.mult, op1=ALU.add)
                kb.op("vector", lambda e: ts1(e, kbt, self.kh[:, tt, :], bsc), reads=[self.b_kh, gs], writes=[b_kbt])
                kb.op("vector", lambda e: ts1(e, qd, self.qh[:, tt, :], e1), reads=[self.b_qh, gs], writes=[b_qd])
                gb = self.gg[:, tt, x:x + 1].to_broadcast([128, 128])
                M, nM = gm[:, d, :], gm[:, 8 + d, :]
                pD, b_pD = regs[("pD", d)].ap, regs[("pD", d)].b
                kb.op("tensor", lambda e: e.matmul(pD[:, 0:128], M, gb, start=True, stop=False), reads=[gs, self.b_gm], writes=[b_pD], sig=False)
                kb.op("tensor", lambda e: e.matmul(pD[:, 0:128], gb, nM, start=False, stop=True), reads=[gs, self.b_gm], writes=[b_pD], sig=False)
                kb.op("tensor", lambda e: e.matmul(pD[:, 128:256], nM, gb, start=True, stop=False), reads=[gs, self.b_gm], writes=[b_pD], sig=False)
                kb.op("tensor", lambda e: e.matmul(pD[:, 128:256], gb, M, start=False, stop=True), reads=[gs, self.b_gm], writes=[b_pD])
                yield
                kb.op("vector", lambda e: ts1(e, kw, kbt[:], e1), reads=[b_kbt, gs], writes=[b_kw])
                kb.op("vector", lambda e: ts1(e, vb, self.vh[:, tt, :], bsc), reads=[self.b_vh, gs], writes=[b_vb])
                kb.op("vector", lambda e: ts1(e, kd, self.kh[:, tt, :], e2), reads=[self.b_kh, gs], writes=[b_kd])
                pt, b_pt = regs[("pt", d)].ap, regs[("pt", d)].b
                kb.op("tensor", lambda e: e.transpose(pt[:, 0:128], kbt[:], self.identf[:]), reads=[b_kbt, self.b_ident], writes=[b_pt], sig=False)
                kb.op("tensor", lambda e: e.transpose(pt[:, 128:256], qd[:], self.identf[:]), reads=[b_qd, self.b_ident], writes=[b_pt])
                yield
                kb.op("scalar", lambda e: e.activation(out=R32(kbT[:]), in_=pt[:, 0:128], func=AF.Identity), reads=[b_pt], writes=[b_kbT])
                kb.op("scalar", lambda e: e.activation(out=qdT[:], in_=pt[:, 128:256], func=AF.Identity), reads=[b_pt], writes=[b_qdT])
                Dls, b_Dls = g("Dls"); DTs, b_DTs = g("DTs"); DTi, b_DTi = g("DTi")
                mD, mDTs, mDTi = (LS, US, UI) if d == 0 else (US, LS, LI)
                trip = [(Dls, b_Dls, pD[:, 0:128], mD), (DTs, b_DTs, pD[:, 128:256], mDTs), (DTi, b_DTi, pD[:, 128:256], mDTi)]
                for (dst, b_dst, src, mk) in trip:
                    kb.op("vector", lambda e, dst=dst, src=src, mk=mk: e.scalar_tensor_tensor(
                        out=dst[:], in0=src, scalar=0.0, in1=gm[:, mk, :], op0=ALU.min, op1=ALU.add), reads=[b_pD, self.b_gm], writes=[b_dst])
                yield
                for (dst, b_dst, src, mk) in trip:
                    kb.op("scalar", lambda e, dst=dst: e.activation(out=dst[:], in_=dst[:], func=AF.Exp), reads=[b_dst], writes=[b_dst])
                pK, b_pK = regs[("pK", d)].ap, regs[("pK", d)].b
                kTt, qTt = self.kT[:, ts_], self.qT[:, ts_]
                kb.op("tensor", lambda e: e.matmul(pK[:, 0:128], R32(kbT[:]), R32(kTt), start=True, stop=True), reads=[b_kbT, self.b_kT], writes=[b_pK], sig=False)
                kb.op("tensor", lambda e: e.matmul(pK[:, 128:256], R32(kTt), R32(kbT[:]), start=True, stop=True), reads=[b_kbT, self.b_kT], writes=[b_pK], sig=False)
                kb.op("tensor", lambda e: e.matmul(pK[:, 256:384], R32(kTt), R32(qTt), start=True, stop=True), reads=[self.b_qT, self.b_kT], writes=[b_pK])
                yield
                A, b_A = g("A"); N, b_N = g("N"); qkT, b_qkT = rings["qkT"][d].get()
                kb.op("vector", lambda e: e.tensor_tensor(out=R32(A[:]), in0=pK[:, 0:128], in1=Dls[:], op=ALU.mult), reads=[b_pK, b_Dls], writes=[b_A])
                kb.op("vector", lambda e: e.tensor_tensor(out=R32(N[:]), in0=pK[:, 128:256], in1=DTs[:], op=ALU.mult), reads=[b_pK, b_DTs], writes=[b_N])
                kb.op("vector", lambda e: e.tensor_tensor(out=qkT[:], in0=pK[:, 256:384], in1=DTi[:], op=ALU.mult), reads=[b_pK, b_DTi], writes=[b_qkT])
                X, b_X = g("X0")
                yield
                kb.op("vector", lambda e: e.tensor_tensor(out=R32(X[:]), in0=self.identf[:], in1=N[:], op=ALU.subtract), reads=[b_N, self.b_ident], writes=[b_X])
                P, b_P, PT, b_PT = N, b_N, A, b_A
                for sidx in range(1, 6):
                    pp, b_pp = regs[("pK", d)].ap, regs[("pK", d)].b
                    if sidx < 5:
                        kb.op("tensor", lambda e: e.matmul(pp[:, 0:128], R32(PT[:]), R32(P[:]), start=True, stop=True), reads=[b_P, b_PT], writes=[b_pp], sig=False)
                    kb.op("tensor", lambda e: e.matmul(pp[:, 128:256], R32(P[:]), R32(PT[:]), start=True, stop=True), reads=[b_P, b_PT], writes=[b_pp])
                    yield
                    nP, b_nP = tmp["P%d" % (sidx % 2)][d].get()
                    nPT, b_nPT = tmp["Q%d" % (sidx % 2)][d].get()
                    kb.op("scalar", lambda e: e.activation(out=R32(nPT[:]), in_=pp[:, 128:256], func=AF.Identity), reads=[b_pp], writes=[b_nPT])
                    if sidx < 5:
                        kb.op("scalar", lambda e: e.activation(out=R32(nP[:]), in_=pp[:, 0:128], func=AF.Identity), reads=[b_pp], writes=[b_nP])
                    yield
                    kb.op("tensor", lambda e: e.matmul(pp[:, 256:384], R32(nPT[:]), R32(X[:]), start=True, stop=True), reads=[b_nPT, b_X], writes=[b_pp])
                    yield
                    nX, b_nX = tmp["X%d" % (sidx % 2)][d].get()
                    kb.op("vector", lambda e: e.tensor_tensor(out=R32(nX[:]), in0=pp[:, 256:384], in1=X[:], op=ALU.add), reads=[b_pp, b_X], writes=[b_nX])
                    P, b_P, PT, b_PT, X, b_X = nP, b_nP, nPT, b_nPT, nX, b_nX
                    yield
                pu, b_pu = regs[("pK", d)].ap, regs[("pK", d)].b
                kb.op("tensor", lambda e: e.matmul(pu[:, 0:128], R32(X[:]), R32(vb[:]), start=True, stop=True), reads=[b_X, b_vb], writes=[b_pu], sig=False)
                kb.op("tensor", lambda e: e.matmul(pu[:, 128:256], R32(kw[:]), R32(X[:]), start=True, stop=True), reads=[b_X, b_kw], writes=[b_pu])
                yield
                ub, b_ub = rings["ub"][d].get(); wT, b_wT = rings["wT"][d].get()
                kb.op("scalar", lambda e: e.activation(out=ub[:], in_=pu[:, 0:128], func=AF.Identity), reads=[b_pu], writes=[b_ub])
                kb.op("scalar", lambda e: e.activation(out=wT[:], in_=pu[:, 128:256], func=AF.Identity), reads=[b_pu], writes=[b_wT])
                ringent[(d, i)] = dict(d=d, tt=tt, x=x, kd=(kd, b_kd), qdT=(qdT, b_qdT), qkT=(qkT, b_qkT), ub=(ub, b_ub), wT=(wT, b_wT))
                prog["prep"][d] = i + 1
                yield

        def chain_gen(d):
            for i in range(NT):
                while (d, i) not in ringent:
                    yield
                en = ringent[(d, i)]
                tt, x = en["tt"], en["x"]
                wT, b_wT = en["wT"]; ub, b_ub = en["ub"]; qdT, b_qdT = en["qdT"]; qkT, b_qkT = en["qkT"]; kd, b_kd = en["kd"]
                for hi in range(2):
                    c2 = hi if d == 0 else 1 - hi
                    r = slice(c2 * 64, c2 * 64 + 64)
                    pw, b_pw = regs[("pw", d)].ap, regs[("pw", d)].b
                    kb.op("tensor", lambda e: e.matmul(pw[r, :], wT[:, r], S[d][:], start=True, stop=True), reads=[b_wT, b_S[d]], writes=[b_pw])
                    if tt >= 2:
                        po, b_po = regs[("po", d)].ap, regs[("po", d)].b
                        kb.op("tensor", lambda e: e.matmul(po[r, :], qdT[:, r], S[d][:], start=True, stop=False),
                              reads=[b_qdT, b_S[d]], writes=[b_po], sig=False)
                    yield
                    vn, b_vn = vnp[d].get()
                    kb.op("vector", lambda e: e.tensor_tensor(out=vn[r, :], in0=ub[r, :], in1=pw[r, :], op=ALU.subtract),
                          reads=[b_ub, b_pw], writes=[b_vn])
                    yield
                    ps_, b_ps = regs[("ps", d)].ap, regs[("ps", d)].b
                    if tt >= 2:
                        kb.op("tensor", lambda e: e.matmul(po[r, :], qkT[r, r], vn[r, :], start=False, stop=True),
                              reads=[b_qkT, b_vn], writes=[b_po])
                    kb.op("tensor", lambda e: e.matmul(ps_[:, :], kd[r, :], vn[r, :], start=True, stop=True), reads=[b_kd, b_vn], writes=[b_ps])
                    yield
                    kb.op("vector", lambda e: e.scalar_tensor_tensor(out=S[d][:], in0=S[d][:], scalar=self.DL[:, c2, tt, x:x + 1], in1=ps_[:, :],
                                                                    op0=ALU.mult, op1=ALU.add), reads=[b_ps, b_S[d], gs], writes=[b_S[d]])
                    if tt >= 2:
                        od = self.osum[r, tt - 2, h * 128:(h + 1) * 128]
                        kb.op("gpsimd" if False else "vector", lambda e: e.tensor_tensor(out=od, in0=po[r, :], in1=od, op=ALU.add),
                              reads=[b_po], writes=[self.b_osum[tt - 2]])
                    yield
                prog["chain"][d] = i + 1

        gens = [prep_gen(0), prep_gen(1), chain_gen(0), chain_gen(1)]
        while gens:
            for g_ in list(gens):
                try:
                    next(g_)
                except StopIteration:
                    gens.remove(g_)

    def phase_gdn(self):
        kb = self.kb
        self.osum = kb.sb("g_osum", [128, 16, 512], F32)
        self.b_osum = [kb.buf("g_osum%d" % t) for t in range(16)]
        for t in range(16):
            kb.op("vector", lambda e, t=t: e.memset(self.osum[:, t, :], 0.0), writes=[self.b_osum[t]])
        for h in range(4):
            with kb.scope():
                self.gdn_head_front(h)
                self.gdn_head_core(h)
            if self.stop == "gdn_h0":
                break
        if "g_osum" in self.dbg:
            self.dump("g_osum", self.osum[:, 0:4, 0:128], self.b_osum[3], [128, 4, 128])
        if "g_osum2" in self.dbg:
            self.dump("g_osum2", self.osum[:, 12:16, 0:128], self.b_osum[15], [128, 4, 128])


    def phase_glu(self):
        kb = self.kb
        wg = kb.sb("wglu", [128, 4, 512], BF16)
        b_wg = kb.buf("wglu")
        self.load_w_bf16(wg, b_wg, self.w_glu, 512, 0)
        bg, b_bg = self._ld("bglu", self.b_glu_t, [128, 4])
        zT = kb.sb("glu_z", [128, 4, L], BF16)
        b_zT = [kb.buf("glu_z%d" % q) for q in range(4)]
        t1 = kb.sb("glu_t1", [128, L], F32)
        t2 = kb.sb("glu_t2", [128, L], F32)
        b_t = kb.buf("glu_t")
        for q in range(4):
            y = self.yT[:, q, :]
            kb.op("vector", lambda e, y=y: e.tensor_tensor(out=t1[:], in0=y, in1=y, op=ALU.mult), reads=[self.b_yT[q]], writes=[b_t])
            kb.op("vector", lambda e: e.tensor_scalar(out=t1[:], in0=t1[:], scalar1=0.044715, scalar2=1.0, op0=ALU.mult, op1=ALU.add), reads=[b_t], writes=[b_t])
            kb.op("vector", lambda e, y=y: e.tensor_tensor(out=t1[:], in0=t1[:], in1=y, op=ALU.mult), reads=[b_t, self.b_yT[q]], writes=[b_t])
            kb.op("scalar", lambda e: e.activation(out=t2[:], in_=t1[:], func=AF.Sigmoid, scale=1.5957691216057308), reads=[b_t], writes=[b_t])
            kb.op("vector", lambda e, y=y, q=q: e.tensor_tensor(out=zT[:, q, :], in0=t2[:], in1=y, op=ALU.mult), reads=[b_t, self.b_yT[q]], writes=[b_zT[q]])
        glp = Pool(kb, "glu_g", [128, 512], F32, 2)
        for oc in range(4):
            for n in range(4):
                pt, b_pt = self.PS.get()
                for kc in range(4):
                    kb.op("tensor", lambda e, kc=kc, pt=pt, oc=oc, n=n: e.matmul(
                        pt[:], wg[:, kc, oc * 128:(oc + 1) * 128], zT[:, kc, n * 512:(n + 1) * 512], start=(kc == 0), stop=(kc == 3)),
                        reads=[b_wg] + b_zT, writes=[b_pt], sig=(kc == 3))
                gl, b_gl = glp.get()
                kb.op("scalar", lambda e, pt=pt, gl=gl, oc=oc: e.activation(out=gl[:], in_=pt[:], func=AF.Sigmoid, bias=bg[:, oc:oc + 1]),
                      reads=[b_pt, b_bg], writes=[b_gl])
                kb.op("vector", lambda e, gl=gl, oc=oc, n=n: e.tensor_tensor(
                    out=self.yaT[:, oc, n * 512:(n + 1) * 512], in0=zT[:, oc, n * 512:(n + 1) * 512], in1=gl[:], op=ALU.mult),
                    reads=[b_gl, b_zT[oc]], writes=[self.b_yaT])
        if "yaT" in self.dbg:
            t = kb.sb("dbg_yaT_t", [128, 4, 512], F32)
            bt = kb.buf("dbg_yaT")
            kb.op("vector", lambda e: e.tensor_copy(out=t[:], in_=self.yaT[:, :, 0:512]), reads=[self.b_yaT], writes=[bt])
            self.dump("yaT", t[:], bt, [128, 4, 512])

    def phase_gdn_out(self):
        kb = self.kb
        wgt = kb.sb("wgate", [128, 8, 512], BF16)
        b_wgt = kb.buf("wgate")
        self.load_w_bf16(wgt, b_wgt, self.w_in, 512, OFF_GATE)
        nw = kb.sb("gnw", [128, 128], F32)
        b_nw = kb.buf("gnw")
        kb.dma("sync", nw[:], self.gdn_norm.partition_broadcast(128), writes=[b_nw])
        ss = kb.sb("go_ss", [128, 16, 4], F32)
        b_ss = kb.buf("go_ss")
        sqp = Pool(kb, "go_sq", [128, 512], F32, 2)
        for t in range(16):
            sq, b_sq = sqp.get()
            kb.op("vector", lambda e, t=t, sq=sq: e.tensor_tensor(out=sq[:], in0=self.osum[:, t, :], in1=self.osum[:, t, :], op=ALU.mult),
                  reads=[self.b_osum[t]], writes=[b_sq])
            kb.op("vector", lambda e, t=t, sq=sq: e.tensor_reduce(out=ss[:, t, :], in_=sq[:].rearrange("p (h c) -> p h c", c=128), axis=AX.X, op=ALU.add),
                  reads=[b_sq], writes=[b_ss])
        ssf = ss[:].rearrange("p t h -> p (t h)")
        kb.op("scalar", lambda e: e.activation(out=ssf, in_=ssf, func=AF.Sqrt, bias=EPS, scale=1.0 / 128), reads=[b_ss], writes=[b_ss])
        kb.op("vector", lambda e: e.reciprocal(out=ssf, in_=ssf), reads=[b_ss], writes=[b_ss])
        sgp = Pool(kb, "go_sg", [128, 512], F32, 2)
        onp = Pool(kb, "go_on", [128, 512], F32, 2)
        for t in range(16):
            pg, b_pg = self.PS.get()
            for kc in range(8):
                kb.op("tensor", lambda e, kc=kc, t=t, pg=pg: e.matmul(pg[:], self.hT[:, kc, (t + 2) * 128:(t + 3) * 128], wgt[:, kc, :],
                                                                     start=(kc == 0), stop=(kc == 7)), reads=[b_wgt, self.b_hT[t + 2]], writes=[b_pg], sig=(kc == 7))
            sg, b_sg = sgp.get()
            kb.op("scalar", lambda e, pg=pg, sg=sg: e.activation(out=sg[:], in_=pg[:], func=AF.Silu), reads=[b_pg], writes=[b_sg])
            on, b_on = onp.get()
            on3 = on[:].rearrange("p (h c) -> p h c", c=128)
            kb.op("vector", lambda e, t=t, on3=on3: e.tensor_tensor(out=on3, in0=self.osum[:, t, :].rearrange("p (h c) -> p h c", c=128),
                                                                   in1=ss[:, t, :].unsqueeze(2).to_broadcast([128, 4, 128]), op=ALU.mult),
                  reads=[self.b_osum[t], b_ss], writes=[b_on])
            kb.op("vector", lambda e, on3=on3: e.tensor_tensor(out=on3, in0=on3, in1=nw[:].unsqueeze(1).to_broadcast([128, 4, 128]), op=ALU.mult),
                  reads=[b_on, b_nw], writes=[b_on])
            kb.op("vector", lambda e, on=on, sg=sg: e.tensor_tensor(out=on[:], in0=on[:], in1=sg[:], op=ALU.mult), reads=[b_on, b_sg], writes=[b_on])
            pt, b_pt = self.PS.get()
            for c in range(4):
                kb.op("tensor", lambda e, c=c, pt=pt, on=on: e.transpose(pt[:, c * 128:(c + 1) * 128], on[:, c * 128:(c + 1) * 128], self.identf[:]),
                      reads=[b_on, self.b_ident], writes=[b_pt])
            kb.op("scalar", lambda e, pt=pt, t=t: e.activation(out=self.ybT[:, :, t * 128:(t + 1) * 128], in_=pt[:].rearrange("p (c k) -> p c k", k=128),
                                                              func=AF.Identity), reads=[b_pt], writes=[self.b_ybT])
        if "ybT" in self.dbg:
            t_ = kb.sb("dbg_ybT_t", [128, 4, 512], F32)
            bt = kb.buf("dbg_ybT")
            kb.op("vector", lambda e: e.tensor_copy(out=t_[:], in_=self.ybT[:, :, 0:512]), reads=[self.b_ybT], writes=[bt])
            self.dump("ybT", t_[:], bt, [128, 4, 512])

    def phase_merge(self):
        kb = self.kb
        mT = kb.sb("mergedT", [128, 8, L], BF16)
        b_mT = [kb.buf("mergedT%d" % n) for n in range(4)]
        with kb.scope():
            wba = kb.sb("wba", [128, 4, D], BF16); b_wba = kb.buf("wba")
            wbb = kb.sb("wbb", [128, 4, D], BF16); b_wbb = kb.buf("wbb")
            self.load_w_bf16(wba, b_wba, self.w_ba, D, 0)
            self.load_w_bf16(wbb, b_wbb, self.w_bb, D, 0)
            wbrall = kb.sb("wbrall", [128, 8, 2 * D], BF16)
            b_wbrall = kb.buf("wbrall")
            self.load_w_bf16(wbrall, b_wbrall, self.w_in, 2 * D, OFF_BR)
            gp = Pool(kb, "mg_g", [128, 2, 512], F32, 2)
            m1p = Pool(kb, "mg_m", [128, 2, 512], F32, 2)
            v = self.w_in.rearrange("(kc p) n -> p kc n", p=128)
            for oc in range(8):
                b_wbr = b_wbrall
                for n in range(4):
                    tiles = [self.b_hT[2 + 4 * n + j] for j in range(4)]
                    tok = slice(n * 512, (n + 1) * 512)
                    stok = slice(CTX + n * 512, CTX + (n + 1) * 512)
                    g, b_g = gp.get()
                    m1, b_m1 = m1p.get()
                    for ab, (wb, b_wb, yT_, b_y) in enumerate([(wba, b_wba, self.yaT, self.b_yaT), (wbb, b_wbb, self.ybT, self.b_ybT)]):
                        pbr, b_pbr = self.PS.get()
                        for kc in range(8):
                            kb.op("tensor", lambda e, kc=kc, pbr=pbr, ab=ab: e.matmul(
                                pbr[:], wbrall[:, kc, ab * D + oc * 128:ab * D + (oc + 1) * 128], self.hT[:, kc, stok], start=(kc == 0), stop=(kc == 7)),
                                reads=[b_wbr] + tiles, writes=[b_pbr], sig=(kc == 7))
                        kb.op("scalar", lambda e, pbr=pbr, g=g, ab=ab: e.activation(out=g[:, ab, :], in_=pbr[:], func=AF.Sigmoid),
                              reads=[b_pbr], writes=[b_g])
                        pp, b_pp = self.PS.get()
                        for kc in range(4):
                            kb.op("tensor", lambda e, kc=kc, pp=pp, wb=wb, yT_=yT_: e.matmul(
                                pp[:], wb[:, kc, oc * 128:(oc + 1) * 128], yT_[:, kc, tok], start=(kc == 0), stop=(kc == 3)),
                                reads=[b_wb, b_y], writes=[b_pp], sig=(kc == 3))
                        kb.op("vector", lambda e, pp=pp, g=g, m1=m1, ab=ab: e.tensor_tensor(out=m1[:, ab, :], in0=pp[:], in1=g[:, ab, :], op=ALU.mult),
                              reads=[b_pp, b_g], writes=[b_m1])
                    kb.op("vector", lambda e, m1=m1, oc=oc: e.tensor_tensor(out=mT[:, oc, tok], in0=m1[:, 0, :], in1=m1[:, 1, :], op=ALU.add),
                          reads=[b_m1], writes=[b_mT[n]])
        if "mergedT" in self.dbg:
            t_ = kb.sb("dbg_mT_t", [128, 8, 256], F32)
            bt = kb.buf("dbg_mT")
            kb.op("vector", lambda e: e.tensor_copy(out=t_[:], in_=mT[:, :, 0:256]), reads=[b_mT[0]], writes=[bt])
            self.dump("mergedT", t_[:], bt, [128, 8, 256])
        with kb.scope():
            wo = kb.sb("wout", [128, 8, D], BF16); b_wo = kb.buf("wout")
            self.load_w_bf16(wo, b_wo, self.w_out, D, 0)
            xp = Pool(kb, "mg_x", [128, D], F32, 3)
            tp = Pool(kb, "mg_t", [128, D], F32, 2)
            self.b_x1s = [kb.buf("x1s%d" % t) for t in range(16)]
            for t in range(16):
                xt, b_xt = xp.get()
                kb.dma("sync" if t % 2 == 0 else "scalar", xt[:], self.x[t * 128:(t + 1) * 128, :], writes=[b_xt])
                tm_, b_tm = tp.get()
                for cb in range(2):
                    pm, b_pm = self.PS.get()
                    for kc in range(8):
                        kb.op("tensor", lambda e, kc=kc, pm=pm, cb=cb, t=t: e.matmul(
                            pm[:], mT[:, kc, t * 128:(t + 1) * 128], wo[:, kc, cb * 512:(cb + 1) * 512], start=(kc == 0), stop=(kc == 7)),
                            reads=[b_wo, b_mT[t // 4]], writes=[b_pm], sig=(kc == 7))
                    cs_ = slice(cb * 512, (cb + 1) * 512)
                    kb.op("vector", lambda e, pm=pm, tm_=tm_, cs_=cs_: e.tensor_tensor(out=tm_[:, cs_], in0=pm[:], in1=self.gt1_bc[:, cs_], op=ALU.mult),
                          reads=[b_pm, self.b_gt1], writes=[b_tm])
                kb.op("vector", lambda e, tm_=tm_, xt=xt: e.tensor_tensor(out=xt[:], in0=tm_[:], in1=xt[:], op=ALU.add), reads=[b_tm, b_xt], writes=[b_xt])
                kb.dma("sync", self.x1s[t * 128:(t + 1) * 128, :], xt[:], reads=[b_xt], writes=[self.b_x1s[t]])
                if t == 0:
                    self.dump("x1", xt[:], b_xt, [128, D])

    def phase_moe_half(self, hp):
        kb = self.kb
        NTL = 8
        X = kb.sb("X%d" % hp, [128, NTL, D], F32)
        b_X = [kb.buf("X%d_%d" % (hp, t)) for t in range(NTL)]
        h2T = kb.sb("h2T%d" % hp, [128, 8, NTL * 128], BF16)
        b_h2 = [kb.buf("h2T%d_%d" % (hp, t)) for t in range(NTL)]
        comb = kb.sb("comb%d" % hp, [128, NTL, NE], F32)
        b_comb = kb.buf("comb%d" % hp)
        combs = kb.sb("combs%d" % hp, [128, NTL, NE], F32)
        combT = kb.sb("combT%d" % hp, [NE, NTL * 128], F32)
        b_combT = kb.buf("combT%d" % hp)
        for t in range(NTL):
            gt = hp * NTL + t
            kb.dma("sync" if t % 2 == 0 else "scalar", X[:, t, :], self.x1s[gt * 128:(gt + 1) * 128, :], reads=[self.b_x1s[gt]], writes=[b_X[t]])
        with kb.scope():
            n2, b_n2 = self._ld("norm2", self.norm2_t, [128, 8])
            A2 = kb.sb("A2", [128, 8, 2], F32); b_A2 = kb.buf("A2")
            kb.op("vector", lambda e: e.scalar_tensor_tensor(out=A2[:], in0=self.modT[:, 24:32, :], scalar=1.0,
                                                            in1=n2[:].unsqueeze(2).to_broadcast([128, 8, 2]), op0=ALU.add, op1=ALU.mult),
                  reads=[self.b_modT, b_n2], writes=[b_A2])
            wr = kb.sb("wrouter", [128, 8, NE], F32); b_wr = kb.buf("wrouter")
            kb.dma("sync", wr[:], self.w_router.rearrange("(kc p) n -> p kc n", p=128), writes=[b_wr])
            br_, b_br = kb.sb("brouter", [128, NE], F32), kb.buf("brouter")
            kb.dma("sync", br_[:], self.b_router.partition_broadcast(128), writes=[b_br])
            xnp = Pool(kb, "m_xn", [128, D], F32, 2)
            junk = Pool(kb, "m_junk", [128, D], F32, 1)
            stat = Pool(kb, "m_stat", [128, 4], F32, 4)
            hfp = Pool(kb, "m_hf", [128, 8, 128], F32, 2)
            lgp = Pool(kb, "m_lg", [128, NE], F32, 2)
            m8p = Pool(kb, "m_m8", [128, 16], F32, 2)
            for t in range(NTL):
                hf, b_hf = hfp.get()
                self._norm_tile2(X[:, t, :], b_X[t], t, A2, b_A2, 16, xnp, junk, stat, h2T, b_h2[t], hf, b_hf)
                pl, b_pl = self.PS.get()
                for kc in range(8):
                    kb.op("tensor", lambda e, kc=kc, pl=pl, hf=hf: e.matmul(pl[:, 0:NE], hf[:, kc, :], wr[:, kc, :], start=(kc == 0), stop=(kc == 7)),
                          reads=[b_hf, b_wr], writes=[b_pl], sig=(kc == 7))
                lg, b_lg = lgp.get()
                m8, b_m8 = m8p.get()
                kb.op("vector", lambda e, pl=pl, lg=lg: e.tensor_tensor(out=lg[:], in0=pl[:, 0:NE], in1=br_[:], op=ALU.add), reads=[b_pl, b_br], writes=[b_lg])
                if t == 0 and hp == 0:
                    self.dump("logits", lg[:], b_lg, [128, NE])
                kb.op("vector", lambda e, lg=lg, m8=m8: e.max(out=m8[:, 0:8], in_=lg[:]), reads=[b_lg], writes=[b_m8])
                kb.op("vector", lambda e, m8=m8: e.tensor_scalar(out=m8[:, 8:9], in0=m8[:, 0:1], scalar1=-1.0, scalar2=0.0, op0=ALU.mult, op1=ALU.add),
                      reads=[b_m8], writes=[b_m8])
                ex, b_ex = lgp.get()
                kb.op("scalar", lambda e, lg=lg, ex=ex, m8=m8: e.activation(out=ex[:], in_=lg[:], func=AF.Exp, bias=m8[:, 8:9]), reads=[b_lg, b_m8], writes=[b_ex])
                kb.op("vector", lambda e, lg=lg, ex=ex, m8=m8: e.scalar_tensor_tensor(out=ex[:], in0=lg[:], scalar=m8[:, 3:4], in1=ex[:], op0=ALU.is_ge, op1=ALU.mult),
                      reads=[b_lg, b_ex, b_m8], writes=[b_ex])
                kb.op("vector", lambda e, ex=ex, m8=m8: e.tensor_reduce(out=m8[:, 9:10], in_=ex[:], axis=AX.X, op=ALU.add), reads=[b_ex], writes=[b_m8])
                kb.op("vector", lambda e, m8=m8: e.reciprocal(out=m8[:, 10:11], in_=m8[:, 9:10]), reads=[b_m8], writes=[b_m8])
                kb.op("vector", lambda e, ex=ex, m8=m8, t=t: e.tensor_scalar(out=comb[:, t, :], in0=ex[:], scalar1=m8[:, 10:11], scalar2=0.0, op0=ALU.mult, op1=ALU.add),
                      reads=[b_ex, b_m8], writes=[b_comb])
                kb.op("vector", lambda e, t=t: e.tensor_scalar(out=combs[:, t, :], in0=comb[:, t, :], scalar1=float(1.0 / 1.702), scalar2=0.0, op0=ALU.mult, op1=ALU.add),
                      reads=[b_comb], writes=[b_comb])
                pT, b_pT = self.PS.get()
                kb.op("tensor", lambda e, pT=pT, t=t: e.transpose(pT[0:NE, 0:128], comb[:, t, :], self.identf[:]), reads=[b_comb, self.b_ident], writes=[b_pT])
                kb.op("scalar", lambda e, pT=pT, t=t: e.activation(out=combT[:, t * 128:(t + 1) * 128], in_=pT[0:NE, 0:128], func=AF.Identity),
                      reads=[b_pT], writes=[b_combT])
            if hp == 0:
                self.dump("comb", comb[:, 0, :], b_comb, [128, NE])
                if "h2T" in self.dbg:
                    t_ = kb.sb("dbg_h2T_t", [128, 8, 256], F32)
                    bt = kb.buf("dbg_h2T")
                    kb.op("vector", lambda e: e.tensor_copy(out=t_[:], in_=h2T[:, :, 0:256]), reads=b_h2[0:2], writes=[bt])
                    self.dump("h2T", t_[:], bt, [128, 8, 256])
        if self.stop == "router":
            return
        with kb.scope():
            bdn, b_bdn = self._ld("bdn", self.b_dn, [NE, D])
            bgu, b_bgu = self._ld("bgu", self.b_gu_t, [128, NE, 16])
            tp = Pool(kb, "moe_t", [128, 512], F32, 3)
            for t in range(NTL):
                for cb in range(2):
                    cs_ = slice(cb * 512, (cb + 1) * 512)
                    pb, b_pb = self.PS.get()
                    kb.op("tensor", lambda e, pb=pb, t=t, cs_=cs_: e.matmul(pb[:], combT[:, t * 128:(t + 1) * 128], bdn[:, cs_], start=True, stop=True),
                          reads=[b_combT, b_bdn], writes=[b_pb])
                    tt_, b_tt = tp.get()
                    kb.op("vector", lambda e, pb=pb, tt_=tt_, cs_=cs_: e.tensor_tensor(out=tt_[:], in0=pb[:], in1=self.gt2_bc[:, cs_], op=ALU.mult),
                          reads=[b_pb, self.b_gt2], writes=[b_tt])
                    kb.op("vector", lambda e, tt_=tt_, t=t, cs_=cs_: e.tensor_tensor(out=X[:, t, cs_], in0=tt_[:], in1=X[:, t, cs_], op=ALU.add),
                          reads=[b_tt], writes=[b_X[t]])
            wgup = Pool(kb, "wgu", [128, 8, 2 * D], BF16, 2)
            wdnp = Pool(kb, "wdn", [128, 8, D], BF16, 2)
            actp = Pool(kb, "actT", [128, 8, 512], BF16, 2)
            gp = Pool(kb, "moe_g", [128, 512], F32, 3)
            sp = Pool(kb, "moe_s", [128, 512], BF16, 3)
            up = Pool(kb, "moe_u", [128, 512], BF16, 3)
            bgu1 = kb.sb("bgu1", [128, NE, 8], F32)
            kb.op("vector", lambda e: e.tensor_scalar(out=bgu1[:], in0=bgu[:, :, 8:16], scalar1=1.0, scalar2=0.0, op0=ALU.add, op1=ALU.add),
                  reads=[b_bgu], writes=[b_bgu])
            NB = NTL * 128 // 512
            nexp = NE if self.stop != "moe1" else 1
            W = {}

            def load_w(ex_):
                wgu, b_wgu = wgup.get()
                wdn, b_wdn = wdnp.get()
                vg = self.w_gu[ex_].rearrange("(kc p) n -> p kc n", p=128)
                for kc in range(8):
                    kb.dma("gpsimd", wgu[:, kc, :], vg[:, kc, :], writes=[b_wgu])
                vd = self.w_dn[ex_].rearrange("(kc p) n -> p kc n", p=128)
                for kc in range(8):
                    kb.dma("gpsimd", wdn[:, kc, :], vd[:, kc, :], writes=[b_wdn])
                W[ex_] = (wgu, b_wgu, wdn, b_wdn)

            def fold(ex_):
                wgu, b_wgu, wdn, b_wdn = W[ex_]
                for q in range(2):
                    kb.op("vector", lambda e, q=q: e.tensor_tensor(out=wdn[:, q::2, :], in0=wdn[:, q::2, :],
                                                                  in1=self.gt2_bc[:].unsqueeze(1).to_broadcast([128, 4, D]), op=ALU.mult),
                          reads=[b_wdn, self.b_gt2], writes=[b_wdn])

            ACT = {}

            def gu(ex_, n):
                wgu, b_wgu, wdn, b_wdn = W[ex_]
                actT, b_act = actp.get()
                ACT[(ex_, n)] = (actT, b_act)
                toks = slice(n * 512, (n + 1) * 512)
                tl = [b_h2[4 * n + j] for j in range(4)]
                for j in range(8):
                    pg, b_pg = self.PS.get()
                    pu, b_pu = self.PS.get()
                    for kc in range(8):
                        kb.op("tensor", lambda e, kc=kc: e.matmul(pg[:], wgu[:, kc, j * 128:(j + 1) * 128], h2T[:, kc, toks],
                                                                  start=(kc == 0), stop=(kc == 7)), reads=[b_wgu] + tl, writes=[b_pg], sig=(kc == 7))
                    for kc in range(8):
                        kb.op("tensor", lambda e, kc=kc: e.matmul(pu[:], wgu[:, kc, D + j * 128:D + (j + 1) * 128], h2T[:, kc, toks],
                                                                  start=(kc == 0), stop=(kc == 7)), reads=[b_wgu] + tl, writes=[b_pu], sig=(kc == 7))
                    g, b_g = gp.get(); sg, b_sg = sp.get(); u, b_u = up.get()
                    kb.op("vector", lambda e: e.tensor_scalar(out=g[:], in0=pg[:], scalar1=bgu[:, ex_, j:j + 1], scalar2=7.0, op0=ALU.add, op1=ALU.min),
                          reads=[b_pg, b_bgu], writes=[b_g])
                    kb.op("scalar", lambda e: e.activation(out=sg[:], in_=g[:], func=AF.Silu, scale=1.702), reads=[b_g], writes=[b_sg])
                    kb.op("vector", lambda e: e.tensor_scalar(out=u[:], in0=pu[:], scalar1=bgu1[:, ex_, j:j + 1], scalar2=8.0, op0=ALU.add, op1=ALU.min),
                          reads=[b_pu, b_bgu], writes=[b_u])
                    kb.op("vector", lambda e: e.scalar_tensor_tensor(out=actT[:, j, :], in0=u[:], scalar=-6.0, in1=sg[:], op0=ALU.max, op1=ALU.mult),
                          reads=[b_u, b_sg], writes=[b_act])
                    if n == 0 and j == 3:
                        fold(ex_)

            def dn(ex_, n):
                wgu, b_wgu, wdn, b_wdn = W[ex_]
                actT, b_act = ACT.pop((ex_, n))
                for tq in range(4):
                    t = n * 4 + tq
                    for cb in range(2):
                        cs_ = slice(cb * 512, (cb + 1) * 512)
                        pd_, b_pd = self.PS.get()
                        for kc in range(8):
                            kb.op("tensor", lambda e, kc=kc: e.matmul(pd_[:], actT[:, kc, tq * 128:(tq + 1) * 128], wdn[:, kc, cs_],
                                                                      start=(kc == 0), stop=(kc == 7)), reads=[b_act, b_wdn], writes=[b_pd], sig=(kc == 7))
                        kb.op("vector", lambda e: e.scalar_tensor_tensor(out=X[:, t, cs_], in0=pd_[:], scalar=combs[:, t, ex_:ex_ + 1], in1=X[:, t, cs_],
                                                                        op0=ALU.mult, op1=ALU.add), reads=[b_pd, b_comb], writes=[b_X[t]])

            items = [(e_, n) for e_ in range(nexp) for n in range(NB)]
            load_w(0)
            if nexp > 1:
                load_w(1)
            gu(*items[0])
            for k, (e_, n) in enumerate(items):
                if k + 1 < len(items):
                    gu(*items[k + 1])
                dn(e_, n)
                if n == NB - 1 and e_ + 2 < nexp:
                    load_w(e_ + 2)
        if hp == 0:
            self.dump("x2", X[:, 0, :], b_X[0], [128, D])
        with kb.scope():
            nf = kb.sb("normf", [128, D], F32); b_nf = kb.buf("normf")
            kb.dma("sync", nf[:], self.norm_f.partition_broadcast(128), writes=[b_nf])
            junk = Pool(kb, "f_junk", [128, D], F32, 1)
            stat = Pool(kb, "f_stat", [128, 4], F32, 4)
            op_ = Pool(kb, "f_o", [128, D], F32, 2)
            for t in range(NTL):
                gt = hp * NTL + t
                jt, b_jt = junk.get(); st, b_st = stat.get()
                kb.op("scalar", lambda e, jt=jt, st=st, t=t: e.activation(out=jt[:], in_=X[:, t, :], func=AF.Square, accum_out=st[:, 0:1]), reads=[b_X[t]], writes=[b_jt, b_st])
                kb.op("scalar", lambda e, st=st: e.activation(out=st[:, 1:2], in_=st[:, 0:1], func=AF.Sqrt, bias=EPS, scale=1.0 / D), reads=[b_st], writes=[b_st])
                kb.op("vector", lambda e, st=st: e.reciprocal(out=st[:, 2:3], in_=st[:, 1:2]), reads=[b_st], writes=[b_st])
                ot, b_ot = op_.get()
                kb.op("vector", lambda e, ot=ot, st=st, t=t: e.scalar_tensor_tensor(out=ot[:], in0=X[:, t, :], scalar=st[:, 2:3], in1=nf[:], op0=ALU.mult, op1=ALU.mult),
                      reads=[b_X[t], b_st, b_nf], writes=[b_ot])
                bo = kb.buf("out%d" % gt)
                kb.dma("sync", self.out[gt * 128:(gt + 1) * 128, :], ot[:], reads=[b_ot], writes=[bo])
                self.fin.append(bo)

    def _norm_tile2(self, xt_ap, b_xt, tt, A, b_A, shift_chunk0, xnp, junk, stat, dstT, b_dst, hf, b_hf):
        kb = self.kb
        jt, b_jt = junk.get()
        st, b_st = stat.get()
        kb.op("scalar", lambda e: e.activation(out=jt[:], in_=xt_ap, func=AF.Square, accum_out=st[:, 0:1]), reads=[b_xt], writes=[b_jt, b_st])
        kb.op("scalar", lambda e: e.activation(out=st[:, 1:2], in_=st[:, 0:1], func=AF.Sqrt, bias=EPS, scale=1.0 / D), reads=[b_st], writes=[b_st])
        kb.op("vector", lambda e: e.reciprocal(out=st[:, 2:3], in_=st[:, 1:2]), reads=[b_st], writes=[b_st])
        xn, b_xn = xnp.get()
        kb.op("scalar", lambda e: e.activation(out=xn[:], in_=xt_ap, func=AF.Identity, scale=st[:, 2:3]), reads=[b_xt, b_st], writes=[b_xn])
        for half in range(2):
            pt, b_pt = self.PS.get()
            for q in range(4):
                kc = half * 4 + q
                kb.op("tensor", lambda e, kc=kc, q=q, pt=pt: e.transpose(pt[:, q * 128:(q + 1) * 128], xn[:, kc * 128:(kc + 1) * 128], self.identf[:]),
                      reads=[b_xn, self.b_ident], writes=[b_pt])
            for q in range(4):
                kc = half * 4 + q
                kb.op("vector", lambda e, kc=kc, q=q, pt=pt: e.tensor_scalar(
                    out=hf[:, kc, :], in0=pt[:, q * 128:(q + 1) * 128], scalar1=A[:, kc, 0:1], scalar2=self.modT[:, shift_chunk0 + kc, 0:1],
                    op0=ALU.mult, op1=ALU.add), reads=[b_pt, b_A, self.b_modT], writes=[b_hf])
        kb.op("scalar", lambda e: e.activation(out=dstT[:, :, tt * 128:(tt + 1) * 128], in_=hf[:], func=AF.Identity), reads=[b_hf], writes=[b_dst])

    def build(self):
        kb = self.kb
        self.declare()
        self.common()
        with kb.scope():
            self.hT = kb.sb("hT", [128, 8, TOK], BF16)
            self.b_hT = [kb.buf("hT%d" % t) for t in range(NT)]
            self.yaT = kb.sb("yaT", [128, 4, L], BF16)
            self.b_yaT = kb.buf("yaT")
            with kb.scope():
                self.defer_s5_body = True
                self.phase_s5_setup()
                with kb.scope():
                    self.phase_mod()
                    self._s5_setup_body()
                if self.stop in ("mod", "s5setup"):
                    return self.finish()
                with kb.scope():
                    self.phase_norm1()
                if self.stop == "norm1":
                    return self.finish()
                self.uT = kb.sb("uT", [128, 4, TOK], BF16)
                self.b_uT = kb.buf("uT")
                self.yT = kb.sb("yT", [128, 4, L], F32)
                self.b_yT = [kb.buf("yT%d" % q) for q in range(4)]
                with kb.scope():
                    self.phase_u()
                if self.stop == "u":
                    return self.finish()
                with kb.scope():
                    self.phase_s5()
                if self.stop == "s5":
                    return self.finish()
                with kb.scope():
                    self.phase_glu()
                if self.stop == "glu":
                    return self.finish()
            self.ybT = kb.sb("ybT", [128, 4, L], BF16)
            self.b_ybT = kb.buf("ybT")
            with kb.scope():
                self.phase_gdn_setup()
                if self.stop in ("gdnsetup",):
                    return self.finish()
                self.phase_gdn()
                if self.stop in ("gdn", "gdn_h0"):
                    return self.finish()
                with kb.scope():
                    self.phase_gdn_out()
                if self.stop == "gdnout":
                    return self.finish()
            with kb.scope():
                self.phase_merge()
            if self.stop == "merge":
                return self.finish()
        for hp in range(2):
            with kb.scope():
                self.phase_moe_half(hp)
            if self.stop in ("router", "moe1", "half"):
                return self.finish()
        return self.finish()

    def finish(self):
        self.kb.finish(self.fin)
        self.kb.finished = True
        for es in reversed(getattr(self.kb, "scopes", [])):
            es.close()
        self.kb.root.close()
        return self.nc


def _fm(v, nch):
    return np.ascontiguousarray(np.asarray(v, np.float32).reshape(nch, 128).T)


def host_inputs(inputs, b):
    f = lambda a: np.ascontiguousarray(np.asarray(a, np.float32))
    m = {}
    m["x"] = f(inputs["x"][b])
    m["ctx"] = f(inputs["ctx"][b])
    cs = np.stack([_fm(inputs["c"][b], 8), _fm(inputs["c_ctx"], 8)], axis=-1)
    m["cs"] = f(cs)
    m["w_mod"] = f(inputs["w_mod"][0])
    bm = np.asarray(inputs["b_mod"][0], np.float32)
    bmt = _fm(bm, 48)
    order = list(range(0, 16)) + list(range(24, 40)) + list(range(16, 24)) + list(range(40, 48))
    m["b_mod_t"] = f(bmt[:, order])
    m["b_mod"] = f(bm)
    m["norm1_t"] = _fm(inputs["norm1"][0], 8)
    m["norm2_t"] = _fm(inputs["norm2"][0], 8)
    m["w_in"] = f(inputs["w_in"][0])
    m["ident"] = np.eye(128, dtype=np.float32)
    m["s5_d_t"] = _fm(inputs["s5_d"][0], 4)
    def pdl(a):
        a = np.asarray(a, np.float32).reshape(2, 16, 2, 64)
        return f(a.transpose(2, 3, 0, 1).reshape(128, 32))
    m["lam_re_t"] = pdl(inputs["s5_lam_re"][0])
    m["lam_im_t"] = pdl(inputs["s5_lam_im"][0])
    m["logstep_t"] = pdl(np.broadcast_to(np.asarray(inputs["s5_log_step"][0], np.float32)[:, :, None], (2, 32, 64)))
    def blk(re, im, cn):
        out = np.zeros((2, 64, 2, 2, 16, 2, 16), np.float32)
        for ri, arr in enumerate((re, im)):
            arr = np.asarray(arr, np.float32)
            arr = arr if cn else arr.transpose(0, 1, 3, 2)
            arr = arr.reshape(2, 16, 2, 64, 16)
            for g2 in range(2):
                out[g2, :, ri, :, :, g2, :] = arr[:, :, g2].transpose(2, 0, 1, 3)
        return f(out.reshape(128, 2, 32, 32))
    m["Bblk"] = blk(inputs["s5_b_re"][0], inputs["s5_b_im"][0], True)
    m["Cblk"] = blk(inputs["s5_c_re"][0], inputs["s5_c_im"][0], False)
    r_ = np.arange(128)[:, None]; c_ = np.arange(128)[None, :]
    same = (r_ // 64) == (c_ // 64)
    NEG = -30000.0
    Mf = (same & (r_ <= c_)).astype(np.float32); Mb = (same & (r_ >= c_)).astype(np.float32)
    gmk = [Mf, Mb, np.where(same & (r_ > c_), 0.0, NEG), np.where(same & (r_ < c_), 0.0, NEG), np.where(same & (r_ <= c_), 0.0, NEG),
           np.tile((r_ < 64), (1, 128)).astype(np.float32), np.tile((r_ >= 64), (1, 128)).astype(np.float32), same.astype(np.float32),
           -Mf, -Mb, np.where(same & (r_ >= c_), 0.0, NEG)]
    m["gmask"] = f(np.stack([np.asarray(a, np.float32) for a in gmk], axis=1))
    cw = np.asarray(inputs["gdn_conv"][0], np.float32)
    m["conv_t"] = f(cw.reshape(5, 12, 128).transpose(2, 1, 0))
    m["alog_dtb"] = f(np.concatenate([np.asarray(inputs["gdn_a_log"][0]).reshape(8), np.asarray(inputs["gdn_dt_bias"][0]).reshape(8)]))
    m["gdn_norm"] = f(inputs["gdn_norm"][0])
    m["w_glu"] = f(inputs["s5_w_glu"][0])
    m["b_glu_t"] = _fm(inputs["s5_b_glu"][0], 4)
    m["w_ba"] = f(inputs["w_branch_a"][0])
    m["w_bb"] = f(inputs["w_branch_b"][0])
    m["w_out"] = f(inputs["w_out"][0])
    m["w_router"] = f(inputs["w_router"][0])
    m["b_router"] = f(inputs["b_router"][0])
    m["w_gu"] = f(inputs["w_gate_up"][0])
    bgu = np.asarray(inputs["b_gate_up"][0], np.float32)
    m["b_gu_t"] = f(bgu.reshape(NE, 16, 128).transpose(2, 0, 1))
    m["w_dn"] = f(inputs["w_down"][0])
    m["b_dn"] = f(inputs["b_down"][0])
    m["norm_f"] = f(inputs["norm_f"])
    m["iota1"] = f(np.tile(np.arange(1, TS5 + 1, dtype=np.float32)[None], (128, 1)))
    return m


_CACHE = {}


def kernel(**inputs):
    nb = 8
    if "nc" not in _CACHE:
        _CACHE["nc"] = Builder().build()
    nc = _CACHE["nc"]
    shared = host_inputs(inputs, 0)
    in_maps = []
    for b in range(nb):
        m = dict(shared)
        if b > 0:
            f = lambda a: np.ascontiguousarray(np.asarray(a, np.float32))
            m["x"] = f(inputs["x"][b])
            m["ctx"] = f(inputs["ctx"][b])
            m["cs"] = f(np.stack([_fm(inputs["c"][b], 8), _fm(inputs["c_ctx"], 8)], axis=-1))
        in_maps.append(m)
    res = run_bass_kernel_spmd(nc, in_maps, core_ids=list(range(nb)))
    out = np.stack([np.asarray(res.results[b]["out"], np.float32) for b in range(nb)], axis=0)
    return out
```
